# Optimizing a Trainium2 kernel written in Bass

```python
import math
import jax, jax.numpy as jnp
from jax import lax
import numpy as np

D_MODEL = 1024
BATCH = 8
SEQ = 4096
DEPTH = 2

HEAD_DIM = 64
ROPE_THETA = 10000.0
LN_EPS = 1e-5
DEEPNORM_ALPHA = (2.0 * DEPTH) ** 0.25
DEEPNORM_BETA = (8.0 * DEPTH) ** -0.25
N_EVEN_LAYERS = (DEPTH + 1) // 2
N_ODD_LAYERS = DEPTH // 2

NSA_HEADS = D_MODEL // 128
NSA_KV_GROUPS = 2
NSA_HEADS_PER_GROUP = NSA_HEADS // NSA_KV_GROUPS
NSA_WIDTH = NSA_HEADS * HEAD_DIM
NSA_KV_WIDTH = NSA_KV_GROUPS * HEAD_DIM
CMP_BLOCK = 32
CMP_STRIDE = 16
CMP_HIDDEN = 2 * HEAD_DIM
SEL_BLOCK = 64
SEL_TOPN = 8
WINDOW = 256
NSA_QCHUNK = 128
NSA_IN_WIDTH = NSA_WIDTH + 6 * NSA_KV_WIDTH + 3 * NSA_HEADS

S5_WIDTH = D_MODEL - NSA_WIDTH
S5_GROUP_CH = 16
S5_GROUPS = S5_WIDTH // S5_GROUP_CH
S5_STATE = 64

EVEN_IN_WIDTH = NSA_IN_WIDTH + S5_WIDTH

MOBA_HEADS = D_MODEL // HEAD_DIM
MOBA_BLOCK = 256
MOBA_TOPK = 3
MOBA_QCHUNK = 64

MOE_GROUPS = 4
MOE_EXPERTS_PER_GROUP = 8
MOE_N_EXPERTS = MOE_GROUPS * MOE_EXPERTS_PER_GROUP
MOE_TOP_IN_GROUP = 2
MOE_FF = 128

kernel_name = 'hybrid_nsa_s5_moba_hmoe_deepnorm'


def layer_norm(x, g, b):
    xf = x.astype(jnp.float32)
    mu = jnp.mean(xf, -1, keepdims=True)
    var = jnp.mean(jnp.square(xf - mu), -1, keepdims=True)
    return ((xf - mu) * lax.rsqrt(var + LN_EPS) * g + b).astype(x.dtype)


def rope_tables(seq):
    inv = 1.0 / (ROPE_THETA ** (jnp.arange(0, HEAD_DIM, 2, dtype=jnp.float32) / HEAD_DIM))
    ang = jnp.arange(seq, dtype=jnp.float32)[:, None] * inv[None, :]
    return jnp.cos(ang), jnp.sin(ang)


def apply_rope(x, cos, sin):
    x1, x2 = jnp.split(x, 2, axis=-1)
    c = cos[:, None, :]
    s = sin[:, None, :]
    return jnp.concatenate([x1 * c - x2 * s, x1 * s + x2 * c], -1).astype(x.dtype)


def masked_softmax(s, mask):
    s = jnp.where(mask, s.astype(jnp.float32), -jnp.inf)
    m = jnp.max(s, -1, keepdims=True)
    m = jnp.where(jnp.isfinite(m), m, 0.0)
    p = jnp.where(mask, jnp.exp(s - m), 0.0)
    return p / jnp.maximum(jnp.sum(p, -1, keepdims=True), 1e-30)


def nsa_mixer(xp, cos, sin, pe_k, pe_v, k_w1, k_w2, v_w1, v_w2):
    B, S, _ = xp.shape
    G, Z, dh = NSA_KV_GROUPS, NSA_HEADS_PER_GROUP, HEAD_DIM
    cuts = [int(v) for v in np.cumsum([NSA_WIDTH] + [NSA_KV_WIDTH] * 6)]
    q, kc, vc, ks, vs, kw, vw, gl = jnp.split(xp, cuts, axis=-1)
    q = apply_rope(q.reshape(B, S, NSA_HEADS, dh), cos, sin)
    q = q.reshape(B, S, G, Z, dh).transpose(0, 2, 3, 1, 4)
    kc, ks, kw = [apply_rope(t.reshape(B, S, G, dh), cos, sin) for t in (kc, ks, kw)]
    vc, vs, vw = [t.reshape(B, S, G, dh) for t in (vc, vs, vw)]
    gates = jax.nn.sigmoid(gl.astype(jnp.float32)).reshape(B, S, G, Z, 3).transpose(0, 2, 3, 1, 4)

    n_cmp = (S - CMP_BLOCK) // CMP_STRIDE + 1
    starts = jnp.arange(n_cmp) * CMP_STRIDE
    idx = starts[:, None] + jnp.arange(CMP_BLOCK)[None, :]

    def compress(t, pe, w1, w2):
        blk = t[:, idx] + pe[None, None, :, None, :]
        blk = blk.transpose(0, 3, 1, 2, 4).reshape(B, G, n_cmp, CMP_BLOCK * dh)
        return jax.nn.gelu(blk @ w1) @ w2

    k_cmp = compress(kc, pe_k, k_w1, k_w2)
    v_cmp = compress(vc, pe_v, v_w1, v_w2)
    cmp_end = starts + CMP_BLOCK - 1

    nbs = S // SEL_BLOCK
    n_sel = min(SEL_TOPN, nbs)
    bstart = jnp.arange(nbs) * SEL_BLOCK
    ovl = jnp.clip(jnp.minimum(starts[:, None] + CMP_BLOCK, bstart[None, :] + SEL_BLOCK)
                   - jnp.maximum(starts[:, None], bstart[None, :]), 0, CMP_BLOCK)
    cmp_to_sel = ovl.astype(jnp.float32) / CMP_BLOCK
    ks_blk = ks.reshape(B, nbs, SEL_BLOCK, G, dh).transpose(0, 3, 1, 2, 4)
    vs_blk = vs.reshape(B, nbs, SEL_BLOCK, G, dh).transpose(0, 3, 1, 2, 4)
    kw_pad = jnp.pad(kw.transpose(0, 2, 1, 3), ((0, 0), (0, 0), (WINDOW, 0), (0, 0)))
    vw_pad = jnp.pad(vw.transpose(0, 2, 1, 3), ((0, 0), (0, 0), (WINDOW, 0), (0, 0)))
    bi = jnp.arange(B)[:, None, None, None]
    gi = jnp.arange(G)[None, :, None, None]
    scale = dh ** -0.5
    QC = NSA_QCHUNK

    def chunk(c):
        t0 = c * QC
        qpos = t0 + jnp.arange(QC)
        qc = lax.dynamic_slice_in_dim(q, t0, QC, axis=3)
        s_c = jnp.einsum('bgzqd,bgnd->bgzqn', qc, k_cmp) * scale
        p_c = masked_softmax(s_c, cmp_end[None, :] <= qpos[:, None])
        o_c = jnp.einsum('bgzqn,bgnd->bgzqd', p_c.astype(v_cmp.dtype), v_cmp)
        imp = jnp.einsum('bgzqn,nj->bgqj', p_c, cmp_to_sel)
        qblk = qpos // SEL_BLOCK
        j = jnp.arange(nbs)
        forced = (j[None, :] == 0) | (j[None, :] == qblk[:, None]) | (j[None, :] == qblk[:, None] - 1)
        future = j[None, :] > qblk[:, None]
        score = jnp.where(future, -1.0, jnp.where(forced, 1e3, imp))
        top_s, sel = lax.top_k(score, n_sel)
        valid = top_s >= 0.0
        k_sel = ks_blk[bi, gi, sel]
        v_sel = vs_blk[bi, gi, sel]
        kpos = sel[..., None] * SEL_BLOCK + jnp.arange(SEL_BLOCK)
        m_s = valid[..., None] & (kpos <= qpos[None, None, :, None, None])
        s_s = jnp.einsum('bgzqd,bgqnkd->bgzqnk', qc, k_sel) * scale
        p_s = masked_softmax(s_s.reshape(B, G, Z, QC, n_sel * SEL_BLOCK),
                             m_s.reshape(B, G, 1, QC, n_sel * SEL_BLOCK))
        o_s = jnp.einsum('bgzqk,bgqkd->bgzqd', p_s.astype(v_sel.dtype),
                         v_sel.reshape(B, G, QC, n_sel * SEL_BLOCK, dh))
        k_w = lax.dynamic_slice_in_dim(kw_pad, t0, QC + WINDOW, axis=2)
        v_w = lax.dynamic_slice_in_dim(vw_pad, t0, QC + WINDOW, axis=2)
        kposw = t0 - WINDOW + jnp.arange(QC + WINDOW)
        diff = qpos[:, None] - kposw[None, :]
        m_w = (diff >= 0) & (diff < WINDOW) & (kposw[None, :] >= 0)
        s_w = jnp.einsum('bgzqd,bgkd->bgzqk', qc, k_w) * scale
        p_w = masked_softmax(s_w, m_w)
        o_w = jnp.einsum('bgzqk,bgkd->bgzqd', p_w.astype(v_w.dtype), v_w)
        g = lax.dynamic_slice_in_dim(gates, t0, QC, axis=3)
        o = g[..., 0:1] * o_c + g[..., 1:2] * o_s + g[..., 2:3] * o_w
        return o.astype(xp.dtype)

    outs = lax.map(chunk, jnp.arange(S // QC))
    return outs.transpose(1, 0, 4, 2, 3, 5).reshape(B, S, NSA_WIDTH)


def ssm_combine(e1, e2):
    a1r, a1i, b1r, b1i = e1
    a2r, a2i, b2r, b2i = e2
    return (a2r * a1r - a2i * a1i,
            a2r * a1i + a2i * a1r,
            a2r * b1r - a2i * b1i + b2r,
            a2r * b1i + a2i * b1r + b2i)


def s5_mixer(u, lam_re, lam_im, log_step, b_re, b_im, c_re, c_im, d_skip, glu_w, glu_b):
    B, S, _ = u.shape
    f32 = jnp.float32
    uf = u.astype(f32)
    ug = uf.reshape(B, S, S5_GROUPS, S5_GROUP_CH)
    step = jnp.exp(log_step.astype(f32))[:, None]
    lr = lam_re.astype(f32)
    li = lam_im.astype(f32)
    mag = jnp.exp(lr * step)
    ar = mag * jnp.cos(li * step)
    ai = mag * jnp.sin(li * step)
    den = lr * lr + li * li
    fr = ((ar - 1.0) * lr + ai * li) / den
    fi = (ai * lr - (ar - 1.0) * li) / den
    br = b_re.astype(f32)
    bim = b_im.astype(f32)
    bbar_r = fr[..., None] * br - fi[..., None] * bim
    bbar_i = fr[..., None] * bim + fi[..., None] * br
    bu_r = jnp.einsum('bsgc,gpc->bsgp', ug, bbar_r)
    bu_i = jnp.einsum('bsgc,gpc->bsgp', ug, bbar_i)
    a_r = jnp.broadcast_to(ar[None, None], (1, S, S5_GROUPS, S5_STATE))
    a_i = jnp.broadcast_to(ai[None, None], (1, S, S5_GROUPS, S5_STATE))
    _, _, xr, xi = lax.associative_scan(ssm_combine, (a_r, a_i, bu_r, bu_i), axis=1)
    y = (jnp.einsum('bsgp,gcp->bsgc', xr, c_re.astype(f32))
         - jnp.einsum('bsgp,gcp->bsgc', xi, c_im.astype(f32)))
    y = y.reshape(B, S, S5_WIDTH) + d_skip.astype(f32) * uf
    g = jax.nn.gelu(y)
    out = g * jax.nn.sigmoid(g @ glu_w.astype(f32) + glu_b.astype(f32))
    return out.astype(u.dtype)


def moba_mixer(xp, cos, sin):
    B, S, _ = xp.shape
    H, dh = MOBA_HEADS, HEAD_DIM
    q, k, v = jnp.split(xp, 3, axis=-1)
    q = apply_rope(q.reshape(B, S, H, dh), cos, sin).transpose(0, 2, 1, 3)
    k = apply_rope(k.reshape(B, S, H, dh), cos, sin).transpose(0, 2, 1, 3)
    v = v.reshape(B, S, H, dh).transpose(0, 2, 1, 3)
    nb = -(-S // MOBA_BLOCK)
    pad = nb * MOBA_BLOCK - S
    k = jnp.pad(k, ((0, 0), (0, 0), (0, pad), (0, 0)))
    v = jnp.pad(v, ((0, 0), (0, 0), (0, pad), (0, 0)))
    k_blk = k.reshape(B, H, nb, MOBA_BLOCK, dh)
    v_blk = v.reshape(B, H, nb, MOBA_BLOCK, dh)
    k_mean = jnp.mean(k_blk.astype(jnp.float32), axis=3)
    top = min(MOBA_TOPK, max(nb - 1, 1))
    bi = jnp.arange(B)[:, None, None, None]
    hi = jnp.arange(H)[None, :, None, None]
    scale = dh ** -0.5
    QC = MOBA_QCHUNK

    def chunk(c):
        t0 = c * QC
        qpos = t0 + jnp.arange(QC)
        qc = lax.dynamic_slice_in_dim(q, t0, QC, axis=2)
        own = t0 // MOBA_BLOCK
        gs = jnp.einsum('bhqd,bhnd->bhqn', qc.astype(jnp.float32), k_mean)
        past = jnp.arange(nb) < own
        gs = jnp.where(past, gs, -jnp.inf)
        top_s, sel = lax.top_k(gs, top)
        valid = jnp.isfinite(top_s)
        k_sel = k_blk[bi, hi, sel]
        v_sel = v_blk[bi, hi, sel]
        s_sel = jnp.einsum('bhqd,bhqnkd->bhqnk', qc, k_sel) * scale
        s_sel = s_sel.reshape(B, H, QC, top * MOBA_BLOCK)
        m_sel = jnp.broadcast_to(valid[..., None], (B, H, QC, top, MOBA_BLOCK)).reshape(B, H, QC, top * MOBA_BLOCK)
        k_own = lax.dynamic_slice_in_dim(k, own * MOBA_BLOCK, MOBA_BLOCK, axis=2)
        v_own = lax.dynamic_slice_in_dim(v, own * MOBA_BLOCK, MOBA_BLOCK, axis=2)
        kpos_own = own * MOBA_BLOCK + jnp.arange(MOBA_BLOCK)
        m_own = jnp.broadcast_to(kpos_own[None, :] <= qpos[:, None], (B, H, QC, MOBA_BLOCK))
        s_own = jnp.einsum('bhqd,bhkd->bhqk', qc, k_own) * scale
        p = masked_softmax(jnp.concatenate([s_own, s_sel], -1), jnp.concatenate([m_own, m_sel], -1))
        p = p.astype(v.dtype)
        o = (jnp.einsum('bhqk,bhkd->bhqd', p[..., :MOBA_BLOCK], v_own)
             + jnp.einsum('bhqnk,bhqnkd->bhqd', p[..., MOBA_BLOCK:].reshape(B, H, QC, top, MOBA_BLOCK), v_sel))
        return o

    outs = lax.map(chunk, jnp.arange(S // QC))
    return outs.transpose(1, 0, 3, 2, 4).reshape(B, S, H * dh)


def hier_moe(x, w_coarse, b_coarse, w_fine, b_fine, w_gate, w_up, w_down):
    B, S, D = x.shape
    t = x.reshape(B * S, D)
    T = t.shape[0]
    pc = jax.nn.softmax((t @ w_coarse + b_coarse).astype(jnp.float32), axis=-1)
    gv, gidx = lax.top_k(pc, 1)
    lf = (jnp.einsum('td,gde->tge', t, w_fine) + b_fine).astype(jnp.float32)
    lf_sel = jnp.take_along_axis(lf, gidx[:, :, None], axis=1)[:, 0]
    pf = jax.nn.softmax(lf_sel, axis=-1)
    fv, fidx = lax.top_k(pf, MOE_TOP_IN_GROUP)
    fv = fv / jnp.sum(fv, -1, keepdims=True)
    w = gv * fv
    eid = gidx * MOE_EXPERTS_PER_GROUP + fidx
    gates = jnp.sum(jax.nn.one_hot(eid, MOE_N_EXPERTS, dtype=jnp.float32) * w[..., None], axis=1)
    gates = gates.reshape(T, MOE_GROUPS, MOE_EXPERTS_PER_GROUP).astype(t.dtype)
    y = jnp.zeros_like(t)
    for g in range(MOE_GROUPS):
        hg = jnp.einsum('td,edf->tef', t, w_gate[g])
        hu = jnp.einsum('td,edf->tef', t, w_up[g])
        h = jax.nn.silu(hg) * hu * gates[:, g, :, None]
        y = y + jnp.einsum('tef,efd->td', h, w_down[g])
    return y.reshape(B, S, D)


def setup_inputs(seed: int = 0) -> dict:
    key = jax.random.key(seed)
    ks = jax.random.split(key, 40)
    f32 = jnp.float32
    NE, NO, L = N_EVEN_LAYERS, N_ODD_LAYERS, DEPTH

    def nrm(i, shape, scale):
        return jax.random.normal(ks[i], shape, f32) * scale

    d = D_MODEL
    return {
        'x': nrm(0, (BATCH, SEQ, d), 1.0),
        'ev_w_in': nrm(1, (NE, d, EVEN_IN_WIDTH), d ** -0.5),
        'nsa_pe_k': nrm(2, (NE, CMP_BLOCK, HEAD_DIM), 0.02),
        'nsa_pe_v': nrm(3, (NE, CMP_BLOCK, HEAD_DIM), 0.02),
        'nsa_cmp_k_w1': nrm(4, (NE, CMP_BLOCK * HEAD_DIM, CMP_HIDDEN), (CMP_BLOCK * HEAD_DIM) ** -0.5),
        'nsa_cmp_k_w2': nrm(5, (NE, CMP_HIDDEN, HEAD_DIM), CMP_HIDDEN ** -0.5),
        'nsa_cmp_v_w1': nrm(6, (NE, CMP_BLOCK * HEAD_DIM, CMP_HIDDEN), (CMP_BLOCK * HEAD_DIM) ** -0.5),
        'nsa_cmp_v_w2': nrm(7, (NE, CMP_HIDDEN, HEAD_DIM), CMP_HIDDEN ** -0.5),
        's5_lambda_re': -0.5 + nrm(8, (NE, S5_GROUPS, S5_STATE), 0.01),
        's5_lambda_im': math.pi * jnp.arange(S5_STATE, dtype=f32) + nrm(9, (NE, S5_GROUPS, S5_STATE), 0.01),
        's5_log_step': jax.random.uniform(ks[10], (NE, S5_GROUPS), f32, math.log(1e-3), math.log(1e-1)),
        's5_b_re': nrm(11, (NE, S5_GROUPS, S5_STATE, S5_GROUP_CH), (2 * S5_GROUP_CH) ** -0.5),
        's5_b_im': nrm(12, (NE, S5_GROUPS, S5_STATE, S5_GROUP_CH), (2 * S5_GROUP_CH) ** -0.5),
        's5_c_re': nrm(13, (NE, S5_GROUPS, S5_GROUP_CH, S5_STATE), S5_STATE ** -0.5),
        's5_c_im': nrm(14, (NE, S5_GROUPS, S5_GROUP_CH, S5_STATE), S5_STATE ** -0.5),
        's5_d': nrm(15, (NE, S5_WIDTH), 1.0),
        's5_glu_w': nrm(16, (NE, S5_WIDTH, S5_WIDTH), S5_WIDTH ** -0.5),
        's5_glu_b': nrm(17, (NE, S5_WIDTH), 0.01),
        'ev_w_out': nrm(18, (NE, d, d), DEEPNORM_BETA * d ** -0.5),
        'od_w_in': nrm(19, (NO, d, 3 * d), d ** -0.5),
        'od_w_out': nrm(20, (NO, d, d), DEEPNORM_BETA * d ** -0.5),
        'ln_mix_g': 1.0 + nrm(21, (L, d), 0.01),
        'ln_mix_b': nrm(22, (L, d), 0.01),
        'ln_ffn_g': 1.0 + nrm(23, (L, d), 0.01),
        'ln_ffn_b': nrm(24, (L, d), 0.01),
        'moe_w_coarse': nrm(25, (L, d, MOE_GROUPS), d ** -0.5),
        'moe_b_coarse': nrm(26, (L, MOE_GROUPS), 0.01),
        'moe_w_fine': nrm(27, (L, MOE_GROUPS, d, MOE_EXPERTS_PER_GROUP), d ** -0.5),
        'moe_b_fine': nrm(28, (L, MOE_GROUPS, MOE_EXPERTS_PER_GROUP), 0.01),
        'moe_w_gate': nrm(29, (L, MOE_GROUPS, MOE_EXPERTS_PER_GROUP, d, MOE_FF), d ** -0.5),
        'moe_w_up': nrm(30, (L, MOE_GROUPS, MOE_EXPERTS_PER_GROUP, d, MOE_FF), d ** -0.5),
        'moe_w_down': nrm(31, (L, MOE_GROUPS, MOE_EXPERTS_PER_GROUP, MOE_FF, d), DEEPNORM_BETA * MOE_FF ** -0.5),
    }


def reference(x, ev_w_in, nsa_pe_k, nsa_pe_v, nsa_cmp_k_w1, nsa_cmp_k_w2, nsa_cmp_v_w1, nsa_cmp_v_w2,
              s5_lambda_re, s5_lambda_im, s5_log_step, s5_b_re, s5_b_im, s5_c_re, s5_c_im, s5_d,
              s5_glu_w, s5_glu_b, ev_w_out, od_w_in, od_w_out,
              ln_mix_g, ln_mix_b, ln_ffn_g, ln_ffn_b,
              moe_w_coarse, moe_b_coarse, moe_w_fine, moe_b_fine, moe_w_gate, moe_w_up, moe_w_down):
    S = x.shape[1]
    cos, sin = rope_tables(S)
    for layer in range(DEPTH):
        if layer % 2 == 0:
            e = layer // 2
            xp = x @ ev_w_in[e]
            o_nsa = nsa_mixer(xp[..., :NSA_IN_WIDTH], cos, sin, nsa_pe_k[e], nsa_pe_v[e],
                              nsa_cmp_k_w1[e], nsa_cmp_k_w2[e], nsa_cmp_v_w1[e], nsa_cmp_v_w2[e])
            o_s5 = s5_mixer(xp[..., NSA_IN_WIDTH:], s5_lambda_re[e], s5_lambda_im[e], s5_log_step[e],
                            s5_b_re[e], s5_b_im[e], s5_c_re[e], s5_c_im[e], s5_d[e], s5_glu_w[e], s5_glu_b[e])
            mix = jnp.concatenate([o_nsa, o_s5], axis=-1) @ ev_w_out[e]
        else:
            o = layer // 2
            mix = moba_mixer(x @ od_w_in[o], cos, sin) @ od_w_out[o]
        x = layer_norm(DEEPNORM_ALPHA * x + mix, ln_mix_g[layer], ln_mix_b[layer])
        ffn = hier_moe(x, moe_w_coarse[layer], moe_b_coarse[layer], moe_w_fine[layer], moe_b_fine[layer],
                       moe_w_gate[layer], moe_w_up[layer], moe_w_down[layer])
        x = layer_norm(DEEPNORM_ALPHA * x + ffn, ln_ffn_g[layer], ln_ffn_b[layer])
    return x
```

```python
import contextlib
import math
import numpy as np
import concourse.bass as bass
import concourse.mybir as mybir
from concourse.bass_utils import run_bass_kernel_spmd

F32 = mybir.dt.float32
BF16 = mybir.dt.bfloat16
I32 = mybir.dt.int32
ALU = mybir.AluOpType
AF = mybir.ActivationFunctionType
AX = mybir.AxisListType

S = 4096
D = 1024
NT = 8
ALPHA = 4.0 ** 0.25
EPS = 1e-5
BIG = 240000.0
MAGIC = 12582912.0
TWO_PI = 2.0 * math.pi


class Buf:
    __slots__ = ("name", "last_w", "readers")

    def __init__(self, name=""):
        self.name = name
        self.last_w = None
        self.readers = {}


class T:
    __slots__ = ("t", "buf")

    def __init__(self, t, name=""):
        self.t = t
        self.buf = Buf(name)

    def __getitem__(self, idx):
        return self.t[idx]


class KB:
    NDMA = 48

    def __init__(self, nc, stack):
        self.nc = nc
        self.stack = stack
        self.eng = {"pe": nc.tensor, "act": nc.scalar, "dve": nc.vector,
                    "pool": nc.gpsimd, "sp": nc.sync}
        self.sems = {}
        for e in self.eng:
            self.sems[e] = stack.enter_context(nc.semaphore("s_" + e))
        self.cnt = {e: 0 for e in self.eng}
        self.dsem = [stack.enter_context(nc.semaphore("d%d" % i)) for i in range(self.NDMA)]
        self.dcnt = [0] * self.NDMA
        self.dnext = 0
        self.dnext_sw = 0
        self.waited = {}
        self.n_ins = 0
        self.n_wait = 0
        self._uid = 0

    def sb(self, shape, dtype=F32, name=None):
        self._uid += 1
        name = (name or "t") + "_%d" % self._uid
        t = self.stack.enter_context(self.nc.sbuf_tensor(name, list(shape), dtype))
        return T(t, name)

    def ps(self, shape, dtype=F32, name=None):
        self._uid += 1
        name = (name or "p") + "_%d" % self._uid
        t = self.stack.enter_context(self.nc.psum_tensor(name, list(shape), dtype))
        return T(t, name)

    def _sem(self, key):
        return self.sems[key] if isinstance(key, str) else self.dsem[key]

    def _wait(self, e, ev):
        key, val = ev
        if e == "pe" and key == "pe":
            return
        k = (e, key)
        if self.waited.get(k, 0) >= val:
            return
        self.waited[k] = val
        self.eng[e].wait_ge(self._sem(key), val)
        self.n_wait += 1

    @staticmethod
    def _b(b):
        return b.buf if isinstance(b, T) else b

    def _deps(self, e, reads, writes):
        for b in reads:
            b = self._b(b)
            if b.last_w is not None:
                self._wait(e, b.last_w)
        for b in writes:
            b = self._b(b)
            if b.last_w is not None:
                self._wait(e, b.last_w)
            for k, v in b.readers.items():
                self._wait(e, (k, v))

    def _mark(self, ev, reads, writes):
        key, val = ev
        for b in reads:
            b = self._b(b)
            if b.readers.get(key, 0) < val:
                b.readers[key] = val
        for b in writes:
            b = self._b(b)
            b.last_w = ev
            b.readers = {}

    def op(self, e, fn, reads=(), writes=()):
        self._deps(e, reads, writes)
        ins = fn(self.eng[e])
        self.cnt[e] += 1
        ins.then_inc(self.sems[e], 1)
        self._mark((e, self.cnt[e]), reads, writes)
        self.n_ins += 1
        return ins

    def dma(self, q, out, in_, reads=(), writes=(), **kw):
        self._deps(q, reads, writes)
        half = self.NDMA // 2
        if q == "pool":
            i = half + self.dnext_sw
            self.dnext_sw = (self.dnext_sw + 1) % (self.NDMA - half)
        else:
            i = self.dnext
            self.dnext = (self.dnext + 1) % half
        if self.dcnt[i] > 0:
            self._wait(q, (i, self.dcnt[i]))
        ins = self.eng[q].dma_start(out=out, in_=in_, **kw)
        self.dcnt[i] += 16
        ins.then_inc(self.dsem[i], 16)
        self._mark((i, self.dcnt[i]), reads, writes)
        self.n_ins += 1
        return ins

    def barrier(self):
        for e in self.eng:
            for k in self.eng:
                if self.cnt[k] > 0:
                    self._wait(e, (k, self.cnt[k]))
            for i in range(self.NDMA):
                if self.dcnt[i] > 0:
                    self._wait(e, (i, self.dcnt[i]))

    @contextlib.contextmanager
    def phase(self):
        with contextlib.ExitStack() as ps:
            old = self.stack
            self.stack = ps
            yield
            self.barrier()
            self.stack = old


class Ring:
    def __init__(self, items):
        self.items = items
        self.i = 0

    def next(self):
        it = self.items[self.i % len(self.items)]
        self.i += 1
        return it


def mm(kb, ot, oap, lt, lap, rt, rap, start=True, stop=True):
    kb.op("pe", lambda e: e.matmul(oap, lhsT=lap, rhs=rap, start=start, stop=stop,
                                   skip_group_check=True), [lt, rt], [ot])


def tt(kb, e, ot, oap, at, aap, bt, bap, op):
    kb.op(e, lambda g: g.tensor_tensor(out=oap, in0=aap, in1=bap, op=op), [at, bt], [ot])


def ts(kb, e, ot, oap, at, aap, s1, s2, op0, op1=None, extra=()):
    if op1 is None:
        kb.op(e, lambda g: g.tensor_scalar(out=oap, in0=aap, scalar1=s1, scalar2=None, op0=op0),
              [at] + list(extra), [ot])
    else:
        kb.op(e, lambda g: g.tensor_scalar(out=oap, in0=aap, scalar1=s1, scalar2=s2, op0=op0, op1=op1),
              [at] + list(extra), [ot])


def stt(kb, e, ot, oap, at, aap, sc, bt, bap, op0, op1, extra=()):
    kb.op(e, lambda g: g.scalar_tensor_tensor(out=oap, in0=aap, scalar=sc, in1=bap, op0=op0, op1=op1),
          [at, bt] + list(extra), [ot])


def act(kb, ot, oap, it, iap, func, scale=1.0, bias=None, extra=()):
    if bias is None:
        kb.op("act", lambda g: g.activation(out=oap, in_=iap, func=func, scale=scale), [it] + list(extra), [ot])
    else:
        kb.op("act", lambda g: g.activation(out=oap, in_=iap, func=func, scale=scale, bias=bias),
              [it] + list(extra), [ot])


def cp(kb, e, ot, oap, it, iap):
    if e == "act":
        kb.op("act", lambda g: g.activation(out=oap, in_=iap, func=AF.Copy), [it], [ot])
    elif e == "dve":
        kb.op("dve", lambda g: g.tensor_scalar(out=oap, in0=iap, scalar1=1.0, scalar2=None, op0=ALU.mult), [it], [ot])
    else:
        kb.op(e, lambda g: g.tensor_copy(out=oap, in_=iap), [it], [ot])


def memset(kb, e, ot, oap, val):
    kb.op(e, lambda g: g.memset(oap, val), [], [ot])


def fm(ap):
    return ap.rearrange("(c p) t -> p c t", p=128)


def build_consts(kb, G):
    with kb.phase():
        row = kb.sb([1, 64], F32)
        make_ramp(kb, row, row[:, 0:32], 1, 32)
        make_ramp(kb, row, row[:, 32:64], 1, 32)
        one1 = kb.sb([1, 1], F32)
        memset(kb, "dve", one1, one1[:], 1.0)
        pidx = kb.ps([64, 1], F32)
        mm(kb, pidx, pidx[:], row, row[:], one1, one1[:])
        idx = kb.sb([64, 1], F32)
        cp(kb, "act", idx, idx[:], pidx, pidx[:])
        inv = kb.sb([64, 1], F32)
        act(kb, inv, inv[:], idx, idx[:], AF.Exp, scale=-math.log(10000.0) / 32.0)
        ts(kb, "dve", inv, inv[:], inv, inv[:], 1.0 / TWO_PI, None, ALU.mult)
        tpos = kb.sb([64, S], F32)
        make_ramp(kb, tpos, tpos[:], 64, S)
        r = kb.sb([64, S], F32)
        rr = kb.sb([64, S], F32)
        tab = kb.sb([64, S], F32)
        for which in ("sin", "cos"):
            if which == "sin":
                ts(kb, "dve", r, r[:], tpos, tpos[:], inv[:, 0:1], None, ALU.mult, extra=[inv])
            else:
                ts(kb, "dve", r, r[:], tpos, tpos[:], inv[:, 0:1], None, ALU.mult, extra=[inv])
                ts(kb, "dve", r, r[:], r, r[:], 0.25, None, ALU.add)
            ts(kb, "dve", rr, rr[:], r, r[:], MAGIC, MAGIC, ALU.add, ALU.subtract)
            tt(kb, "dve", r, r[:], r, r[:], rr, rr[:], ALU.subtract)
            act(kb, tab, tab[:], r, r[:], AF.Sin, scale=TWO_PI)
            if which == "sin":
                ts(kb, "dve", tab, tab[0:32, :], tab, tab[0:32, :], -1.0, None, ALU.mult)
                kb.dma("sp", G["ropeS"], tab[:], reads=[tab], writes=[G["b_rope"]])
            else:
                kb.dma("sp", G["ropeC"], tab[:], reads=[tab], writes=[G["b_rope"]])


def linear_fm(kb, G, inT, w_dram, outT, n_in_chunks=8, n_out_chunks=8, in_buf=None, out_buf=None):
    with kb.phase():
        w = kb.sb([128, n_in_chunks, n_out_chunks * 128], BF16)
        kb.dma("pool", w[:], w_dram.rearrange("(c p) n -> p c n", p=128), reads=[], writes=[w])
        xin = Ring([kb.sb([128, n_in_chunks, 512], BF16) for _ in range(2)])
        ob = Ring([kb.sb([128, n_out_chunks, 512], F32) for _ in range(2)])
        pss = Ring([kb.ps([128, 512], F32) for _ in range(4)])
        inv = fm(inT)
        outv = fm(outT)
        for t in range(NT):
            x = xin.next()
            kb.dma("sp", x[:], inv[:, :, t * 512:(t + 1) * 512], reads=[in_buf], writes=[x])
            o = ob.next()
            for f in range(n_out_chunks):
                p = pss.next()
                for c in range(n_in_chunks):
                    mm(kb, p, p[:], w, w[:, c, f * 128:(f + 1) * 128], x, x[:, c, :],
                       start=(c == 0), stop=(c == n_in_chunks - 1))
                cp(kb, "act" if f % 2 == 0 else "dve", o, o[:, f, :], p, p[:])
            kb.dma("sp", outv[:, :, t * 512:(t + 1) * 512], o[:], reads=[o], writes=[out_buf])


def res_ln(kb, G, resT, addT, g_dram, b_dram, outT, res_buf, add_buf, out_buf):
    with kb.phase():
        ones = kb.sb([128, 128], F32)
        memset(kb, "dve", ones, ones[:], 1.0 / D)
        gb = kb.sb([128, 2, 8], F32)
        with kb.nc.allow_non_contiguous_dma(reason="tiny ln params"):
            kb.dma("sp", gb[:, 0, :], g_dram.rearrange("(c p) -> p c", p=128), writes=[gb])
            kb.dma("sp", gb[:, 1, :], b_dram.rearrange("(c p) -> p c", p=128), writes=[gb])
        rin = Ring([kb.sb([128, 8, 512], F32) for _ in range(2)])
        ain = Ring([kb.sb([128, 8, 512], F32) for _ in range(2)])
        zsq = Ring([kb.sb([128, 512], F32) for _ in range(2)])
        oo = Ring([kb.sb([128, 8, 512], F32) for _ in range(2)])
        ps1 = Ring([kb.ps([128, 512], F32) for _ in range(2)])
        ps2 = Ring([kb.ps([128, 512], F32) for _ in range(2)])
        mean = kb.sb([128, 512], F32)
        rstd = kb.sb([128, 512], F32)
        tmp = kb.sb([128, 512], F32)
        rv, av, ov = fm(resT), fm(addT), fm(outT)
        for t in range(NT):
            sl = slice(t * 512, (t + 1) * 512)
            r = rin.next()
            a = ain.next()
            kb.dma("sp", r[:], rv[:, :, sl], reads=[res_buf], writes=[r])
            kb.dma("act", a[:], av[:, :, sl], reads=[add_buf], writes=[a])
            p1, p2 = ps1.next(), ps2.next()
            for c in range(8):
                stt(kb, "dve", r, r[:, c, :], r, r[:, c, :], ALPHA, a, a[:, c, :], ALU.mult, ALU.add)
                z2 = zsq.next()
                act(kb, z2, z2[:], r, r[:, c, :], AF.Square)
                mm(kb, p1, p1[:], ones, ones[:], r, r[:, c, :], start=(c == 0), stop=(c == 7))
                mm(kb, p2, p2[:], ones, ones[:], z2, z2[:], start=(c == 0), stop=(c == 7))
            cp(kb, "act", mean, mean[:], p1, p1[:])
            tt(kb, "dve", tmp, tmp[:], mean, mean[:], mean, mean[:], ALU.mult)
            tt(kb, "dve", tmp, tmp[:], p2, p2[:], tmp, tmp[:], ALU.subtract)
            ts(kb, "dve", tmp, tmp[:], tmp, tmp[:], EPS, None, ALU.add)
            act(kb, tmp, tmp[:], tmp, tmp[:], AF.Sqrt)
            kb.op("dve", lambda g: g.reciprocal(out=rstd[:], in_=tmp[:]), [tmp], [rstd])
            o = oo.next()
            for c in range(8):
                e = "dve" if c % 2 == 0 else "pool"
                tt(kb, e, r, r[:, c, :], r, r[:, c, :], mean, mean[:], ALU.subtract)
                tt(kb, e, r, r[:, c, :], r, r[:, c, :], rstd, rstd[:], ALU.mult)
                ts(kb, e, o, o[:, c, :], r, r[:, c, :], gb[:, 0, c:c + 1], gb[:, 1, c:c + 1], ALU.mult, ALU.add,
                   extra=[gb])
            kb.dma("sp", ov[:, :, sl], o[:], reads=[o], writes=[out_buf])


def inproj(kb, G, xT, x_buf, w_rope, w_swap, n_rope, rope_dst, w_fm, n_fm_chunks, fm_dst, w_tm, tm_cols, tm_cb, tm_splits):
    with kb.phase():
        wr = kb.sb([128, 8, n_rope * 64], BF16)
        ws = kb.sb([128, 8, n_rope * 64], BF16)
        kb.dma("pool", wr[:], w_rope.rearrange("(c p) n -> p c n", p=128), writes=[wr])
        kb.dma("pool", ws[:], w_swap.rearrange("(c p) n -> p c n", p=128), writes=[ws])
        if n_fm_chunks:
            wf = kb.sb([128, 8, n_fm_chunks * 128], BF16)
            kb.dma("pool", wf[:], w_fm.rearrange("(c p) n -> p c n", p=128), writes=[wf])
        wt = kb.sb([128, 8, tm_cols], BF16)
        kb.dma("pool", wt[:], w_tm.rearrange("(c p) n -> p c n", p=128), writes=[wt])
        xin = Ring([kb.sb([128, 8, 512], BF16) for _ in range(2)])
        cc = Ring([kb.sb([64, 512], F32) for _ in range(2)])
        ss = Ring([kb.sb([64, 512], F32) for _ in range(2)])
        pa = Ring([kb.ps([64, 512], F32) for _ in range(2)])
        pb = Ring([kb.ps([64, 512], F32) for _ in range(2)])
        pf = Ring([kb.ps([128, 512], F32) for _ in range(2)])
        ntm = len(tm_splits)
        pt = [kb.ps([128, 512], F32) for _ in range(ntm)]
        t1 = Ring([kb.sb([64, 512], F32) for _ in range(2)])
        t2 = Ring([kb.sb([64, 512], F32) for _ in range(2)])
        ro = Ring([kb.sb([64, 512], BF16) for _ in range(3)])
        fo = Ring([kb.sb([128, 512], BF16) for _ in range(2)])
        xv = fm(xT)
        import os
        SK = os.environ.get("INPROJ_SKIP", "")
        for t in range(NT):
            sl = slice(t * 512, (t + 1) * 512)
            x = xin.next()
            kb.dma("pool", x[:], xv[:, :, sl], reads=[x_buf], writes=[x])
            c_, s_ = cc.next(), ss.next()
            kb.dma("sp", c_[:], G["ropeC"][:, sl], reads=[G["b_rope"]], writes=[c_])
            kb.dma("sp", s_[:], G["ropeS"][:, sl], reads=[G["b_rope"]], writes=[s_])
            for h in range(0 if "r" in SK else n_rope):
                a, b = pa.next(), pb.next()
                for c in range(8):
                    mm(kb, a, a[:], wr, wr[:, c, h * 64:(h + 1) * 64], x, x[:, c, :], start=(c == 0), stop=(c == 7))
                for c in range(8):
                    mm(kb, b, b[:], ws, ws[:, c, h * 64:(h + 1) * 64], x, x[:, c, :], start=(c == 0), stop=(c == 7))
                u1, u2, o = t1.next(), t2.next(), ro.next()
                tt(kb, "dve", u1, u1[:], a, a[:], c_, c_[:], ALU.mult)
                tt(kb, "dve", u2, u2[:], b, b[:], s_, s_[:], ALU.mult)
                tt(kb, "dve" if "p" in SK else "pool", o, o[:], u1, u1[:], u2, u2[:], ALU.add)
                dst, dbuf = rope_dst(h)
                kb.dma("sp", dst[:, sl], o[:], reads=[o], writes=[dbuf])
            for f in range(0 if "f" in SK else n_fm_chunks):
                p = pf.next()
                for c in range(8):
                    mm(kb, p, p[:], wf, wf[:, c, f * 128:(f + 1) * 128], x, x[:, c, :], start=(c == 0), stop=(c == 7))
                o = fo.next()
                cp(kb, "act", o, o[:], p, p[:])
                dst, dbuf = fm_dst(f)
                kb.dma("sp", dst[:, sl], o[:], reads=[o], writes=[dbuf])
            for sub in range(0 if "t" in SK else 4):
                for j in range(ntm):
                    n0, n1 = tm_splits[j]
                    for c in range(8):
                        mm(kb, pt[j], pt[j][:, 0:n1 - n0], x, x[:, c, sub * 128:(sub + 1) * 128], wt, wt[:, c, n0:n1],
                           start=(c == 0), stop=(c == 7))
                tm_cb(kb, pt, t * 4 + sub)


def moe(kb, G, xT, x_buf, L, outT, out_buf):
    wr_d, br_d = G["moe_wr"][L], G["moe_br"][L]
    wg_d, wu_d, wd_d = G["moe_wg"][L], G["moe_wu"][L], G["moe_wd"][L]
    xv = fm(xT)
    with kb.phase():
        gT = kb.sb([32, S], BF16)
        with contextlib.ExitStack() as rs:
            old = kb.stack
            kb.stack = rs
            wr = kb.sb([128, 8, 36], F32)
            kb.dma("sp", wr[:], wr_d.rearrange("(c p) n -> p c n", p=128), writes=[wr])
            br = kb.sb([1, 36], F32)
            kb.dma("sp", br[:], br_d, writes=[br])
            ones1 = kb.sb([1, 128], F32)
            memset(kb, "dve", ones1, ones1[:], 1.0)
            ident = kb.sb([128, 128], F32)
            memset(kb, "dve", ident, ident[:], 1.0)
            kb.op("pool", lambda g: g.affine_select(out=ident[:], in_=ident[:], pattern=[[-1, 128]],
                                                    compare_op=ALU.is_equal, fill=0.0, base=0, channel_multiplier=1),
                  [ident], [ident])
            xin = Ring([kb.sb([128, 8, 512], F32) for _ in range(2)])
            pl = Ring([kb.ps([128, 36], F32) for _ in range(2)])
            ptr = Ring([kb.ps([32, 128], F32) for _ in range(2)])
            sm = Ring([kb.sb([128, 160], F32) for _ in range(2)])
            gt = Ring([kb.sb([128, 32], F32) for _ in range(2)])
            for t in range(NT):
                x = xin.next()
                kb.dma("sp", x[:], xv[:, :, t * 512:(t + 1) * 512], reads=[x_buf], writes=[x])
                for sub in range(4):
                    p = pl.next()
                    for c in range(8):
                        mm(kb, p, p[:], x, x[:, c, sub * 128:(sub + 1) * 128], wr, wr[:, c, :], start=(c == 0), stop=False)
                    mm(kb, p, p[:], ones1, ones1[:], br, br[:], start=False, stop=True)
                    w = sm.next()
                    cp(kb, "act", w, w[:, 0:36], p, p[:])
                    kb.op("dve", lambda g: g.tensor_reduce(out=w[:, 36:37], in_=w[:, 0:4], axis=AX.X, op=ALU.max), [w], [w])
                    ts(kb, "dve", w, w[:, 37:38], w, w[:, 36:37], -1.0, None, ALU.mult)
                    act(kb, w, w[:, 38:42], w, w[:, 0:4], AF.Exp, bias=w[:, 37:38])
                    kb.op("dve", lambda g: g.tensor_reduce(out=w[:, 42:43], in_=w[:, 38:42], axis=AX.X, op=ALU.add), [w], [w])
                    kb.op("dve", lambda g: g.reciprocal(out=w[:, 43:44], in_=w[:, 42:43]), [w], [w])
                    ts(kb, "dve", w, w[:, 44:48], w, w[:, 0:4], w[:, 36:37], None, ALU.is_equal)
                    tt(kb, "dve", w, w[:, 48:80].rearrange("p (g e) -> p g e", g=4),
                       w, w[:, 4:36].rearrange("p (g e) -> p g e", g=4),
                       w, w[:, 44:48].unsqueeze(2).to_broadcast([128, 4, 8]), ALU.mult)
                    kb.op("dve", lambda g: g.tensor_reduce(out=w[:, 80:88], in_=w[:, 48:80].rearrange("p (g e) -> p e g", g=4),
                                                           axis=AX.X, op=ALU.add), [w], [w])
                    kb.op("dve", lambda g: g.max(out=w[:, 88:96], in_=w[:, 80:88]), [w], [w])
                    tt(kb, "dve", w, w[:, 96:97], w, w[:, 88:89], w, w[:, 89:90], ALU.subtract)
                    act(kb, w, w[:, 97:98], w, w[:, 96:97], AF.Sigmoid)
                    tt(kb, "dve", w, w[:, 98:99], w, w[:, 97:98], w, w[:, 43:44], ALU.mult)
                    tt(kb, "dve", w, w[:, 99:100], w, w[:, 43:44], w, w[:, 98:99], ALU.subtract)
                    ts(kb, "dve", w, w[:, 100:108], w, w[:, 80:88], w[:, 88:89], w[:, 98:99], ALU.is_equal, ALU.mult)
                    ts(kb, "dve", w, w[:, 108:116], w, w[:, 80:88], w[:, 89:90], w[:, 99:100], ALU.is_equal, ALU.mult)
                    tt(kb, "dve", w, w[:, 116:124], w, w[:, 100:108], w, w[:, 108:116], ALU.add)
                    gg = gt.next()
                    tt(kb, "dve", gg, gg[:].rearrange("p (g e) -> p g e", g=4),
                       w, w[:, 44:48].unsqueeze(2).to_broadcast([128, 4, 8]),
                       w, w[:, 116:124].unsqueeze(1).to_broadcast([128, 4, 8]), ALU.mult)
                    pT = ptr.next()
                    kb.op("pe", lambda e: e.transpose(pT[:], gg[:], ident[:]), [gg, ident], [pT])
                    q0 = (t * 4 + sub) * 128
                    cp(kb, "act", gT, gT[:, q0:q0 + 128], pT, pT[:])
            kb.barrier()
            kb.stack = old
        sel = kb.sb([32, 32, 128], BF16)
        memset(kb, "dve", sel, sel[:], 1.0)
        kb.op("pool", lambda g: g.affine_select(out=sel[:], in_=sel[:], pattern=[[-1, 32], [0, 128]],
                                                compare_op=ALU.is_equal, fill=0.0, base=0, channel_multiplier=1),
              [sel], [sel])
        TS = 2048
        xb = kb.sb([128, 8, TS], BF16)
        yacc = kb.sb([128, 8, TS], F32)
        hq = kb.sb([128, 4, TS], BF16)
        wgr = Ring([kb.sb([128, 8, 128], BF16) for _ in range(2)])
        wur = Ring([kb.sb([128, 8, 128], BF16) for _ in range(2)])
        wdr = Ring([kb.sb([128, 1024], BF16) for _ in range(8)])
        pg = Ring([kb.ps([128, 512], F32) for _ in range(2)])
        pu = Ring([kb.ps([128, 512], F32) for _ in range(2)])
        pc = Ring([kb.ps([128, 512], F32) for _ in range(2)])
        py = Ring([kb.ps([128, 512], F32) for _ in range(2)])
        sl_ = Ring([kb.sb([128, 512], F32) for _ in range(2)])
        t1_ = Ring([kb.sb([128, 512], F32) for _ in range(2)])
        ov = fm(outT)
        for st in range(S // TS):
            kb.dma("pool", xb[:], xv[:, :, st * TS:(st + 1) * TS], reads=[x_buf], writes=[xb])
            for q in range(8):
                wds = []
                for ei in range(4):
                    e = q * 4 + ei
                    wg, wu, wd = wgr.next(), wur.next(), wdr.next()
                    kb.dma("pool", wg[:], wg_d[e].rearrange("(c p) n -> p c n", p=128), writes=[wg])
                    kb.dma("pool", wu[:], wu_d[e].rearrange("(c p) n -> p c n", p=128), writes=[wu])
                    kb.dma("pool", wd[:], wd_d[e], writes=[wd])
                    wds.append(wd)
                    for tq in range(TS // 512):
                        tsl = slice(tq * 512, (tq + 1) * 512)
                        a, b, c_ = pg.next(), pu.next(), pc.next()
                        for c in range(8):
                            mm(kb, a, a[:], wg, wg[:, c, :], xb, xb[:, c, tsl], start=(c == 0), stop=(c == 7))
                        for c in range(8):
                            mm(kb, b, b[:], wu, wu[:, c, :], xb, xb[:, c, tsl], start=(c == 0), stop=(c == 7))
                        g0 = st * TS + tq * 512
                        mm(kb, c_, c_[:], sel, sel[:, e, :], gT, gT[:, g0:g0 + 512])
                        s1, u1 = sl_.next(), t1_.next()
                        act(kb, s1, s1[:], a, a[:], AF.Silu)
                        tt(kb, "dve", u1, u1[:], c_, c_[:], s1, s1[:], ALU.mult)
                        tt(kb, "dve", hq, hq[:, ei, tsl], b, b[:], u1, u1[:], ALU.mult)
                for tq in range(TS // 512):
                    tsl = slice(tq * 512, (tq + 1) * 512)
                    for f in range(8):
                        y = py.next()
                        for ei in range(4):
                            mm(kb, y, y[:], wds[ei], wds[ei][:, f * 128:(f + 1) * 128], hq, hq[:, ei, tsl],
                               start=(ei == 0), stop=(ei == 3))
                        if q == 0:
                            cp(kb, "act", yacc, yacc[:, f, tsl], y, y[:])
                        else:
                            tt(kb, "pool" if False else "dve", yacc, yacc[:, f, tsl], y, y[:], yacc, yacc[:, f, tsl], ALU.add)
            kb.dma("sp", ov[:, :, st * TS:(st + 1) * TS], yacc[:], reads=[yacc], writes=[out_buf])


def sched_causal(Q):
    out = []
    for kt in range(4 * Q + 4):
        r = kt - 4 * Q
        out.append((kt, max(0, r), 4, [(r, "tri")] if r >= 0 else [], None))
    return out


def sched_win(Q):
    out = []
    for kt in range(max(0, 4 * Q - 2), 4 * Q + 4):
        r = kt - 4 * Q
        s0, s1 = max(0, r), min(3, r + 2) + 1
        m = []
        if r >= 0:
            m.append((r, "tri"))
        if 0 <= r + 2 <= 3:
            m.append((r + 2, "ntri"))
        out.append((kt, s0, s1, m, None))
    return out


def sched_cmp(Q):
    out = [(0, 0, 4, [], "cmp")]
    if Q >= 4:
        out.append((1, 0, 4, [], "cmp"))
    return out


class AttnCtx:
    def __init__(self, kb, need_cmp):
        self.kb = kb
        self.tri = kb.sb([128, 128], BF16)
        self.ntri = kb.sb([128, 128], BF16)
        memset(kb, "dve", self.tri, self.tri[:], 1.0)
        memset(kb, "dve", self.ntri, self.ntri[:], 1.0)
        kb.op("pool", lambda g: g.affine_select(out=self.tri[:], in_=self.tri[:], pattern=[[1, 128]],
                                                compare_op=ALU.is_ge, fill=0.0, base=0, channel_multiplier=-1),
              [self.tri], [self.tri])
        kb.op("pool", lambda g: g.affine_select(out=self.ntri[:], in_=self.ntri[:], pattern=[[-1, 128]],
                                                compare_op=ALU.is_gt, fill=0.0, base=0, channel_multiplier=1),
              [self.ntri], [self.ntri])
        self.cmpm = None
        if need_cmp:
            self.cmpm = kb.sb([128, 2, S], BF16)
            memset(kb, "dve", self.cmpm, self.cmpm[:], 1.0)
            for j in range(2):
                kb.op("pool", lambda g, j=j: g.affine_select(out=self.cmpm[:, j, :], in_=self.cmpm[:, j, :], pattern=[[1, S]],
                                                             compare_op=ALU.is_ge, fill=0.0, base=-31 - 2048 * j,
                                                             channel_multiplier=-16), [self.cmpm], [self.cmpm])
        self.zl = kb.sb([128, 128], BF16)
        self.zr = kb.sb([128, 512], BF16)
        memset(kb, "dve", self.zl, self.zl[:], 0.0)
        memset(kb, "dve", self.zr, self.zr[:], 0.0)
        self.psS = Ring([kb.ps([128, 512], F32) for _ in range(3)])
        self.psO = Ring([kb.ps([128, 4, 128], F32) for _ in range(3)])
        self.pT = Ring([kb.sb([128, 512], BF16) for _ in range(3)])
        self.den = Ring([kb.sb([128, 4], F32) for _ in range(3)])
        self.coef = Ring([kb.sb([128, 4], F32) for _ in range(3)])
        self.tmp = Ring([kb.sb([128, 4, 64], F32) for _ in range(2)])
        self.mask_eng = Ring(["pool", "dve"])


def attn_branch(A, Kc, q_t, k_t, v_t, nkeys_last, sched, Q, acc_t, first, gate_t=None, gate_ap=None):
    kb = A.kb
    po = A.psO.next()
    mm(kb, po, po[:].rearrange("p a b -> p (a b)"), A.zl, A.zl[:], A.zr, A.zr[:], start=True, stop=True)
    for (kt, s0, s1, masks, full) in sched(Q):
        ksz = 128
        if nkeys_last is not None and (kt + 1) * 128 > nkeys_last:
            ksz = nkeys_last - kt * 128
        c0, c1 = s0 * 128, s1 * 128
        ps = A.psS.next()
        mm(kb, ps, ps[0:ksz, c0:c1], k_t, k_t[0:Kc, kt * 128:kt * 128 + ksz], q_t, q_t[0:Kc, Q * 512 + c0:Q * 512 + c1])
        p = A.pT.next()
        act(kb, p, p[0:ksz, c0:c1], ps, ps[0:ksz, c0:c1], AF.Exp, scale=0.125)
        for (s, name) in masks:
            m = A.tri if name == "tri" else A.ntri
            tt(kb, A.mask_eng.next(), p, p[0:ksz, s * 128:(s + 1) * 128], p, p[0:ksz, s * 128:(s + 1) * 128], m, m[0:ksz, :], ALU.mult)
        if full == "cmp":
            tt(kb, A.mask_eng.next(), p, p[0:ksz, c0:c1], p, p[0:ksz, c0:c1], A.cmpm, A.cmpm[0:ksz, kt, Q * 512 + c0:Q * 512 + c1], ALU.mult)
        for s in range(s0, s1):
            mm(kb, po, po[:, s, 0:65], p, p[0:ksz, s * 128:(s + 1) * 128], v_t, v_t[0:ksz, kt, :], start=False, stop=False)
    den, coef = A.den.next(), A.coef.next()
    ts(kb, "dve", den, den[:], po, po[:, :, 64], 1e-30, None, ALU.max)
    kb.op("dve", lambda g: g.reciprocal(out=coef[:], in_=den[:]), [den], [coef])
    if gate_t is not None:
        tt(kb, "dve", coef, coef[:], coef, coef[:], gate_t, gate_ap, ALU.mult)
    cb = coef[:].unsqueeze(2).to_broadcast([128, 4, 64])
    if first:
        tt(kb, "dve", acc_t, acc_t[:], po, po[:, :, 0:64], coef, cb, ALU.mult)
    else:
        tmp = A.tmp.next()
        tt(kb, "dve", tmp, tmp[:], po, po[:, :, 0:64], coef, cb, ALU.mult)
        tt(kb, "pool", acc_t, acc_t[:], acc_t, acc_t[:], tmp, tmp[:], ALU.add)


def make_ramp(kb, t, ap, parts, n, start=0.0):
    ones = kb.sb([parts, n], F32)
    memset(kb, "dve", ones, ones[:], 1.0)
    kb.op("dve", lambda g: g.tensor_tensor_scan(out=ap, data0=ones[:], data1=ones[:], initial=float(start) - 1.0,
                                                op0=ALU.mult, op1=ALU.add), [ones], [t])


def make_ident(kb, dtype=F32):
    ident = kb.sb([128, 128], dtype)
    memset(kb, "dve", ident, ident[:], 1.0)
    kb.op("pool", lambda g: g.affine_select(out=ident[:], in_=ident[:], pattern=[[-1, 128]],
                                            compare_op=ALU.is_equal, fill=0.0, base=0, channel_multiplier=1),
          [ident], [ident])
    return ident


def gelu_tanh(kb, z, zap, out_t, out_ap, shape, tmp1, tmp2):
    a1 = tmp1[tuple(slice(0, s) for s in shape)]
    a2 = tmp2[tuple(slice(0, s) for s in shape)]
    act(kb, tmp1, a1, z, zap, AF.Square)
    ts(kb, "dve", tmp1, a1, tmp1, a1, 0.044715, 1.0, ALU.mult, ALU.add)
    tt(kb, "dve", tmp1, a1, tmp1, a1, z, zap, ALU.mult)
    act(kb, tmp2, a2, tmp1, a1, AF.Sigmoid, scale=1.5957691216057308)
    tt(kb, "dve", out_t, out_ap, z, zap, tmp2, a2, ALU.mult)


def nsa_compress(kb, G):
    with kb.phase():
        ident = make_ident(kb)
        pps = kb.ps([64, 32], F32)
        pb = kb.ps([128, 1], F32)
        ph = kb.ps([128, 256], F32)
        pk = kb.ps([64, 256], F32)
        pv = kb.ps([128, 64], F32)
        for which in ("k", "v"):
            w1_d, w2_d, pe_d = G["cmp_%s_w1" % which], G["cmp_%s_w2" % which], G["pe_%s" % which]
            w1 = kb.sb([128, 32, 128], BF16)
            w1v = w1_d.rearrange("(l d) f -> d l f", d=64)
            kb.dma("pool", w1[0:64], w1v, writes=[w1])
            kb.dma("pool", w1[64:128], w1v, writes=[w1])
            w2 = kb.sb([128, 64], BF16)
            kb.dma("pool", w2[:], w2_d, writes=[w2])
            pe = kb.sb([32, 64], F32)
            kb.dma("sp", pe[:], pe_d, writes=[pe])
            kb.op("pe", lambda e: e.transpose(pps[:], pe[:], ident[0:32, 0:32]), [pe, ident], [pps])
            peT = kb.sb([64, 32], BF16)
            cp(kb, "act", peT, peT[:], pps, pps[:])
            for l in range(32):
                mm(kb, pb, pb[:], w1, w1[0:64, l, :], peT, peT[:, l:l + 1], start=(l == 0), stop=(l == 31))
            bias = kb.sb([128, 1], F32)
            cp(kb, "act", bias, bias[:], pb, pb[:])
            src = kb.sb([128, S], BF16)
            if which == "k":
                kb.dma("sp", src[0:64], G["kcT"][0], reads=[G["b_qk0"]], writes=[src])
                kb.dma("sp", src[64:128], G["kcT"][1], reads=[G["b_qk0"]], writes=[src])
            else:
                kb.dma("sp", src[:], G["vcT"], reads=[G["b_qk0"]], writes=[src])
            for g in range(2):
                for l in range(32):
                    mm(kb, ph, ph[:, 0:255], w1, w1[g * 64:(g + 1) * 64, l, :], src, src[g * 64:(g + 1) * 64, l:l + 4065:16],
                       start=(l == 0), stop=(l == 31))
                z = kb.sb([128, 255], F32)
                act(kb, z, z[:], ph, ph[:, 0:255], AF.Identity, bias=bias[:, 0:1], extra=[bias])
                hid = kb.sb([128, 256], BF16)
                t1, t2 = kb.sb([128, 255], F32), kb.sb([128, 255], F32)
                gelu_tanh(kb, z, z[:], hid, hid[:, 0:255], (128, 255), t1, t2)
                if which == "k":
                    mm(kb, pk, pk[:, 0:255], w2, w2[:], hid, hid[:, 0:255])
                    kc = kb.sb([64, 256], BF16)
                    memset(kb, "dve", kc, kc[:], 0.0)
                    cp(kb, "act", kc, kc[:, 0:255], pk, pk[:, 0:255])
                    kb.dma("sp", G["kcmpT"][g], kc[:], reads=[kc], writes=[G["b_cmp"]])
                else:
                    vcs = kb.sb([128, 2, 65], BF16)
                    memset(kb, "dve", vcs, vcs[:], 0.0)
                    memset(kb, "dve", vcs, vcs[:, :, 64:65], 1.0)
                    for j in range(2):
                        nsz = 128 if j == 0 else 127
                        mm(kb, pv, pv[0:nsz, :], hid, hid[:, j * 128:j * 128 + nsz], w2, w2[:])
                        cp(kb, "act", vcs, vcs[0:nsz, j, 0:64], pv, pv[0:nsz, :])
                    kb.dma("sp", G["vcmp"][g], vcs[:], reads=[vcs], writes=[G["b_cmp"]])


def nsa_select(kb, G):
    with kb.phase():
        identb = make_ident(kb, BF16)
        val = kb.sb([128, 128], F32)
        make_ramp(kb, val, val[:], 128, 128, start=-64.0)
        ts(kb, "dve", val, val[64:128, :], val, val[64:128, :], -1.0, None, ALU.add)
        KM, AM, fut = kb.sb([128, 128], F32), kb.sb([128, 128], F32), kb.sb([128, 128], F32)
        ts(kb, "dve", KM, KM[:], val, val[:], -2.0, None, ALU.is_le)
        ts(kb, "dve", fut, fut[:], val, val[:], 1.0, None, ALU.is_ge)
        ts(kb, "dve", AM, AM[:], KM, KM[:], -1000.0, 1000.0, ALU.mult, ALU.add)
        stt(kb, "dve", AM, AM[:], fut, fut[:], -1001.0, AM, AM[:], ALU.mult, ALU.add)
        Mc = kb.sb([128, 8], F32)
        memset(kb, "dve", Mc, Mc[:], 1.0)
        kb.op("pool", lambda g: g.affine_select(out=Mc[:], in_=Mc[:], pattern=[[-16, 8]], compare_op=ALU.is_ge,
                                                fill=0.0, base=-15, channel_multiplier=1), [Mc], [Mc])
        q4 = kb.sb([64, 4, S], BF16)
        kcm = kb.sb([64, 256], BF16)
        negT = kb.sb([64, S], BF16)
        psc = Ring([kb.ps([128, 4, 256], F32) for _ in range(2)])
        ptr = Ring([kb.ps([64, 128], BF16) for _ in range(2)])
        pcs = Ring([kb.sb([128, 4, 256], F32) for _ in range(2)])
        Psum = Ring([kb.sb([128, 256], F32) for _ in range(2)])
        sm = Ring([kb.sb([128, 160], F32) for _ in range(2)])
        sc = Ring([kb.sb([128, 64], F32) for _ in range(2)])
        ngb = Ring([kb.sb([128, 64], BF16) for _ in range(2)])
        for g in range(2):
            for z in range(4):
                kb.dma("sp", q4[:, z, :], G["qaug0"][g * 4 + z, 0:64, :], reads=[G["b_qk0"]], writes=[q4])
            kb.dma("sp", kcm[:], G["kcmpT"][g], reads=[G["b_cmp"]], writes=[kcm])
            for it in pcs.items + Psum.items:
                memset(kb, "pool", it, it[:], 0.0)
            for c in range(32):
                ncv = min(255, 8 * c + 7)
                p = psc.next()
                for z in range(4):
                    mm(kb, p, p[:, z, 0:ncv], q4, q4[:, z, c * 128:(c + 1) * 128], kcm, kcm[:, 0:ncv])
                pc = pcs.next()
                act(kb, pc, pc[:, :, 0:ncv], p, p[:, :, 0:ncv], AF.Exp, scale=0.125)
                if c == 0:
                    tt(kb, "dve", pc, pc[:, :, 0:7], pc, pc[:, :, 0:7], Mc, Mc[:, 1:8].unsqueeze(1).to_broadcast([128, 4, 7]), ALU.mult)
                else:
                    tt(kb, "dve", pc, pc[:, :, ncv - 8:ncv], pc, pc[:, :, ncv - 8:ncv], Mc,
                       Mc[:, 0:8].unsqueeze(1).to_broadcast([128, 4, 8]), ALU.mult)
                w = sm.next()
                kb.op("dve", lambda e: e.tensor_reduce(out=w[:, 0:4], in_=pc[:, :, 0:ncv], axis=AX.X, op=ALU.add), [pc], [w])
                ts(kb, "dve", w, w[:, 4:8], w, w[:, 0:4], 1e-30, None, ALU.max)
                kb.op("dve", lambda e: e.reciprocal(out=w[:, 8:12], in_=w[:, 4:8]), [w], [w])
                P_ = Psum.next()
                ts(kb, "dve", P_, P_[:, 0:ncv], pc, pc[:, 0, 0:ncv], w[:, 8:9], None, ALU.mult, extra=[w])
                for z in range(1, 4):
                    stt(kb, "dve", P_, P_[:, 0:ncv], pc, pc[:, z, 0:ncv], w[:, 8 + z:9 + z], P_, P_[:, 0:ncv], ALU.mult, ALU.add,
                        extra=[w])
                s_ = sc.next()
                kb.op("dve", lambda e: e.tensor_reduce(out=w[:, 16:80], in_=P_[:].rearrange("p (j r) -> p j r", r=4),
                                                       axis=AX.X, op=ALU.add), [P_], [w])
                stt(kb, "dve", s_, s_[:], P_, P_[:, 3:256:4], -0.5, w, w[:, 16:80], ALU.mult, ALU.add)
                stt(kb, "dve", s_, s_[:, 1:64], P_, P_[:, 3:252:4], 0.5, s_, s_[:, 1:64], ALU.mult, ALU.add)
                tt(kb, "dve", s_, s_[:], s_, s_[:], KM, KM[:, 64 - 2 * c:128 - 2 * c], ALU.mult)
                tt(kb, "dve", s_, s_[:], s_, s_[:], AM, AM[:, 64 - 2 * c:128 - 2 * c], ALU.add)
                memset(kb, "dve", s_, s_[:, 0:1], 1000.0)
                kb.op("dve", lambda e: e.max(out=w[:, 80:88], in_=s_[:]), [s_], [w])
                ts(kb, "dve", w, w[:, 88:89], w, w[:, 87:88], 0.0, None, ALU.max)
                ts(kb, "dve", s_, s_[:], s_, s_[:], w[:, 88:89], None, ALU.is_ge, extra=[w])
                nb = ngb.next()
                ts(kb, "dve", nb, nb[:], s_, s_[:], -1.0, BIG, ALU.add, ALU.mult)
                pT = ptr.next()
                kb.op("pe", lambda e: e.transpose(pT[:], nb[:], identb[:]), [nb, identb], [pT])
                cp(kb, "act", negT, negT[:, c * 128:(c + 1) * 128], pT, pT[:])
            for z in range(4):
                kb.dma("sp", G["qaug0"][g * 4 + z, 64:128, :], negT[:], reads=[negT], writes=[G["b_neg0"]])


def nsa_attn(kb, G):
    with kb.phase():
        A = AttnCtx(kb, need_cmp=True)
        identb = make_ident(kb, BF16)
        Ec = kb.sb([128, S], BF16)
        memset(kb, "dve", Ec, Ec[:], 1.0)
        kb.op("pool", lambda g: g.affine_select(out=Ec[64:128, :], in_=Ec[64:128, :], pattern=[[1, S]], compare_op=ALU.is_ge,
                                                fill=0.0, base=0, channel_multiplier=-64), [Ec], [Ec])
        kb.op("pool", lambda g: g.affine_select(out=Ec[64:128, :], in_=Ec[64:128, :], pattern=[[-1, S]], compare_op=ALU.is_ge,
                                                fill=0.0, base=63, channel_multiplier=64), [Ec], [Ec])
        gates = kb.sb([128, 32, 24], F32)
        kb.dma("sp", gates[:], G["gates"].rearrange("(c p) n -> p c n", p=128), reads=[G["b_qk0"]], writes=[gates])
        ks = kb.sb([128, S], BF16)
        kw = kb.sb([64, S], BF16)
        kcm = kb.sb([64, 256], BF16)
        vs = kb.sb([128, 32, 65], BF16)
        vw = kb.sb([128, 32, 65], BF16)
        vcm = kb.sb([128, 2, 65], BF16)
        qr = Ring([kb.sb([128, S], BF16) for _ in range(2)])
        accr = Ring([kb.sb([128, 4, 64], F32) for _ in range(2)])
        otok = kb.sb([128, 8, 4, 128], BF16)
        ptr = Ring([kb.ps([128, 4, 128], BF16) for _ in range(2)])
        oT = Ring([kb.sb([128, 512], BF16) for _ in range(2)])
        v3v = G["v3"].rearrange("(kt p) b c -> p kt b c", p=128)
        for hp in range(4):
            g = hp // 2
            if hp % 2 == 0:
                kb.dma("sp", ks[0:64], G["ksT"][g], reads=[G["b_qk0"]], writes=[ks])
                cp(kb, "pool", ks, ks[64:128, :], Ec, Ec[64:128, :])
                kb.dma("sp", kw[:], G["kwT"][g], reads=[G["b_qk0"]], writes=[kw])
                kb.dma("sp", kcm[:], G["kcmpT"][g], reads=[G["b_cmp"]], writes=[kcm])
                kb.dma("sp", vs[:], v3v[:, :, 2 + g, :], reads=[G["b_qk0"]], writes=[vs])
                kb.dma("sp", vw[:], v3v[:, :, 4 + g, :], reads=[G["b_qk0"]], writes=[vw])
                kb.dma("sp", vcm[:], G["vcmp"][g], reads=[G["b_cmp"]], writes=[vcm])
            for hh in range(2):
                h = hp * 2 + hh
                q = qr.next()
                kb.dma("sp", q[:], G["qaug0"][h], reads=[G["b_qk0"], G["b_neg0"]], writes=[q])
                for Q in range(8):
                    acc = accr.next()
                    attn_branch(A, 64, q, kcm, vcm, 255, sched_cmp, Q, acc, True, gates, gates[:, 4 * Q:4 * Q + 4, h * 3 + 0])
                    attn_branch(A, 128, q, ks, vs, None, sched_causal, Q, acc, False, gates, gates[:, 4 * Q:4 * Q + 4, h * 3 + 1])
                    attn_branch(A, 64, q, kw, vw, None, sched_win, Q, acc, False, gates, gates[:, 4 * Q:4 * Q + 4, h * 3 + 2])
                    cp(kb, "act", otok, otok[:, Q, :, hh * 64:(hh + 1) * 64], acc, acc[:])
            for Q in range(8):
                pT = ptr.next()
                for s in range(4):
                    kb.op("pe", lambda e, s=s: e.transpose(pT[:, s, :], otok[:, Q, s, :], identb[:]), [otok, identb], [pT])
                o = oT.next()
                cp(kb, "dve", o, o[:].rearrange("p (s q) -> p s q", s=4), pT, pT[:])
                kb.dma("sp", G["o0T"][hp * 128:(hp + 1) * 128, Q * 512:(Q + 1) * 512], o[:], reads=[o], writes=[G["b_o0"]])


def sincos_from_rev(kb, r, rap, shape, tmp, out_sin, out_sin_ap, out_cos, out_cos_ap):
    a1 = tmp[0][tuple(slice(0, s) for s in shape)]
    a2 = tmp[1][tuple(slice(0, s) for s in shape)]
    ts(kb, "dve", tmp[0], a1, r, rap, MAGIC, MAGIC, ALU.add, ALU.subtract)
    tt(kb, "dve", tmp[0], a1, r, rap, tmp[0], a1, ALU.subtract)
    act(kb, out_sin, out_sin_ap, tmp[0], a1, AF.Sin, scale=TWO_PI)
    ts(kb, "dve", tmp[1], a2, r, rap, 0.25, None, ALU.add)
    ts(kb, "dve", tmp[0], a1, tmp[1], a2, MAGIC, MAGIC, ALU.add, ALU.subtract)
    tt(kb, "dve", tmp[0], a1, tmp[1], a2, tmp[0], a1, ALU.subtract)
    act(kb, out_cos, out_cos_ap, tmp[0], a1, AF.Sin, scale=TWO_PI)


def s5(kb, G):
    with kb.phase():
        ident = make_ident(kb)
        identb = make_ident(kb, BF16)
        par = kb.sb([128, 4, 16], F32)
        with contextlib.ExitStack() as rs:
            old = kb.stack
            kb.stack = rs
            lr, li = kb.sb([16, 128], F32), kb.sb([16, 128], F32)
            kb.dma("sp", lr[:], G["s5_lre"].rearrange("(s w) p -> s (w p)", w=2), writes=[lr])
            kb.dma("sp", li[:], G["s5_lim"].rearrange("(s w) p -> s (w p)", w=2), writes=[li])
            ls = kb.sb([16, 2], F32)
            kb.dma("sp", ls[:], G["s5_ls"].rearrange("(s w) -> s w", w=2), writes=[ls])
            act(kb, ls, ls[:], ls, ls[:], AF.Exp)
            W = [kb.sb([16, 128], F32) for _ in range(12)]
            stepb, mag, thr, sn, cs, ar1, ai, den, fr, fi, ta, tb = W
            cp(kb, "dve", stepb, stepb[:].rearrange("s (w p) -> s w p", w=2), ls, ls[:].unsqueeze(2).to_broadcast([16, 2, 64]))
            tt(kb, "dve", ta, ta[:], lr, lr[:], stepb, stepb[:], ALU.mult)
            act(kb, mag, mag[:], ta, ta[:], AF.Exp)
            tt(kb, "dve", thr, thr[:], li, li[:], stepb, stepb[:], ALU.mult)
            ts(kb, "dve", thr, thr[:], thr, thr[:], 1.0 / TWO_PI, None, ALU.mult)
            sincos_from_rev(kb, thr, thr[:], (16, 128), (ta, tb), sn, sn[:], cs, cs[:])
            tt(kb, "dve", ai, ai[:], mag, mag[:], sn, sn[:], ALU.mult)
            tt(kb, "dve", ar1, ar1[:], mag, mag[:], cs, cs[:], ALU.mult)
            ts(kb, "dve", ar1, ar1[:], ar1, ar1[:], -1.0, None, ALU.add)
            tt(kb, "dve", ta, ta[:], lr, lr[:], lr, lr[:], ALU.mult)
            tt(kb, "dve", tb, tb[:], li, li[:], li, li[:], ALU.mult)
            tt(kb, "dve", den, den[:], ta, ta[:], tb, tb[:], ALU.add)
            kb.op("dve", lambda e: e.reciprocal(out=den[:], in_=den[:]), [den], [den])
            tt(kb, "dve", ta, ta[:], ar1, ar1[:], lr, lr[:], ALU.mult)
            tt(kb, "dve", tb, tb[:], ai, ai[:], li, li[:], ALU.mult)
            tt(kb, "dve", fr, fr[:], ta, ta[:], tb, tb[:], ALU.add)
            tt(kb, "dve", fr, fr[:], fr, fr[:], den, den[:], ALU.mult)
            tt(kb, "dve", ta, ta[:], ai, ai[:], lr, lr[:], ALU.mult)
            tt(kb, "dve", tb, tb[:], ar1, ar1[:], li, li[:], ALU.mult)
            tt(kb, "dve", fi, fi[:], ta, ta[:], tb, tb[:], ALU.subtract)
            tt(kb, "dve", fi, fi[:], fi, fi[:], den, den[:], ALU.mult)
            pp = kb.ps([128, 4, 16], F32)
            for i, src in enumerate((mag, thr, fr, fi)):
                kb.op("pe", lambda e, i=i, src=src: e.transpose(pp[:, i, :], src[:], ident[0:16, 0:16]), [src, ident], [pp])
            cp(kb, "act", par, par[:], pp, pp[:])
            kb.barrier()
            kb.stack = old
        BTr, BTi = kb.sb([128, 4, 128], BF16), kb.sb([128, 4, 128], BF16)
        CTr, CTi = kb.sb([128, 4, 128], BF16), kb.sb([128, 4, 128], BF16)
        with contextlib.ExitStack() as rs:
            old = kb.stack
            kb.stack = rs
            br, bi = kb.sb([128, 16, 16], F32), kb.sb([128, 16, 16], F32)
            kb.dma("sp", br[:], G["s5_bre"].rearrange("(s w) p c -> (w p) s c", w=2), writes=[br])
            kb.dma("sp", bi[:], G["s5_bim"].rearrange("(s w) p c -> (w p) s c", w=2), writes=[bi])
            frb = par[:, 2, :].unsqueeze(2).to_broadcast([128, 16, 16])
            fib = par[:, 3, :].unsqueeze(2).to_broadcast([128, 16, 16])
            t1, t2 = kb.sb([128, 16, 16], F32), kb.sb([128, 16, 16], F32)
            Bb = [kb.sb([128, 16, 16], F32), kb.sb([128, 16, 16], F32)]
            tt(kb, "dve", t1, t1[:], br, br[:], par, frb, ALU.mult)
            tt(kb, "dve", t2, t2[:], bi, bi[:], par, fib, ALU.mult)
            tt(kb, "dve", Bb[0], Bb[0][:], t1, t1[:], t2, t2[:], ALU.subtract)
            tt(kb, "dve", t1, t1[:], bi, bi[:], par, frb, ALU.mult)
            tt(kb, "dve", t2, t2[:], br, br[:], par, fib, ALU.mult)
            tt(kb, "dve", Bb[1], Bb[1][:], t1, t1[:], t2, t2[:], ALU.add)
            for ri, dstT in ((0, BTr), (1, BTi)):
                BD = kb.sb([128, 16, 32], BF16)
                memset(kb, "dve", BD, BD[:], 0.0)
                cp(kb, "dve", BD, BD[0:64, :, 0:16], Bb[ri], Bb[ri][0:64])
                cp(kb, "dve", BD, BD[64:128, :, 16:32], Bb[ri], Bb[ri][64:128])
                pT = kb.ps([128, 4, 128], BF16)
                for j in range(4):
                    kb.op("pe", lambda e, j=j: e.transpose(pT[:, j, :], BD[:, 4 * j:4 * j + 4, :].rearrange("p a b -> p (a b)"), identb[:]),
                          [BD, identb], [pT])
                cp(kb, "act", dstT, dstT[:], pT, pT[:])
            m0, m1 = kb.sb([128, 1], F32), kb.sb([128, 1], F32)
            memset(kb, "dve", m0, m0[:], 0.0)
            memset(kb, "dve", m1, m1[:], 1.0)
            for q in range(4):
                memset(kb, "dve", m0, m0[32 * q:32 * q + 16, :], 1.0)
                memset(kb, "dve", m1, m1[32 * q:32 * q + 16, :], 0.0)
            for ri, dstT, key in ((0, CTr, "s5_cre"), (1, CTi, "s5_cim")):
                ct = kb.sb([128, 4, 64], F32)
                kb.dma("sp", ct[:], G[key].rearrange("g c p -> (g c) p").rearrange("(j r) p -> r j p", r=128), writes=[ct])
                BD = kb.sb([128, 4, 128], BF16)
                ts(kb, "dve", BD, BD[:, :, 0:64], ct, ct[:], m0[:, 0:1], None, ALU.mult, extra=[m0])
                ts(kb, "dve", BD, BD[:, :, 64:128], ct, ct[:], m1[:, 0:1], None, ALU.mult, extra=[m1])
                pT = kb.ps([128, 4, 128], BF16)
                for j in range(4):
                    kb.op("pe", lambda e, j=j: e.transpose(pT[:, j, :], BD[:, j, :], identb[:]), [BD, identb], [pT])
                if ri == 0:
                    cp(kb, "act", dstT, dstT[:], pT, pT[:])
                else:
                    ts(kb, "dve", dstT, dstT[:], pT, pT[:], -1.0, None, ALU.mult)
            kb.barrier()
            kb.stack = old
        with contextlib.ExitStack() as rs:
            old = kb.stack
            kb.stack = rs
            uT = kb.sb([128, 4, S], BF16)
            kb.dma("sp", uT[:], fm(G["uT"]), reads=[G["b_qk0"]], writes=[uT])
            uTa = kb.sb([32, 4, S], BF16)
            cp(kb, "pool", uTa, uTa[:], uT, uT[96:128])
            BTra, BTia = kb.sb([32, 4, 128], BF16), kb.sb([32, 4, 128], BF16)
            cp(kb, "pool", BTra, BTra[:], BTr, BTr[96:128])
            cp(kb, "pool", BTia, BTia[:], BTi, BTi[96:128])
            jt = kb.sb([128, 513], F32)
            make_ramp(kb, jt, jt[:], 128, 513)
            rT = kb.sb([128, 513], F32)
            tmpA, tmpB = kb.sb([128, 513], F32), kb.sb([128, 513], F32)
            cTr = Ring([kb.sb([128, 513], F32) for _ in range(2)])
            sTr = Ring([kb.sb([128, 513], F32) for _ in range(2)])
            pbr = Ring([kb.ps([128, 512], F32) for _ in range(2)])
            pbi = Ring([kb.ps([128, 512], F32) for _ in range(2)])
            pyr = Ring([kb.ps([128, 4, 32], F32) for _ in range(2)])
            E = [Ring([kb.sb([128, 512], F32) for _ in range(2)]) for _ in range(6)]
            zrr = Ring([kb.sb([128, 512], F32) for _ in range(2)])
            zir = Ring([kb.sb([128, 512], F32) for _ in range(2)])
            xrr = Ring([kb.sb([128, 512], BF16) for _ in range(2)])
            xir = Ring([kb.sb([128, 512], BF16) for _ in range(2)])
            car = Ring([kb.sb([128, 4], F32) for _ in range(2)])
            ysr = Ring([kb.sb([128, 4, 32], F32) for _ in range(2)])
            yv = G["ytm"].rearrange("(c p) f -> p c f", p=128)
            for st in range(16):
                ts(kb, "dve", rT, rT[:], jt, jt[:], par[:, 1, st:st + 1], None, ALU.mult, extra=[par])
                cT, sT = cTr.next(), sTr.next()
                sincos_from_rev(kb, rT, rT[:], (128, 513), (tmpA, tmpB), sT, sT[:], cT, cT[:])
                magb = par[:, 0, st:st + 1].to_broadcast([128, 512])
                po = (st % 4) * 32
                prev = None
                for ck in range(8):
                    a, b = pbr.next(), pbi.next()
                    csl = slice(ck * 512, (ck + 1) * 512)
                    if st % 4 == 3:
                        mm(kb, a, a[:], BTra, BTra[0:32, st // 4, :], uTa, uTa[0:32, st // 4, csl])
                        mm(kb, b, b[:], BTia, BTia[0:32, st // 4, :], uTa, uTa[0:32, st // 4, csl])
                    else:
                        mm(kb, a, a[:], BTr, BTr[po:po + 32, st // 4, :], uT, uT[po:po + 32, st // 4, csl])
                        mm(kb, b, b[:], BTi, BTi[po:po + 32, st // 4, :], uT, uT[po:po + 32, st // 4, csl])
                    e = [r.next() for r in E]
                    tt(kb, "dve", e[0], e[0][:], a, a[:], cT, cT[:, 0:512], ALU.mult)
                    tt(kb, "dve", e[1], e[1][:], b, b[:], sT, sT[:, 0:512], ALU.mult)
                    tt(kb, "pool", e[0], e[0][:], e[0], e[0][:], e[1], e[1][:], ALU.add)
                    tt(kb, "dve", e[2], e[2][:], b, b[:], cT, cT[:, 0:512], ALU.mult)
                    tt(kb, "dve", e[3], e[3][:], a, a[:], sT, sT[:, 0:512], ALU.mult)
                    tt(kb, "pool", e[2], e[2][:], e[2], e[2][:], e[3], e[3][:], ALU.subtract)
                    zr, zi = zrr.next(), zir.next()
                    if prev is None:
                        i0, i1, ex = 0.0, 0.0, []
                    else:
                        pzr, pzi = prev
                        ca = car.next()
                        ts(kb, "dve", ca, ca[:, 0:1], pzi, pzi[:, 511:512], sT[:, 512:513], None, ALU.mult, extra=[sT])
                        stt(kb, "dve", ca, ca[:, 1:2], pzr, pzr[:, 511:512], cT[:, 512:513], ca, ca[:, 0:1], ALU.mult, ALU.subtract,
                            extra=[cT])
                        ts(kb, "dve", ca, ca[:, 2:3], pzi, pzi[:, 511:512], cT[:, 512:513], None, ALU.mult, extra=[cT])
                        stt(kb, "dve", ca, ca[:, 3:4], pzr, pzr[:, 511:512], sT[:, 512:513], ca, ca[:, 2:3], ALU.mult, ALU.add,
                            extra=[sT])
                        i0, i1, ex = ca[:, 1:2], ca[:, 3:4], [ca]
                    kb.op("dve", lambda g, i0=i0: g.tensor_tensor_scan(out=zr[:], data0=magb, data1=e[0][:], initial=i0,
                                                                       op0=ALU.mult, op1=ALU.add), [e[0], par] + ex, [zr])
                    kb.op("dve", lambda g, i1=i1: g.tensor_tensor_scan(out=zi[:], data0=magb, data1=e[2][:], initial=i1,
                                                                       op0=ALU.mult, op1=ALU.add), [e[2], par] + ex, [zi])
                    prev = (zr, zi)
                    xr, xi = xrr.next(), xir.next()
                    tt(kb, "pool", e[4], e[4][:], zr, zr[:], cT, cT[:, 0:512], ALU.mult)
                    tt(kb, "pool", e[5], e[5][:], zi, zi[:], sT, sT[:, 0:512], ALU.mult)
                    tt(kb, "pool", xr, xr[:], e[4], e[4][:], e[5], e[5][:], ALU.subtract)
                    tt(kb, "dve", e[1], e[1][:], zr, zr[:], sT, sT[:, 0:512], ALU.mult)
                    tt(kb, "pool", e[3], e[3][:], zi, zi[:], cT, cT[:, 0:512], ALU.mult)
                    tt(kb, "pool", xi, xi[:], e[1], e[1][:], e[3], e[3][:], ALU.add)
                    yp = pyr.next()
                    for tq in range(4):
                        mm(kb, yp, yp[:, tq, :], xr, xr[:, tq * 128:(tq + 1) * 128], CTr, CTr[:, st // 4, po:po + 32], start=True, stop=False)
                        mm(kb, yp, yp[:, tq, :], xi, xi[:, tq * 128:(tq + 1) * 128], CTi, CTi[:, st // 4, po:po + 32], start=False, stop=True)
                    ys = ysr.next()
                    cp(kb, "act", ys, ys[:], yp, yp[:])
                    kb.dma("sp", yv[:, ck * 4:ck * 4 + 4, st * 32:(st + 1) * 32], ys[:], reads=[ys], writes=[G["b_y"]])
            kb.barrier()
            kb.stack = old
        dbc = kb.sb([128, 512], F32)
        kb.dma("sp", dbc[:], G["s5_d"].to_broadcast([128, 512]), writes=[dbc])
        glw = kb.sb([128, 4, 512], BF16)
        kb.dma("pool", glw[:], G["s5_glw"].rearrange("(c p) n -> p c n", p=128), writes=[glw])
        glb = kb.sb([128, 4], F32)
        with kb.nc.allow_non_contiguous_dma(reason="tiny"):
            kb.dma("sp", glb[:], G["s5_glb"].rearrange("(c p) -> p c", p=128), writes=[glb])
        yr = Ring([kb.sb([128, 512], F32) for _ in range(2)])
        ur = Ring([kb.sb([128, 512], F32) for _ in range(2)])
        g1, g2 = kb.sb([128, 512], F32), kb.sb([128, 512], F32)
        gbr = Ring([kb.sb([128, 512], BF16) for _ in range(2)])
        pT = Ring([kb.ps([128, 4, 128], BF16) for _ in range(2)])
        gTr = Ring([kb.sb([128, 4, 128], BF16) for _ in range(2)])
        pz = Ring([kb.ps([128, 128], F32) for _ in range(2)])
        sgr = Ring([kb.sb([128, 128], F32) for _ in range(2)])
        osr = Ring([kb.sb([128, 4, 128], BF16) for _ in range(2)])
        ytv = G["ytm"].rearrange("(c p) f -> p c f", p=128)
        utv = G["utm"].rearrange("(c p) f -> p c f", p=128)
        o0v = G["o0T"][512:1024, :].rearrange("(c p) t -> p c t", p=128)
        for tk in range(32):
            y, u = yr.next(), ur.next()
            kb.dma("sp", y[:], ytv[:, tk, :], reads=[G["b_y"]], writes=[y])
            kb.dma("act", u[:], utv[:, tk, :], reads=[G["b_qk0"]], writes=[u])
            tt(kb, "dve", u, u[:], u, u[:], dbc, dbc[:], ALU.mult)
            tt(kb, "dve", y, y[:], y, y[:], u, u[:], ALU.add)
            gb_ = gbr.next()
            gelu_tanh(kb, y, y[:], gb_, gb_[:], (128, 512), g1, g2)
            p = pT.next()
            for c in range(4):
                kb.op("pe", lambda e, c=c: e.transpose(p[:, c, :], gb_[:, c * 128:(c + 1) * 128], identb[:]), [gb_, identb], [p])
            gT = gTr.next()
            cp(kb, "act", gT, gT[:], p, p[:])
            os_ = osr.next()
            for fo in range(4):
                z = pz.next()
                for c in range(4):
                    mm(kb, z, z[:], glw, glw[:, c, fo * 128:(fo + 1) * 128], gT, gT[:, c, :], start=(c == 0), stop=(c == 3))
                sg = sgr.next()
                act(kb, sg, sg[:], z, z[:], AF.Sigmoid, bias=glb[:, fo:fo + 1], extra=[glb])
                tt(kb, "dve", os_, os_[:, fo, :], gT, gT[:, fo, :], sg, sg[:], ALU.mult)
            kb.dma("sp", o0v[:, :, tk * 128:(tk + 1) * 128], os_[:], reads=[os_], writes=[G["b_o0"]])


def moba_select(kb, G):
    with kb.phase():
        identb = make_ident(kb, BF16)
        val = kb.sb([128, 32, 16], F32)
        nv = kb.sb([128, 16], F32)
        make_ramp(kb, nv, nv[:], 128, 16)
        tt(kb, "dve", val, val[:].rearrange("p (a b) n -> p a b n", b=2),
           nv, nv[:].unsqueeze(1).unsqueeze(1).to_broadcast([128, 16, 2, 16]),
           nv, nv[:].unsqueeze(2).unsqueeze(3).to_broadcast([128, 16, 2, 16]), ALU.subtract)
        addm, notown = kb.sb([128, 32, 16], F32), kb.sb([128, 32, 16], F32)
        ts(kb, "dve", addm, addm[:], val, val[:], 0.0, -1e30, ALU.is_ge, ALU.mult)
        ts(kb, "dve", notown, notown[:], val, val[:], 0.0, None, ALU.not_equal)
        qr = Ring([kb.sb([64, S], BF16) for _ in range(2)])
        kr = Ring([kb.sb([64, S], BF16) for _ in range(2)])
        km = Ring([kb.sb([64, 16], F32) for _ in range(2)])
        kmb = Ring([kb.sb([64, 16], BF16) for _ in range(2)])
        pg = Ring([kb.ps([128, 32, 16], F32) for _ in range(2)])
        gs = Ring([kb.sb([128, 32, 16], F32) for _ in range(2)])
        eq = kb.sb([128, 32, 16], F32)
        mx = Ring([kb.sb([128, 32], F32) for _ in range(2)])
        ngr = Ring([kb.sb([128, 32, 16], BF16) for _ in range(2)])
        ptr = Ring([kb.ps([16, 8, 128], BF16) for _ in range(2)])
        ngT = Ring([kb.sb([16, S], BF16) for _ in range(2)])
        for h in range(16):
            q, k = qr.next(), kr.next()
            kb.dma("sp", q[:], G["qaug1"][h, 0:64, :], reads=[G["b_qk1"]], writes=[q])
            kb.dma("act", k[:], G["kaug1"][h, 0:64, :], reads=[G["b_qk1"]], writes=[k])
            m_, mb = km.next(), kmb.next()
            kb.op("dve", lambda e: e.tensor_reduce(out=m_[:], in_=k[:].rearrange("p (n j) -> p n j", j=256), axis=AX.X, op=ALU.add),
                  [k], [m_])
            ts(kb, "dve", mb, mb[:], m_, m_[:], 1.0 / 256.0, None, ALU.mult)
            p = pg.next()
            for c in range(32):
                mm(kb, p, p[:, c, :], q, q[:, c * 128:(c + 1) * 128], mb, mb[:])
            g_ = gs.next()
            tt(kb, "dve", g_, g_[:], p, p[:], addm, addm[:], ALU.add)
            m = mx.next()
            for it in range(3):
                kb.op("dve", lambda e: e.tensor_reduce(out=m[:], in_=g_[:], axis=AX.X, op=ALU.max), [g_], [m])
                if it < 2:
                    tt(kb, "dve", eq, eq[:], g_, g_[:], m, m[:].unsqueeze(2).to_broadcast([128, 32, 16]), ALU.is_equal)
                    stt(kb, "dve", g_, g_[:], eq, eq[:], -1e30, g_, g_[:], ALU.mult, ALU.add)
            ts(kb, "dve", m, m[:], m, m[:], -1e29, None, ALU.max)
            tt(kb, "dve", eq, eq[:], p, p[:], addm, addm[:], ALU.add)
            tt(kb, "dve", eq, eq[:], eq, eq[:], m, m[:].unsqueeze(2).to_broadcast([128, 32, 16]), ALU.is_ge)
            ts(kb, "dve", eq, eq[:], eq, eq[:], -1.0, BIG, ALU.add, ALU.mult)
            ng = ngr.next()
            tt(kb, "dve", ng, ng[:], eq, eq[:], notown, notown[:], ALU.mult)
            nT = ngT.next()
            for c8 in range(4):
                pT = ptr.next()
                for j in range(8):
                    c = c8 * 8 + j
                    kb.op("pe", lambda e, c=c, j=j: e.transpose(pT[:, j, :], ng[:, c, :], identb[:]), [ng, identb], [pT])
                cp(kb, "act", nT, nT[:, c8 * 1024:(c8 + 1) * 1024].rearrange("p (j q) -> p j q", j=8), pT, pT[:])
            kb.dma("sp", G["qaug1"][h, 64:80, :], nT[:], reads=[nT], writes=[G["b_neg1"]])


def moba_attn(kb, G):
    with kb.phase():
        A = AttnCtx(kb, need_cmp=False)
        identb = make_ident(kb, BF16)
        Ec = kb.sb([128, S], BF16)
        memset(kb, "dve", Ec, Ec[:], 1.0)
        kb.op("pool", lambda g: g.affine_select(out=Ec[64:80, :], in_=Ec[64:80, :], pattern=[[1, S]], compare_op=ALU.is_ge,
                                                fill=0.0, base=0, channel_multiplier=-256), [Ec], [Ec])
        kb.op("pool", lambda g: g.affine_select(out=Ec[64:80, :], in_=Ec[64:80, :], pattern=[[-1, S]], compare_op=ALU.is_ge,
                                                fill=0.0, base=255, channel_multiplier=256), [Ec], [Ec])
        qr = Ring([kb.sb([80, S], BF16) for _ in range(2)])
        kr = Ring([kb.sb([80, S], BF16) for _ in range(2)])
        vr = Ring([kb.sb([128, 32, 65], BF16) for _ in range(2)])
        accr = Ring([kb.sb([128, 4, 64], F32) for _ in range(2)])
        otok = kb.sb([128, 8, 4, 128], BF16)
        ptr = Ring([kb.ps([128, 4, 128], BF16) for _ in range(2)])
        oT = Ring([kb.sb([128, 512], BF16) for _ in range(2)])
        v1v = G["v1"].rearrange("(kt p) b c -> p kt b c", p=128)
        for hp in range(8):
            for hh in range(2):
                h = hp * 2 + hh
                q, k, v = qr.next(), kr.next(), vr.next()
                kb.dma("sp", q[:], G["qaug1"][h], reads=[G["b_qk1"], G["b_neg1"]], writes=[q])
                kb.dma("act", k[0:64], G["kaug1"][h, 0:64, :], reads=[G["b_qk1"]], writes=[k])
                cp(kb, "pool", k, k[64:80, :], Ec, Ec[64:80, :])
                kb.dma("sp", v[:], v1v[:, :, h, :], reads=[G["b_qk1"]], writes=[v])
                for Q in range(8):
                    acc = accr.next()
                    attn_branch(A, 80, q, k, v, None, sched_causal, Q, acc, True)
                    cp(kb, "act", otok, otok[:, Q, :, hh * 64:(hh + 1) * 64], acc, acc[:])
            for Q in range(8):
                pT = ptr.next()
                for s in range(4):
                    kb.op("pe", lambda e, s=s: e.transpose(pT[:, s, :], otok[:, Q, s, :], identb[:]), [otok, identb], [pT])
                o = oT.next()
                cp(kb, "dve", o, o[:].rearrange("p (s q) -> p s q", s=4), pT, pT[:])
                kb.dma("sp", G["o1T"][hp * 128:(hp + 1) * 128, Q * 512:(Q + 1) * 512], o[:], reads=[o], writes=[G["b_o1"]])


def build_program(debug=False, upto=99):
    nc = bass.Bass("TRN2", target_bir_lowering=False)
    G = {}

    def din(name, shape, dt=F32):
        return nc.dram_tensor(name, list(shape), dt, kind="ExternalInput").ap()

    def scr(name, shape, dt=F32, out=False):
        isout = out or (debug and (debug is True or name in debug))
        return nc.dram_tensor(name, list(shape), dt, kind="ExternalOutput" if isout else "Internal").ap()

    xT = din("xT", [D, S])
    w0r, w0s = din("w0r", [D, 14 * 64]), din("w0s", [D, 14 * 64])
    w0f, w0t = din("w0f", [D, 5 * 128]), din("w0t", [D, 920])
    w1r, w1s, w1t = din("w1r", [D, 32 * 64]), din("w1s", [D, 32 * 64]), din("w1t", [D, 1024])
    for wh in ("k", "v"):
        G["cmp_%s_w1" % wh] = din("cmp_%s_w1" % wh, [2048, 128])
        G["cmp_%s_w2" % wh] = din("cmp_%s_w2" % wh, [128, 64])
        G["pe_%s" % wh] = din("pe_%s" % wh, [32, 64])
    G["s5_lre"], G["s5_lim"], G["s5_ls"] = din("s5_lre", [32, 64]), din("s5_lim", [32, 64]), din("s5_ls", [32])
    G["s5_bre"], G["s5_bim"] = din("s5_bre", [32, 64, 16]), din("s5_bim", [32, 64, 16])
    G["s5_cre"], G["s5_cim"] = din("s5_cre", [32, 16, 64]), din("s5_cim", [32, 16, 64])
    G["s5_d"], G["s5_glw"], G["s5_glb"] = din("s5_d", [1, 512]), din("s5_glw", [512, 512]), din("s5_glb", [512])
    ev_wo, od_wo = din("ev_wo", [D, D]), din("od_wo", [D, D])
    ln = {k: din(k, [2, D]) for k in ("ln_mix_g", "ln_mix_b", "ln_ffn_g", "ln_ffn_b")}
    G["moe_wr"] = [din("moe_wr%d" % L, [D, 36]) for L in range(2)]
    G["moe_br"] = [din("moe_br%d" % L, [1, 36]) for L in range(2)]
    G["moe_wg"] = [din("moe_wg%d" % L, [32, D, 128]) for L in range(2)]
    G["moe_wu"] = [din("moe_wu%d" % L, [32, D, 128]) for L in range(2)]
    G["moe_wd"] = [din("moe_wd%d" % L, [32, 128, D]) for L in range(2)]
    yT = scr("yT", [D, S], out=True)
    G["ropeC"], G["ropeS"] = scr("ropeC", [64, S]), scr("ropeS", [64, S])
    G["qaug0"] = scr("qaug0", [8, 128, S], BF16)
    G["ksT"], G["kwT"], G["kcT"] = scr("ksT", [2, 64, S], BF16), scr("kwT", [2, 64, S], BF16), scr("kcT", [2, 64, S], BF16)
    G["vcT"], G["uT"] = scr("vcT", [128, S], BF16), scr("uT", [512, S], BF16)
    G["v3"] = scr("v3", [S, 6, 65], BF16)
    G["gates"], G["utm"], G["ytm"] = scr("gates", [S, 24]), scr("utm", [S, 512]), scr("ytm", [S, 512])
    G["kcmpT"], G["vcmp"] = scr("kcmpT", [2, 64, 256], BF16), scr("vcmp", [2, 128, 2, 65], BF16)
    G["o0T"], G["o1T"] = scr("o0T", [D, S], BF16), scr("o1T", [D, S], BF16)
    mixT = scr("mixT", [D, S])
    x1T, x2T, x3T = scr("x1T", [D, S]), scr("x2T", [D, S]), scr("x3T", [D, S])
    G["qaug1"], G["kaug1"] = scr("qaug1", [16, 80, S], BF16), scr("kaug1", [16, 64, S], BF16)
    G["v1"] = scr("v1", [S, 16, 65], BF16)
    for b in ("b_rope", "b_qk0", "b_cmp", "b_neg0", "b_o0", "b_y", "b_qk1", "b_neg1", "b_o1"):
        G[b] = Buf(b)
    bx, bmix, b1, b2, b3, by = Buf(), Buf(), Buf(), Buf(), Buf(), Buf()

    with contextlib.ExitStack() as st:
        kb = KB(nc, st)
        build_consts(kb, G)
        v3r = Ring([kb.sb([128, 6, 65], BF16) for _ in range(2)])
        gtr = Ring([kb.sb([128, 24], F32) for _ in range(2)])
        utr = Ring([kb.sb([128, 512], F32) for _ in range(2)])
        for it in v3r.items:
            memset(kb, "dve", it, it[:], 1.0)

        def rope_dst0(h):
            if h < 8:
                return G["qaug0"][h, 0:64, :], G["b_qk0"]
            g = (h - 8) % 2
            return (G["kcT"], G["ksT"], G["kwT"])[(h - 8) // 2][g], G["b_qk0"]

        def fm_dst0(c):
            if c == 0:
                return G["vcT"], G["b_qk0"]
            return G["uT"][(c - 1) * 128:c * 128, :], G["b_qk0"]

        def tm_cb0(kb, pt, ti):
            import os
            TM = os.environ.get("TM_PARTS", "vgu")
            v, g_, u = v3r.next(), gtr.next(), utr.next()
            rs = slice(ti * 128, (ti + 1) * 128)
            if "v" in TM:
                cp(kb, "act", v, v[:, :, 0:64], pt[0], pt[0][:, 0:384].rearrange("p (b c) -> p b c", b=6))
                kb.dma("sp", G["v3"][rs], v[:], reads=[v], writes=[G["b_qk0"]])
            if "g" in TM:
                act(kb, g_, g_[:], pt[0], pt[0][:, 384:408], AF.Sigmoid)
                kb.dma("sp", G["gates"][rs], g_[:], reads=[g_], writes=[G["b_qk0"]])
            if "u" in TM:
                cp(kb, "dve", u, u[:], pt[1], pt[1][:])
                kb.dma("sp", G["utm"][rs], u[:], reads=[u], writes=[G["b_qk0"]])

        if upto >= 1:
            inproj(kb, G, xT, bx, w0r, w0s, 14, rope_dst0, w0f, 5, fm_dst0, w0t, 920, tm_cb0, [(0, 408), (408, 920)])
        if upto >= 2:
            nsa_compress(kb, G)
            nsa_select(kb, G)
        if upto >= 3:
            nsa_attn(kb, G)
        if upto >= 4:
            s5(kb, G)
        if upto >= 5:
            linear_fm(kb, G, G["o0T"], ev_wo, mixT, in_buf=G["b_o0"], out_buf=bmix)
            res_ln(kb, G, xT, mixT, ln["ln_mix_g"][0], ln["ln_mix_b"][0], x1T, bx, bmix, b1)
            moe(kb, G, x1T, b1, 0, mixT, bmix)
            res_ln(kb, G, x1T, mixT, ln["ln_ffn_g"][0], ln["ln_ffn_b"][0], x2T, b1, bmix, b2)
        v1r = Ring([kb.sb([128, 16, 65], BF16) for _ in range(2)])
        for it in v1r.items:
            memset(kb, "dve", it, it[:], 1.0)

        def rope_dst1(h):
            if h < 16:
                return G["qaug1"][h, 0:64, :], G["b_qk1"]
            return G["kaug1"][h - 16], G["b_qk1"]

        def tm_cb1(kb, pt, ti):
            v = v1r.next()
            cp(kb, "act", v, v[:, 0:8, 0:64], pt[0], pt[0][:].rearrange("p (b c) -> p b c", b=8))
            cp(kb, "dve", v, v[:, 8:16, 0:64], pt[1], pt[1][:].rearrange("p (b c) -> p b c", b=8))
            kb.dma("sp", G["v1"][ti * 128:(ti + 1) * 128], v[:], reads=[v], writes=[G["b_qk1"]])

        if upto >= 6:
            inproj(kb, G, x2T, b2, w1r, w1s, 32, rope_dst1, None, 0, None, w1t, 1024, tm_cb1, [(0, 512), (512, 1024)])
            moba_select(kb, G)
            moba_attn(kb, G)
        if upto >= 7:
            linear_fm(kb, G, G["o1T"], od_wo, mixT, in_buf=G["b_o1"], out_buf=bmix)
            res_ln(kb, G, x2T, mixT, ln["ln_mix_g"][1], ln["ln_mix_b"][1], x3T, b2, bmix, b3)
            moe(kb, G, x3T, b3, 1, mixT, bmix)
            res_ln(kb, G, x3T, mixT, ln["ln_ffn_g"][1], ln["ln_ffn_b"][1], yT, b3, bmix, by)
        kb.barrier()
        print("instructions", kb.n_ins, "waits", kb.n_wait, flush=True)
    return nc


def _swap_cols(w, nh):
    w = w.reshape(w.shape[0], nh, 2, 32)
    return np.ascontiguousarray(w[:, :, ::-1, :].reshape(w.shape[0], nh * 64))


def prep_weights(inp):
    c = np.ascontiguousarray
    W = inp["ev_w_in"][0]
    q, kc, vc, ks, vs, kw, vw, gl, u = np.split(W, [512, 640, 768, 896, 1024, 1152, 1280, 1304], axis=1)
    m = {}
    rope = np.concatenate([q, kc, ks, kw], axis=1)
    m["w0r"] = c(rope)
    m["w0s"] = _swap_cols(rope, 14)
    m["w0f"] = c(np.concatenate([vc, u], axis=1))
    m["w0t"] = c(np.concatenate([vc, vs, vw, gl, u], axis=1))
    W1 = inp["od_w_in"][0]
    m["w1r"] = c(W1[:, 0:2048])
    m["w1s"] = _swap_cols(W1[:, 0:2048], 32)
    m["w1t"] = c(W1[:, 2048:3072])
    m["cmp_k_w1"], m["cmp_k_w2"], m["pe_k"] = c(inp["nsa_cmp_k_w1"][0]), c(inp["nsa_cmp_k_w2"][0]), c(inp["nsa_pe_k"][0])
    m["cmp_v_w1"], m["cmp_v_w2"], m["pe_v"] = c(inp["nsa_cmp_v_w1"][0]), c(inp["nsa_cmp_v_w2"][0]), c(inp["nsa_pe_v"][0])
    m["s5_lre"], m["s5_lim"], m["s5_ls"] = c(inp["s5_lambda_re"][0]), c(inp["s5_lambda_im"][0]), c(inp["s5_log_step"][0])
    m["s5_bre"], m["s5_bim"] = c(inp["s5_b_re"][0]), c(inp["s5_b_im"][0])
    m["s5_cre"], m["s5_cim"] = c(inp["s5_c_re"][0]), c(inp["s5_c_im"][0])
    m["s5_d"], m["s5_glw"], m["s5_glb"] = c(inp["s5_d"][0][None, :]), c(inp["s5_glu_w"][0]), c(inp["s5_glu_b"][0])
    m["ev_wo"], m["od_wo"] = c(inp["ev_w_out"][0]), c(inp["od_w_out"][0])
    for k in ("ln_mix_g", "ln_mix_b", "ln_ffn_g", "ln_ffn_b"):
        m[k] = c(inp[k])
    for L in range(2):
        m["moe_wr%d" % L] = c(np.concatenate([inp["moe_w_coarse"][L]] + [inp["moe_w_fine"][L, g] for g in range(4)], axis=1))
        m["moe_br%d" % L] = c(np.concatenate([inp["moe_b_coarse"][L]] + [inp["moe_b_fine"][L, g] for g in range(4)])[None, :])
        m["moe_wg%d" % L] = c(inp["moe_w_gate"][L].reshape(32, D, 128))
        m["moe_wu%d" % L] = c(inp["moe_w_up"][L].reshape(32, D, 128))
        m["moe_wd%d" % L] = c(inp["moe_w_down"][L].reshape(32, 128, D))
    return {k: np.asarray(v, dtype=np.float32) for k, v in m.items()}


def kernel(**inputs):
    inp = {k: np.asarray(v) for k, v in inputs.items()}
    nc = build_program()
    wm = prep_weights(inp)
    in_maps = []
    for b in range(8):
        m = dict(wm)
        m["xT"] = np.ascontiguousarray(inp["x"][b].T)
        in_maps.append(m)
    res = run_bass_kernel_spmd(nc, in_maps, core_ids=list(range(8)))
    out = np.stack([np.ascontiguousarray(res.results[b]["yT"].T) for b in range(8)], axis=0)
    return out.astype(np.float32)
```

```python
import contextlib
import math
import numpy as np
import concourse.bass as bass
import concourse.mybir as mybir
from concourse.bass_utils import run_bass_kernel_spmd

F32 = mybir.dt.float32
BF16 = mybir.dt.bfloat16
I32 = mybir.dt.int32
ALU = mybir.AluOpType
AF = mybir.ActivationFunctionType
AX = mybir.AxisListType

S = 4096
D = 1024
NT = 8
ALPHA = 4.0 ** 0.25
EPS = 1e-5
BIG = 240000.0
MAGIC = 12582912.0
TWO_PI = 2.0 * math.pi


class Buf:
    __slots__ = ("name", "last_w", "readers")

    def __init__(self, name=""):
        self.name = name
        self.last_w = None
        self.readers = {}


class T:
    __slots__ = ("t", "buf")

    def __init__(self, t, name=""):
        self.t = t
        self.buf = Buf(name)

    def __getitem__(self, idx):
        return self.t[idx]


class KB:
    NDMA = 48

    def __init__(self, nc, stack):
        self.nc = nc
        self.stack = stack
        self.eng = {"pe": nc.tensor, "act": nc.scalar, "dve": nc.vector,
                    "pool": nc.gpsimd, "sp": nc.sync}
        self.sems = {}
        for e in self.eng:
            self.sems[e] = stack.enter_context(nc.semaphore("s_" + e))
        self.cnt = {e: 0 for e in self.eng}
        self.dsem = [stack.enter_context(nc.semaphore("d%d" % i)) for i in range(self.NDMA)]
        self.dcnt = [0] * self.NDMA
        self.dnext = 0
        self.dnext_sw = 0
        self.waited = {}
        self.n_ins = 0
        self.n_wait = 0
        self._uid = 0

    def sb(self, shape, dtype=F32, name=None):
        self._uid += 1
        name = (name or "t") + "_%d" % self._uid
        t = self.stack.enter_context(self.nc.sbuf_tensor(name, list(shape), dtype))
        return T(t, name)

    def ps(self, shape, dtype=F32, name=None):
        self._uid += 1
        name = (name or "p") + "_%d" % self._uid
        t = self.stack.enter_context(self.nc.psum_tensor(name, list(shape), dtype))
        return T(t, name)

    def _sem(self, key):
        return self.sems[key] if isinstance(key, str) else self.dsem[key]

    def _wait(self, e, ev):
        key, val = ev
        if e == "pe" and key == "pe":
            return
        k = (e, key)
        if self.waited.get(k, 0) >= val:
            return
        self.waited[k] = val
        self.eng[e].wait_ge(self._sem(key), val)
        self.n_wait += 1

    @staticmethod
    def _b(b):
        return b.buf if isinstance(b, T) else b

    def _deps(self, e, reads, writes):
        for b in reads:
            b = self._b(b)
            if b.last_w is not None:
                self._wait(e, b.last_w)
        for b in writes:
            b = self._b(b)
            if b.last_w is not None:
                self._wait(e, b.last_w)
            for k, v in b.readers.items():
                self._wait(e, (k, v))

    def _mark(self, ev, reads, writes):
        key, val = ev
        for b in reads:
            b = self._b(b)
            if b.readers.get(key, 0) < val:
                b.readers[key] = val
        for b in writes:
            b = self._b(b)
            b.last_w = ev
            b.readers = {}

    def op(self, e, fn, reads=(), writes=()):
        self._deps(e, reads, writes)
        ins = fn(self.eng[e])
        self.cnt[e] += 1
        ins.then_inc(self.sems[e], 1)
        self._mark((e, self.cnt[e]), reads, writes)
        self.n_ins += 1
        return ins

    def dma(self, q, out, in_, reads=(), writes=(), **kw):
        self._deps(q, reads, writes)
        half = self.NDMA // 2
        if q == "pool":
            i = half + self.dnext_sw
            self.dnext_sw = (self.dnext_sw + 1) % (self.NDMA - half)
        else:
            i = self.dnext
            self.dnext = (self.dnext + 1) % half
        if self.dcnt[i] > 0:
            self._wait(q, (i, self.dcnt[i]))
        ins = self.eng[q].dma_start(out=out, in_=in_, **kw)
        self.dcnt[i] += 16
        ins.then_inc(self.dsem[i], 16)
        self._mark((i, self.dcnt[i]), reads, writes)
        self.n_ins += 1
        return ins

    def barrier(self):
        for e in self.eng:
            for k in self.eng:
                if self.cnt[k] > 0:
                    self._wait(e, (k, self.cnt[k]))
            for i in range(self.NDMA):
                if self.dcnt[i] > 0:
                    self._wait(e, (i, self.dcnt[i]))

    @contextlib.contextmanager
    def phase(self):
        with contextlib.ExitStack() as ps:
            old = self.stack
            self.stack = ps
            yield
            self.barrier()
            self.stack = old


class Ring:
    def __init__(self, items):
        self.items = items
        self.i = 0

    def next(self):
        it = self.items[self.i % len(self.items)]
        self.i += 1
        return it


def mm(kb, ot, oap, lt, lap, rt, rap, start=True, stop=True):
    kb.op("pe", lambda e: e.matmul(oap, lhsT=lap, rhs=rap, start=start, stop=stop,
                                   skip_group_check=True), [lt, rt], [ot])


def tt(kb, e, ot, oap, at, aap, bt, bap, op):
    kb.op(e, lambda g: g.tensor_tensor(out=oap, in0=aap, in1=bap, op=op), [at, bt], [ot])


def ts(kb, e, ot, oap, at, aap, s1, s2, op0, op1=None, extra=()):
    if op1 is None:
        kb.op(e, lambda g: g.tensor_scalar(out=oap, in0=aap, scalar1=s1, scalar2=None, op0=op0),
              [at] + list(extra), [ot])
    else:
        kb.op(e, lambda g: g.tensor_scalar(out=oap, in0=aap, scalar1=s1, scalar2=s2, op0=op0, op1=op1),
              [at] + list(extra), [ot])


def stt(kb, e, ot, oap, at, aap, sc, bt, bap, op0, op1, extra=()):
    kb.op(e, lambda g: g.scalar_tensor_tensor(out=oap, in0=aap, scalar=sc, in1=bap, op0=op0, op1=op1),
          [at, bt] + list(extra), [ot])


def act(kb, ot, oap, it, iap, func, scale=1.0, bias=None, extra=()):
    if bias is None:
        kb.op("act", lambda g: g.activation(out=oap, in_=iap, func=func, scale=scale), [it] + list(extra), [ot])
    else:
        kb.op("act", lambda g: g.activation(out=oap, in_=iap, func=func, scale=scale, bias=bias),
              [it] + list(extra), [ot])


def cp(kb, e, ot, oap, it, iap):
    if e == "act":
        kb.op("act", lambda g: g.activation(out=oap, in_=iap, func=AF.Copy), [it], [ot])
    elif e == "dve":
        kb.op("dve", lambda g: g.tensor_scalar(out=oap, in0=iap, scalar1=1.0, scalar2=None, op0=ALU.mult), [it], [ot])
    else:
        kb.op(e, lambda g: g.tensor_copy(out=oap, in_=iap), [it], [ot])


def memset(kb, e, ot, oap, val):
    kb.op(e, lambda g: g.memset(oap, val), [], [ot])


def fm(ap):
    return ap.rearrange("(c p) t -> p c t", p=128)


def build_consts(kb, G):
    with kb.phase():
        row = kb.sb([1, 64], F32)
        make_ramp(kb, row, row[:, 0:32], 1, 32)
        make_ramp(kb, row, row[:, 32:64], 1, 32)
        one1 = kb.sb([1, 1], F32)
        memset(kb, "dve", one1, one1[:], 1.0)
        pidx = kb.ps([64, 1], F32)
        mm(kb, pidx, pidx[:], row, row[:], one1, one1[:])
        idx = kb.sb([64, 1], F32)
        cp(kb, "act", idx, idx[:], pidx, pidx[:])
        inv = kb.sb([64, 1], F32)
        act(kb, inv, inv[:], idx, idx[:], AF.Exp, scale=-math.log(10000.0) / 32.0)
        ts(kb, "dve", inv, inv[:], inv, inv[:], 1.0 / TWO_PI, None, ALU.mult)
        tpos = kb.sb([64, S], F32)
        make_ramp(kb, tpos, tpos[:], 64, S)
        r = kb.sb([64, S], F32)
        rr = kb.sb([64, S], F32)
        tab = kb.sb([64, S], F32)
        for which in ("sin", "cos"):
            if which == "sin":
                ts(kb, "dve", r, r[:], tpos, tpos[:], inv[:, 0:1], None, ALU.mult, extra=[inv])
            else:
                ts(kb, "dve", r, r[:], tpos, tpos[:], inv[:, 0:1], None, ALU.mult, extra=[inv])
                ts(kb, "dve", r, r[:], r, r[:], 0.25, None, ALU.add)
            ts(kb, "dve", rr, rr[:], r, r[:], MAGIC, MAGIC, ALU.add, ALU.subtract)
            tt(kb, "dve", r, r[:], r, r[:], rr, rr[:], ALU.subtract)
            act(kb, tab, tab[:], r, r[:], AF.Sin, scale=TWO_PI)
            if which == "sin":
                ts(kb, "dve", tab, tab[0:32, :], tab, tab[0:32, :], -1.0, None, ALU.mult)
                kb.dma("sp", G["ropeS"], tab[:], reads=[tab], writes=[G["b_rope"]])
            else:
                kb.dma("sp", G["ropeC"], tab[:], reads=[tab], writes=[G["b_rope"]])


def linear_fm(kb, G, inT, w_dram, outT, n_in_chunks=8, n_out_chunks=8, in_buf=None, out_buf=None):
    with kb.phase():
        w = kb.sb([128, n_in_chunks, n_out_chunks * 128], BF16)
        kb.dma("pool", w[:], w_dram.rearrange("(c p) n -> p c n", p=128), reads=[], writes=[w])
        xin = Ring([kb.sb([128, n_in_chunks, 512], BF16) for _ in range(2)])
        ob = Ring([kb.sb([128, n_out_chunks, 512], F32) for _ in range(2)])
        pss = Ring([kb.ps([128, 512], F32) for _ in range(4)])
        inv = fm(inT)
        outv = fm(outT)
        for t in range(NT):
            x = xin.next()
            kb.dma("sp", x[:], inv[:, :, t * 512:(t + 1) * 512], reads=[in_buf], writes=[x])
            o = ob.next()
            for f in range(n_out_chunks):
                p = pss.next()
                for c in range(n_in_chunks):
                    mm(kb, p, p[:], w, w[:, c, f * 128:(f + 1) * 128], x, x[:, c, :],
                       start=(c == 0), stop=(c == n_in_chunks - 1))
                cp(kb, "act" if f % 2 == 0 else "dve", o, o[:, f, :], p, p[:])
            kb.dma("sp", outv[:, :, t * 512:(t + 1) * 512], o[:], reads=[o], writes=[out_buf])


def res_ln(kb, G, resT, addT, g_dram, b_dram, outT, res_buf, add_buf, out_buf):
    with kb.phase():
        ones = kb.sb([128, 128], F32)
        memset(kb, "dve", ones, ones[:], 1.0 / D)
        gb = kb.sb([128, 2, 8], F32)
        with kb.nc.allow_non_contiguous_dma(reason="tiny ln params"):
            kb.dma("sp", gb[:, 0, :], g_dram.rearrange("(c p) -> p c", p=128), writes=[gb])
            kb.dma("sp", gb[:, 1, :], b_dram.rearrange("(c p) -> p c", p=128), writes=[gb])
        rin = Ring([kb.sb([128, 8, 512], F32) for _ in range(2)])
        ain = Ring([kb.sb([128, 8, 512], F32) for _ in range(2)])
        zsq = Ring([kb.sb([128, 512], F32) for _ in range(2)])
        oo = Ring([kb.sb([128, 8, 512], F32) for _ in range(2)])
        ps1 = Ring([kb.ps([128, 512], F32) for _ in range(2)])
        ps2 = Ring([kb.ps([128, 512], F32) for _ in range(2)])
        mean = kb.sb([128, 512], F32)
        rstd = kb.sb([128, 512], F32)
        tmp = kb.sb([128, 512], F32)
        rv, av, ov = fm(resT), fm(addT), fm(outT)
        for t in range(NT):
            sl = slice(t * 512, (t + 1) * 512)
            r = rin.next()
            a = ain.next()
            kb.dma("sp", r[:], rv[:, :, sl], reads=[res_buf], writes=[r])
            kb.dma("act", a[:], av[:, :, sl], reads=[add_buf], writes=[a])
            p1, p2 = ps1.next(), ps2.next()
            for c in range(8):
                stt(kb, "dve", r, r[:, c, :], r, r[:, c, :], ALPHA, a, a[:, c, :], ALU.mult, ALU.add)
                z2 = zsq.next()
                act(kb, z2, z2[:], r, r[:, c, :], AF.Square)
                mm(kb, p1, p1[:], ones, ones[:], r, r[:, c, :], start=(c == 0), stop=(c == 7))
                mm(kb, p2, p2[:], ones, ones[:], z2, z2[:], start=(c == 0), stop=(c == 7))
            cp(kb, "act", mean, mean[:], p1, p1[:])
            tt(kb, "dve", tmp, tmp[:], mean, mean[:], mean, mean[:], ALU.mult)
            tt(kb, "dve", tmp, tmp[:], p2, p2[:], tmp, tmp[:], ALU.subtract)
            ts(kb, "dve", tmp, tmp[:], tmp, tmp[:], EPS, None, ALU.add)
            act(kb, tmp, tmp[:], tmp, tmp[:], AF.Sqrt)
            kb.op("dve", lambda g: g.reciprocal(out=rstd[:], in_=tmp[:]), [tmp], [rstd])
            o = oo.next()
            for c in range(8):
                e = "dve" if c % 2 == 0 else "pool"
                tt(kb, e, r, r[:, c, :], r, r[:, c, :], mean, mean[:], ALU.subtract)
                tt(kb, e, r, r[:, c, :], r, r[:, c, :], rstd, rstd[:], ALU.mult)
                ts(kb, e, o, o[:, c, :], r, r[:, c, :], gb[:, 0, c:c + 1], gb[:, 1, c:c + 1], ALU.mult, ALU.add,
                   extra=[gb])
            kb.dma("sp", ov[:, :, sl], o[:], reads=[o], writes=[out_buf])


def inproj(kb, G, xT, x_buf, w_rope, w_swap, n_rope, rope_dst, w_fm, n_fm_chunks, fm_dst, w_tm, tm_cols, tm_cb, tm_splits):
    with kb.phase():
        wr = kb.sb([128, 8, n_rope * 64], BF16)
        ws = kb.sb([128, 8, n_rope * 64], BF16)
        kb.dma("pool", wr[:], w_rope.rearrange("(c p) n -> p c n", p=128), writes=[wr])
        kb.dma("pool", ws[:], w_swap.rearrange("(c p) n -> p c n", p=128), writes=[ws])
        if n_fm_chunks:
            wf = kb.sb([128, 8, n_fm_chunks * 128], BF16)
            kb.dma("pool", wf[:], w_fm.rearrange("(c p) n -> p c n", p=128), writes=[wf])
        wt = kb.sb([128, 8, tm_cols], BF16)
        kb.dma("pool", wt[:], w_tm.rearrange("(c p) n -> p c n", p=128), writes=[wt])
        xin = Ring([kb.sb([128, 8, 512], BF16) for _ in range(2)])
        cc = Ring([kb.sb([64, 512], F32) for _ in range(2)])
        ss = Ring([kb.sb([64, 512], F32) for _ in range(2)])
        pa = Ring([kb.ps([64, 512], F32) for _ in range(2)])
        pb = Ring([kb.ps([64, 512], F32) for _ in range(2)])
        pf = Ring([kb.ps([128, 512], F32) for _ in range(2)])
        ntm = len(tm_splits)
        pt = [kb.ps([128, 512], F32) for _ in range(ntm)]
        t1 = Ring([kb.sb([64, 512], F32) for _ in range(2)])
        t2 = Ring([kb.sb([64, 512], F32) for _ in range(2)])
        ro = Ring([kb.sb([64, 512], BF16) for _ in range(3)])
        fo = Ring([kb.sb([128, 512], BF16) for _ in range(2)])
        xv = fm(xT)
        import os
        SK = os.environ.get("INPROJ_SKIP", "")
        for t in range(NT):
            sl = slice(t * 512, (t + 1) * 512)
            x = xin.next()
            kb.dma("pool", x[:], xv[:, :, sl], reads=[x_buf], writes=[x])
            c_, s_ = cc.next(), ss.next()
            kb.dma("sp", c_[:], G["ropeC"][:, sl], reads=[G["b_rope"]], writes=[c_])
            kb.dma("sp", s_[:], G["ropeS"][:, sl], reads=[G["b_rope"]], writes=[s_])
            for h in range(0 if "r" in SK else n_rope):
                a, b = pa.next(), pb.next()
                for c in range(8):
                    mm(kb, a, a[:], wr, wr[:, c, h * 64:(h + 1) * 64], x, x[:, c, :], start=(c == 0), stop=(c == 7))
                for c in range(8):
                    mm(kb, b, b[:], ws, ws[:, c, h * 64:(h + 1) * 64], x, x[:, c, :], start=(c == 0), stop=(c == 7))
                u1, u2, o = t1.next(), t2.next(), ro.next()
                tt(kb, "dve", u1, u1[:], a, a[:], c_, c_[:], ALU.mult)
                tt(kb, "dve", u2, u2[:], b, b[:], s_, s_[:], ALU.mult)
                tt(kb, "dve" if "p" in SK else "pool", o, o[:], u1, u1[:], u2, u2[:], ALU.add)
                dst, dbuf = rope_dst(h)
                kb.dma("sp", dst[:, sl], o[:], reads=[o], writes=[dbuf])
            for f in range(0 if "f" in SK else n_fm_chunks):
                p = pf.next()
                for c in range(8):
                    mm(kb, p, p[:], wf, wf[:, c, f * 128:(f + 1) * 128], x, x[:, c, :], start=(c == 0), stop=(c == 7))
                o = fo.next()
                cp(kb, "act", o, o[:], p, p[:])
                dst, dbuf = fm_dst(f)
                kb.dma("sp", dst[:, sl], o[:], reads=[o], writes=[dbuf])
            for sub in range(0 if "t" in SK else 4):
                for j in range(ntm):
                    n0, n1 = tm_splits[j]
                    for c in range(8):
                        mm(kb, pt[j], pt[j][:, 0:n1 - n0], x, x[:, c, sub * 128:(sub + 1) * 128], wt, wt[:, c, n0:n1],
                           start=(c == 0), stop=(c == 7))
                tm_cb(kb, pt, t * 4 + sub)


def moe(kb, G, xT, x_buf, L, outT, out_buf):
    wr_d, br_d = G["moe_wr"][L], G["moe_br"][L]
    wg_d, wu_d, wd_d = G["moe_wg"][L], G["moe_wu"][L], G["moe_wd"][L]
    xv = fm(xT)
    with kb.phase():
        gT = kb.sb([32, S], BF16)
        with contextlib.ExitStack() as rs:
            old = kb.stack
            kb.stack = rs
            wr = kb.sb([128, 8, 36], F32)
            kb.dma("sp", wr[:], wr_d.rearrange("(c p) n -> p c n", p=128), writes=[wr])
            br = kb.sb([1, 36], F32)
            kb.dma("sp", br[:], br_d, writes=[br])
            ones1 = kb.sb([1, 128], F32)
            memset(kb, "dve", ones1, ones1[:], 1.0)
            ident = kb.sb([128, 128], F32)
            memset(kb, "dve", ident, ident[:], 1.0)
            kb.op("pool", lambda g: g.affine_select(out=ident[:], in_=ident[:], pattern=[[-1, 128]],
                                                    compare_op=ALU.is_equal, fill=0.0, base=0, channel_multiplier=1),
                  [ident], [ident])
            xin = Ring([kb.sb([128, 8, 512], F32) for _ in range(2)])
            pl = Ring([kb.ps([128, 36], F32) for _ in range(2)])
            ptr = Ring([kb.ps([32, 128], F32) for _ in range(2)])
            sm = Ring([kb.sb([128, 160], F32) for _ in range(2)])
            gt = Ring([kb.sb([128, 32], F32) for _ in range(2)])
            for t in range(NT):
                x = xin.next()
                kb.dma("sp", x[:], xv[:, :, t * 512:(t + 1) * 512], reads=[x_buf], writes=[x])
                for sub in range(4):
                    p = pl.next()
                    for c in range(8):
                        mm(kb, p, p[:], x, x[:, c, sub * 128:(sub + 1) * 128], wr, wr[:, c, :], start=(c == 0), stop=False)
                    mm(kb, p, p[:], ones1, ones1[:], br, br[:], start=False, stop=True)
                    w = sm.next()
                    cp(kb, "act", w, w[:, 0:36], p, p[:])
                    kb.op("dve", lambda g: g.tensor_reduce(out=w[:, 36:37], in_=w[:, 0:4], axis=AX.X, op=ALU.max), [w], [w])
                    ts(kb, "dve", w, w[:, 37:38], w, w[:, 36:37], -1.0, None, ALU.mult)
                    act(kb, w, w[:, 38:42], w, w[:, 0:4], AF.Exp, bias=w[:, 37:38])
                    kb.op("dve", lambda g: g.tensor_reduce(out=w[:, 42:43], in_=w[:, 38:42], axis=AX.X, op=ALU.add), [w], [w])
                    kb.op("dve", lambda g: g.reciprocal(out=w[:, 43:44], in_=w[:, 42:43]), [w], [w])
                    ts(kb, "dve", w, w[:, 44:48], w, w[:, 0:4], w[:, 36:37], None, ALU.is_equal)
                    tt(kb, "dve", w, w[:, 48:80].rearrange("p (g e) -> p g e", g=4),
                       w, w[:, 4:36].rearrange("p (g e) -> p g e", g=4),
                       w, w[:, 44:48].unsqueeze(2).to_broadcast([128, 4, 8]), ALU.mult)
                    kb.op("dve", lambda g: g.tensor_reduce(out=w[:, 80:88], in_=w[:, 48:80].rearrange("p (g e) -> p e g", g=4),
                                                           axis=AX.X, op=ALU.add), [w], [w])
                    kb.op("dve", lambda g: g.max(out=w[:, 88:96], in_=w[:, 80:88]), [w], [w])
                    tt(kb, "dve", w, w[:, 96:97], w, w[:, 88:89], w, w[:, 89:90], ALU.subtract)
                    act(kb, w, w[:, 97:98], w, w[:, 96:97], AF.Sigmoid)
                    tt(kb, "dve", w, w[:, 98:99], w, w[:, 97:98], w, w[:, 43:44], ALU.mult)
                    tt(kb, "dve", w, w[:, 99:100], w, w[:, 43:44], w, w[:, 98:99], ALU.subtract)
                    ts(kb, "dve", w, w[:, 100:108], w, w[:, 80:88], w[:, 88:89], w[:, 98:99], ALU.is_equal, ALU.mult)
                    ts(kb, "dve", w, w[:, 108:116], w, w[:, 80:88], w[:, 89:90], w[:, 99:100], ALU.is_equal, ALU.mult)
                    tt(kb, "dve", w, w[:, 116:124], w, w[:, 100:108], w, w[:, 108:116], ALU.add)
                    gg = gt.next()
                    tt(kb, "dve", gg, gg[:].rearrange("p (g e) -> p g e", g=4),
                       w, w[:, 44:48].unsqueeze(2).to_broadcast([128, 4, 8]),
                       w, w[:, 116:124].unsqueeze(1).to_broadcast([128, 4, 8]), ALU.mult)
                    pT = ptr.next()
                    kb.op("pe", lambda e: e.transpose(pT[:], gg[:], ident[:]), [gg, ident], [pT])
                    q0 = (t * 4 + sub) * 128
                    cp(kb, "act", gT, gT[:, q0:q0 + 128], pT, pT[:])
            kb.barrier()
            kb.stack = old
        sel = kb.sb([32, 32, 128], BF16)
        memset(kb, "dve", sel, sel[:], 1.0)
        kb.op("pool", lambda g: g.affine_select(out=sel[:], in_=sel[:], pattern=[[-1, 32], [0, 128]],
                                                compare_op=ALU.is_equal, fill=0.0, base=0, channel_multiplier=1),
              [sel], [sel])
        TS = 2048
        xb = kb.sb([128, 8, TS], BF16)
        yacc = kb.sb([128, 8, TS], F32)
        hq = kb.sb([128, 4, TS], BF16)
        wgr = Ring([kb.sb([128, 8, 128], BF16) for _ in range(2)])
        wur = Ring([kb.sb([128, 8, 128], BF16) for _ in range(2)])
        wdr = Ring([kb.sb([128, 1024], BF16) for _ in range(8)])
        pg = Ring([kb.ps([128, 512], F32) for _ in range(2)])
        pu = Ring([kb.ps([128, 512], F32) for _ in range(2)])
        pc = Ring([kb.ps([128, 512], F32) for _ in range(2)])
        py = Ring([kb.ps([128, 512], F32) for _ in range(2)])
        sl_ = Ring([kb.sb([128, 512], F32) for _ in range(2)])
        t1_ = Ring([kb.sb([128, 512], F32) for _ in range(2)])
        ov = fm(outT)
        for st in range(S // TS):
            kb.dma("pool", xb[:], xv[:, :, st * TS:(st + 1) * TS], reads=[x_buf], writes=[xb])
            for q in range(8):
                wds = []
                for ei in range(4):
                    e = q * 4 + ei
                    wg, wu, wd = wgr.next(), wur.next(), wdr.next()
                    kb.dma("pool", wg[:], wg_d[e].rearrange("(c p) n -> p c n", p=128), writes=[wg])
                    kb.dma("pool", wu[:], wu_d[e].rearrange("(c p) n -> p c n", p=128), writes=[wu])
                    kb.dma("pool", wd[:], wd_d[e], writes=[wd])
                    wds.append(wd)
                    for tq in range(TS // 512):
                        tsl = slice(tq * 512, (tq + 1) * 512)
                        a, b, c_ = pg.next(), pu.next(), pc.next()
                        for c in range(8):
                            mm(kb, a, a[:], wg, wg[:, c, :], xb, xb[:, c, tsl], start=(c == 0), stop=(c == 7))
                        for c in range(8):
                            mm(kb, b, b[:], wu, wu[:, c, :], xb, xb[:, c, tsl], start=(c == 0), stop=(c == 7))
                        g0 = st * TS + tq * 512
                        mm(kb, c_, c_[:], sel, sel[:, e, :], gT, gT[:, g0:g0 + 512])
                        s1, u1 = sl_.next(), t1_.next()
                        act(kb, s1, s1[:], a, a[:], AF.Silu)
                        tt(kb, "dve", u1, u1[:], c_, c_[:], s1, s1[:], ALU.mult)
                        tt(kb, "dve", hq, hq[:, ei, tsl], b, b[:], u1, u1[:], ALU.mult)
                for tq in range(TS // 512):
                    tsl = slice(tq * 512, (tq + 1) * 512)
                    for f in range(8):
                        y = py.next()
                        for ei in range(4):
                            mm(kb, y, y[:], wds[ei], wds[ei][:, f * 128:(f + 1) * 128], hq, hq[:, ei, tsl],
                               start=(ei == 0), stop=(ei == 3))
                        if q == 0:
                            cp(kb, "act", yacc, yacc[:, f, tsl], y, y[:])
                        else:
                            tt(kb, "pool" if False else "dve", yacc, yacc[:, f, tsl], y, y[:], yacc, yacc[:, f, tsl], ALU.add)
            kb.dma("sp", ov[:, :, st * TS:(st + 1) * TS], yacc[:], reads=[yacc], writes=[out_buf])


def sched_causal(Q):
    out = []
    for kt in range(4 * Q + 4):
        r = kt - 4 * Q
        out.append((kt, max(0, r), 4, [(r, "tri")] if r >= 0 else [], None))
    return out


def sched_win(Q):
    out = []
    for kt in range(max(0, 4 * Q - 2), 4 * Q + 4):
        r = kt - 4 * Q
        s0, s1 = max(0, r), min(3, r + 2) + 1
        m = []
        if r >= 0:
            m.append((r, "tri"))
        if 0 <= r + 2 <= 3:
            m.append((r + 2, "ntri"))
        out.append((kt, s0, s1, m, None))
    return out


def sched_cmp(Q):
    out = [(0, 0, 4, [], "cmp")]
    if Q >= 4:
        out.append((1, 0, 4, [], "cmp"))
    return out


class AttnCtx:
    def __init__(self, kb, need_cmp):
        self.kb = kb
        self.tri = kb.sb([128, 128], BF16)
        self.ntri = kb.sb([128, 128], BF16)
        memset(kb, "dve", self.tri, self.tri[:], 1.0)
        memset(kb, "dve", self.ntri, self.ntri[:], 1.0)
        kb.op("pool", lambda g: g.affine_select(out=self.tri[:], in_=self.tri[:], pattern=[[1, 128]],
                                                compare_op=ALU.is_ge, fill=0.0, base=0, channel_multiplier=-1),
              [self.tri], [self.tri])
        kb.op("pool", lambda g: g.affine_select(out=self.ntri[:], in_=self.ntri[:], pattern=[[-1, 128]],
                                                compare_op=ALU.is_gt, fill=0.0, base=0, channel_multiplier=1),
              [self.ntri], [self.ntri])
        self.cmpm = None
        if need_cmp:
            self.cmpm = kb.sb([128, 2, S], BF16)
            memset(kb, "dve", self.cmpm, self.cmpm[:], 1.0)
            for j in range(2):
                kb.op("pool", lambda g, j=j: g.affine_select(out=self.cmpm[:, j, :], in_=self.cmpm[:, j, :], pattern=[[1, S]],
                                                             compare_op=ALU.is_ge, fill=0.0, base=-31 - 2048 * j,
                                                             channel_multiplier=-16), [self.cmpm], [self.cmpm])
        self.zl = kb.sb([128, 128], BF16)
        self.zr = kb.sb([128, 512], BF16)
        memset(kb, "dve", self.zl, self.zl[:], 0.0)
        memset(kb, "dve", self.zr, self.zr[:], 0.0)
        self.psS = Ring([kb.ps([128, 512], F32) for _ in range(3)])
        self.psO = Ring([kb.ps([128, 4, 128], F32) for _ in range(3)])
        self.pT = Ring([kb.sb([128, 512], BF16) for _ in range(3)])
        self.den = Ring([kb.sb([128, 4], F32) for _ in range(3)])
        self.coef = Ring([kb.sb([128, 4], F32) for _ in range(3)])
        self.tmp = Ring([kb.sb([128, 4, 64], F32) for _ in range(2)])
        self.mask_eng = Ring(["pool", "dve"])
        self.pend = []
        self.LA = 2

    def push(self, fn):
        self.pend.append(fn)
        while len(self.pend) > self.LA:
            self.pend.pop(0)()

    def flush_all(self):
        while self.pend:
            self.pend.pop(0)()


def attn_branch(A, Kc, q_t, k_t, v_t, nkeys_last, sched, Q, acc_t, first, gate_t=None, gate_ap=None, after=None):
    kb = A.kb
    po = A.psO.next()
    mm(kb, po, po[:].rearrange("p a b -> p (a b)"), A.zl, A.zl[:], A.zr, A.zr[:], start=True, stop=True)

    def fin():
        den, coef = A.den.next(), A.coef.next()
        ts(kb, "dve", den, den[:], po, po[:, :, 64], 1e-30, None, ALU.max)
        kb.op("dve", lambda g: g.reciprocal(out=coef[:], in_=den[:]), [den], [coef])
        if gate_t is not None:
            tt(kb, "dve", coef, coef[:], coef, coef[:], gate_t, gate_ap, ALU.mult)
        cb = coef[:].unsqueeze(2).to_broadcast([128, 4, 64])
        if first:
            tt(kb, "dve", acc_t, acc_t[:], po, po[:, :, 0:64], coef, cb, ALU.mult)
        else:
            tmp = A.tmp.next()
            tt(kb, "dve", tmp, tmp[:], po, po[:, :, 0:64], coef, cb, ALU.mult)
            tt(kb, "pool", acc_t, acc_t[:], acc_t, acc_t[:], tmp, tmp[:], ALU.add)
        if after is not None:
            after()

    items = sched(Q)
    for idx, (kt, s0, s1, masks, full) in enumerate(items):
        ksz = 128
        if nkeys_last is not None and (kt + 1) * 128 > nkeys_last:
            ksz = nkeys_last - kt * 128
        c0, c1 = s0 * 128, s1 * 128
        ps = A.psS.next()
        mm(kb, ps, ps[0:ksz, c0:c1], k_t, k_t[0:Kc, kt * 128:kt * 128 + ksz], q_t, q_t[0:Kc, Q * 512 + c0:Q * 512 + c1])
        p = A.pT.next()
        act(kb, p, p[0:ksz, c0:c1], ps, ps[0:ksz, c0:c1], AF.Exp, scale=0.125)
        for (s, name) in masks:
            m = A.tri if name == "tri" else A.ntri
            tt(kb, A.mask_eng.next(), p, p[0:ksz, s * 128:(s + 1) * 128], p, p[0:ksz, s * 128:(s + 1) * 128], m, m[0:ksz, :], ALU.mult)
        if full == "cmp":
            tt(kb, A.mask_eng.next(), p, p[0:ksz, c0:c1], p, p[0:ksz, c0:c1], A.cmpm, A.cmpm[0:ksz, kt, Q * 512 + c0:Q * 512 + c1], ALU.mult)
        last = idx == len(items) - 1

        def pv(p=p, ksz=ksz, kt=kt, s0=s0, s1=s1, last=last):
            for s in range(s0, s1):
                mm(kb, po, po[:, s, 0:65], p, p[0:ksz, s * 128:(s + 1) * 128], v_t, v_t[0:ksz, kt, :], start=False, stop=False)
            if last:
                fin()

        A.push(pv)


def make_ramp(kb, t, ap, parts, n, start=0.0):
    ones = kb.sb([parts, n], F32)
    memset(kb, "dve", ones, ones[:], 1.0)
    kb.op("dve", lambda g: g.tensor_tensor_scan(out=ap, data0=ones[:], data1=ones[:], initial=float(start) - 1.0,
                                                op0=ALU.mult, op1=ALU.add), [ones], [t])


def make_ident(kb, dtype=F32):
    ident = kb.sb([128, 128], dtype)
    memset(kb, "dve", ident, ident[:], 1.0)
    kb.op("pool", lambda g: g.affine_select(out=ident[:], in_=ident[:], pattern=[[-1, 128]],
                                            compare_op=ALU.is_equal, fill=0.0, base=0, channel_multiplier=1),
          [ident], [ident])
    return ident


def gelu_tanh(kb, z, zap, out_t, out_ap, shape, tmp1, tmp2):
    a1 = tmp1[tuple(slice(0, s) for s in shape)]
    a2 = tmp2[tuple(slice(0, s) for s in shape)]
    act(kb, tmp1, a1, z, zap, AF.Square)
    ts(kb, "dve", tmp1, a1, tmp1, a1, 0.044715, 1.0, ALU.mult, ALU.add)
    tt(kb, "dve", tmp1, a1, tmp1, a1, z, zap, ALU.mult)
    act(kb, tmp2, a2, tmp1, a1, AF.Sigmoid, scale=1.5957691216057308)
    tt(kb, "dve", out_t, out_ap, z, zap, tmp2, a2, ALU.mult)


def nsa_compress(kb, G):
    with kb.phase():
        ident = make_ident(kb)
        pps = kb.ps([64, 32], F32)
        pb = kb.ps([128, 1], F32)
        ph = kb.ps([128, 256], F32)
        pk = kb.ps([64, 256], F32)
        pv = kb.ps([128, 64], F32)
        for which in ("k", "v"):
            w1_d, w2_d, pe_d = G["cmp_%s_w1" % which], G["cmp_%s_w2" % which], G["pe_%s" % which]
            w1 = kb.sb([128, 32, 128], BF16)
            w1v = w1_d.rearrange("(l d) f -> d l f", d=64)
            kb.dma("pool", w1[0:64], w1v, writes=[w1])
            kb.dma("pool", w1[64:128], w1v, writes=[w1])
            w2 = kb.sb([128, 64], BF16)
            kb.dma("pool", w2[:], w2_d, writes=[w2])
            pe = kb.sb([32, 64], F32)
            kb.dma("sp", pe[:], pe_d, writes=[pe])
            kb.op("pe", lambda e: e.transpose(pps[:], pe[:], ident[0:32, 0:32]), [pe, ident], [pps])
            peT = kb.sb([64, 32], BF16)
            cp(kb, "act", peT, peT[:], pps, pps[:])
            for l in range(32):
                mm(kb, pb, pb[:], w1, w1[0:64, l, :], peT, peT[:, l:l + 1], start=(l == 0), stop=(l == 31))
            bias = kb.sb([128, 1], F32)
            cp(kb, "act", bias, bias[:], pb, pb[:])
            src = kb.sb([128, S], BF16)
            if which == "k":
                kb.dma("sp", src[0:64], G["kcT"][0], reads=[G["b_qk0"]], writes=[src])
                kb.dma("sp", src[64:128], G["kcT"][1], reads=[G["b_qk0"]], writes=[src])
            else:
                kb.dma("sp", src[:], G["vcT"], reads=[G["b_qk0"]], writes=[src])
            for g in range(2):
                for l in range(32):
                    mm(kb, ph, ph[:, 0:255], w1, w1[g * 64:(g + 1) * 64, l, :], src, src[g * 64:(g + 1) * 64, l:l + 4065:16],
                       start=(l == 0), stop=(l == 31))
                z = kb.sb([128, 255], F32)
                act(kb, z, z[:], ph, ph[:, 0:255], AF.Identity, bias=bias[:, 0:1], extra=[bias])
                hid = kb.sb([128, 256], BF16)
                t1, t2 = kb.sb([128, 255], F32), kb.sb([128, 255], F32)
                gelu_tanh(kb, z, z[:], hid, hid[:, 0:255], (128, 255), t1, t2)
                if which == "k":
                    mm(kb, pk, pk[:, 0:255], w2, w2[:], hid, hid[:, 0:255])
                    kc = kb.sb([64, 256], BF16)
                    memset(kb, "dve", kc, kc[:], 0.0)
                    cp(kb, "act", kc, kc[:, 0:255], pk, pk[:, 0:255])
                    kb.dma("sp", G["kcmpT"][g], kc[:], reads=[kc], writes=[G["b_cmp"]])
                else:
                    vcs = kb.sb([128, 2, 65], BF16)
                    memset(kb, "dve", vcs, vcs[:], 0.0)
                    memset(kb, "dve", vcs, vcs[:, :, 64:65], 1.0)
                    for j in range(2):
                        nsz = 128 if j == 0 else 127
                        mm(kb, pv, pv[0:nsz, :], hid, hid[:, j * 128:j * 128 + nsz], w2, w2[:])
                        cp(kb, "act", vcs, vcs[0:nsz, j, 0:64], pv, pv[0:nsz, :])
                    kb.dma("sp", G["vcmp"][g], vcs[:], reads=[vcs], writes=[G["b_cmp"]])


def nsa_select(kb, G):
    with kb.phase():
        identb = make_ident(kb, BF16)
        val = kb.sb([128, 128], F32)
        make_ramp(kb, val, val[:], 128, 128, start=-64.0)
        ts(kb, "dve", val, val[64:128, :], val, val[64:128, :], -1.0, None, ALU.add)
        KM, AM, fut = kb.sb([128, 128], F32), kb.sb([128, 128], F32), kb.sb([128, 128], F32)
        ts(kb, "dve", KM, KM[:], val, val[:], -2.0, None, ALU.is_le)
        ts(kb, "dve", fut, fut[:], val, val[:], 1.0, None, ALU.is_ge)
        ts(kb, "dve", AM, AM[:], KM, KM[:], -1000.0, 1000.0, ALU.mult, ALU.add)
        stt(kb, "dve", AM, AM[:], fut, fut[:], -1001.0, AM, AM[:], ALU.mult, ALU.add)
        Mc = kb.sb([128, 8], F32)
        memset(kb, "dve", Mc, Mc[:], 1.0)
        kb.op("pool", lambda g: g.affine_select(out=Mc[:], in_=Mc[:], pattern=[[-16, 8]], compare_op=ALU.is_ge,
                                                fill=0.0, base=-15, channel_multiplier=1), [Mc], [Mc])
        q4 = kb.sb([64, 4, S], BF16)
        kcm = kb.sb([64, 256], BF16)
        negT = kb.sb([64, S], BF16)
        psc = Ring([kb.ps([128, 4, 256], F32) for _ in range(2)])
        ptr = Ring([kb.ps([64, 128], BF16) for _ in range(2)])
        pcs = Ring([kb.sb([128, 4, 256], F32) for _ in range(2)])
        Psum = Ring([kb.sb([128, 256], F32) for _ in range(2)])
        sm = Ring([kb.sb([128, 160], F32) for _ in range(2)])
        sc = Ring([kb.sb([128, 64], F32) for _ in range(2)])
        ngb = Ring([kb.sb([128, 64], BF16) for _ in range(2)])
        for g in range(2):
            for z in range(4):
                kb.dma("sp", q4[:, z, :], G["qaug0"][g * 4 + z, 0:64, :], reads=[G["b_qk0"]], writes=[q4])
            kb.dma("sp", kcm[:], G["kcmpT"][g], reads=[G["b_cmp"]], writes=[kcm])
            for it in pcs.items + Psum.items:
                memset(kb, "pool", it, it[:], 0.0)
            for c in range(32):
                ncv = min(255, 8 * c + 7)
                p = psc.next()
                for z in range(4):
                    mm(kb, p, p[:, z, 0:ncv], q4, q4[:, z, c * 128:(c + 1) * 128], kcm, kcm[:, 0:ncv])
                pc = pcs.next()
                act(kb, pc, pc[:, :, 0:ncv], p, p[:, :, 0:ncv], AF.Exp, scale=0.125)
                if c == 0:
                    tt(kb, "dve", pc, pc[:, :, 0:7], pc, pc[:, :, 0:7], Mc, Mc[:, 1:8].unsqueeze(1).to_broadcast([128, 4, 7]), ALU.mult)
                else:
                    tt(kb, "dve", pc, pc[:, :, ncv - 8:ncv], pc, pc[:, :, ncv - 8:ncv], Mc,
                       Mc[:, 0:8].unsqueeze(1).to_broadcast([128, 4, 8]), ALU.mult)
                w = sm.next()
                kb.op("dve", lambda e: e.tensor_reduce(out=w[:, 0:4], in_=pc[:, :, 0:ncv], axis=AX.X, op=ALU.add), [pc], [w])
                ts(kb, "dve", w, w[:, 4:8], w, w[:, 0:4], 1e-30, None, ALU.max)
                kb.op("dve", lambda e: e.reciprocal(out=w[:, 8:12], in_=w[:, 4:8]), [w], [w])
                P_ = Psum.next()
                ts(kb, "dve", P_, P_[:, 0:ncv], pc, pc[:, 0, 0:ncv], w[:, 8:9], None, ALU.mult, extra=[w])
                for z in range(1, 4):
                    stt(kb, "dve", P_, P_[:, 0:ncv], pc, pc[:, z, 0:ncv], w[:, 8 + z:9 + z], P_, P_[:, 0:ncv], ALU.mult, ALU.add,
                        extra=[w])
                s_ = sc.next()
                kb.op("dve", lambda e: e.tensor_reduce(out=w[:, 16:80], in_=P_[:].rearrange("p (j r) -> p j r", r=4),
                                                       axis=AX.X, op=ALU.add), [P_], [w])
                stt(kb, "dve", s_, s_[:], P_, P_[:, 3:256:4], -0.5, w, w[:, 16:80], ALU.mult, ALU.add)
                stt(kb, "dve", s_, s_[:, 1:64], P_, P_[:, 3:252:4], 0.5, s_, s_[:, 1:64], ALU.mult, ALU.add)
                tt(kb, "dve", s_, s_[:], s_, s_[:], KM, KM[:, 64 - 2 * c:128 - 2 * c], ALU.mult)
                tt(kb, "dve", s_, s_[:], s_, s_[:], AM, AM[:, 64 - 2 * c:128 - 2 * c], ALU.add)
                memset(kb, "dve", s_, s_[:, 0:1], 1000.0)
                kb.op("dve", lambda e: e.max(out=w[:, 80:88], in_=s_[:]), [s_], [w])
                ts(kb, "dve", w, w[:, 88:89], w, w[:, 87:88], 0.0, None, ALU.max)
                ts(kb, "dve", s_, s_[:], s_, s_[:], w[:, 88:89], None, ALU.is_ge, extra=[w])
                nb = ngb.next()
                ts(kb, "dve", nb, nb[:], s_, s_[:], -1.0, BIG, ALU.add, ALU.mult)
                pT = ptr.next()
                kb.op("pe", lambda e: e.transpose(pT[:], nb[:], identb[:]), [nb, identb], [pT])
                cp(kb, "act", negT, negT[:, c * 128:(c + 1) * 128], pT, pT[:])
            for z in range(4):
                kb.dma("sp", G["qaug0"][g * 4 + z, 64:128, :], negT[:], reads=[negT], writes=[G["b_neg0"]])


def nsa_attn(kb, G):
    with kb.phase():
        A = AttnCtx(kb, need_cmp=True)
        identb = make_ident(kb, BF16)
        Ec = kb.sb([128, S], BF16)
        memset(kb, "dve", Ec, Ec[:], 1.0)
        kb.op("pool", lambda g: g.affine_select(out=Ec[64:128, :], in_=Ec[64:128, :], pattern=[[1, S]], compare_op=ALU.is_ge,
                                                fill=0.0, base=0, channel_multiplier=-64), [Ec], [Ec])
        kb.op("pool", lambda g: g.affine_select(out=Ec[64:128, :], in_=Ec[64:128, :], pattern=[[-1, S]], compare_op=ALU.is_ge,
                                                fill=0.0, base=63, channel_multiplier=64), [Ec], [Ec])
        gates = kb.sb([128, 32, 24], F32)
        kb.dma("sp", gates[:], G["gates"].rearrange("(c p) n -> p c n", p=128), reads=[G["b_qk0"]], writes=[gates])
        ks = kb.sb([128, S], BF16)
        kw = kb.sb([64, S], BF16)
        kcm = kb.sb([64, 256], BF16)
        vs = kb.sb([128, 32, 65], BF16)
        vw = kb.sb([128, 32, 65], BF16)
        vcm = kb.sb([128, 2, 65], BF16)
        qr = Ring([kb.sb([128, S], BF16) for _ in range(2)])
        accr = Ring([kb.sb([128, 4, 64], F32) for _ in range(2)])
        otok = kb.sb([128, 8, 4, 128], BF16)
        ptr = Ring([kb.ps([128, 4, 128], BF16) for _ in range(2)])
        oT = Ring([kb.sb([128, 512], BF16) for _ in range(2)])
        v3v = G["v3"].rearrange("(kt p) b c -> p kt b c", p=128)
        for hp in range(4):
            g = hp // 2
            if hp % 2 == 0:
                kb.dma("sp", ks[0:64], G["ksT"][g], reads=[G["b_qk0"]], writes=[ks])
                cp(kb, "pool", ks, ks[64:128, :], Ec, Ec[64:128, :])
                kb.dma("sp", kw[:], G["kwT"][g], reads=[G["b_qk0"]], writes=[kw])
                kb.dma("sp", kcm[:], G["kcmpT"][g], reads=[G["b_cmp"]], writes=[kcm])
                kb.dma("sp", vs[:], v3v[:, :, 2 + g, :], reads=[G["b_qk0"]], writes=[vs])
                kb.dma("sp", vw[:], v3v[:, :, 4 + g, :], reads=[G["b_qk0"]], writes=[vw])
                kb.dma("sp", vcm[:], G["vcmp"][g], reads=[G["b_cmp"]], writes=[vcm])
            for hh in range(2):
                h = hp * 2 + hh
                q = qr.next()
                kb.dma("sp", q[:], G["qaug0"][h], reads=[G["b_qk0"], G["b_neg0"]], writes=[q])
                for Q in range(8):
                    acc = accr.next()
                    attn_branch(A, 64, q, kcm, vcm, 255, sched_cmp, Q, acc, True, gates, gates[:, 4 * Q:4 * Q + 4, h * 3 + 0])
                    attn_branch(A, 128, q, ks, vs, None, sched_causal, Q, acc, False, gates, gates[:, 4 * Q:4 * Q + 4, h * 3 + 1])
                    attn_branch(A, 64, q, kw, vw, None, sched_win, Q, acc, False, gates, gates[:, 4 * Q:4 * Q + 4, h * 3 + 2],
                                after=lambda acc=acc, Q=Q, hh=hh: cp(kb, "act", otok, otok[:, Q, :, hh * 64:(hh + 1) * 64], acc, acc[:]))
            A.flush_all()
            for Q in range(8):
                pT = ptr.next()
                for s in range(4):
                    kb.op("pe", lambda e, s=s: e.transpose(pT[:, s, :], otok[:, Q, s, :], identb[:]), [otok, identb], [pT])
                o = oT.next()
                cp(kb, "dve", o, o[:].rearrange("p (s q) -> p s q", s=4), pT, pT[:])
                kb.dma("sp", G["o0T"][hp * 128:(hp + 1) * 128, Q * 512:(Q + 1) * 512], o[:], reads=[o], writes=[G["b_o0"]])


def sincos_from_rev(kb, r, rap, shape, tmp, out_sin, out_sin_ap, out_cos, out_cos_ap):
    a1 = tmp[0][tuple(slice(0, s) for s in shape)]
    a2 = tmp[1][tuple(slice(0, s) for s in shape)]
    ts(kb, "dve", tmp[0], a1, r, rap, MAGIC, MAGIC, ALU.add, ALU.subtract)
    tt(kb, "dve", tmp[0], a1, r, rap, tmp[0], a1, ALU.subtract)
    act(kb, out_sin, out_sin_ap, tmp[0], a1, AF.Sin, scale=TWO_PI)
    ts(kb, "dve", tmp[1], a2, r, rap, 0.25, None, ALU.add)
    ts(kb, "dve", tmp[0], a1, tmp[1], a2, MAGIC, MAGIC, ALU.add, ALU.subtract)
    tt(kb, "dve", tmp[0], a1, tmp[1], a2, tmp[0], a1, ALU.subtract)
    act(kb, out_cos, out_cos_ap, tmp[0], a1, AF.Sin, scale=TWO_PI)


def s5(kb, G):
    with kb.phase():
        ident = make_ident(kb)
        identb = make_ident(kb, BF16)
        par = kb.sb([128, 4, 16], F32)
        with contextlib.ExitStack() as rs:
            old = kb.stack
            kb.stack = rs
            lr, li = kb.sb([16, 128], F32), kb.sb([16, 128], F32)
            kb.dma("sp", lr[:], G["s5_lre"].rearrange("(s w) p -> s (w p)", w=2), writes=[lr])
            kb.dma("sp", li[:], G["s5_lim"].rearrange("(s w) p -> s (w p)", w=2), writes=[li])
            ls = kb.sb([16, 2], F32)
            kb.dma("sp", ls[:], G["s5_ls"].rearrange("(s w) -> s w", w=2), writes=[ls])
            act(kb, ls, ls[:], ls, ls[:], AF.Exp)
            W = [kb.sb([16, 128], F32) for _ in range(12)]
            stepb, mag, thr, sn, cs, ar1, ai, den, fr, fi, ta, tb = W
            cp(kb, "dve", stepb, stepb[:].rearrange("s (w p) -> s w p", w=2), ls, ls[:].unsqueeze(2).to_broadcast([16, 2, 64]))
            tt(kb, "dve", ta, ta[:], lr, lr[:], stepb, stepb[:], ALU.mult)
            act(kb, mag, mag[:], ta, ta[:], AF.Exp)
            tt(kb, "dve", thr, thr[:], li, li[:], stepb, stepb[:], ALU.mult)
            ts(kb, "dve", thr, thr[:], thr, thr[:], 1.0 / TWO_PI, None, ALU.mult)
            sincos_from_rev(kb, thr, thr[:], (16, 128), (ta, tb), sn, sn[:], cs, cs[:])
            tt(kb, "dve", ai, ai[:], mag, mag[:], sn, sn[:], ALU.mult)
            tt(kb, "dve", ar1, ar1[:], mag, mag[:], cs, cs[:], ALU.mult)
            ts(kb, "dve", ar1, ar1[:], ar1, ar1[:], -1.0, None, ALU.add)
            tt(kb, "dve", ta, ta[:], lr, lr[:], lr, lr[:], ALU.mult)
            tt(kb, "dve", tb, tb[:], li, li[:], li, li[:], ALU.mult)
            tt(kb, "dve", den, den[:], ta, ta[:], tb, tb[:], ALU.add)
            kb.op("dve", lambda e: e.reciprocal(out=den[:], in_=den[:]), [den], [den])
            tt(kb, "dve", ta, ta[:], ar1, ar1[:], lr, lr[:], ALU.mult)
            tt(kb, "dve", tb, tb[:], ai, ai[:], li, li[:], ALU.mult)
            tt(kb, "dve", fr, fr[:], ta, ta[:], tb, tb[:], ALU.add)
            tt(kb, "dve", fr, fr[:], fr, fr[:], den, den[:], ALU.mult)
            tt(kb, "dve", ta, ta[:], ai, ai[:], lr, lr[:], ALU.mult)
            tt(kb, "dve", tb, tb[:], ar1, ar1[:], li, li[:], ALU.mult)
            tt(kb, "dve", fi, fi[:], ta, ta[:], tb, tb[:], ALU.subtract)
            tt(kb, "dve", fi, fi[:], fi, fi[:], den, den[:], ALU.mult)
            pp = kb.ps([128, 4, 16], F32)
            for i, src in enumerate((mag, thr, fr, fi)):
                kb.op("pe", lambda e, i=i, src=src: e.transpose(pp[:, i, :], src[:], ident[0:16, 0:16]), [src, ident], [pp])
            cp(kb, "act", par, par[:], pp, pp[:])
            kb.barrier()
            kb.stack = old
        BTr, BTi = kb.sb([128, 4, 128], BF16), kb.sb([128, 4, 128], BF16)
        CTr, CTi = kb.sb([128, 4, 128], BF16), kb.sb([128, 4, 128], BF16)
        with contextlib.ExitStack() as rs:
            old = kb.stack
            kb.stack = rs
            br, bi = kb.sb([128, 16, 16], F32), kb.sb([128, 16, 16], F32)
            kb.dma("sp", br[:], G["s5_bre"].rearrange("(s w) p c -> (w p) s c", w=2), writes=[br])
            kb.dma("sp", bi[:], G["s5_bim"].rearrange("(s w) p c -> (w p) s c", w=2), writes=[bi])
            frb = par[:, 2, :].unsqueeze(2).to_broadcast([128, 16, 16])
            fib = par[:, 3, :].unsqueeze(2).to_broadcast([128, 16, 16])
            t1, t2 = kb.sb([128, 16, 16], F32), kb.sb([128, 16, 16], F32)
            Bb = [kb.sb([128, 16, 16], F32), kb.sb([128, 16, 16], F32)]
            tt(kb, "dve", t1, t1[:], br, br[:], par, frb, ALU.mult)
            tt(kb, "dve", t2, t2[:], bi, bi[:], par, fib, ALU.mult)
            tt(kb, "dve", Bb[0], Bb[0][:], t1, t1[:], t2, t2[:], ALU.subtract)
            tt(kb, "dve", t1, t1[:], bi, bi[:], par, frb, ALU.mult)
            tt(kb, "dve", t2, t2[:], br, br[:], par, fib, ALU.mult)
            tt(kb, "dve", Bb[1], Bb[1][:], t1, t1[:], t2, t2[:], ALU.add)
            for ri, dstT in ((0, BTr), (1, BTi)):
                BD = kb.sb([128, 16, 32], BF16)
                memset(kb, "dve", BD, BD[:], 0.0)
                cp(kb, "dve", BD, BD[0:64, :, 0:16], Bb[ri], Bb[ri][0:64])
                cp(kb, "dve", BD, BD[64:128, :, 16:32], Bb[ri], Bb[ri][64:128])
                pT = kb.ps([128, 4, 128], BF16)
                for j in range(4):
                    kb.op("pe", lambda e, j=j: e.transpose(pT[:, j, :], BD[:, 4 * j:4 * j + 4, :].rearrange("p a b -> p (a b)"), identb[:]),
                          [BD, identb], [pT])
                cp(kb, "act", dstT, dstT[:], pT, pT[:])
            m0, m1 = kb.sb([128, 1], F32), kb.sb([128, 1], F32)
            memset(kb, "dve", m0, m0[:], 0.0)
            memset(kb, "dve", m1, m1[:], 1.0)
            for q in range(4):
                memset(kb, "dve", m0, m0[32 * q:32 * q + 16, :], 1.0)
                memset(kb, "dve", m1, m1[32 * q:32 * q + 16, :], 0.0)
            for ri, dstT, key in ((0, CTr, "s5_cre"), (1, CTi, "s5_cim")):
                ct = kb.sb([128, 4, 64], F32)
                kb.dma("sp", ct[:], G[key].rearrange("g c p -> (g c) p").rearrange("(j r) p -> r j p", r=128), writes=[ct])
                BD = kb.sb([128, 4, 128], BF16)
                ts(kb, "dve", BD, BD[:, :, 0:64], ct, ct[:], m0[:, 0:1], None, ALU.mult, extra=[m0])
                ts(kb, "dve", BD, BD[:, :, 64:128], ct, ct[:], m1[:, 0:1], None, ALU.mult, extra=[m1])
                pT = kb.ps([128, 4, 128], BF16)
                for j in range(4):
                    kb.op("pe", lambda e, j=j: e.transpose(pT[:, j, :], BD[:, j, :], identb[:]), [BD, identb], [pT])
                if ri == 0:
                    cp(kb, "act", dstT, dstT[:], pT, pT[:])
                else:
                    ts(kb, "dve", dstT, dstT[:], pT, pT[:], -1.0, None, ALU.mult)
            kb.barrier()
            kb.stack = old
        with contextlib.ExitStack() as rs:
            old = kb.stack
            kb.stack = rs
            uT = kb.sb([128, 4, S], BF16)
            kb.dma("sp", uT[:], fm(G["uT"]), reads=[G["b_qk0"]], writes=[uT])
            uTa = kb.sb([32, 4, S], BF16)
            cp(kb, "pool", uTa, uTa[:], uT, uT[96:128])
            BTra, BTia = kb.sb([32, 4, 128], BF16), kb.sb([32, 4, 128], BF16)
            cp(kb, "pool", BTra, BTra[:], BTr, BTr[96:128])
            cp(kb, "pool", BTia, BTia[:], BTi, BTi[96:128])
            jt = kb.sb([128, 513], F32)
            make_ramp(kb, jt, jt[:], 128, 513)
            rT = kb.sb([128, 513], F32)
            tmpA, tmpB = kb.sb([128, 513], F32), kb.sb([128, 513], F32)
            cTr = Ring([kb.sb([128, 513], F32) for _ in range(2)])
            sTr = Ring([kb.sb([128, 513], F32) for _ in range(2)])
            pbr = Ring([kb.ps([128, 512], F32) for _ in range(2)])
            pbi = Ring([kb.ps([128, 512], F32) for _ in range(2)])
            pyr = Ring([kb.ps([128, 4, 32], F32) for _ in range(2)])
            E = [Ring([kb.sb([128, 512], F32) for _ in range(2)]) for _ in range(6)]
            zrr = Ring([kb.sb([128, 512], F32) for _ in range(2)])
            zir = Ring([kb.sb([128, 512], F32) for _ in range(2)])
            xrr = Ring([kb.sb([128, 512], BF16) for _ in range(2)])
            xir = Ring([kb.sb([128, 512], BF16) for _ in range(2)])
            car = Ring([kb.sb([128, 4], F32) for _ in range(2)])
            ysr = Ring([kb.sb([128, 4, 32], F32) for _ in range(2)])
            yv = G["ytm"].rearrange("(c p) f -> p c f", p=128)
            for st in range(16):
                ts(kb, "dve", rT, rT[:], jt, jt[:], par[:, 1, st:st + 1], None, ALU.mult, extra=[par])
                cT, sT = cTr.next(), sTr.next()
                sincos_from_rev(kb, rT, rT[:], (128, 513), (tmpA, tmpB), sT, sT[:], cT, cT[:])
                magb = par[:, 0, st:st + 1].to_broadcast([128, 512])
                po = (st % 4) * 32
                prev = None
                for ck in range(8):
                    a, b = pbr.next(), pbi.next()
                    csl = slice(ck * 512, (ck + 1) * 512)
                    if st % 4 == 3:
                        mm(kb, a, a[:], BTra, BTra[0:32, st // 4, :], uTa, uTa[0:32, st // 4, csl])
                        mm(kb, b, b[:], BTia, BTia[0:32, st // 4, :], uTa, uTa[0:32, st // 4, csl])
                    else:
                        mm(kb, a, a[:], BTr, BTr[po:po + 32, st // 4, :], uT, uT[po:po + 32, st // 4, csl])
                        mm(kb, b, b[:], BTi, BTi[po:po + 32, st // 4, :], uT, uT[po:po + 32, st // 4, csl])
                    e = [r.next() for r in E]
                    tt(kb, "dve", e[0], e[0][:], a, a[:], cT, cT[:, 0:512], ALU.mult)
                    tt(kb, "dve", e[1], e[1][:], b, b[:], sT, sT[:, 0:512], ALU.mult)
                    tt(kb, "pool", e[0], e[0][:], e[0], e[0][:], e[1], e[1][:], ALU.add)
                    tt(kb, "dve", e[2], e[2][:], b, b[:], cT, cT[:, 0:512], ALU.mult)
                    tt(kb, "dve", e[3], e[3][:], a, a[:], sT, sT[:, 0:512], ALU.mult)
                    tt(kb, "pool", e[2], e[2][:], e[2], e[2][:], e[3], e[3][:], ALU.subtract)
                    zr, zi = zrr.next(), zir.next()
                    if prev is None:
                        i0, i1, ex = 0.0, 0.0, []
                    else:
                        pzr, pzi = prev
                        ca = car.next()
                        ts(kb, "dve", ca, ca[:, 0:1], pzi, pzi[:, 511:512], sT[:, 512:513], None, ALU.mult, extra=[sT])
                        stt(kb, "dve", ca, ca[:, 1:2], pzr, pzr[:, 511:512], cT[:, 512:513], ca, ca[:, 0:1], ALU.mult, ALU.subtract,
                            extra=[cT])
                        ts(kb, "dve", ca, ca[:, 2:3], pzi, pzi[:, 511:512], cT[:, 512:513], None, ALU.mult, extra=[cT])
                        stt(kb, "dve", ca, ca[:, 3:4], pzr, pzr[:, 511:512], sT[:, 512:513], ca, ca[:, 2:3], ALU.mult, ALU.add,
                            extra=[sT])
                        i0, i1, ex = ca[:, 1:2], ca[:, 3:4], [ca]
                    kb.op("dve", lambda g, i0=i0: g.tensor_tensor_scan(out=zr[:], data0=magb, data1=e[0][:], initial=i0,
                                                                       op0=ALU.mult, op1=ALU.add), [e[0], par] + ex, [zr])
                    kb.op("dve", lambda g, i1=i1: g.tensor_tensor_scan(out=zi[:], data0=magb, data1=e[2][:], initial=i1,
                                                                       op0=ALU.mult, op1=ALU.add), [e[2], par] + ex, [zi])
                    prev = (zr, zi)
                    xr, xi = xrr.next(), xir.next()
                    tt(kb, "pool", e[4], e[4][:], zr, zr[:], cT, cT[:, 0:512], ALU.mult)
                    tt(kb, "pool", e[5], e[5][:], zi, zi[:], sT, sT[:, 0:512], ALU.mult)
                    tt(kb, "pool", xr, xr[:], e[4], e[4][:], e[5], e[5][:], ALU.subtract)
                    tt(kb, "dve", e[1], e[1][:], zr, zr[:], sT, sT[:, 0:512], ALU.mult)
                    tt(kb, "pool", e[3], e[3][:], zi, zi[:], cT, cT[:, 0:512], ALU.mult)
                    tt(kb, "pool", xi, xi[:], e[1], e[1][:], e[3], e[3][:], ALU.add)
                    yp = pyr.next()
                    for tq in range(4):
                        mm(kb, yp, yp[:, tq, :], xr, xr[:, tq * 128:(tq + 1) * 128], CTr, CTr[:, st // 4, po:po + 32], start=True, stop=False)
                        mm(kb, yp, yp[:, tq, :], xi, xi[:, tq * 128:(tq + 1) * 128], CTi, CTi[:, st // 4, po:po + 32], start=False, stop=True)
                    ys = ysr.next()
                    cp(kb, "act", ys, ys[:], yp, yp[:])
                    kb.dma("sp", yv[:, ck * 4:ck * 4 + 4, st * 32:(st + 1) * 32], ys[:], reads=[ys], writes=[G["b_y"]])
            kb.barrier()
            kb.stack = old
        dbc = kb.sb([128, 512], F32)
        kb.dma("sp", dbc[:], G["s5_d"].to_broadcast([128, 512]), writes=[dbc])
        glw = kb.sb([128, 4, 512], BF16)
        kb.dma("pool", glw[:], G["s5_glw"].rearrange("(c p) n -> p c n", p=128), writes=[glw])
        glb = kb.sb([128, 4], F32)
        with kb.nc.allow_non_contiguous_dma(reason="tiny"):
            kb.dma("sp", glb[:], G["s5_glb"].rearrange("(c p) -> p c", p=128), writes=[glb])
        yr = Ring([kb.sb([128, 512], F32) for _ in range(2)])
        ur = Ring([kb.sb([128, 512], F32) for _ in range(2)])
        g1, g2 = kb.sb([128, 512], F32), kb.sb([128, 512], F32)
        gbr = Ring([kb.sb([128, 512], BF16) for _ in range(2)])
        pT = Ring([kb.ps([128, 4, 128], BF16) for _ in range(2)])
        gTr = Ring([kb.sb([128, 4, 128], BF16) for _ in range(2)])
        pz = Ring([kb.ps([128, 128], F32) for _ in range(2)])
        sgr = Ring([kb.sb([128, 128], F32) for _ in range(2)])
        osr = Ring([kb.sb([128, 4, 128], BF16) for _ in range(2)])
        ytv = G["ytm"].rearrange("(c p) f -> p c f", p=128)
        utv = G["utm"].rearrange("(c p) f -> p c f", p=128)
        o0v = G["o0T"][512:1024, :].rearrange("(c p) t -> p c t", p=128)
        for tk in range(32):
            y, u = yr.next(), ur.next()
            kb.dma("sp", y[:], ytv[:, tk, :], reads=[G["b_y"]], writes=[y])
            kb.dma("act", u[:], utv[:, tk, :], reads=[G["b_qk0"]], writes=[u])
            tt(kb, "dve", u, u[:], u, u[:], dbc, dbc[:], ALU.mult)
            tt(kb, "dve", y, y[:], y, y[:], u, u[:], ALU.add)
            gb_ = gbr.next()
            gelu_tanh(kb, y, y[:], gb_, gb_[:], (128, 512), g1, g2)
            p = pT.next()
            for c in range(4):
                kb.op("pe", lambda e, c=c: e.transpose(p[:, c, :], gb_[:, c * 128:(c + 1) * 128], identb[:]), [gb_, identb], [p])
            gT = gTr.next()
            cp(kb, "act", gT, gT[:], p, p[:])
            os_ = osr.next()
            for fo in range(4):
                z = pz.next()
                for c in range(4):
                    mm(kb, z, z[:], glw, glw[:, c, fo * 128:(fo + 1) * 128], gT, gT[:, c, :], start=(c == 0), stop=(c == 3))
                sg = sgr.next()
                act(kb, sg, sg[:], z, z[:], AF.Sigmoid, bias=glb[:, fo:fo + 1], extra=[glb])
                tt(kb, "dve", os_, os_[:, fo, :], gT, gT[:, fo, :], sg, sg[:], ALU.mult)
            kb.dma("sp", o0v[:, :, tk * 128:(tk + 1) * 128], os_[:], reads=[os_], writes=[G["b_o0"]])


def moba_select(kb, G):
    with kb.phase():
        identb = make_ident(kb, BF16)
        val = kb.sb([128, 32, 16], F32)
        nv = kb.sb([128, 16], F32)
        make_ramp(kb, nv, nv[:], 128, 16)
        tt(kb, "dve", val, val[:].rearrange("p (a b) n -> p a b n", b=2),
           nv, nv[:].unsqueeze(1).unsqueeze(1).to_broadcast([128, 16, 2, 16]),
           nv, nv[:].unsqueeze(2).unsqueeze(3).to_broadcast([128, 16, 2, 16]), ALU.subtract)
        addm, notown = kb.sb([128, 32, 16], F32), kb.sb([128, 32, 16], F32)
        ts(kb, "dve", addm, addm[:], val, val[:], 0.0, -1e30, ALU.is_ge, ALU.mult)
        ts(kb, "dve", notown, notown[:], val, val[:], 0.0, None, ALU.not_equal)
        qr = Ring([kb.sb([64, S], BF16) for _ in range(2)])
        kr = Ring([kb.sb([64, S], BF16) for _ in range(2)])
        km = Ring([kb.sb([64, 16], F32) for _ in range(2)])
        kmb = Ring([kb.sb([64, 16], BF16) for _ in range(2)])
        pg = Ring([kb.ps([128, 32, 16], F32) for _ in range(2)])
        gs = Ring([kb.sb([128, 32, 16], F32) for _ in range(2)])
        eq = kb.sb([128, 32, 16], F32)
        mx = Ring([kb.sb([128, 32], F32) for _ in range(2)])
        ngr = Ring([kb.sb([128, 32, 16], BF16) for _ in range(2)])
        ptr = Ring([kb.ps([16, 8, 128], BF16) for _ in range(2)])
        ngT = Ring([kb.sb([16, S], BF16) for _ in range(2)])
        for h in range(16):
            q, k = qr.next(), kr.next()
            kb.dma("sp", q[:], G["qaug1"][h, 0:64, :], reads=[G["b_qk1"]], writes=[q])
            kb.dma("act", k[:], G["kaug1"][h, 0:64, :], reads=[G["b_qk1"]], writes=[k])
            m_, mb = km.next(), kmb.next()
            kb.op("dve", lambda e: e.tensor_reduce(out=m_[:], in_=k[:].rearrange("p (n j) -> p n j", j=256), axis=AX.X, op=ALU.add),
                  [k], [m_])
            ts(kb, "dve", mb, mb[:], m_, m_[:], 1.0 / 256.0, None, ALU.mult)
            p = pg.next()
            for c in range(32):
                mm(kb, p, p[:, c, :], q, q[:, c * 128:(c + 1) * 128], mb, mb[:])
            g_ = gs.next()
            tt(kb, "dve", g_, g_[:], p, p[:], addm, addm[:], ALU.add)
            m = mx.next()
            for it in range(3):
                kb.op("dve", lambda e: e.tensor_reduce(out=m[:], in_=g_[:], axis=AX.X, op=ALU.max), [g_], [m])
                if it < 2:
                    tt(kb, "dve", eq, eq[:], g_, g_[:], m, m[:].unsqueeze(2).to_broadcast([128, 32, 16]), ALU.is_equal)
                    stt(kb, "dve", g_, g_[:], eq, eq[:], -1e30, g_, g_[:], ALU.mult, ALU.add)
            ts(kb, "dve", m, m[:], m, m[:], -1e29, None, ALU.max)
            tt(kb, "dve", eq, eq[:], p, p[:], addm, addm[:], ALU.add)
            tt(kb, "dve", eq, eq[:], eq, eq[:], m, m[:].unsqueeze(2).to_broadcast([128, 32, 16]), ALU.is_ge)
            ts(kb, "dve", eq, eq[:], eq, eq[:], -1.0, BIG, ALU.add, ALU.mult)
            ng = ngr.next()
            tt(kb, "dve", ng, ng[:], eq, eq[:], notown, notown[:], ALU.mult)
            nT = ngT.next()
            for c8 in range(4):
                pT = ptr.next()
                for j in range(8):
                    c = c8 * 8 + j
                    kb.op("pe", lambda e, c=c, j=j: e.transpose(pT[:, j, :], ng[:, c, :], identb[:]), [ng, identb], [pT])
                cp(kb, "act", nT, nT[:, c8 * 1024:(c8 + 1) * 1024].rearrange("p (j q) -> p j q", j=8), pT, pT[:])
            kb.dma("sp", G["qaug1"][h, 64:80, :], nT[:], reads=[nT], writes=[G["b_neg1"]])


def moba_attn(kb, G):
    with kb.phase():
        A = AttnCtx(kb, need_cmp=False)
        identb = make_ident(kb, BF16)
        Ec = kb.sb([128, S], BF16)
        memset(kb, "dve", Ec, Ec[:], 1.0)
        kb.op("pool", lambda g: g.affine_select(out=Ec[64:80, :], in_=Ec[64:80, :], pattern=[[1, S]], compare_op=ALU.is_ge,
                                                fill=0.0, base=0, channel_multiplier=-256), [Ec], [Ec])
        kb.op("pool", lambda g: g.affine_select(out=Ec[64:80, :], in_=Ec[64:80, :], pattern=[[-1, S]], compare_op=ALU.is_ge,
                                                fill=0.0, base=255, channel_multiplier=256), [Ec], [Ec])
        qr = Ring([kb.sb([80, S], BF16) for _ in range(2)])
        kr = Ring([kb.sb([80, S], BF16) for _ in range(2)])
        vr = Ring([kb.sb([128, 32, 65], BF16) for _ in range(2)])
        accr = Ring([kb.sb([128, 4, 64], F32) for _ in range(2)])
        otok = kb.sb([128, 8, 4, 128], BF16)
        ptr = Ring([kb.ps([128, 4, 128], BF16) for _ in range(2)])
        oT = Ring([kb.sb([128, 512], BF16) for _ in range(2)])
        v1v = G["v1"].rearrange("(kt p) b c -> p kt b c", p=128)
        for hp in range(8):
            for hh in range(2):
                h = hp * 2 + hh
                q, k, v = qr.next(), kr.next(), vr.next()
                kb.dma("sp", q[:], G["qaug1"][h], reads=[G["b_qk1"], G["b_neg1"]], writes=[q])
                kb.dma("act", k[0:64], G["kaug1"][h, 0:64, :], reads=[G["b_qk1"]], writes=[k])
                cp(kb, "pool", k, k[64:80, :], Ec, Ec[64:80, :])
                kb.dma("sp", v[:], v1v[:, :, h, :], reads=[G["b_qk1"]], writes=[v])
                for Q in range(8):
                    acc = accr.next()
                    attn_branch(A, 80, q, k, v, None, sched_causal, Q, acc, True,
                                after=lambda acc=acc, Q=Q, hh=hh: cp(kb, "act", otok, otok[:, Q, :, hh * 64:(hh + 1) * 64], acc, acc[:]))
            A.flush_all()
            for Q in range(8):
                pT = ptr.next()
                for s in range(4):
                    kb.op("pe", lambda e, s=s: e.transpose(pT[:, s, :], otok[:, Q, s, :], identb[:]), [otok, identb], [pT])
                o = oT.next()
                cp(kb, "dve", o, o[:].rearrange("p (s q) -> p s q", s=4), pT, pT[:])
                kb.dma("sp", G["o1T"][hp * 128:(hp + 1) * 128, Q * 512:(Q + 1) * 512], o[:], reads=[o], writes=[G["b_o1"]])


def build_program(debug=False, upto=99):
    nc = bass.Bass("TRN2", target_bir_lowering=False)
    G = {}

    def din(name, shape, dt=F32):
        return nc.dram_tensor(name, list(shape), dt, kind="ExternalInput").ap()

    def scr(name, shape, dt=F32, out=False):
        isout = out or (debug and (debug is True or name in debug))
        return nc.dram_tensor(name, list(shape), dt, kind="ExternalOutput" if isout else "Internal").ap()

    xT = din("xT", [D, S])
    w0r, w0s = din("w0r", [D, 14 * 64]), din("w0s", [D, 14 * 64])
    w0f, w0t = din("w0f", [D, 5 * 128]), din("w0t", [D, 920])
    w1r, w1s, w1t = din("w1r", [D, 32 * 64]), din("w1s", [D, 32 * 64]), din("w1t", [D, 1024])
    for wh in ("k", "v"):
        G["cmp_%s_w1" % wh] = din("cmp_%s_w1" % wh, [2048, 128])
        G["cmp_%s_w2" % wh] = din("cmp_%s_w2" % wh, [128, 64])
        G["pe_%s" % wh] = din("pe_%s" % wh, [32, 64])
    G["s5_lre"], G["s5_lim"], G["s5_ls"] = din("s5_lre", [32, 64]), din("s5_lim", [32, 64]), din("s5_ls", [32])
    G["s5_bre"], G["s5_bim"] = din("s5_bre", [32, 64, 16]), din("s5_bim", [32, 64, 16])
    G["s5_cre"], G["s5_cim"] = din("s5_cre", [32, 16, 64]), din("s5_cim", [32, 16, 64])
    G["s5_d"], G["s5_glw"], G["s5_glb"] = din("s5_d", [1, 512]), din("s5_glw", [512, 512]), din("s5_glb", [512])
    ev_wo, od_wo = din("ev_wo", [D, D]), din("od_wo", [D, D])
    ln = {k: din(k, [2, D]) for k in ("ln_mix_g", "ln_mix_b", "ln_ffn_g", "ln_ffn_b")}
    G["moe_wr"] = [din("moe_wr%d" % L, [D, 36]) for L in range(2)]
    G["moe_br"] = [din("moe_br%d" % L, [1, 36]) for L in range(2)]
    G["moe_wg"] = [din("moe_wg%d" % L, [32, D, 128]) for L in range(2)]
    G["moe_wu"] = [din("moe_wu%d" % L, [32, D, 128]) for L in range(2)]
    G["moe_wd"] = [din("moe_wd%d" % L, [32, 128, D]) for L in range(2)]
    yT = scr("yT", [D, S], out=True)
    G["ropeC"], G["ropeS"] = scr("ropeC", [64, S]), scr("ropeS", [64, S])
    G["qaug0"] = scr("qaug0", [8, 128, S], BF16)
    G["ksT"], G["kwT"], G["kcT"] = scr("ksT", [2, 64, S], BF16), scr("kwT", [2, 64, S], BF16), scr("kcT", [2, 64, S], BF16)
    G["vcT"], G["uT"] = scr("vcT", [128, S], BF16), scr("uT", [512, S], BF16)
    G["v3"] = scr("v3", [S, 6, 65], BF16)
    G["gates"], G["utm"], G["ytm"] = scr("gates", [S, 24]), scr("utm", [S, 512]), scr("ytm", [S, 512])
    G["kcmpT"], G["vcmp"] = scr("kcmpT", [2, 64, 256], BF16), scr("vcmp", [2, 128, 2, 65], BF16)
    G["o0T"], G["o1T"] = scr("o0T", [D, S], BF16), scr("o1T", [D, S], BF16)
    mixT = scr("mixT", [D, S])
    x1T, x2T, x3T = scr("x1T", [D, S]), scr("x2T", [D, S]), scr("x3T", [D, S])
    G["qaug1"], G["kaug1"] = scr("qaug1", [16, 80, S], BF16), scr("kaug1", [16, 64, S], BF16)
    G["v1"] = scr("v1", [S, 16, 65], BF16)
    for b in ("b_rope", "b_qk0", "b_cmp", "b_neg0", "b_o0", "b_y", "b_qk1", "b_neg1", "b_o1"):
        G[b] = Buf(b)
    bx, bmix, b1, b2, b3, by = Buf(), Buf(), Buf(), Buf(), Buf(), Buf()

    with contextlib.ExitStack() as st:
        kb = KB(nc, st)
        build_consts(kb, G)
        v3r = Ring([kb.sb([128, 6, 65], BF16) for _ in range(2)])
        gtr = Ring([kb.sb([128, 24], F32) for _ in range(2)])
        utr = Ring([kb.sb([128, 512], F32) for _ in range(2)])
        for it in v3r.items:
            memset(kb, "dve", it, it[:], 1.0)

        def rope_dst0(h):
            if h < 8:
                return G["qaug0"][h, 0:64, :], G["b_qk0"]
            g = (h - 8) % 2
            return (G["kcT"], G["ksT"], G["kwT"])[(h - 8) // 2][g], G["b_qk0"]

        def fm_dst0(c):
            if c == 0:
                return G["vcT"], G["b_qk0"]
            return G["uT"][(c - 1) * 128:c * 128, :], G["b_qk0"]

        def tm_cb0(kb, pt, ti):
            import os
            TM = os.environ.get("TM_PARTS", "vgu")
            v, g_, u = v3r.next(), gtr.next(), utr.next()
            rs = slice(ti * 128, (ti + 1) * 128)
            if "v" in TM:
                cp(kb, "act", v, v[:, :, 0:64], pt[0], pt[0][:, 0:384].rearrange("p (b c) -> p b c", b=6))
                kb.dma("sp", G["v3"][rs], v[:], reads=[v], writes=[G["b_qk0"]])
            if "g" in TM:
                act(kb, g_, g_[:], pt[0], pt[0][:, 384:408], AF.Sigmoid)
                kb.dma("sp", G["gates"][rs], g_[:], reads=[g_], writes=[G["b_qk0"]])
            if "u" in TM:
                cp(kb, "dve", u, u[:], pt[1], pt[1][:])
                kb.dma("sp", G["utm"][rs], u[:], reads=[u], writes=[G["b_qk0"]])

        if upto >= 1:
            inproj(kb, G, xT, bx, w0r, w0s, 14, rope_dst0, w0f, 5, fm_dst0, w0t, 920, tm_cb0, [(0, 408), (408, 920)])
        if upto >= 2:
            nsa_compress(kb, G)
            nsa_select(kb, G)
        if upto >= 3:
            nsa_attn(kb, G)
        if upto >= 4:
            s5(kb, G)
        if upto >= 5:
            linear_fm(kb, G, G["o0T"], ev_wo, mixT, in_buf=G["b_o0"], out_buf=bmix)
            res_ln(kb, G, xT, mixT, ln["ln_mix_g"][0], ln["ln_mix_b"][0], x1T, bx, bmix, b1)
            moe(kb, G, x1T, b1, 0, mixT, bmix)
            res_ln(kb, G, x1T, mixT, ln["ln_ffn_g"][0], ln["ln_ffn_b"][0], x2T, b1, bmix, b2)
        v1r = Ring([kb.sb([128, 16, 65], BF16) for _ in range(2)])
        for it in v1r.items:
            memset(kb, "dve", it, it[:], 1.0)

        def rope_dst1(h):
            if h < 16:
                return G["qaug1"][h, 0:64, :], G["b_qk1"]
            return G["kaug1"][h - 16], G["b_qk1"]

        def tm_cb1(kb, pt, ti):
            v = v1r.next()
            cp(kb, "act", v, v[:, 0:8, 0:64], pt[0], pt[0][:].rearrange("p (b c) -> p b c", b=8))
            cp(kb, "dve", v, v[:, 8:16, 0:64], pt[1], pt[1][:].rearrange("p (b c) -> p b c", b=8))
            kb.dma("sp", G["v1"][ti * 128:(ti + 1) * 128], v[:], reads=[v], writes=[G["b_qk1"]])

        if upto >= 6:
            inproj(kb, G, x2T, b2, w1r, w1s, 32, rope_dst1, None, 0, None, w1t, 1024, tm_cb1, [(0, 512), (512, 1024)])
            moba_select(kb, G)
            moba_attn(kb, G)
        if upto >= 7:
            linear_fm(kb, G, G["o1T"], od_wo, mixT, in_buf=G["b_o1"], out_buf=bmix)
            res_ln(kb, G, x2T, mixT, ln["ln_mix_g"][1], ln["ln_mix_b"][1], x3T, b2, bmix, b3)
            moe(kb, G, x3T, b3, 1, mixT, bmix)
            res_ln(kb, G, x3T, mixT, ln["ln_ffn_g"][1], ln["ln_ffn_b"][1], yT, b3, bmix, by)
        kb.barrier()
        print("instructions", kb.n_ins, "waits", kb.n_wait, flush=True)
    return nc


def _swap_cols(w, nh):
    w = w.reshape(w.shape[0], nh, 2, 32)
    return np.ascontiguousarray(w[:, :, ::-1, :].reshape(w.shape[0], nh * 64))


def prep_weights(inp):
    c = np.ascontiguousarray
    W = inp["ev_w_in"][0]
    q, kc, vc, ks, vs, kw, vw, gl, u = np.split(W, [512, 640, 768, 896, 1024, 1152, 1280, 1304], axis=1)
    m = {}
    rope = np.concatenate([q, kc, ks, kw], axis=1)
    m["w0r"] = c(rope)
    m["w0s"] = _swap_cols(rope, 14)
    m["w0f"] = c(np.concatenate([vc, u], axis=1))
    m["w0t"] = c(np.concatenate([vc, vs, vw, gl, u], axis=1))
    W1 = inp["od_w_in"][0]
    m["w1r"] = c(W1[:, 0:2048])
    m["w1s"] = _swap_cols(W1[:, 0:2048], 32)
    m["w1t"] = c(W1[:, 2048:3072])
    m["cmp_k_w1"], m["cmp_k_w2"], m["pe_k"] = c(inp["nsa_cmp_k_w1"][0]), c(inp["nsa_cmp_k_w2"][0]), c(inp["nsa_pe_k"][0])
    m["cmp_v_w1"], m["cmp_v_w2"], m["pe_v"] = c(inp["nsa_cmp_v_w1"][0]), c(inp["nsa_cmp_v_w2"][0]), c(inp["nsa_pe_v"][0])
    m["s5_lre"], m["s5_lim"], m["s5_ls"] = c(inp["s5_lambda_re"][0]), c(inp["s5_lambda_im"][0]), c(inp["s5_log_step"][0])
    m["s5_bre"], m["s5_bim"] = c(inp["s5_b_re"][0]), c(inp["s5_b_im"][0])
    m["s5_cre"], m["s5_cim"] = c(inp["s5_c_re"][0]), c(inp["s5_c_im"][0])
    m["s5_d"], m["s5_glw"], m["s5_glb"] = c(inp["s5_d"][0][None, :]), c(inp["s5_glu_w"][0]), c(inp["s5_glu_b"][0])
    m["ev_wo"], m["od_wo"] = c(inp["ev_w_out"][0]), c(inp["od_w_out"][0])
    for k in ("ln_mix_g", "ln_mix_b", "ln_ffn_g", "ln_ffn_b"):
        m[k] = c(inp[k])
    for L in range(2):
        m["moe_wr%d" % L] = c(np.concatenate([inp["moe_w_coarse"][L]] + [inp["moe_w_fine"][L, g] for g in range(4)], axis=1))
        m["moe_br%d" % L] = c(np.concatenate([inp["moe_b_coarse"][L]] + [inp["moe_b_fine"][L, g] for g in range(4)])[None, :])
        m["moe_wg%d" % L] = c(inp["moe_w_gate"][L].reshape(32, D, 128))
        m["moe_wu%d" % L] = c(inp["moe_w_up"][L].reshape(32, D, 128))
        m["moe_wd%d" % L] = c(inp["moe_w_down"][L].reshape(32, 128, D))
    return {k: np.asarray(v, dtype=np.float32) for k, v in m.items()}


def kernel(**inputs):
    inp = {k: np.asarray(v) for k, v in inputs.items()}
    nc = build_program()
    wm = prep_weights(inp)
    in_maps = []
    for b in range(8):
        m = dict(wm)
        m["xT"] = np.ascontiguousarray(inp["x"][b].T)
        in_maps.append(m)
    res = run_bass_kernel_spmd(nc, in_maps, core_ids=list(range(8)))
    out = np.stack([np.ascontiguousarray(res.results[b]["yT"].T) for b in range(8)], axis=0)
    return out.astype(np.float32)
```

```python
import contextlib
import math
import numpy as np
import concourse.bass as bass
import concourse.mybir as mybir
from concourse.bass_utils import run_bass_kernel_spmd

F32 = mybir.dt.float32
BF16 = mybir.dt.bfloat16
I32 = mybir.dt.int32
ALU = mybir.AluOpType
AF = mybir.ActivationFunctionType
AX = mybir.AxisListType

S = 4096
D = 1024
NT = 8
ALPHA = 4.0 ** 0.25
EPS = 1e-5
BIG = 240000.0
MAGIC = 12582912.0
TWO_PI = 2.0 * math.pi


class Buf:
    __slots__ = ("name", "last_w", "readers")

    def __init__(self, name=""):
        self.name = name
        self.last_w = None
        self.readers = {}


class T:
    __slots__ = ("t", "buf")

    def __init__(self, t, name=""):
        self.t = t
        self.buf = Buf(name)

    def __getitem__(self, idx):
        return self.t[idx]


class KB:
    NDMA = 48

    def __init__(self, nc, stack):
        self.nc = nc
        self.stack = stack
        self.eng = {"pe": nc.tensor, "act": nc.scalar, "dve": nc.vector,
                    "pool": nc.gpsimd, "sp": nc.sync}
        self.sems = {}
        for e in self.eng:
            self.sems[e] = stack.enter_context(nc.semaphore("s_" + e))
        self.cnt = {e: 0 for e in self.eng}
        self.dsem = [stack.enter_context(nc.semaphore("d%d" % i)) for i in range(self.NDMA)]
        self.dcnt = [0] * self.NDMA
        self.dnext = 0
        self.dnext_sw = 0
        self.waited = {}
        self.n_ins = 0
        self.n_wait = 0
        self._uid = 0

    def sb(self, shape, dtype=F32, name=None):
        self._uid += 1
        name = (name or "t") + "_%d" % self._uid
        t = self.stack.enter_context(self.nc.sbuf_tensor(name, list(shape), dtype))
        return T(t, name)

    def ps(self, shape, dtype=F32, name=None):
        self._uid += 1
        name = (name or "p") + "_%d" % self._uid
        t = self.stack.enter_context(self.nc.psum_tensor(name, list(shape), dtype))
        return T(t, name)

    def _sem(self, key):
        return self.sems[key] if isinstance(key, str) else self.dsem[key]

    def _wait(self, e, ev):
        key, val = ev
        if e == "pe" and key == "pe":
            return
        k = (e, key)
        if self.waited.get(k, 0) >= val:
            return
        self.waited[k] = val
        self.eng[e].wait_ge(self._sem(key), val)
        self.n_wait += 1

    @staticmethod
    def _b(b):
        return b.buf if isinstance(b, T) else b

    def _deps(self, e, reads, writes):
        for b in reads:
            b = self._b(b)
            if b.last_w is not None:
                self._wait(e, b.last_w)
        for b in writes:
            b = self._b(b)
            if b.last_w is not None:
                self._wait(e, b.last_w)
            for k, v in b.readers.items():
                self._wait(e, (k, v))

    def _mark(self, ev, reads, writes):
        key, val = ev
        for b in reads:
            b = self._b(b)
            if b.readers.get(key, 0) < val:
                b.readers[key] = val
        for b in writes:
            b = self._b(b)
            b.last_w = ev
            b.readers = {}

    def op(self, e, fn, reads=(), writes=()):
        self._deps(e, reads, writes)
        ins = fn(self.eng[e])
        self.cnt[e] += 1
        ins.then_inc(self.sems[e], 1)
        self._mark((e, self.cnt[e]), reads, writes)
        self.n_ins += 1
        return ins

    def dma(self, q, out, in_, reads=(), writes=(), **kw):
        self._deps(q, reads, writes)
        half = self.NDMA // 2
        if q == "pool":
            i = half + self.dnext_sw
            self.dnext_sw = (self.dnext_sw + 1) % (self.NDMA - half)
        else:
            i = self.dnext
            self.dnext = (self.dnext + 1) % half
        if self.dcnt[i] > 0:
            self._wait(q, (i, self.dcnt[i]))
        ins = self.eng[q].dma_start(out=out, in_=in_, **kw)
        self.dcnt[i] += 16
        ins.then_inc(self.dsem[i], 16)
        self._mark((i, self.dcnt[i]), reads, writes)
        self.n_ins += 1
        return ins

    def barrier(self):
        for e in self.eng:
            for k in self.eng:
                if self.cnt[k] > 0:
                    self._wait(e, (k, self.cnt[k]))
            for i in range(self.NDMA):
                if self.dcnt[i] > 0:
                    self._wait(e, (i, self.dcnt[i]))

    @contextlib.contextmanager
    def phase(self):
        with contextlib.ExitStack() as ps:
            old = self.stack
            self.stack = ps
            yield
            self.barrier()
            self.stack = old


class Ring:
    def __init__(self, items):
        self.items = items
        self.i = 0

    def next(self):
        it = self.items[self.i % len(self.items)]
        self.i += 1
        return it


def mm(kb, ot, oap, lt, lap, rt, rap, start=True, stop=True):
    kb.op("pe", lambda e: e.matmul(oap, lhsT=lap, rhs=rap, start=start, stop=stop,
                                   skip_group_check=True), [lt, rt], [ot])


def tt(kb, e, ot, oap, at, aap, bt, bap, op):
    kb.op(e, lambda g: g.tensor_tensor(out=oap, in0=aap, in1=bap, op=op), [at, bt], [ot])


def ts(kb, e, ot, oap, at, aap, s1, s2, op0, op1=None, extra=()):
    if op1 is None:
        kb.op(e, lambda g: g.tensor_scalar(out=oap, in0=aap, scalar1=s1, scalar2=None, op0=op0),
              [at] + list(extra), [ot])
    else:
        kb.op(e, lambda g: g.tensor_scalar(out=oap, in0=aap, scalar1=s1, scalar2=s2, op0=op0, op1=op1),
              [at] + list(extra), [ot])


def stt(kb, e, ot, oap, at, aap, sc, bt, bap, op0, op1, extra=()):
    kb.op(e, lambda g: g.scalar_tensor_tensor(out=oap, in0=aap, scalar=sc, in1=bap, op0=op0, op1=op1),
          [at, bt] + list(extra), [ot])


def act(kb, ot, oap, it, iap, func, scale=1.0, bias=None, extra=()):
    if bias is None:
        kb.op("act", lambda g: g.activation(out=oap, in_=iap, func=func, scale=scale), [it] + list(extra), [ot])
    else:
        kb.op("act", lambda g: g.activation(out=oap, in_=iap, func=func, scale=scale, bias=bias),
              [it] + list(extra), [ot])


def cp(kb, e, ot, oap, it, iap):
    if e == "act":
        kb.op("act", lambda g: g.activation(out=oap, in_=iap, func=AF.Copy), [it], [ot])
    elif e == "dve":
        kb.op("dve", lambda g: g.tensor_scalar(out=oap, in0=iap, scalar1=1.0, scalar2=None, op0=ALU.mult), [it], [ot])
    else:
        kb.op(e, lambda g: g.tensor_copy(out=oap, in_=iap), [it], [ot])


def memset(kb, e, ot, oap, val):
    kb.op(e, lambda g: g.memset(oap, val), [], [ot])


def fm(ap):
    return ap.rearrange("(c p) t -> p c t", p=128)


def build_consts(kb, G):
    with kb.phase():
        row = kb.sb([1, 64], F32)
        make_ramp(kb, row, row[:, 0:32], 1, 32)
        make_ramp(kb, row, row[:, 32:64], 1, 32)
        one1 = kb.sb([1, 1], F32)
        memset(kb, "dve", one1, one1[:], 1.0)
        pidx = kb.ps([64, 1], F32)
        mm(kb, pidx, pidx[:], row, row[:], one1, one1[:])
        idx = kb.sb([64, 1], F32)
        cp(kb, "act", idx, idx[:], pidx, pidx[:])
        inv = kb.sb([64, 1], F32)
        act(kb, inv, inv[:], idx, idx[:], AF.Exp, scale=-math.log(10000.0) / 32.0)
        ts(kb, "dve", inv, inv[:], inv, inv[:], 1.0 / TWO_PI, None, ALU.mult)
        tpos = kb.sb([64, S], F32)
        make_ramp(kb, tpos, tpos[:], 64, S)
        r = kb.sb([64, S], F32)
        rr = kb.sb([64, S], F32)
        tab = kb.sb([64, S], F32)
        for which in ("sin", "cos"):
            if which == "sin":
                ts(kb, "dve", r, r[:], tpos, tpos[:], inv[:, 0:1], None, ALU.mult, extra=[inv])
            else:
                ts(kb, "dve", r, r[:], tpos, tpos[:], inv[:, 0:1], None, ALU.mult, extra=[inv])
                ts(kb, "dve", r, r[:], r, r[:], 0.25, None, ALU.add)
            ts(kb, "dve", rr, rr[:], r, r[:], MAGIC, MAGIC, ALU.add, ALU.subtract)
            tt(kb, "dve", r, r[:], r, r[:], rr, rr[:], ALU.subtract)
            act(kb, tab, tab[:], r, r[:], AF.Sin, scale=TWO_PI)
            if which == "sin":
                ts(kb, "dve", tab, tab[0:32, :], tab, tab[0:32, :], -1.0, None, ALU.mult)
                kb.dma("sp", G["ropeS"], tab[:], reads=[tab], writes=[G["b_rope"]])
            else:
                kb.dma("sp", G["ropeC"], tab[:], reads=[tab], writes=[G["b_rope"]])


def linear_fm(kb, G, inT, w_dram, outT, n_in_chunks=8, n_out_chunks=8, in_buf=None, out_buf=None):
    with kb.phase():
        w = kb.sb([128, n_in_chunks, n_out_chunks * 128], BF16)
        kb.dma("pool", w[:], w_dram.rearrange("(c p) n -> p c n", p=128), reads=[], writes=[w])
        xin = Ring([kb.sb([128, n_in_chunks, 512], BF16) for _ in range(2)])
        ob = Ring([kb.sb([128, n_out_chunks, 512], F32) for _ in range(2)])
        pss = Ring([kb.ps([128, 512], F32) for _ in range(4)])
        inv = fm(inT)
        outv = fm(outT)
        for t in range(NT):
            x = xin.next()
            kb.dma("sp", x[:], inv[:, :, t * 512:(t + 1) * 512], reads=[in_buf], writes=[x])
            o = ob.next()
            for f in range(n_out_chunks):
                p = pss.next()
                for c in range(n_in_chunks):
                    mm(kb, p, p[:], w, w[:, c, f * 128:(f + 1) * 128], x, x[:, c, :],
                       start=(c == 0), stop=(c == n_in_chunks - 1))
                cp(kb, "act" if f % 2 == 0 else "dve", o, o[:, f, :], p, p[:])
            kb.dma("sp", outv[:, :, t * 512:(t + 1) * 512], o[:], reads=[o], writes=[out_buf])


def res_ln(kb, G, resT, addT, g_dram, b_dram, outT, res_buf, add_buf, out_buf):
    with kb.phase():
        ones = kb.sb([128, 128], F32)
        memset(kb, "dve", ones, ones[:], 1.0 / D)
        gb = kb.sb([128, 2, 8], F32)
        with kb.nc.allow_non_contiguous_dma(reason="tiny ln params"):
            kb.dma("sp", gb[:, 0, :], g_dram.rearrange("(c p) -> p c", p=128), writes=[gb])
            kb.dma("sp", gb[:, 1, :], b_dram.rearrange("(c p) -> p c", p=128), writes=[gb])
        rin = Ring([kb.sb([128, 8, 512], F32) for _ in range(2)])
        ain = Ring([kb.sb([128, 8, 512], F32) for _ in range(2)])
        zsq = Ring([kb.sb([128, 512], F32) for _ in range(2)])
        oo = Ring([kb.sb([128, 8, 512], F32) for _ in range(2)])
        ps1 = Ring([kb.ps([128, 512], F32) for _ in range(2)])
        ps2 = Ring([kb.ps([128, 512], F32) for _ in range(2)])
        mean = kb.sb([128, 512], F32)
        rstd = kb.sb([128, 512], F32)
        tmp = kb.sb([128, 512], F32)
        rv, av, ov = fm(resT), fm(addT), fm(outT)
        for t in range(NT):
            sl = slice(t * 512, (t + 1) * 512)
            r = rin.next()
            a = ain.next()
            kb.dma("sp", r[:], rv[:, :, sl], reads=[res_buf], writes=[r])
            kb.dma("act", a[:], av[:, :, sl], reads=[add_buf], writes=[a])
            p1, p2 = ps1.next(), ps2.next()
            for c in range(8):
                stt(kb, "dve", r, r[:, c, :], r, r[:, c, :], ALPHA, a, a[:, c, :], ALU.mult, ALU.add)
                z2 = zsq.next()
                act(kb, z2, z2[:], r, r[:, c, :], AF.Square)
                mm(kb, p1, p1[:], ones, ones[:], r, r[:, c, :], start=(c == 0), stop=(c == 7))
                mm(kb, p2, p2[:], ones, ones[:], z2, z2[:], start=(c == 0), stop=(c == 7))
            cp(kb, "act", mean, mean[:], p1, p1[:])
            tt(kb, "dve", tmp, tmp[:], mean, mean[:], mean, mean[:], ALU.mult)
            tt(kb, "dve", tmp, tmp[:], p2, p2[:], tmp, tmp[:], ALU.subtract)
            ts(kb, "dve", tmp, tmp[:], tmp, tmp[:], EPS, None, ALU.add)
            act(kb, tmp, tmp[:], tmp, tmp[:], AF.Sqrt)
            kb.op("dve", lambda g: g.reciprocal(out=rstd[:], in_=tmp[:]), [tmp], [rstd])
            o = oo.next()
            for c in range(8):
                e = "dve" if c % 2 == 0 else "pool"
                tt(kb, e, r, r[:, c, :], r, r[:, c, :], mean, mean[:], ALU.subtract)
                tt(kb, e, r, r[:, c, :], r, r[:, c, :], rstd, rstd[:], ALU.mult)
                ts(kb, e, o, o[:, c, :], r, r[:, c, :], gb[:, 0, c:c + 1], gb[:, 1, c:c + 1], ALU.mult, ALU.add,
                   extra=[gb])
            kb.dma("sp", ov[:, :, sl], o[:], reads=[o], writes=[out_buf])


def inproj(kb, G, xT, x_buf, w_rope, w_swap, n_rope, rope_dst, w_fm, n_fm_chunks, fm_dst, w_tm, tm_cols, tm_cb, tm_splits):
    with kb.phase():
        wr = kb.sb([128, 8, n_rope * 64], BF16)
        ws = kb.sb([128, 8, n_rope * 64], BF16)
        kb.dma("pool", wr[:], w_rope.rearrange("(c p) n -> p c n", p=128), writes=[wr])
        kb.dma("pool", ws[:], w_swap.rearrange("(c p) n -> p c n", p=128), writes=[ws])
        if n_fm_chunks:
            wf = kb.sb([128, 8, n_fm_chunks * 128], BF16)
            kb.dma("pool", wf[:], w_fm.rearrange("(c p) n -> p c n", p=128), writes=[wf])
        wt = kb.sb([128, 8, tm_cols], BF16)
        kb.dma("pool", wt[:], w_tm.rearrange("(c p) n -> p c n", p=128), writes=[wt])
        xin = Ring([kb.sb([128, 8, 512], BF16) for _ in range(2)])
        assert n_rope % 2 == 0
        cc = Ring([kb.sb([128, 512], F32) for _ in range(2)])
        ss = Ring([kb.sb([128, 512], F32) for _ in range(2)])
        pa = Ring([kb.ps([128, 512], F32) for _ in range(2)])
        pb = Ring([kb.ps([128, 512], F32) for _ in range(2)])
        pf = Ring([kb.ps([128, 512], F32) for _ in range(2)])
        ntm = len(tm_splits)
        pt = [kb.ps([128, 512], F32) for _ in range(ntm)]
        t1 = Ring([kb.sb([128, 512], F32) for _ in range(2)])
        t2 = Ring([kb.sb([128, 512], F32) for _ in range(2)])
        ro = Ring([kb.sb([128, 512], BF16) for _ in range(3)])
        fo = Ring([kb.sb([128, 512], BF16) for _ in range(2)])
        xv = fm(xT)
        import os
        SK = os.environ.get("INPROJ_SKIP", "")
        for t in range(NT):
            sl = slice(t * 512, (t + 1) * 512)
            x = xin.next()
            kb.dma("pool", x[:], xv[:, :, sl], reads=[x_buf], writes=[x])
            c_, s_ = cc.next(), ss.next()
            for k in range(2):
                kb.dma("sp", c_[64 * k:64 * k + 64], G["ropeC"][:, sl], reads=[G["b_rope"]], writes=[c_])
                kb.dma("sp", s_[64 * k:64 * k + 64], G["ropeS"][:, sl], reads=[G["b_rope"]], writes=[s_])
            for hp in range(0 if "r" in SK else n_rope // 2):
                a, b = pa.next(), pb.next()
                for c in range(8):
                    mm(kb, a, a[:], wr, wr[:, c, hp * 128:(hp + 1) * 128], x, x[:, c, :], start=(c == 0), stop=(c == 7))
                for c in range(8):
                    mm(kb, b, b[:], ws, ws[:, c, hp * 128:(hp + 1) * 128], x, x[:, c, :], start=(c == 0), stop=(c == 7))
                u1, u2, o = t1.next(), t2.next(), ro.next()
                tt(kb, "dve", u1, u1[:], a, a[:], c_, c_[:], ALU.mult)
                tt(kb, "dve", u2, u2[:], b, b[:], s_, s_[:], ALU.mult)
                tt(kb, "dve" if "p" in SK else "pool", o, o[:], u1, u1[:], u2, u2[:], ALU.add)
                for k in range(2):
                    dst, dbuf = rope_dst(2 * hp + k)
                    kb.dma("sp", dst[:, sl], o[64 * k:64 * k + 64, :], reads=[o], writes=[dbuf])
            for f in range(0 if "f" in SK else n_fm_chunks):
                p = pf.next()
                for c in range(8):
                    mm(kb, p, p[:], wf, wf[:, c, f * 128:(f + 1) * 128], x, x[:, c, :], start=(c == 0), stop=(c == 7))
                o = fo.next()
                cp(kb, "act", o, o[:], p, p[:])
                dst, dbuf = fm_dst(f)
                kb.dma("sp", dst[:, sl], o[:], reads=[o], writes=[dbuf])
            for sub in range(0 if "t" in SK else 4):
                for j in range(ntm):
                    n0, n1 = tm_splits[j]
                    for c in range(8):
                        mm(kb, pt[j], pt[j][:, 0:n1 - n0], x, x[:, c, sub * 128:(sub + 1) * 128], wt, wt[:, c, n0:n1],
                           start=(c == 0), stop=(c == 7))
                tm_cb(kb, pt, t * 4 + sub)


def moe(kb, G, xT, x_buf, L, outT, out_buf):
    wr_d, br_d = G["moe_wr"][L], G["moe_br"][L]
    wg_d, wu_d, wd_d = G["moe_wg"][L], G["moe_wu"][L], G["moe_wd"][L]
    xv = fm(xT)
    with kb.phase():
        gT = kb.sb([32, S], BF16)
        with contextlib.ExitStack() as rs:
            old = kb.stack
            kb.stack = rs
            wr = kb.sb([128, 8, 36], F32)
            kb.dma("sp", wr[:], wr_d.rearrange("(c p) n -> p c n", p=128), writes=[wr])
            br = kb.sb([1, 36], F32)
            kb.dma("sp", br[:], br_d, writes=[br])
            ones1 = kb.sb([1, 128], F32)
            memset(kb, "dve", ones1, ones1[:], 1.0)
            ident = kb.sb([128, 128], F32)
            memset(kb, "dve", ident, ident[:], 1.0)
            kb.op("pool", lambda g: g.affine_select(out=ident[:], in_=ident[:], pattern=[[-1, 128]],
                                                    compare_op=ALU.is_equal, fill=0.0, base=0, channel_multiplier=1),
                  [ident], [ident])
            xin = Ring([kb.sb([128, 8, 512], F32) for _ in range(2)])
            pl = Ring([kb.ps([128, 36], F32) for _ in range(2)])
            ptr = Ring([kb.ps([32, 128], F32) for _ in range(2)])
            sm = Ring([kb.sb([128, 160], F32) for _ in range(2)])
            gt = Ring([kb.sb([128, 32], F32) for _ in range(2)])
            for t in range(NT):
                x = xin.next()
                kb.dma("sp", x[:], xv[:, :, t * 512:(t + 1) * 512], reads=[x_buf], writes=[x])
                for sub in range(4):
                    p = pl.next()
                    for c in range(8):
                        mm(kb, p, p[:], x, x[:, c, sub * 128:(sub + 1) * 128], wr, wr[:, c, :], start=(c == 0), stop=False)
                    mm(kb, p, p[:], ones1, ones1[:], br, br[:], start=False, stop=True)
                    w = sm.next()
                    cp(kb, "act", w, w[:, 0:36], p, p[:])
                    kb.op("dve", lambda g: g.tensor_reduce(out=w[:, 36:37], in_=w[:, 0:4], axis=AX.X, op=ALU.max), [w], [w])
                    ts(kb, "dve", w, w[:, 37:38], w, w[:, 36:37], -1.0, None, ALU.mult)
                    act(kb, w, w[:, 38:42], w, w[:, 0:4], AF.Exp, bias=w[:, 37:38])
                    kb.op("dve", lambda g: g.tensor_reduce(out=w[:, 42:43], in_=w[:, 38:42], axis=AX.X, op=ALU.add), [w], [w])
                    kb.op("dve", lambda g: g.reciprocal(out=w[:, 43:44], in_=w[:, 42:43]), [w], [w])
                    ts(kb, "dve", w, w[:, 44:48], w, w[:, 0:4], w[:, 36:37], None, ALU.is_equal)
                    tt(kb, "dve", w, w[:, 48:80].rearrange("p (g e) -> p g e", g=4),
                       w, w[:, 4:36].rearrange("p (g e) -> p g e", g=4),
                       w, w[:, 44:48].unsqueeze(2).to_broadcast([128, 4, 8]), ALU.mult)
                    kb.op("dve", lambda g: g.tensor_reduce(out=w[:, 80:88], in_=w[:, 48:80].rearrange("p (g e) -> p e g", g=4),
                                                           axis=AX.X, op=ALU.add), [w], [w])
                    kb.op("dve", lambda g: g.max(out=w[:, 88:96], in_=w[:, 80:88]), [w], [w])
                    tt(kb, "dve", w, w[:, 96:97], w, w[:, 88:89], w, w[:, 89:90], ALU.subtract)
                    act(kb, w, w[:, 97:98], w, w[:, 96:97], AF.Sigmoid)
                    tt(kb, "dve", w, w[:, 98:99], w, w[:, 97:98], w, w[:, 43:44], ALU.mult)
                    tt(kb, "dve", w, w[:, 99:100], w, w[:, 43:44], w, w[:, 98:99], ALU.subtract)
                    ts(kb, "dve", w, w[:, 100:108], w, w[:, 80:88], w[:, 88:89], w[:, 98:99], ALU.is_equal, ALU.mult)
                    ts(kb, "dve", w, w[:, 108:116], w, w[:, 80:88], w[:, 89:90], w[:, 99:100], ALU.is_equal, ALU.mult)
                    tt(kb, "dve", w, w[:, 116:124], w, w[:, 100:108], w, w[:, 108:116], ALU.add)
                    gg = gt.next()
                    tt(kb, "dve", gg, gg[:].rearrange("p (g e) -> p g e", g=4),
                       w, w[:, 44:48].unsqueeze(2).to_broadcast([128, 4, 8]),
                       w, w[:, 116:124].unsqueeze(1).to_broadcast([128, 4, 8]), ALU.mult)
                    pT = ptr.next()
                    kb.op("pe", lambda e: e.transpose(pT[:], gg[:], ident[:]), [gg, ident], [pT])
                    q0 = (t * 4 + sub) * 128
                    cp(kb, "act", gT, gT[:, q0:q0 + 128], pT, pT[:])
            kb.barrier()
            kb.stack = old
        sel = kb.sb([32, 32, 128], BF16)
        memset(kb, "dve", sel, sel[:], 1.0)
        kb.op("pool", lambda g: g.affine_select(out=sel[:], in_=sel[:], pattern=[[-1, 32], [0, 128]],
                                                compare_op=ALU.is_equal, fill=0.0, base=0, channel_multiplier=1),
              [sel], [sel])
        TS = 2048
        xb = kb.sb([128, 8, TS], BF16)
        yacc = kb.sb([128, 8, TS], F32)
        hq = kb.sb([128, 4, TS], BF16)
        wgr = Ring([kb.sb([128, 8, 128], BF16) for _ in range(2)])
        wur = Ring([kb.sb([128, 8, 128], BF16) for _ in range(2)])
        wdr = Ring([kb.sb([128, 1024], BF16) for _ in range(8)])
        pg = Ring([kb.ps([128, 512], F32) for _ in range(2)])
        pu = Ring([kb.ps([128, 512], F32) for _ in range(2)])
        pc = Ring([kb.ps([128, 512], F32) for _ in range(2)])
        py = Ring([kb.ps([128, 512], F32) for _ in range(2)])
        sl_ = Ring([kb.sb([128, 512], F32) for _ in range(2)])
        t1_ = Ring([kb.sb([128, 512], F32) for _ in range(2)])
        ov = fm(outT)
        for st in range(S // TS):
            kb.dma("pool", xb[:], xv[:, :, st * TS:(st + 1) * TS], reads=[x_buf], writes=[xb])
            for q in range(8):
                wds = []
                for ei in range(4):
                    e = q * 4 + ei
                    wg, wu, wd = wgr.next(), wur.next(), wdr.next()
                    kb.dma("pool", wg[:], wg_d[e].rearrange("(c p) n -> p c n", p=128), writes=[wg])
                    kb.dma("pool", wu[:], wu_d[e].rearrange("(c p) n -> p c n", p=128), writes=[wu])
                    kb.dma("pool", wd[:], wd_d[e], writes=[wd])
                    wds.append(wd)
                    for tq in range(TS // 512):
                        tsl = slice(tq * 512, (tq + 1) * 512)
                        a, b, c_ = pg.next(), pu.next(), pc.next()
                        for c in range(8):
                            mm(kb, a, a[:], wg, wg[:, c, :], xb, xb[:, c, tsl], start=(c == 0), stop=(c == 7))
                        for c in range(8):
                            mm(kb, b, b[:], wu, wu[:, c, :], xb, xb[:, c, tsl], start=(c == 0), stop=(c == 7))
                        g0 = st * TS + tq * 512
                        mm(kb, c_, c_[:], sel, sel[:, e, :], gT, gT[:, g0:g0 + 512])
                        s1, u1 = sl_.next(), t1_.next()
                        act(kb, s1, s1[:], a, a[:], AF.Silu)
                        tt(kb, "dve", u1, u1[:], c_, c_[:], s1, s1[:], ALU.mult)
                        tt(kb, "dve", hq, hq[:, ei, tsl], b, b[:], u1, u1[:], ALU.mult)
                for tq in range(TS // 512):
                    tsl = slice(tq * 512, (tq + 1) * 512)
                    for f in range(8):
                        y = py.next()
                        for ei in range(4):
                            mm(kb, y, y[:], wds[ei], wds[ei][:, f * 128:(f + 1) * 128], hq, hq[:, ei, tsl],
                               start=(ei == 0), stop=(ei == 3))
                        if q == 0:
                            cp(kb, "act", yacc, yacc[:, f, tsl], y, y[:])
                        else:
                            tt(kb, "pool" if False else "dve", yacc, yacc[:, f, tsl], y, y[:], yacc, yacc[:, f, tsl], ALU.add)
            kb.dma("sp", ov[:, :, st * TS:(st + 1) * TS], yacc[:], reads=[yacc], writes=[out_buf])


def sched_causal(Q):
    out = []
    for kt in range(4 * Q + 4):
        r = kt - 4 * Q
        out.append((kt, max(0, r), 4, [(r, "tri")] if r >= 0 else [], None))
    return out


def sched_win(Q):
    out = []
    for kt in range(max(0, 4 * Q - 2), 4 * Q + 4):
        r = kt - 4 * Q
        s0, s1 = max(0, r), min(3, r + 2) + 1
        m = []
        if r >= 0:
            m.append((r, "tri"))
        if 0 <= r + 2 <= 3:
            m.append((r + 2, "ntri"))
        out.append((kt, s0, s1, m, None))
    return out


def sched_cmp(Q):
    out = [(0, 0, 4, [], "cmp")]
    if Q >= 4:
        out.append((1, 0, 4, [], "cmp"))
    return out


class AttnCtx:
    def __init__(self, kb, need_cmp):
        self.kb = kb
        self.tri = kb.sb([128, 128], BF16)
        self.ntri = kb.sb([128, 128], BF16)
        memset(kb, "dve", self.tri, self.tri[:], 1.0)
        memset(kb, "dve", self.ntri, self.ntri[:], 1.0)
        kb.op("pool", lambda g: g.affine_select(out=self.tri[:], in_=self.tri[:], pattern=[[1, 128]],
                                                compare_op=ALU.is_ge, fill=0.0, base=0, channel_multiplier=-1),
              [self.tri], [self.tri])
        kb.op("pool", lambda g: g.affine_select(out=self.ntri[:], in_=self.ntri[:], pattern=[[-1, 128]],
                                                compare_op=ALU.is_gt, fill=0.0, base=0, channel_multiplier=1),
              [self.ntri], [self.ntri])
        self.cmpm = None
        if need_cmp:
            self.cmpm = kb.sb([128, 2, S], BF16)
            memset(kb, "dve", self.cmpm, self.cmpm[:], 1.0)
            for j in range(2):
                kb.op("pool", lambda g, j=j: g.affine_select(out=self.cmpm[:, j, :], in_=self.cmpm[:, j, :], pattern=[[1, S]],
                                                             compare_op=ALU.is_ge, fill=0.0, base=-31 - 2048 * j,
                                                             channel_multiplier=-16), [self.cmpm], [self.cmpm])
        self.zl = kb.sb([128, 128], BF16)
        self.zr = kb.sb([128, 512], BF16)
        memset(kb, "dve", self.zl, self.zl[:], 0.0)
        memset(kb, "dve", self.zr, self.zr[:], 0.0)
        self.psS = Ring([kb.ps([128, 512], F32) for _ in range(3)])
        self.psO = Ring([kb.ps([128, 4, 128], F32) for _ in range(3)])
        self.pT = Ring([kb.sb([128, 512], BF16) for _ in range(3)])
        self.den = Ring([kb.sb([128, 4], F32) for _ in range(3)])
        self.coef = Ring([kb.sb([128, 4], F32) for _ in range(3)])
        self.tmp = Ring([kb.sb([128, 4, 64], F32) for _ in range(2)])
        self.mask_eng = Ring(["pool", "dve"])
        self.pend = []
        self.LA = 2

    def push(self, fn):
        self.pend.append(fn)
        while len(self.pend) > self.LA:
            self.pend.pop(0)()

    def flush_all(self):
        while self.pend:
            self.pend.pop(0)()


def attn_branch(A, Kc, q_t, k_t, v_t, nkeys_last, sched, Q, acc_t, first, gate_t=None, gate_ap=None, after=None):
    kb = A.kb
    po = A.psO.next()
    mm(kb, po, po[:].rearrange("p a b -> p (a b)"), A.zl, A.zl[:], A.zr, A.zr[:], start=True, stop=True)

    def fin():
        den, coef = A.den.next(), A.coef.next()
        ts(kb, "dve", den, den[:], po, po[:, :, 64], 1e-30, None, ALU.max)
        kb.op("dve", lambda g: g.reciprocal(out=coef[:], in_=den[:]), [den], [coef])
        if gate_t is not None:
            tt(kb, "dve", coef, coef[:], coef, coef[:], gate_t, gate_ap, ALU.mult)
        cb = coef[:].unsqueeze(2).to_broadcast([128, 4, 64])
        if first:
            tt(kb, "dve", acc_t, acc_t[:], po, po[:, :, 0:64], coef, cb, ALU.mult)
        else:
            tmp = A.tmp.next()
            tt(kb, "dve", tmp, tmp[:], po, po[:, :, 0:64], coef, cb, ALU.mult)
            tt(kb, "pool", acc_t, acc_t[:], acc_t, acc_t[:], tmp, tmp[:], ALU.add)
        if after is not None:
            after()

    items = sched(Q)
    for idx, (kt, s0, s1, masks, full) in enumerate(items):
        ksz = 128
        if nkeys_last is not None and (kt + 1) * 128 > nkeys_last:
            ksz = nkeys_last - kt * 128
        c0, c1 = s0 * 128, s1 * 128
        ps = A.psS.next()
        mm(kb, ps, ps[0:ksz, c0:c1], k_t, k_t[0:Kc, kt * 128:kt * 128 + ksz], q_t, q_t[0:Kc, Q * 512 + c0:Q * 512 + c1])
        p = A.pT.next()
        act(kb, p, p[0:ksz, c0:c1], ps, ps[0:ksz, c0:c1], AF.Exp, scale=0.125)
        for (s, name) in masks:
            m = A.tri if name == "tri" else A.ntri
            tt(kb, A.mask_eng.next(), p, p[0:ksz, s * 128:(s + 1) * 128], p, p[0:ksz, s * 128:(s + 1) * 128], m, m[0:ksz, :], ALU.mult)
        if full == "cmp":
            tt(kb, A.mask_eng.next(), p, p[0:ksz, c0:c1], p, p[0:ksz, c0:c1], A.cmpm, A.cmpm[0:ksz, kt, Q * 512 + c0:Q * 512 + c1], ALU.mult)
        last = idx == len(items) - 1

        def pv(p=p, ksz=ksz, kt=kt, s0=s0, s1=s1, last=last):
            for s in range(s0, s1):
                mm(kb, po, po[:, s, 0:65], p, p[0:ksz, s * 128:(s + 1) * 128], v_t, v_t[0:ksz, kt, :], start=False, stop=False)
            if last:
                fin()

        A.push(pv)


def make_ramp(kb, t, ap, parts, n, start=0.0):
    ones = kb.sb([parts, n], F32)
    memset(kb, "dve", ones, ones[:], 1.0)
    kb.op("dve", lambda g: g.tensor_tensor_scan(out=ap, data0=ones[:], data1=ones[:], initial=float(start) - 1.0,
                                                op0=ALU.mult, op1=ALU.add), [ones], [t])


def make_ident(kb, dtype=F32):
    ident = kb.sb([128, 128], dtype)
    memset(kb, "dve", ident, ident[:], 1.0)
    kb.op("pool", lambda g: g.affine_select(out=ident[:], in_=ident[:], pattern=[[-1, 128]],
                                            compare_op=ALU.is_equal, fill=0.0, base=0, channel_multiplier=1),
          [ident], [ident])
    return ident


def gelu_tanh(kb, z, zap, out_t, out_ap, shape, tmp1, tmp2):
    a1 = tmp1[tuple(slice(0, s) for s in shape)]
    a2 = tmp2[tuple(slice(0, s) for s in shape)]
    act(kb, tmp1, a1, z, zap, AF.Square)
    ts(kb, "dve", tmp1, a1, tmp1, a1, 0.044715, 1.0, ALU.mult, ALU.add)
    tt(kb, "dve", tmp1, a1, tmp1, a1, z, zap, ALU.mult)
    act(kb, tmp2, a2, tmp1, a1, AF.Sigmoid, scale=1.5957691216057308)
    tt(kb, "dve", out_t, out_ap, z, zap, tmp2, a2, ALU.mult)


def nsa_compress(kb, G):
    with kb.phase():
        ident = make_ident(kb)
        pps = kb.ps([64, 32], F32)
        pb = kb.ps([128, 1], F32)
        ph = kb.ps([128, 256], F32)
        pk = kb.ps([64, 256], F32)
        pv = kb.ps([128, 64], F32)
        for which in ("k", "v"):
            w1_d, w2_d, pe_d = G["cmp_%s_w1" % which], G["cmp_%s_w2" % which], G["pe_%s" % which]
            w1 = kb.sb([128, 32, 128], BF16)
            w1v = w1_d.rearrange("(l d) f -> d l f", d=64)
            kb.dma("pool", w1[0:64], w1v, writes=[w1])
            kb.dma("pool", w1[64:128], w1v, writes=[w1])
            w2 = kb.sb([128, 64], BF16)
            kb.dma("pool", w2[:], w2_d, writes=[w2])
            pe = kb.sb([32, 64], F32)
            kb.dma("sp", pe[:], pe_d, writes=[pe])
            kb.op("pe", lambda e: e.transpose(pps[:], pe[:], ident[0:32, 0:32]), [pe, ident], [pps])
            peT = kb.sb([64, 32], BF16)
            cp(kb, "act", peT, peT[:], pps, pps[:])
            for l in range(32):
                mm(kb, pb, pb[:], w1, w1[0:64, l, :], peT, peT[:, l:l + 1], start=(l == 0), stop=(l == 31))
            bias = kb.sb([128, 1], F32)
            cp(kb, "act", bias, bias[:], pb, pb[:])
            src = kb.sb([128, S], BF16)
            if which == "k":
                kb.dma("sp", src[0:64], G["kcT"][0], reads=[G["b_qk0"]], writes=[src])
                kb.dma("sp", src[64:128], G["kcT"][1], reads=[G["b_qk0"]], writes=[src])
            else:
                kb.dma("sp", src[:], G["vcT"], reads=[G["b_qk0"]], writes=[src])
            for g in range(2):
                for l in range(32):
                    mm(kb, ph, ph[:, 0:255], w1, w1[g * 64:(g + 1) * 64, l, :], src, src[g * 64:(g + 1) * 64, l:l + 4065:16],
                       start=(l == 0), stop=(l == 31))
                z = kb.sb([128, 255], F32)
                act(kb, z, z[:], ph, ph[:, 0:255], AF.Identity, bias=bias[:, 0:1], extra=[bias])
                hid = kb.sb([128, 256], BF16)
                t1, t2 = kb.sb([128, 255], F32), kb.sb([128, 255], F32)
                gelu_tanh(kb, z, z[:], hid, hid[:, 0:255], (128, 255), t1, t2)
                if which == "k":
                    mm(kb, pk, pk[:, 0:255], w2, w2[:], hid, hid[:, 0:255])
                    kc = kb.sb([64, 256], BF16)
                    memset(kb, "dve", kc, kc[:], 0.0)
                    cp(kb, "act", kc, kc[:, 0:255], pk, pk[:, 0:255])
                    kb.dma("sp", G["kcmpT"][g], kc[:], reads=[kc], writes=[G["b_cmp"]])
                else:
                    vcs = kb.sb([128, 2, 65], BF16)
                    memset(kb, "dve", vcs, vcs[:], 0.0)
                    memset(kb, "dve", vcs, vcs[:, :, 64:65], 1.0)
                    for j in range(2):
                        nsz = 128 if j == 0 else 127
                        mm(kb, pv, pv[0:nsz, :], hid, hid[:, j * 128:j * 128 + nsz], w2, w2[:])
                        cp(kb, "act", vcs, vcs[0:nsz, j, 0:64], pv, pv[0:nsz, :])
                    kb.dma("sp", G["vcmp"][g], vcs[:], reads=[vcs], writes=[G["b_cmp"]])


def nsa_select(kb, G):
    with kb.phase():
        identb = make_ident(kb, BF16)
        val = kb.sb([128, 128], F32)
        make_ramp(kb, val, val[:], 128, 128, start=-64.0)
        ts(kb, "dve", val, val[64:128, :], val, val[64:128, :], -1.0, None, ALU.add)
        KM, AM, fut = kb.sb([128, 128], F32), kb.sb([128, 128], F32), kb.sb([128, 128], F32)
        ts(kb, "dve", KM, KM[:], val, val[:], -2.0, None, ALU.is_le)
        ts(kb, "dve", fut, fut[:], val, val[:], 1.0, None, ALU.is_ge)
        ts(kb, "dve", AM, AM[:], KM, KM[:], -1000.0, 1000.0, ALU.mult, ALU.add)
        stt(kb, "dve", AM, AM[:], fut, fut[:], -1001.0, AM, AM[:], ALU.mult, ALU.add)
        Mc = kb.sb([128, 8], F32)
        memset(kb, "dve", Mc, Mc[:], 1.0)
        kb.op("pool", lambda g: g.affine_select(out=Mc[:], in_=Mc[:], pattern=[[-16, 8]], compare_op=ALU.is_ge,
                                                fill=0.0, base=-15, channel_multiplier=1), [Mc], [Mc])
        q4 = kb.sb([64, 4, S], BF16)
        kcm = kb.sb([64, 256], BF16)
        negT = kb.sb([64, S], BF16)
        psc = Ring([kb.ps([128, 4, 256], F32) for _ in range(2)])
        ptr = Ring([kb.ps([64, 128], BF16) for _ in range(2)])
        pcs = Ring([kb.sb([128, 4, 256], F32) for _ in range(2)])
        Psum = Ring([kb.sb([128, 256], F32) for _ in range(2)])
        sm = Ring([kb.sb([128, 160], F32) for _ in range(2)])
        sc = Ring([kb.sb([128, 64], F32) for _ in range(2)])
        ngb = Ring([kb.sb([128, 64], BF16) for _ in range(2)])
        for g in range(2):
            for z in range(4):
                kb.dma("sp", q4[:, z, :], G["qaug0"][g * 4 + z, 0:64, :], reads=[G["b_qk0"]], writes=[q4])
            kb.dma("sp", kcm[:], G["kcmpT"][g], reads=[G["b_cmp"]], writes=[kcm])
            for it in pcs.items + Psum.items:
                memset(kb, "pool", it, it[:], 0.0)
            for c in range(32):
                ncv = min(255, 8 * c + 7)
                p = psc.next()
                for z in range(4):
                    mm(kb, p, p[:, z, 0:ncv], q4, q4[:, z, c * 128:(c + 1) * 128], kcm, kcm[:, 0:ncv])
                pc = pcs.next()
                act(kb, pc, pc[:, :, 0:ncv], p, p[:, :, 0:ncv], AF.Exp, scale=0.125)
                if c == 0:
                    tt(kb, "dve", pc, pc[:, :, 0:7], pc, pc[:, :, 0:7], Mc, Mc[:, 1:8].unsqueeze(1).to_broadcast([128, 4, 7]), ALU.mult)
                else:
                    tt(kb, "dve", pc, pc[:, :, ncv - 8:ncv], pc, pc[:, :, ncv - 8:ncv], Mc,
                       Mc[:, 0:8].unsqueeze(1).to_broadcast([128, 4, 8]), ALU.mult)
                w = sm.next()
                kb.op("dve", lambda e: e.tensor_reduce(out=w[:, 0:4], in_=pc[:, :, 0:ncv], axis=AX.X, op=ALU.add), [pc], [w])
                ts(kb, "dve", w, w[:, 4:8], w, w[:, 0:4], 1e-30, None, ALU.max)
                kb.op("dve", lambda e: e.reciprocal(out=w[:, 8:12], in_=w[:, 4:8]), [w], [w])
                P_ = Psum.next()
                ts(kb, "dve", P_, P_[:, 0:ncv], pc, pc[:, 0, 0:ncv], w[:, 8:9], None, ALU.mult, extra=[w])
                for z in range(1, 4):
                    stt(kb, "dve", P_, P_[:, 0:ncv], pc, pc[:, z, 0:ncv], w[:, 8 + z:9 + z], P_, P_[:, 0:ncv], ALU.mult, ALU.add,
                        extra=[w])
                s_ = sc.next()
                kb.op("dve", lambda e: e.tensor_reduce(out=w[:, 16:80], in_=P_[:].rearrange("p (j r) -> p j r", r=4),
                                                       axis=AX.X, op=ALU.add), [P_], [w])
                stt(kb, "dve", s_, s_[:], P_, P_[:, 3:256:4], -0.5, w, w[:, 16:80], ALU.mult, ALU.add)
                stt(kb, "dve", s_, s_[:, 1:64], P_, P_[:, 3:252:4], 0.5, s_, s_[:, 1:64], ALU.mult, ALU.add)
                tt(kb, "dve", s_, s_[:], s_, s_[:], KM, KM[:, 64 - 2 * c:128 - 2 * c], ALU.mult)
                tt(kb, "dve", s_, s_[:], s_, s_[:], AM, AM[:, 64 - 2 * c:128 - 2 * c], ALU.add)
                memset(kb, "dve", s_, s_[:, 0:1], 1000.0)
                kb.op("dve", lambda e: e.max(out=w[:, 80:88], in_=s_[:]), [s_], [w])
                ts(kb, "dve", w, w[:, 88:89], w, w[:, 87:88], 0.0, None, ALU.max)
                ts(kb, "dve", s_, s_[:], s_, s_[:], w[:, 88:89], None, ALU.is_ge, extra=[w])
                nb = ngb.next()
                ts(kb, "dve", nb, nb[:], s_, s_[:], -1.0, BIG, ALU.add, ALU.mult)
                pT = ptr.next()
                kb.op("pe", lambda e: e.transpose(pT[:], nb[:], identb[:]), [nb, identb], [pT])
                cp(kb, "act", negT, negT[:, c * 128:(c + 1) * 128], pT, pT[:])
            for z in range(4):
                kb.dma("sp", G["qaug0"][g * 4 + z, 64:128, :], negT[:], reads=[negT], writes=[G["b_neg0"]])


def nsa_attn(kb, G):
    with kb.phase():
        A = AttnCtx(kb, need_cmp=True)
        identb = make_ident(kb, BF16)
        Ec = kb.sb([128, S], BF16)
        memset(kb, "dve", Ec, Ec[:], 1.0)
        kb.op("pool", lambda g: g.affine_select(out=Ec[64:128, :], in_=Ec[64:128, :], pattern=[[1, S]], compare_op=ALU.is_ge,
                                                fill=0.0, base=0, channel_multiplier=-64), [Ec], [Ec])
        kb.op("pool", lambda g: g.affine_select(out=Ec[64:128, :], in_=Ec[64:128, :], pattern=[[-1, S]], compare_op=ALU.is_ge,
                                                fill=0.0, base=63, channel_multiplier=64), [Ec], [Ec])
        gates = kb.sb([128, 32, 24], F32)
        kb.dma("sp", gates[:], G["gates"].rearrange("(c p) n -> p c n", p=128), reads=[G["b_qk0"]], writes=[gates])
        ks = kb.sb([128, S], BF16)
        kw = kb.sb([64, S], BF16)
        kcm = kb.sb([64, 256], BF16)
        vs = kb.sb([128, 32, 65], BF16)
        vw = kb.sb([128, 32, 65], BF16)
        vcm = kb.sb([128, 2, 65], BF16)
        qr = Ring([kb.sb([128, S], BF16) for _ in range(2)])
        accr = Ring([kb.sb([128, 4, 64], F32) for _ in range(2)])
        otok = kb.sb([128, 8, 4, 128], BF16)
        ptr = Ring([kb.ps([128, 4, 128], BF16) for _ in range(2)])
        oT = Ring([kb.sb([128, 512], BF16) for _ in range(2)])
        v3v = G["v3"].rearrange("(kt p) b c -> p kt b c", p=128)
        for hp in range(4):
            g = hp // 2
            if hp % 2 == 0:
                kb.dma("sp", ks[0:64], G["ksT"][g], reads=[G["b_qk0"]], writes=[ks])
                cp(kb, "pool", ks, ks[64:128, :], Ec, Ec[64:128, :])
                kb.dma("sp", kw[:], G["kwT"][g], reads=[G["b_qk0"]], writes=[kw])
                kb.dma("sp", kcm[:], G["kcmpT"][g], reads=[G["b_cmp"]], writes=[kcm])
                kb.dma("sp", vs[:], v3v[:, :, 2 + g, :], reads=[G["b_qk0"]], writes=[vs])
                kb.dma("sp", vw[:], v3v[:, :, 4 + g, :], reads=[G["b_qk0"]], writes=[vw])
                kb.dma("sp", vcm[:], G["vcmp"][g], reads=[G["b_cmp"]], writes=[vcm])
            for hh in range(2):
                h = hp * 2 + hh
                q = qr.next()
                kb.dma("sp", q[:], G["qaug0"][h], reads=[G["b_qk0"], G["b_neg0"]], writes=[q])
                for Q in range(8):
                    acc = accr.next()
                    attn_branch(A, 64, q, kcm, vcm, 255, sched_cmp, Q, acc, True, gates, gates[:, 4 * Q:4 * Q + 4, h * 3 + 0])
                    attn_branch(A, 128, q, ks, vs, None, sched_causal, Q, acc, False, gates, gates[:, 4 * Q:4 * Q + 4, h * 3 + 1])
                    attn_branch(A, 64, q, kw, vw, None, sched_win, Q, acc, False, gates, gates[:, 4 * Q:4 * Q + 4, h * 3 + 2],
                                after=lambda acc=acc, Q=Q, hh=hh: cp(kb, "act", otok, otok[:, Q, :, hh * 64:(hh + 1) * 64], acc, acc[:]))
            A.flush_all()
            for Q in range(8):
                pT = ptr.next()
                for s in range(4):
                    kb.op("pe", lambda e, s=s: e.transpose(pT[:, s, :], otok[:, Q, s, :], identb[:]), [otok, identb], [pT])
                o = oT.next()
                cp(kb, "dve", o, o[:].rearrange("p (s q) -> p s q", s=4), pT, pT[:])
                kb.dma("sp", G["o0T"][hp * 128:(hp + 1) * 128, Q * 512:(Q + 1) * 512], o[:], reads=[o], writes=[G["b_o0"]])


def sincos_from_rev(kb, r, rap, shape, tmp, out_sin, out_sin_ap, out_cos, out_cos_ap):
    a1 = tmp[0][tuple(slice(0, s) for s in shape)]
    a2 = tmp[1][tuple(slice(0, s) for s in shape)]
    ts(kb, "dve", tmp[0], a1, r, rap, MAGIC, MAGIC, ALU.add, ALU.subtract)
    tt(kb, "dve", tmp[0], a1, r, rap, tmp[0], a1, ALU.subtract)
    act(kb, out_sin, out_sin_ap, tmp[0], a1, AF.Sin, scale=TWO_PI)
    ts(kb, "dve", tmp[1], a2, r, rap, 0.25, None, ALU.add)
    ts(kb, "dve", tmp[0], a1, tmp[1], a2, MAGIC, MAGIC, ALU.add, ALU.subtract)
    tt(kb, "dve", tmp[0], a1, tmp[1], a2, tmp[0], a1, ALU.subtract)
    act(kb, out_cos, out_cos_ap, tmp[0], a1, AF.Sin, scale=TWO_PI)


def s5(kb, G):
    with kb.phase():
        ident = make_ident(kb)
        identb = make_ident(kb, BF16)
        par = kb.sb([128, 4, 16], F32)
        with contextlib.ExitStack() as rs:
            old = kb.stack
            kb.stack = rs
            lr, li = kb.sb([16, 128], F32), kb.sb([16, 128], F32)
            kb.dma("sp", lr[:], G["s5_lre"].rearrange("(s w) p -> s (w p)", w=2), writes=[lr])
            kb.dma("sp", li[:], G["s5_lim"].rearrange("(s w) p -> s (w p)", w=2), writes=[li])
            ls = kb.sb([16, 2], F32)
            kb.dma("sp", ls[:], G["s5_ls"].rearrange("(s w) -> s w", w=2), writes=[ls])
            act(kb, ls, ls[:], ls, ls[:], AF.Exp)
            W = [kb.sb([16, 128], F32) for _ in range(12)]
            stepb, mag, thr, sn, cs, ar1, ai, den, fr, fi, ta, tb = W
            cp(kb, "dve", stepb, stepb[:].rearrange("s (w p) -> s w p", w=2), ls, ls[:].unsqueeze(2).to_broadcast([16, 2, 64]))
            tt(kb, "dve", ta, ta[:], lr, lr[:], stepb, stepb[:], ALU.mult)
            act(kb, mag, mag[:], ta, ta[:], AF.Exp)
            tt(kb, "dve", thr, thr[:], li, li[:], stepb, stepb[:], ALU.mult)
            ts(kb, "dve", thr, thr[:], thr, thr[:], 1.0 / TWO_PI, None, ALU.mult)
            sincos_from_rev(kb, thr, thr[:], (16, 128), (ta, tb), sn, sn[:], cs, cs[:])
            tt(kb, "dve", ai, ai[:], mag, mag[:], sn, sn[:], ALU.mult)
            tt(kb, "dve", ar1, ar1[:], mag, mag[:], cs, cs[:], ALU.mult)
            ts(kb, "dve", ar1, ar1[:], ar1, ar1[:], -1.0, None, ALU.add)
            tt(kb, "dve", ta, ta[:], lr, lr[:], lr, lr[:], ALU.mult)
            tt(kb, "dve", tb, tb[:], li, li[:], li, li[:], ALU.mult)
            tt(kb, "dve", den, den[:], ta, ta[:], tb, tb[:], ALU.add)
            kb.op("dve", lambda e: e.reciprocal(out=den[:], in_=den[:]), [den], [den])
            tt(kb, "dve", ta, ta[:], ar1, ar1[:], lr, lr[:], ALU.mult)
            tt(kb, "dve", tb, tb[:], ai, ai[:], li, li[:], ALU.mult)
            tt(kb, "dve", fr, fr[:], ta, ta[:], tb, tb[:], ALU.add)
            tt(kb, "dve", fr, fr[:], fr, fr[:], den, den[:], ALU.mult)
            tt(kb, "dve", ta, ta[:], ai, ai[:], lr, lr[:], ALU.mult)
            tt(kb, "dve", tb, tb[:], ar1, ar1[:], li, li[:], ALU.mult)
            tt(kb, "dve", fi, fi[:], ta, ta[:], tb, tb[:], ALU.subtract)
            tt(kb, "dve", fi, fi[:], fi, fi[:], den, den[:], ALU.mult)
            pp = kb.ps([128, 4, 16], F32)
            for i, src in enumerate((mag, thr, fr, fi)):
                kb.op("pe", lambda e, i=i, src=src: e.transpose(pp[:, i, :], src[:], ident[0:16, 0:16]), [src, ident], [pp])
            cp(kb, "act", par, par[:], pp, pp[:])
            kb.barrier()
            kb.stack = old
        BTr, BTi = kb.sb([128, 4, 128], BF16), kb.sb([128, 4, 128], BF16)
        CTr, CTi = kb.sb([128, 4, 128], BF16), kb.sb([128, 4, 128], BF16)
        with contextlib.ExitStack() as rs:
            old = kb.stack
            kb.stack = rs
            br, bi = kb.sb([128, 16, 16], F32), kb.sb([128, 16, 16], F32)
            kb.dma("sp", br[:], G["s5_bre"].rearrange("(s w) p c -> (w p) s c", w=2), writes=[br])
            kb.dma("sp", bi[:], G["s5_bim"].rearrange("(s w) p c -> (w p) s c", w=2), writes=[bi])
            frb = par[:, 2, :].unsqueeze(2).to_broadcast([128, 16, 16])
            fib = par[:, 3, :].unsqueeze(2).to_broadcast([128, 16, 16])
            t1, t2 = kb.sb([128, 16, 16], F32), kb.sb([128, 16, 16], F32)
            Bb = [kb.sb([128, 16, 16], F32), kb.sb([128, 16, 16], F32)]
            tt(kb, "dve", t1, t1[:], br, br[:], par, frb, ALU.mult)
            tt(kb, "dve", t2, t2[:], bi, bi[:], par, fib, ALU.mult)
            tt(kb, "dve", Bb[0], Bb[0][:], t1, t1[:], t2, t2[:], ALU.subtract)
            tt(kb, "dve", t1, t1[:], bi, bi[:], par, frb, ALU.mult)
            tt(kb, "dve", t2, t2[:], br, br[:], par, fib, ALU.mult)
            tt(kb, "dve", Bb[1], Bb[1][:], t1, t1[:], t2, t2[:], ALU.add)
            for ri, dstT in ((0, BTr), (1, BTi)):
                BD = kb.sb([128, 16, 32], BF16)
                memset(kb, "dve", BD, BD[:], 0.0)
                cp(kb, "dve", BD, BD[0:64, :, 0:16], Bb[ri], Bb[ri][0:64])
                cp(kb, "dve", BD, BD[64:128, :, 16:32], Bb[ri], Bb[ri][64:128])
                pT = kb.ps([128, 4, 128], BF16)
                for j in range(4):
                    kb.op("pe", lambda e, j=j: e.transpose(pT[:, j, :], BD[:, 4 * j:4 * j + 4, :].rearrange("p a b -> p (a b)"), identb[:]),
                          [BD, identb], [pT])
                cp(kb, "act", dstT, dstT[:], pT, pT[:])
            m0, m1 = kb.sb([128, 1], F32), kb.sb([128, 1], F32)
            memset(kb, "dve", m0, m0[:], 0.0)
            memset(kb, "dve", m1, m1[:], 1.0)
            for q in range(4):
                memset(kb, "dve", m0, m0[32 * q:32 * q + 16, :], 1.0)
                memset(kb, "dve", m1, m1[32 * q:32 * q + 16, :], 0.0)
            for ri, dstT, key in ((0, CTr, "s5_cre"), (1, CTi, "s5_cim")):
                ct = kb.sb([128, 4, 64], F32)
                kb.dma("sp", ct[:], G[key].rearrange("g c p -> (g c) p").rearrange("(j r) p -> r j p", r=128), writes=[ct])
                BD = kb.sb([128, 4, 128], BF16)
                ts(kb, "dve", BD, BD[:, :, 0:64], ct, ct[:], m0[:, 0:1], None, ALU.mult, extra=[m0])
                ts(kb, "dve", BD, BD[:, :, 64:128], ct, ct[:], m1[:, 0:1], None, ALU.mult, extra=[m1])
                pT = kb.ps([128, 4, 128], BF16)
                for j in range(4):
                    kb.op("pe", lambda e, j=j: e.transpose(pT[:, j, :], BD[:, j, :], identb[:]), [BD, identb], [pT])
                if ri == 0:
                    cp(kb, "act", dstT, dstT[:], pT, pT[:])
                else:
                    ts(kb, "dve", dstT, dstT[:], pT, pT[:], -1.0, None, ALU.mult)
            kb.barrier()
            kb.stack = old
        with contextlib.ExitStack() as rs:
            old = kb.stack
            kb.stack = rs
            uT = kb.sb([128, 4, S], BF16)
            kb.dma("sp", uT[:], fm(G["uT"]), reads=[G["b_qk0"]], writes=[uT])
            uTa = kb.sb([32, 4, S], BF16)
            cp(kb, "pool", uTa, uTa[:], uT, uT[96:128])
            BTra, BTia = kb.sb([32, 4, 128], BF16), kb.sb([32, 4, 128], BF16)
            cp(kb, "pool", BTra, BTra[:], BTr, BTr[96:128])
            cp(kb, "pool", BTia, BTia[:], BTi, BTi[96:128])
            jt = kb.sb([128, 513], F32)
            make_ramp(kb, jt, jt[:], 128, 513)
            rT = kb.sb([128, 513], F32)
            tmpA, tmpB = kb.sb([128, 513], F32), kb.sb([128, 513], F32)
            cTr = Ring([kb.sb([128, 513], F32) for _ in range(2)])
            sTr = Ring([kb.sb([128, 513], F32) for _ in range(2)])
            pbr = Ring([kb.ps([128, 512], F32) for _ in range(2)])
            pbi = Ring([kb.ps([128, 512], F32) for _ in range(2)])
            pyr = Ring([kb.ps([128, 4, 32], F32) for _ in range(2)])
            E = [Ring([kb.sb([128, 512], F32) for _ in range(2)]) for _ in range(6)]
            zrr = Ring([kb.sb([128, 512], F32) for _ in range(2)])
            zir = Ring([kb.sb([128, 512], F32) for _ in range(2)])
            xrr = Ring([kb.sb([128, 512], BF16) for _ in range(2)])
            xir = Ring([kb.sb([128, 512], BF16) for _ in range(2)])
            car = Ring([kb.sb([128, 4], F32) for _ in range(2)])
            ysr = Ring([kb.sb([128, 4, 32], F32) for _ in range(2)])
            yv = G["ytm"].rearrange("(c p) f -> p c f", p=128)
            for st in range(16):
                ts(kb, "dve", rT, rT[:], jt, jt[:], par[:, 1, st:st + 1], None, ALU.mult, extra=[par])
                cT, sT = cTr.next(), sTr.next()
                sincos_from_rev(kb, rT, rT[:], (128, 513), (tmpA, tmpB), sT, sT[:], cT, cT[:])
                magb = par[:, 0, st:st + 1].to_broadcast([128, 512])
                po = (st % 4) * 32
                prev = None
                for ck in range(8):
                    a, b = pbr.next(), pbi.next()
                    csl = slice(ck * 512, (ck + 1) * 512)
                    if st % 4 == 3:
                        mm(kb, a, a[:], BTra, BTra[0:32, st // 4, :], uTa, uTa[0:32, st // 4, csl])
                        mm(kb, b, b[:], BTia, BTia[0:32, st // 4, :], uTa, uTa[0:32, st // 4, csl])
                    else:
                        mm(kb, a, a[:], BTr, BTr[po:po + 32, st // 4, :], uT, uT[po:po + 32, st // 4, csl])
                        mm(kb, b, b[:], BTi, BTi[po:po + 32, st // 4, :], uT, uT[po:po + 32, st // 4, csl])
                    e = [r.next() for r in E]
                    tt(kb, "dve", e[0], e[0][:], a, a[:], cT, cT[:, 0:512], ALU.mult)
                    tt(kb, "dve", e[1], e[1][:], b, b[:], sT, sT[:, 0:512], ALU.mult)
                    tt(kb, "pool", e[0], e[0][:], e[0], e[0][:], e[1], e[1][:], ALU.add)
                    tt(kb, "dve", e[2], e[2][:], b, b[:], cT, cT[:, 0:512], ALU.mult)
                    tt(kb, "dve", e[3], e[3][:], a, a[:], sT, sT[:, 0:512], ALU.mult)
                    tt(kb, "pool", e[2], e[2][:], e[2], e[2][:], e[3], e[3][:], ALU.subtract)
                    zr, zi = zrr.next(), zir.next()
                    if prev is None:
                        i0, i1, ex = 0.0, 0.0, []
                    else:
                        pzr, pzi = prev
                        ca = car.next()
                        ts(kb, "dve", ca, ca[:, 0:1], pzi, pzi[:, 511:512], sT[:, 512:513], None, ALU.mult, extra=[sT])
                        stt(kb, "dve", ca, ca[:, 1:2], pzr, pzr[:, 511:512], cT[:, 512:513], ca, ca[:, 0:1], ALU.mult, ALU.subtract,
                            extra=[cT])
                        ts(kb, "dve", ca, ca[:, 2:3], pzi, pzi[:, 511:512], cT[:, 512:513], None, ALU.mult, extra=[cT])
                        stt(kb, "dve", ca, ca[:, 3:4], pzr, pzr[:, 511:512], sT[:, 512:513], ca, ca[:, 2:3], ALU.mult, ALU.add,
                            extra=[sT])
                        i0, i1, ex = ca[:, 1:2], ca[:, 3:4], [ca]
                    kb.op("dve", lambda g, i0=i0: g.tensor_tensor_scan(out=zr[:], data0=magb, data1=e[0][:], initial=i0,
                                                                       op0=ALU.mult, op1=ALU.add), [e[0], par] + ex, [zr])
                    kb.op("dve", lambda g, i1=i1: g.tensor_tensor_scan(out=zi[:], data0=magb, data1=e[2][:], initial=i1,
                                                                       op0=ALU.mult, op1=ALU.add), [e[2], par] + ex, [zi])
                    prev = (zr, zi)
                    xr, xi = xrr.next(), xir.next()
                    tt(kb, "pool", e[4], e[4][:], zr, zr[:], cT, cT[:, 0:512], ALU.mult)
                    tt(kb, "pool", e[5], e[5][:], zi, zi[:], sT, sT[:, 0:512], ALU.mult)
                    tt(kb, "pool", xr, xr[:], e[4], e[4][:], e[5], e[5][:], ALU.subtract)
                    tt(kb, "dve", e[1], e[1][:], zr, zr[:], sT, sT[:, 0:512], ALU.mult)
                    tt(kb, "pool", e[3], e[3][:], zi, zi[:], cT, cT[:, 0:512], ALU.mult)
                    tt(kb, "pool", xi, xi[:], e[1], e[1][:], e[3], e[3][:], ALU.add)
                    yp = pyr.next()
                    for tq in range(4):
                        mm(kb, yp, yp[:, tq, :], xr, xr[:, tq * 128:(tq + 1) * 128], CTr, CTr[:, st // 4, po:po + 32], start=True, stop=False)
                        mm(kb, yp, yp[:, tq, :], xi, xi[:, tq * 128:(tq + 1) * 128], CTi, CTi[:, st // 4, po:po + 32], start=False, stop=True)
                    ys = ysr.next()
                    cp(kb, "act", ys, ys[:], yp, yp[:])
                    kb.dma("sp", yv[:, ck * 4:ck * 4 + 4, st * 32:(st + 1) * 32], ys[:], reads=[ys], writes=[G["b_y"]])
            kb.barrier()
            kb.stack = old
        dbc = kb.sb([128, 512], F32)
        kb.dma("sp", dbc[:], G["s5_d"].to_broadcast([128, 512]), writes=[dbc])
        glw = kb.sb([128, 4, 512], BF16)
        kb.dma("pool", glw[:], G["s5_glw"].rearrange("(c p) n -> p c n", p=128), writes=[glw])
        glb = kb.sb([128, 4], F32)
        with kb.nc.allow_non_contiguous_dma(reason="tiny"):
            kb.dma("sp", glb[:], G["s5_glb"].rearrange("(c p) -> p c", p=128), writes=[glb])
        yr = Ring([kb.sb([128, 512], F32) for _ in range(2)])
        ur = Ring([kb.sb([128, 512], F32) for _ in range(2)])
        g1, g2 = kb.sb([128, 512], F32), kb.sb([128, 512], F32)
        gbr = Ring([kb.sb([128, 512], BF16) for _ in range(2)])
        pT = Ring([kb.ps([128, 4, 128], BF16) for _ in range(2)])
        gTr = Ring([kb.sb([128, 4, 128], BF16) for _ in range(2)])
        pz = Ring([kb.ps([128, 128], F32) for _ in range(2)])
        sgr = Ring([kb.sb([128, 128], F32) for _ in range(2)])
        osr = Ring([kb.sb([128, 4, 128], BF16) for _ in range(2)])
        ytv = G["ytm"].rearrange("(c p) f -> p c f", p=128)
        utv = G["utm"].rearrange("(c p) f -> p c f", p=128)
        o0v = G["o0T"][512:1024, :].rearrange("(c p) t -> p c t", p=128)
        for tk in range(32):
            y, u = yr.next(), ur.next()
            kb.dma("sp", y[:], ytv[:, tk, :], reads=[G["b_y"]], writes=[y])
            kb.dma("act", u[:], utv[:, tk, :], reads=[G["b_qk0"]], writes=[u])
            tt(kb, "dve", u, u[:], u, u[:], dbc, dbc[:], ALU.mult)
            tt(kb, "dve", y, y[:], y, y[:], u, u[:], ALU.add)
            gb_ = gbr.next()
            gelu_tanh(kb, y, y[:], gb_, gb_[:], (128, 512), g1, g2)
            p = pT.next()
            for c in range(4):
                kb.op("pe", lambda e, c=c: e.transpose(p[:, c, :], gb_[:, c * 128:(c + 1) * 128], identb[:]), [gb_, identb], [p])
            gT = gTr.next()
            cp(kb, "act", gT, gT[:], p, p[:])
            os_ = osr.next()
            for fo in range(4):
                z = pz.next()
                for c in range(4):
                    mm(kb, z, z[:], glw, glw[:, c, fo * 128:(fo + 1) * 128], gT, gT[:, c, :], start=(c == 0), stop=(c == 3))
                sg = sgr.next()
                act(kb, sg, sg[:], z, z[:], AF.Sigmoid, bias=glb[:, fo:fo + 1], extra=[glb])
                tt(kb, "dve", os_, os_[:, fo, :], gT, gT[:, fo, :], sg, sg[:], ALU.mult)
            kb.dma("sp", o0v[:, :, tk * 128:(tk + 1) * 128], os_[:], reads=[os_], writes=[G["b_o0"]])


def moba_select(kb, G):
    with kb.phase():
        identb = make_ident(kb, BF16)
        val = kb.sb([128, 32, 16], F32)
        nv = kb.sb([128, 16], F32)
        make_ramp(kb, nv, nv[:], 128, 16)
        tt(kb, "dve", val, val[:].rearrange("p (a b) n -> p a b n", b=2),
           nv, nv[:].unsqueeze(1).unsqueeze(1).to_broadcast([128, 16, 2, 16]),
           nv, nv[:].unsqueeze(2).unsqueeze(3).to_broadcast([128, 16, 2, 16]), ALU.subtract)
        addm, notown = kb.sb([128, 32, 16], F32), kb.sb([128, 32, 16], F32)
        ts(kb, "dve", addm, addm[:], val, val[:], 0.0, -1e30, ALU.is_ge, ALU.mult)
        ts(kb, "dve", notown, notown[:], val, val[:], 0.0, None, ALU.not_equal)
        qr = Ring([kb.sb([64, S], BF16) for _ in range(2)])
        kr = Ring([kb.sb([64, S], BF16) for _ in range(2)])
        km = Ring([kb.sb([64, 16], F32) for _ in range(2)])
        kmb = Ring([kb.sb([64, 16], BF16) for _ in range(2)])
        pg = Ring([kb.ps([128, 32, 16], F32) for _ in range(2)])
        gs = Ring([kb.sb([128, 32, 16], F32) for _ in range(2)])
        eq = kb.sb([128, 32, 16], F32)
        mx = Ring([kb.sb([128, 32], F32) for _ in range(2)])
        ngr = Ring([kb.sb([128, 32, 16], BF16) for _ in range(2)])
        ptr = Ring([kb.ps([16, 8, 128], BF16) for _ in range(2)])
        ngT = Ring([kb.sb([16, S], BF16) for _ in range(2)])
        for h in range(16):
            q, k = qr.next(), kr.next()
            kb.dma("sp", q[:], G["qaug1"][h, 0:64, :], reads=[G["b_qk1"]], writes=[q])
            kb.dma("act", k[:], G["kaug1"][h, 0:64, :], reads=[G["b_qk1"]], writes=[k])
            m_, mb = km.next(), kmb.next()
            kb.op("dve", lambda e: e.tensor_reduce(out=m_[:], in_=k[:].rearrange("p (n j) -> p n j", j=256), axis=AX.X, op=ALU.add),
                  [k], [m_])
            ts(kb, "dve", mb, mb[:], m_, m_[:], 1.0 / 256.0, None, ALU.mult)
            p = pg.next()
            for c in range(32):
                mm(kb, p, p[:, c, :], q, q[:, c * 128:(c + 1) * 128], mb, mb[:])
            g_ = gs.next()
            tt(kb, "dve", g_, g_[:], p, p[:], addm, addm[:], ALU.add)
            m = mx.next()
            for it in range(3):
                kb.op("dve", lambda e: e.tensor_reduce(out=m[:], in_=g_[:], axis=AX.X, op=ALU.max), [g_], [m])
                if it < 2:
                    tt(kb, "dve", eq, eq[:], g_, g_[:], m, m[:].unsqueeze(2).to_broadcast([128, 32, 16]), ALU.is_equal)
                    stt(kb, "dve", g_, g_[:], eq, eq[:], -1e30, g_, g_[:], ALU.mult, ALU.add)
            ts(kb, "dve", m, m[:], m, m[:], -1e29, None, ALU.max)
            tt(kb, "dve", eq, eq[:], p, p[:], addm, addm[:], ALU.add)
            tt(kb, "dve", eq, eq[:], eq, eq[:], m, m[:].unsqueeze(2).to_broadcast([128, 32, 16]), ALU.is_ge)
            ts(kb, "dve", eq, eq[:], eq, eq[:], -1.0, BIG, ALU.add, ALU.mult)
            ng = ngr.next()
            tt(kb, "dve", ng, ng[:], eq, eq[:], notown, notown[:], ALU.mult)
            nT = ngT.next()
            for c8 in range(4):
                pT = ptr.next()
                for j in range(8):
                    c = c8 * 8 + j
                    kb.op("pe", lambda e, c=c, j=j: e.transpose(pT[:, j, :], ng[:, c, :], identb[:]), [ng, identb], [pT])
                cp(kb, "act", nT, nT[:, c8 * 1024:(c8 + 1) * 1024].rearrange("p (j q) -> p j q", j=8), pT, pT[:])
            kb.dma("sp", G["qaug1"][h, 64:80, :], nT[:], reads=[nT], writes=[G["b_neg1"]])


def moba_attn(kb, G):
    with kb.phase():
        A = AttnCtx(kb, need_cmp=False)
        identb = make_ident(kb, BF16)
        Ec = kb.sb([128, S], BF16)
        memset(kb, "dve", Ec, Ec[:], 1.0)
        kb.op("pool", lambda g: g.affine_select(out=Ec[64:80, :], in_=Ec[64:80, :], pattern=[[1, S]], compare_op=ALU.is_ge,
                                                fill=0.0, base=0, channel_multiplier=-256), [Ec], [Ec])
        kb.op("pool", lambda g: g.affine_select(out=Ec[64:80, :], in_=Ec[64:80, :], pattern=[[-1, S]], compare_op=ALU.is_ge,
                                                fill=0.0, base=255, channel_multiplier=256), [Ec], [Ec])
        qr = Ring([kb.sb([80, S], BF16) for _ in range(2)])
        kr = Ring([kb.sb([80, S], BF16) for _ in range(2)])
        vr = Ring([kb.sb([128, 32, 65], BF16) for _ in range(2)])
        accr = Ring([kb.sb([128, 4, 64], F32) for _ in range(2)])
        otok = kb.sb([128, 8, 4, 128], BF16)
        ptr = Ring([kb.ps([128, 4, 128], BF16) for _ in range(2)])
        oT = Ring([kb.sb([128, 512], BF16) for _ in range(2)])
        v1v = G["v1"].rearrange("(kt p) b c -> p kt b c", p=128)
        for hp in range(8):
            for hh in range(2):
                h = hp * 2 + hh
                q, k, v = qr.next(), kr.next(), vr.next()
                kb.dma("sp", q[:], G["qaug1"][h], reads=[G["b_qk1"], G["b_neg1"]], writes=[q])
                kb.dma("act", k[0:64], G["kaug1"][h, 0:64, :], reads=[G["b_qk1"]], writes=[k])
                cp(kb, "pool", k, k[64:80, :], Ec, Ec[64:80, :])
                kb.dma("sp", v[:], v1v[:, :, h, :], reads=[G["b_qk1"]], writes=[v])
                for Q in range(8):
                    acc = accr.next()
                    attn_branch(A, 80, q, k, v, None, sched_causal, Q, acc, True,
                                after=lambda acc=acc, Q=Q, hh=hh: cp(kb, "act", otok, otok[:, Q, :, hh * 64:(hh + 1) * 64], acc, acc[:]))
            A.flush_all()
            for Q in range(8):
                pT = ptr.next()
                for s in range(4):
                    kb.op("pe", lambda e, s=s: e.transpose(pT[:, s, :], otok[:, Q, s, :], identb[:]), [otok, identb], [pT])
                o = oT.next()
                cp(kb, "dve", o, o[:].rearrange("p (s q) -> p s q", s=4), pT, pT[:])
                kb.dma("sp", G["o1T"][hp * 128:(hp + 1) * 128, Q * 512:(Q + 1) * 512], o[:], reads=[o], writes=[G["b_o1"]])


def build_program(debug=False, upto=99):
    nc = bass.Bass("TRN2", target_bir_lowering=False)
    G = {}

    def din(name, shape, dt=F32):
        return nc.dram_tensor(name, list(shape), dt, kind="ExternalInput").ap()

    def scr(name, shape, dt=F32, out=False):
        isout = out or (debug and (debug is True or name in debug))
        return nc.dram_tensor(name, list(shape), dt, kind="ExternalOutput" if isout else "Internal").ap()

    xT = din("xT", [D, S])
    w0r, w0s = din("w0r", [D, 14 * 64]), din("w0s", [D, 14 * 64])
    w0f, w0t = din("w0f", [D, 5 * 128]), din("w0t", [D, 920])
    w1r, w1s, w1t = din("w1r", [D, 32 * 64]), din("w1s", [D, 32 * 64]), din("w1t", [D, 1024])
    for wh in ("k", "v"):
        G["cmp_%s_w1" % wh] = din("cmp_%s_w1" % wh, [2048, 128])
        G["cmp_%s_w2" % wh] = din("cmp_%s_w2" % wh, [128, 64])
        G["pe_%s" % wh] = din("pe_%s" % wh, [32, 64])
    G["s5_lre"], G["s5_lim"], G["s5_ls"] = din("s5_lre", [32, 64]), din("s5_lim", [32, 64]), din("s5_ls", [32])
    G["s5_bre"], G["s5_bim"] = din("s5_bre", [32, 64, 16]), din("s5_bim", [32, 64, 16])
    G["s5_cre"], G["s5_cim"] = din("s5_cre", [32, 16, 64]), din("s5_cim", [32, 16, 64])
    G["s5_d"], G["s5_glw"], G["s5_glb"] = din("s5_d", [1, 512]), din("s5_glw", [512, 512]), din("s5_glb", [512])
    ev_wo, od_wo = din("ev_wo", [D, D]), din("od_wo", [D, D])
    ln = {k: din(k, [2, D]) for k in ("ln_mix_g", "ln_mix_b", "ln_ffn_g", "ln_ffn_b")}
    G["moe_wr"] = [din("moe_wr%d" % L, [D, 36]) for L in range(2)]
    G["moe_br"] = [din("moe_br%d" % L, [1, 36]) for L in range(2)]
    G["moe_wg"] = [din("moe_wg%d" % L, [32, D, 128]) for L in range(2)]
    G["moe_wu"] = [din("moe_wu%d" % L, [32, D, 128]) for L in range(2)]
    G["moe_wd"] = [din("moe_wd%d" % L, [32, 128, D]) for L in range(2)]
    yT = scr("yT", [D, S], out=True)
    G["ropeC"], G["ropeS"] = scr("ropeC", [64, S]), scr("ropeS", [64, S])
    G["qaug0"] = scr("qaug0", [8, 128, S], BF16)
    G["ksT"], G["kwT"], G["kcT"] = scr("ksT", [2, 64, S], BF16), scr("kwT", [2, 64, S], BF16), scr("kcT", [2, 64, S], BF16)
    G["vcT"], G["uT"] = scr("vcT", [128, S], BF16), scr("uT", [512, S], BF16)
    G["v3"] = scr("v3", [S, 6, 65], BF16)
    G["gates"], G["utm"], G["ytm"] = scr("gates", [S, 24]), scr("utm", [S, 512]), scr("ytm", [S, 512])
    G["kcmpT"], G["vcmp"] = scr("kcmpT", [2, 64, 256], BF16), scr("vcmp", [2, 128, 2, 65], BF16)
    G["o0T"], G["o1T"] = scr("o0T", [D, S], BF16), scr("o1T", [D, S], BF16)
    mixT = scr("mixT", [D, S])
    x1T, x2T, x3T = scr("x1T", [D, S]), scr("x2T", [D, S]), scr("x3T", [D, S])
    G["qaug1"], G["kaug1"] = scr("qaug1", [16, 80, S], BF16), scr("kaug1", [16, 64, S], BF16)
    G["v1"] = scr("v1", [S, 16, 65], BF16)
    for b in ("b_rope", "b_qk0", "b_cmp", "b_neg0", "b_o0", "b_y", "b_qk1", "b_neg1", "b_o1"):
        G[b] = Buf(b)
    bx, bmix, b1, b2, b3, by = Buf(), Buf(), Buf(), Buf(), Buf(), Buf()

    with contextlib.ExitStack() as st:
        kb = KB(nc, st)
        build_consts(kb, G)
        v3r = Ring([kb.sb([128, 6, 65], BF16) for _ in range(2)])
        gtr = Ring([kb.sb([128, 24], F32) for _ in range(2)])
        utr = Ring([kb.sb([128, 512], F32) for _ in range(2)])
        for it in v3r.items:
            memset(kb, "dve", it, it[:], 1.0)

        def rope_dst0(h):
            if h < 8:
                return G["qaug0"][h, 0:64, :], G["b_qk0"]
            g = (h - 8) % 2
            return (G["kcT"], G["ksT"], G["kwT"])[(h - 8) // 2][g], G["b_qk0"]

        def fm_dst0(c):
            if c == 0:
                return G["vcT"], G["b_qk0"]
            return G["uT"][(c - 1) * 128:c * 128, :], G["b_qk0"]

        def tm_cb0(kb, pt, ti):
            import os
            TM = os.environ.get("TM_PARTS", "vgu")
            v, g_, u = v3r.next(), gtr.next(), utr.next()
            rs = slice(ti * 128, (ti + 1) * 128)
            if "v" in TM:
                cp(kb, "act", v, v[:, :, 0:64], pt[0], pt[0][:, 0:384].rearrange("p (b c) -> p b c", b=6))
                kb.dma("sp", G["v3"][rs], v[:], reads=[v], writes=[G["b_qk0"]])
            if "g" in TM:
                act(kb, g_, g_[:], pt[0], pt[0][:, 384:408], AF.Sigmoid)
                kb.dma("sp", G["gates"][rs], g_[:], reads=[g_], writes=[G["b_qk0"]])
            if "u" in TM:
                cp(kb, "dve", u, u[:], pt[1], pt[1][:])
                kb.dma("sp", G["utm"][rs], u[:], reads=[u], writes=[G["b_qk0"]])

        if upto >= 1:
            inproj(kb, G, xT, bx, w0r, w0s, 14, rope_dst0, w0f, 5, fm_dst0, w0t, 920, tm_cb0, [(0, 408), (408, 920)])
        if upto >= 2:
            nsa_compress(kb, G)
            nsa_select(kb, G)
        if upto >= 3:
            nsa_attn(kb, G)
        if upto >= 4:
            s5(kb, G)
        if upto >= 5:
            linear_fm(kb, G, G["o0T"], ev_wo, mixT, in_buf=G["b_o0"], out_buf=bmix)
            res_ln(kb, G, xT, mixT, ln["ln_mix_g"][0], ln["ln_mix_b"][0], x1T, bx, bmix, b1)
            moe(kb, G, x1T, b1, 0, mixT, bmix)
            res_ln(kb, G, x1T, mixT, ln["ln_ffn_g"][0], ln["ln_ffn_b"][0], x2T, b1, bmix, b2)
        v1r = Ring([kb.sb([128, 16, 65], BF16) for _ in range(2)])
        for it in v1r.items:
            memset(kb, "dve", it, it[:], 1.0)

        def rope_dst1(h):
            if h < 16:
                return G["qaug1"][h, 0:64, :], G["b_qk1"]
            return G["kaug1"][h - 16], G["b_qk1"]

        def tm_cb1(kb, pt, ti):
            v = v1r.next()
            cp(kb, "act", v, v[:, 0:8, 0:64], pt[0], pt[0][:].rearrange("p (b c) -> p b c", b=8))
            cp(kb, "dve", v, v[:, 8:16, 0:64], pt[1], pt[1][:].rearrange("p (b c) -> p b c", b=8))
            kb.dma("sp", G["v1"][ti * 128:(ti + 1) * 128], v[:], reads=[v], writes=[G["b_qk1"]])

        if upto >= 6:
            inproj(kb, G, x2T, b2, w1r, w1s, 32, rope_dst1, None, 0, None, w1t, 1024, tm_cb1, [(0, 512), (512, 1024)])
            moba_select(kb, G)
            moba_attn(kb, G)
        if upto >= 7:
            linear_fm(kb, G, G["o1T"], od_wo, mixT, in_buf=G["b_o1"], out_buf=bmix)
            res_ln(kb, G, x2T, mixT, ln["ln_mix_g"][1], ln["ln_mix_b"][1], x3T, b2, bmix, b3)
            moe(kb, G, x3T, b3, 1, mixT, bmix)
            res_ln(kb, G, x3T, mixT, ln["ln_ffn_g"][1], ln["ln_ffn_b"][1], yT, b3, bmix, by)
        kb.barrier()
        print("instructions", kb.n_ins, "waits", kb.n_wait, flush=True)
    return nc


def _swap_cols(w, nh):
    w = w.reshape(w.shape[0], nh, 2, 32)
    return np.ascontiguousarray(w[:, :, ::-1, :].reshape(w.shape[0], nh * 64))


def prep_weights(inp):
    c = np.ascontiguousarray
    W = inp["ev_w_in"][0]
    q, kc, vc, ks, vs, kw, vw, gl, u = np.split(W, [512, 640, 768, 896, 1024, 1152, 1280, 1304], axis=1)
    m = {}
    rope = np.concatenate([q, kc, ks, kw], axis=1)
    m["w0r"] = c(rope)
    m["w0s"] = _swap_cols(rope, 14)
    m["w0f"] = c(np.concatenate([vc, u], axis=1))
    m["w0t"] = c(np.concatenate([vc, vs, vw, gl, u], axis=1))
    W1 = inp["od_w_in"][0]
    m["w1r"] = c(W1[:, 0:2048])
    m["w1s"] = _swap_cols(W1[:, 0:2048], 32)
    m["w1t"] = c(W1[:, 2048:3072])
    m["cmp_k_w1"], m["cmp_k_w2"], m["pe_k"] = c(inp["nsa_cmp_k_w1"][0]), c(inp["nsa_cmp_k_w2"][0]), c(inp["nsa_pe_k"][0])
    m["cmp_v_w1"], m["cmp_v_w2"], m["pe_v"] = c(inp["nsa_cmp_v_w1"][0]), c(inp["nsa_cmp_v_w2"][0]), c(inp["nsa_pe_v"][0])
    m["s5_lre"], m["s5_lim"], m["s5_ls"] = c(inp["s5_lambda_re"][0]), c(inp["s5_lambda_im"][0]), c(inp["s5_log_step"][0])
    m["s5_bre"], m["s5_bim"] = c(inp["s5_b_re"][0]), c(inp["s5_b_im"][0])
    m["s5_cre"], m["s5_cim"] = c(inp["s5_c_re"][0]), c(inp["s5_c_im"][0])
    m["s5_d"], m["s5_glw"], m["s5_glb"] = c(inp["s5_d"][0][None, :]), c(inp["s5_glu_w"][0]), c(inp["s5_glu_b"][0])
    m["ev_wo"], m["od_wo"] = c(inp["ev_w_out"][0]), c(inp["od_w_out"][0])
    for k in ("ln_mix_g", "ln_mix_b", "ln_ffn_g", "ln_ffn_b"):
        m[k] = c(inp[k])
    for L in range(2):
        m["moe_wr%d" % L] = c(np.concatenate([inp["moe_w_coarse"][L]] + [inp["moe_w_fine"][L, g] for g in range(4)], axis=1))
        m["moe_br%d" % L] = c(np.concatenate([inp["moe_b_coarse"][L]] + [inp["moe_b_fine"][L, g] for g in range(4)])[None, :])
        m["moe_wg%d" % L] = c(inp["moe_w_gate"][L].reshape(32, D, 128))
        m["moe_wu%d" % L] = c(inp["moe_w_up"][L].reshape(32, D, 128))
        m["moe_wd%d" % L] = c(inp["moe_w_down"][L].reshape(32, 128, D))
    return {k: np.asarray(v, dtype=np.float32) for k, v in m.items()}


def kernel(**inputs):
    inp = {k: np.asarray(v) for k, v in inputs.items()}
    nc = build_program()
    wm = prep_weights(inp)
    in_maps = []
    for b in range(8):
        m = dict(wm)
        m["xT"] = np.ascontiguousarray(inp["x"][b].T)
        in_maps.append(m)
    res = run_bass_kernel_spmd(nc, in_maps, core_ids=list(range(8)))
    out = np.stack([np.ascontiguousarray(res.results[b]["yT"].T) for b in range(8)], axis=0)
    return out.astype(np.float32)
```

```python
import contextlib
import math
import numpy as np
import concourse.bass as bass
import concourse.mybir as mybir
from concourse.bass_utils import run_bass_kernel_spmd

F32 = mybir.dt.float32
BF16 = mybir.dt.bfloat16
I32 = mybir.dt.int32
ALU = mybir.AluOpType
AF = mybir.ActivationFunctionType
AX = mybir.AxisListType

S = 4096
D = 1024
NT = 8
ALPHA = 4.0 ** 0.25
EPS = 1e-5
BIG = 240000.0
MAGIC = 12582912.0
TWO_PI = 2.0 * math.pi


class Buf:
    __slots__ = ("name", "last_w", "readers")

    def __init__(self, name=""):
        self.name = name
        self.last_w = None
        self.readers = {}


class T:
    __slots__ = ("t", "buf")

    def __init__(self, t, name=""):
        self.t = t
        self.buf = Buf(name)

    def __getitem__(self, idx):
        return self.t[idx]


class KB:
    NDMA = 48

    def __init__(self, nc, stack):
        self.nc = nc
        self.stack = stack
        self.eng = {"pe": nc.tensor, "act": nc.scalar, "dve": nc.vector,
                    "pool": nc.gpsimd, "sp": nc.sync}
        self.sems = {}
        for e in self.eng:
            self.sems[e] = stack.enter_context(nc.semaphore("s_" + e))
        self.cnt = {e: 0 for e in self.eng}
        self.dsem = [stack.enter_context(nc.semaphore("d%d" % i)) for i in range(self.NDMA)]
        self.dcnt = [0] * self.NDMA
        self.dnext = 0
        self.dnext_sw = 0
        self.waited = {}
        self.n_ins = 0
        self.n_wait = 0
        self._uid = 0

    def sb(self, shape, dtype=F32, name=None):
        self._uid += 1
        name = (name or "t") + "_%d" % self._uid
        t = self.stack.enter_context(self.nc.sbuf_tensor(name, list(shape), dtype))
        return T(t, name)

    def ps(self, shape, dtype=F32, name=None):
        self._uid += 1
        name = (name or "p") + "_%d" % self._uid
        t = self.stack.enter_context(self.nc.psum_tensor(name, list(shape), dtype))
        return T(t, name)

    def _sem(self, key):
        return self.sems[key] if isinstance(key, str) else self.dsem[key]

    def _wait(self, e, ev):
        key, val = ev
        if e == "pe" and key == "pe":
            return
        k = (e, key)
        if self.waited.get(k, 0) >= val:
            return
        self.waited[k] = val
        self.eng[e].wait_ge(self._sem(key), val)
        self.n_wait += 1

    @staticmethod
    def _b(b):
        return b.buf if isinstance(b, T) else b

    def _deps(self, e, reads, writes):
        for b in reads:
            b = self._b(b)
            if b.last_w is not None:
                self._wait(e, b.last_w)
        for b in writes:
            b = self._b(b)
            if b.last_w is not None:
                self._wait(e, b.last_w)
            for k, v in b.readers.items():
                self._wait(e, (k, v))

    def _mark(self, ev, reads, writes):
        key, val = ev
        for b in reads:
            b = self._b(b)
            if b.readers.get(key, 0) < val:
                b.readers[key] = val
        for b in writes:
            b = self._b(b)
            b.last_w = ev
            b.readers = {}

    def op(self, e, fn, reads=(), writes=()):
        self._deps(e, reads, writes)
        ins = fn(self.eng[e])
        self.cnt[e] += 1
        ins.then_inc(self.sems[e], 1)
        self._mark((e, self.cnt[e]), reads, writes)
        self.n_ins += 1
        return ins

    def dma(self, q, out, in_, reads=(), writes=(), **kw):
        self._deps(q, reads, writes)
        half = self.NDMA // 2
        if q == "pool":
            i = half + self.dnext_sw
            self.dnext_sw = (self.dnext_sw + 1) % (self.NDMA - half)
        else:
            i = self.dnext
            self.dnext = (self.dnext + 1) % half
        if self.dcnt[i] > 0:
            self._wait(q, (i, self.dcnt[i]))
        ins = self.eng[q].dma_start(out=out, in_=in_, **kw)
        self.dcnt[i] += 16
        ins.then_inc(self.dsem[i], 16)
        self._mark((i, self.dcnt[i]), reads, writes)
        self.n_ins += 1
        return ins

    def barrier(self):
        for e in self.eng:
            for k in self.eng:
                if self.cnt[k] > 0:
                    self._wait(e, (k, self.cnt[k]))
            for i in range(self.NDMA):
                if self.dcnt[i] > 0:
                    self._wait(e, (i, self.dcnt[i]))

    @contextlib.contextmanager
    def phase(self):
        with contextlib.ExitStack() as ps:
            old = self.stack
            self.stack = ps
            yield
            self.barrier()
            self.stack = old


class Ring:
    def __init__(self, items):
        self.items = items
        self.i = 0

    def next(self):
        it = self.items[self.i % len(self.items)]
        self.i += 1
        return it


def mm(kb, ot, oap, lt, lap, rt, rap, start=True, stop=True):
    kb.op("pe", lambda e: e.matmul(oap, lhsT=lap, rhs=rap, start=start, stop=stop,
                                   skip_group_check=True), [lt, rt], [ot])


def tt(kb, e, ot, oap, at, aap, bt, bap, op):
    kb.op(e, lambda g: g.tensor_tensor(out=oap, in0=aap, in1=bap, op=op), [at, bt], [ot])


def ts(kb, e, ot, oap, at, aap, s1, s2, op0, op1=None, extra=()):
    if op1 is None:
        kb.op(e, lambda g: g.tensor_scalar(out=oap, in0=aap, scalar1=s1, scalar2=None, op0=op0),
              [at] + list(extra), [ot])
    else:
        kb.op(e, lambda g: g.tensor_scalar(out=oap, in0=aap, scalar1=s1, scalar2=s2, op0=op0, op1=op1),
              [at] + list(extra), [ot])


def stt(kb, e, ot, oap, at, aap, sc, bt, bap, op0, op1, extra=()):
    kb.op(e, lambda g: g.scalar_tensor_tensor(out=oap, in0=aap, scalar=sc, in1=bap, op0=op0, op1=op1),
          [at, bt] + list(extra), [ot])


def act(kb, ot, oap, it, iap, func, scale=1.0, bias=None, extra=()):
    if bias is None:
        kb.op("act", lambda g: g.activation(out=oap, in_=iap, func=func, scale=scale), [it] + list(extra), [ot])
    else:
        kb.op("act", lambda g: g.activation(out=oap, in_=iap, func=func, scale=scale, bias=bias),
              [it] + list(extra), [ot])


def cp(kb, e, ot, oap, it, iap):
    if e == "act":
        kb.op("act", lambda g: g.activation(out=oap, in_=iap, func=AF.Copy), [it], [ot])
    elif e == "dve":
        kb.op("dve", lambda g: g.tensor_scalar(out=oap, in0=iap, scalar1=1.0, scalar2=None, op0=ALU.mult), [it], [ot])
    else:
        kb.op(e, lambda g: g.tensor_copy(out=oap, in_=iap), [it], [ot])


def memset(kb, e, ot, oap, val):
    kb.op(e, lambda g: g.memset(oap, val), [], [ot])


def fm(ap):
    return ap.rearrange("(c p) t -> p c t", p=128)


def build_consts(kb, G):
    with kb.phase():
        row = kb.sb([1, 64], F32)
        make_ramp(kb, row, row[:, 0:32], 1, 32)
        make_ramp(kb, row, row[:, 32:64], 1, 32)
        one1 = kb.sb([1, 1], F32)
        memset(kb, "dve", one1, one1[:], 1.0)
        pidx = kb.ps([64, 1], F32)
        mm(kb, pidx, pidx[:], row, row[:], one1, one1[:])
        idx = kb.sb([64, 1], F32)
        cp(kb, "act", idx, idx[:], pidx, pidx[:])
        inv = kb.sb([64, 1], F32)
        act(kb, inv, inv[:], idx, idx[:], AF.Exp, scale=-math.log(10000.0) / 32.0)
        ts(kb, "dve", inv, inv[:], inv, inv[:], 1.0 / TWO_PI, None, ALU.mult)
        tpos = kb.sb([64, S], F32)
        make_ramp(kb, tpos, tpos[:], 64, S)
        r = kb.sb([64, S], F32)
        rr = kb.sb([64, S], F32)
        tab = kb.sb([64, S], F32)
        for which in ("sin", "cos"):
            if which == "sin":
                ts(kb, "dve", r, r[:], tpos, tpos[:], inv[:, 0:1], None, ALU.mult, extra=[inv])
            else:
                ts(kb, "dve", r, r[:], tpos, tpos[:], inv[:, 0:1], None, ALU.mult, extra=[inv])
                ts(kb, "dve", r, r[:], r, r[:], 0.25, None, ALU.add)
            ts(kb, "dve", rr, rr[:], r, r[:], MAGIC, MAGIC, ALU.add, ALU.subtract)
            tt(kb, "dve", r, r[:], r, r[:], rr, rr[:], ALU.subtract)
            act(kb, tab, tab[:], r, r[:], AF.Sin, scale=TWO_PI)
            if which == "sin":
                ts(kb, "dve", tab, tab[0:32, :], tab, tab[0:32, :], -1.0, None, ALU.mult)
                kb.dma("sp", G["ropeS"], tab[:], reads=[tab], writes=[G["b_rope"]])
            else:
                kb.dma("sp", G["ropeC"], tab[:], reads=[tab], writes=[G["b_rope"]])


def linear_fm(kb, G, inT, w_dram, outT, n_in_chunks=8, n_out_chunks=8, in_buf=None, out_buf=None):
    with kb.phase():
        w = kb.sb([128, n_in_chunks, n_out_chunks * 128], BF16)
        kb.dma("pool", w[:], w_dram.rearrange("(c p) n -> p c n", p=128), reads=[], writes=[w])
        xin = Ring([kb.sb([128, n_in_chunks, 512], BF16) for _ in range(2)])
        ob = Ring([kb.sb([128, n_out_chunks, 512], F32) for _ in range(2)])
        pss = Ring([kb.ps([128, 512], F32) for _ in range(4)])
        inv = fm(inT)
        outv = fm(outT)
        for t in range(NT):
            x = xin.next()
            kb.dma("sp", x[:], inv[:, :, t * 512:(t + 1) * 512], reads=[in_buf], writes=[x])
            o = ob.next()
            for f in range(n_out_chunks):
                p = pss.next()
                for c in range(n_in_chunks):
                    mm(kb, p, p[:], w, w[:, c, f * 128:(f + 1) * 128], x, x[:, c, :],
                       start=(c == 0), stop=(c == n_in_chunks - 1))
                cp(kb, "act" if f % 2 == 0 else "dve", o, o[:, f, :], p, p[:])
            kb.dma("sp", outv[:, :, t * 512:(t + 1) * 512], o[:], reads=[o], writes=[out_buf])


def res_ln(kb, G, resT, addT, g_dram, b_dram, outT, res_buf, add_buf, out_buf):
    with kb.phase():
        ones = kb.sb([128, 128], F32)
        memset(kb, "dve", ones, ones[:], 1.0 / D)
        gb = kb.sb([128, 2, 8], F32)
        with kb.nc.allow_non_contiguous_dma(reason="tiny ln params"):
            kb.dma("sp", gb[:, 0, :], g_dram.rearrange("(c p) -> p c", p=128), writes=[gb])
            kb.dma("sp", gb[:, 1, :], b_dram.rearrange("(c p) -> p c", p=128), writes=[gb])
        rin = Ring([kb.sb([128, 8, 512], F32) for _ in range(2)])
        ain = Ring([kb.sb([128, 8, 512], F32) for _ in range(2)])
        zsq = Ring([kb.sb([128, 512], F32) for _ in range(2)])
        oo = Ring([kb.sb([128, 8, 512], F32) for _ in range(2)])
        ps1 = Ring([kb.ps([128, 512], F32) for _ in range(2)])
        ps2 = Ring([kb.ps([128, 512], F32) for _ in range(2)])
        mean = kb.sb([128, 512], F32)
        rstd = kb.sb([128, 512], F32)
        tmp = kb.sb([128, 512], F32)
        rv, av, ov = fm(resT), fm(addT), fm(outT)
        for t in range(NT):
            sl = slice(t * 512, (t + 1) * 512)
            r = rin.next()
            a = ain.next()
            kb.dma("sp", r[:], rv[:, :, sl], reads=[res_buf], writes=[r])
            kb.dma("act", a[:], av[:, :, sl], reads=[add_buf], writes=[a])
            p1, p2 = ps1.next(), ps2.next()
            for c in range(8):
                stt(kb, "dve", r, r[:, c, :], r, r[:, c, :], ALPHA, a, a[:, c, :], ALU.mult, ALU.add)
                z2 = zsq.next()
                act(kb, z2, z2[:], r, r[:, c, :], AF.Square)
                mm(kb, p1, p1[:], ones, ones[:], r, r[:, c, :], start=(c == 0), stop=(c == 7))
                mm(kb, p2, p2[:], ones, ones[:], z2, z2[:], start=(c == 0), stop=(c == 7))
            cp(kb, "act", mean, mean[:], p1, p1[:])
            tt(kb, "dve", tmp, tmp[:], mean, mean[:], mean, mean[:], ALU.mult)
            tt(kb, "dve", tmp, tmp[:], p2, p2[:], tmp, tmp[:], ALU.subtract)
            ts(kb, "dve", tmp, tmp[:], tmp, tmp[:], EPS, None, ALU.add)
            act(kb, tmp, tmp[:], tmp, tmp[:], AF.Sqrt)
            kb.op("dve", lambda g: g.reciprocal(out=rstd[:], in_=tmp[:]), [tmp], [rstd])
            o = oo.next()
            for c in range(8):
                e = "dve" if c % 2 == 0 else "pool"
                tt(kb, e, r, r[:, c, :], r, r[:, c, :], mean, mean[:], ALU.subtract)
                tt(kb, e, r, r[:, c, :], r, r[:, c, :], rstd, rstd[:], ALU.mult)
                ts(kb, e, o, o[:, c, :], r, r[:, c, :], gb[:, 0, c:c + 1], gb[:, 1, c:c + 1], ALU.mult, ALU.add,
                   extra=[gb])
            kb.dma("sp", ov[:, :, sl], o[:], reads=[o], writes=[out_buf])


def linear_res_ln(kb, G, inT, w_dram, resT, g_dram, b_dram, outT, in_buf, res_buf, out_buf):
    with kb.phase():
        w = kb.sb([128, 8, 1024], BF16)
        kb.dma("pool", w[:], w_dram.rearrange("(c p) n -> p c n", p=128), reads=[], writes=[w])
        ones = kb.sb([128, 128], F32)
        memset(kb, "dve", ones, ones[:], 1.0 / D)
        gb = kb.sb([128, 2, 8], F32)
        with kb.nc.allow_non_contiguous_dma(reason="tiny ln params"):
            kb.dma("sp", gb[:, 0, :], g_dram.rearrange("(c p) -> p c", p=128), writes=[gb])
            kb.dma("sp", gb[:, 1, :], b_dram.rearrange("(c p) -> p c", p=128), writes=[gb])
        xin = Ring([kb.sb([128, 8, 512], BF16) for _ in range(2)])
        rin = Ring([kb.sb([128, 8, 512], F32) for _ in range(2)])
        zsq = Ring([kb.sb([128, 512], F32) for _ in range(2)])
        oo = Ring([kb.sb([128, 8, 512], F32) for _ in range(2)])
        pss = Ring([kb.ps([128, 512], F32) for _ in range(4)])
        ps1 = Ring([kb.ps([128, 512], F32) for _ in range(2)])
        ps2 = Ring([kb.ps([128, 512], F32) for _ in range(2)])
        mean = kb.sb([128, 512], F32)
        rstd = kb.sb([128, 512], F32)
        tmp = kb.sb([128, 512], F32)
        inv, rv, ov = fm(inT), fm(resT), fm(outT)
        for t in range(NT):
            sl = slice(t * 512, (t + 1) * 512)
            x = xin.next()
            kb.dma("sp", x[:], inv[:, :, sl], reads=[in_buf], writes=[x])
            r = rin.next()
            kb.dma("act", r[:], rv[:, :, sl], reads=[res_buf], writes=[r])
            p1, p2 = ps1.next(), ps2.next()
            pend = None
            for f in range(8):
                p = pss.next()
                for c in range(8):
                    mm(kb, p, p[:], w, w[:, c, f * 128:(f + 1) * 128], x, x[:, c, :], start=(c == 0), stop=(c == 7))
                if pend is not None:
                    pend()
                stt(kb, "dve", r, r[:, f, :], r, r[:, f, :], ALPHA, p, p[:], ALU.mult, ALU.add)
                z2 = zsq.next()
                act(kb, z2, z2[:], r, r[:, f, :], AF.Square)

                def pend(f=f, z2=z2, r=r, p1=p1, p2=p2):
                    mm(kb, p1, p1[:], ones, ones[:], r, r[:, f, :], start=(f == 0), stop=(f == 7))
                    mm(kb, p2, p2[:], ones, ones[:], z2, z2[:], start=(f == 0), stop=(f == 7))
            pend()
            cp(kb, "act", mean, mean[:], p1, p1[:])
            tt(kb, "dve", tmp, tmp[:], mean, mean[:], mean, mean[:], ALU.mult)
            tt(kb, "dve", tmp, tmp[:], p2, p2[:], tmp, tmp[:], ALU.subtract)
            ts(kb, "dve", tmp, tmp[:], tmp, tmp[:], EPS, None, ALU.add)
            act(kb, tmp, tmp[:], tmp, tmp[:], AF.Sqrt)
            kb.op("dve", lambda g: g.reciprocal(out=rstd[:], in_=tmp[:]), [tmp], [rstd])
            o = oo.next()
            for c in range(8):
                e = "dve" if c % 2 == 0 else "pool"
                tt(kb, e, r, r[:, c, :], r, r[:, c, :], mean, mean[:], ALU.subtract)
                tt(kb, e, r, r[:, c, :], r, r[:, c, :], rstd, rstd[:], ALU.mult)
                ts(kb, e, o, o[:, c, :], r, r[:, c, :], gb[:, 0, c:c + 1], gb[:, 1, c:c + 1], ALU.mult, ALU.add,
                   extra=[gb])
            kb.dma("sp", ov[:, :, sl], o[:], reads=[o], writes=[out_buf])


def inproj(kb, G, xT, x_buf, w_rope, w_swap, n_rope, rope_dst, w_fm, n_fm_chunks, fm_dst, w_tm, tm_cols, tm_cb, tm_splits):
    with kb.phase():
        wr = kb.sb([128, 8, n_rope * 64], BF16)
        ws = kb.sb([128, 8, n_rope * 64], BF16)
        kb.dma("pool", wr[:], w_rope.rearrange("(c p) n -> p c n", p=128), writes=[wr])
        kb.dma("pool", ws[:], w_swap.rearrange("(c p) n -> p c n", p=128), writes=[ws])
        if n_fm_chunks:
            wf = kb.sb([128, 8, n_fm_chunks * 128], BF16)
            kb.dma("pool", wf[:], w_fm.rearrange("(c p) n -> p c n", p=128), writes=[wf])
        wt = kb.sb([128, 8, tm_cols], BF16)
        kb.dma("pool", wt[:], w_tm.rearrange("(c p) n -> p c n", p=128), writes=[wt])
        xin = Ring([kb.sb([128, 8, 512], BF16) for _ in range(2)])
        assert n_rope % 2 == 0
        cc = Ring([kb.sb([128, 512], F32) for _ in range(2)])
        ss = Ring([kb.sb([128, 512], F32) for _ in range(2)])
        pa = Ring([kb.ps([128, 512], F32) for _ in range(2)])
        pb = Ring([kb.ps([128, 512], F32) for _ in range(2)])
        pf = Ring([kb.ps([128, 512], F32) for _ in range(2)])
        ntm = len(tm_splits)
        pt = [kb.ps([128, 512], F32) for _ in range(ntm)]
        t1 = Ring([kb.sb([128, 512], F32) for _ in range(2)])
        t2 = Ring([kb.sb([128, 512], F32) for _ in range(2)])
        ro = Ring([kb.sb([128, 512], BF16) for _ in range(3)])
        fo = Ring([kb.sb([128, 512], BF16) for _ in range(2)])
        xv = fm(xT)
        import os
        SK = os.environ.get("INPROJ_SKIP", "")
        for t in range(NT):
            sl = slice(t * 512, (t + 1) * 512)
            x = xin.next()
            kb.dma("pool", x[:], xv[:, :, sl], reads=[x_buf], writes=[x])
            c_, s_ = cc.next(), ss.next()
            for k in range(2):
                kb.dma("sp", c_[64 * k:64 * k + 64], G["ropeC"][:, sl], reads=[G["b_rope"]], writes=[c_])
                kb.dma("sp", s_[64 * k:64 * k + 64], G["ropeS"][:, sl], reads=[G["b_rope"]], writes=[s_])
            for hp in range(0 if "r" in SK else n_rope // 2):
                a, b = pa.next(), pb.next()
                for c in range(8):
                    mm(kb, a, a[:], wr, wr[:, c, hp * 128:(hp + 1) * 128], x, x[:, c, :], start=(c == 0), stop=(c == 7))
                for c in range(8):
                    mm(kb, b, b[:], ws, ws[:, c, hp * 128:(hp + 1) * 128], x, x[:, c, :], start=(c == 0), stop=(c == 7))
                u1, u2, o = t1.next(), t2.next(), ro.next()
                tt(kb, "dve", u1, u1[:], a, a[:], c_, c_[:], ALU.mult)
                tt(kb, "dve", u2, u2[:], b, b[:], s_, s_[:], ALU.mult)
                tt(kb, "dve" if "p" in SK else "pool", o, o[:], u1, u1[:], u2, u2[:], ALU.add)
                for k in range(2):
                    dst, dbuf = rope_dst(2 * hp + k)
                    kb.dma("sp", dst[:, sl], o[64 * k:64 * k + 64, :], reads=[o], writes=[dbuf])
            for f in range(0 if "f" in SK else n_fm_chunks):
                p = pf.next()
                for c in range(8):
                    mm(kb, p, p[:], wf, wf[:, c, f * 128:(f + 1) * 128], x, x[:, c, :], start=(c == 0), stop=(c == 7))
                o = fo.next()
                cp(kb, "act", o, o[:], p, p[:])
                dst, dbuf = fm_dst(f)
                kb.dma("sp", dst[:, sl], o[:], reads=[o], writes=[dbuf])
            for sub in range(0 if "t" in SK else 4):
                for j in range(ntm):
                    n0, n1 = tm_splits[j]
                    for c in range(8):
                        mm(kb, pt[j], pt[j][:, 0:n1 - n0], x, x[:, c, sub * 128:(sub + 1) * 128], wt, wt[:, c, n0:n1],
                           start=(c == 0), stop=(c == 7))
                tm_cb(kb, pt, t * 4 + sub)


def moe(kb, G, xT, x_buf, L, outT, out_buf):
    wr_d, br_d = G["moe_wr"][L], G["moe_br"][L]
    wg_d, wu_d, wd_d = G["moe_wg"][L], G["moe_wu"][L], G["moe_wd"][L]
    xv = fm(xT)
    with kb.phase():
        gT = kb.sb([32, S], BF16)
        with contextlib.ExitStack() as rs:
            old = kb.stack
            kb.stack = rs
            wr = kb.sb([128, 8, 36], F32)
            kb.dma("sp", wr[:], wr_d.rearrange("(c p) n -> p c n", p=128), writes=[wr])
            br = kb.sb([1, 36], F32)
            kb.dma("sp", br[:], br_d, writes=[br])
            ones1 = kb.sb([1, 128], F32)
            memset(kb, "dve", ones1, ones1[:], 1.0)
            ident = kb.sb([128, 128], F32)
            memset(kb, "dve", ident, ident[:], 1.0)
            kb.op("pool", lambda g: g.affine_select(out=ident[:], in_=ident[:], pattern=[[-1, 128]],
                                                    compare_op=ALU.is_equal, fill=0.0, base=0, channel_multiplier=1),
                  [ident], [ident])
            xin = Ring([kb.sb([128, 8, 512], F32) for _ in range(2)])
            pl = Ring([kb.ps([128, 36], F32) for _ in range(2)])
            ptr = Ring([kb.ps([32, 128], F32) for _ in range(2)])
            sm = Ring([kb.sb([128, 160], F32) for _ in range(2)])
            gt = Ring([kb.sb([128, 32], F32) for _ in range(2)])
            for t in range(NT):
                x = xin.next()
                kb.dma("sp", x[:], xv[:, :, t * 512:(t + 1) * 512], reads=[x_buf], writes=[x])
                for sub in range(4):
                    p = pl.next()
                    for c in range(8):
                        mm(kb, p, p[:], x, x[:, c, sub * 128:(sub + 1) * 128], wr, wr[:, c, :], start=(c == 0), stop=False)
                    mm(kb, p, p[:], ones1, ones1[:], br, br[:], start=False, stop=True)
                    w = sm.next()
                    cp(kb, "act", w, w[:, 0:36], p, p[:])
                    kb.op("dve", lambda g: g.tensor_reduce(out=w[:, 36:37], in_=w[:, 0:4], axis=AX.X, op=ALU.max), [w], [w])
                    ts(kb, "dve", w, w[:, 37:38], w, w[:, 36:37], -1.0, None, ALU.mult)
                    act(kb, w, w[:, 38:42], w, w[:, 0:4], AF.Exp, bias=w[:, 37:38])
                    kb.op("dve", lambda g: g.tensor_reduce(out=w[:, 42:43], in_=w[:, 38:42], axis=AX.X, op=ALU.add), [w], [w])
                    kb.op("dve", lambda g: g.reciprocal(out=w[:, 43:44], in_=w[:, 42:43]), [w], [w])
                    ts(kb, "dve", w, w[:, 44:48], w, w[:, 0:4], w[:, 36:37], None, ALU.is_equal)
                    tt(kb, "dve", w, w[:, 48:80].rearrange("p (g e) -> p g e", g=4),
                       w, w[:, 4:36].rearrange("p (g e) -> p g e", g=4),
                       w, w[:, 44:48].unsqueeze(2).to_broadcast([128, 4, 8]), ALU.mult)
                    kb.op("dve", lambda g: g.tensor_reduce(out=w[:, 80:88], in_=w[:, 48:80].rearrange("p (g e) -> p e g", g=4),
                                                           axis=AX.X, op=ALU.add), [w], [w])
                    kb.op("dve", lambda g: g.max(out=w[:, 88:96], in_=w[:, 80:88]), [w], [w])
                    tt(kb, "dve", w, w[:, 96:97], w, w[:, 88:89], w, w[:, 89:90], ALU.subtract)
                    act(kb, w, w[:, 97:98], w, w[:, 96:97], AF.Sigmoid)
                    tt(kb, "dve", w, w[:, 98:99], w, w[:, 97:98], w, w[:, 43:44], ALU.mult)
                    tt(kb, "dve", w, w[:, 99:100], w, w[:, 43:44], w, w[:, 98:99], ALU.subtract)
                    ts(kb, "dve", w, w[:, 100:108], w, w[:, 80:88], w[:, 88:89], w[:, 98:99], ALU.is_equal, ALU.mult)
                    ts(kb, "dve", w, w[:, 108:116], w, w[:, 80:88], w[:, 89:90], w[:, 99:100], ALU.is_equal, ALU.mult)
                    tt(kb, "dve", w, w[:, 116:124], w, w[:, 100:108], w, w[:, 108:116], ALU.add)
                    gg = gt.next()
                    tt(kb, "dve", gg, gg[:].rearrange("p (g e) -> p g e", g=4),
                       w, w[:, 44:48].unsqueeze(2).to_broadcast([128, 4, 8]),
                       w, w[:, 116:124].unsqueeze(1).to_broadcast([128, 4, 8]), ALU.mult)
                    pT = ptr.next()
                    kb.op("pe", lambda e: e.transpose(pT[:], gg[:], ident[:]), [gg, ident], [pT])
                    q0 = (t * 4 + sub) * 128
                    cp(kb, "act", gT, gT[:, q0:q0 + 128], pT, pT[:])
            kb.barrier()
            kb.stack = old
        sel = kb.sb([32, 32, 128], BF16)
        memset(kb, "dve", sel, sel[:], 1.0)
        kb.op("pool", lambda g: g.affine_select(out=sel[:], in_=sel[:], pattern=[[-1, 32], [0, 128]],
                                                compare_op=ALU.is_equal, fill=0.0, base=0, channel_multiplier=1),
              [sel], [sel])
        TS = 2048
        xb = kb.sb([128, 8, TS], BF16)
        yacc = kb.sb([128, 8, TS], F32)
        hq = kb.sb([128, 4, TS], BF16)
        wgr = Ring([kb.sb([128, 8, 128], BF16) for _ in range(2)])
        wur = Ring([kb.sb([128, 8, 128], BF16) for _ in range(2)])
        wdr = Ring([kb.sb([128, 1024], BF16) for _ in range(8)])
        pg = Ring([kb.ps([128, 512], F32) for _ in range(2)])
        pu = Ring([kb.ps([128, 512], F32) for _ in range(2)])
        pc = Ring([kb.ps([128, 512], F32) for _ in range(2)])
        py = Ring([kb.ps([128, 512], F32) for _ in range(2)])
        sl_ = Ring([kb.sb([128, 512], F32) for _ in range(2)])
        t1_ = Ring([kb.sb([128, 512], F32) for _ in range(2)])
        ov = fm(outT)
        for st in range(S // TS):
            kb.dma("pool", xb[:], xv[:, :, st * TS:(st + 1) * TS], reads=[x_buf], writes=[xb])
            for q in range(8):
                wds = []
                for ei in range(4):
                    e = q * 4 + ei
                    wg, wu, wd = wgr.next(), wur.next(), wdr.next()
                    kb.dma("pool", wg[:], wg_d[e].rearrange("(c p) n -> p c n", p=128), writes=[wg])
                    kb.dma("pool", wu[:], wu_d[e].rearrange("(c p) n -> p c n", p=128), writes=[wu])
                    kb.dma("pool", wd[:], wd_d[e], writes=[wd])
                    wds.append(wd)
                    for tq in range(TS // 512):
                        tsl = slice(tq * 512, (tq + 1) * 512)
                        a, b, c_ = pg.next(), pu.next(), pc.next()
                        for c in range(8):
                            mm(kb, a, a[:], wg, wg[:, c, :], xb, xb[:, c, tsl], start=(c == 0), stop=(c == 7))
                        for c in range(8):
                            mm(kb, b, b[:], wu, wu[:, c, :], xb, xb[:, c, tsl], start=(c == 0), stop=(c == 7))
                        g0 = st * TS + tq * 512
                        mm(kb, c_, c_[:], sel, sel[:, e, :], gT, gT[:, g0:g0 + 512])
                        s1, u1 = sl_.next(), t1_.next()
                        act(kb, s1, s1[:], a, a[:], AF.Silu)
                        tt(kb, "dve", u1, u1[:], c_, c_[:], s1, s1[:], ALU.mult)
                        tt(kb, "dve", hq, hq[:, ei, tsl], b, b[:], u1, u1[:], ALU.mult)
                for tq in range(TS // 512):
                    tsl = slice(tq * 512, (tq + 1) * 512)
                    for f in range(8):
                        y = py.next()
                        for ei in range(4):
                            mm(kb, y, y[:], wds[ei], wds[ei][:, f * 128:(f + 1) * 128], hq, hq[:, ei, tsl],
                               start=(ei == 0), stop=(ei == 3))
                        if q == 0:
                            cp(kb, "act", yacc, yacc[:, f, tsl], y, y[:])
                        else:
                            tt(kb, "pool" if False else "dve", yacc, yacc[:, f, tsl], y, y[:], yacc, yacc[:, f, tsl], ALU.add)
            kb.dma("sp", ov[:, :, st * TS:(st + 1) * TS], yacc[:], reads=[yacc], writes=[out_buf])


def sched_causal(Q):
    out = []
    for kt in range(4 * Q + 4):
        r = kt - 4 * Q
        out.append((kt, max(0, r), 4, [(r, "tri")] if r >= 0 else [], None))
    return out


def sched_win(Q):
    out = []
    for kt in range(max(0, 4 * Q - 2), 4 * Q + 4):
        r = kt - 4 * Q
        s0, s1 = max(0, r), min(3, r + 2) + 1
        m = []
        if r >= 0:
            m.append((r, "tri"))
        if 0 <= r + 2 <= 3:
            m.append((r + 2, "ntri"))
        out.append((kt, s0, s1, m, None))
    return out


def sched_cmp(Q):
    out = [(0, 0, 4, [], "cmp")]
    if Q >= 4:
        out.append((1, 0, 4, [], "cmp"))
    return out


class AttnCtx:
    def __init__(self, kb, need_cmp):
        self.kb = kb
        self.tri = kb.sb([128, 128], BF16)
        self.ntri = kb.sb([128, 128], BF16)
        memset(kb, "dve", self.tri, self.tri[:], 1.0)
        memset(kb, "dve", self.ntri, self.ntri[:], 1.0)
        kb.op("pool", lambda g: g.affine_select(out=self.tri[:], in_=self.tri[:], pattern=[[1, 128]],
                                                compare_op=ALU.is_ge, fill=0.0, base=0, channel_multiplier=-1),
              [self.tri], [self.tri])
        kb.op("pool", lambda g: g.affine_select(out=self.ntri[:], in_=self.ntri[:], pattern=[[-1, 128]],
                                                compare_op=ALU.is_gt, fill=0.0, base=0, channel_multiplier=1),
              [self.ntri], [self.ntri])
        self.cmpm = None
        if need_cmp:
            self.cmpm = kb.sb([128, 2, S], BF16)
            memset(kb, "dve", self.cmpm, self.cmpm[:], 1.0)
            for j in range(2):
                kb.op("pool", lambda g, j=j: g.affine_select(out=self.cmpm[:, j, :], in_=self.cmpm[:, j, :], pattern=[[1, S]],
                                                             compare_op=ALU.is_ge, fill=0.0, base=-31 - 2048 * j,
                                                             channel_multiplier=-16), [self.cmpm], [self.cmpm])
        self.zl = kb.sb([128, 128], BF16)
        self.zr = kb.sb([128, 512], BF16)
        memset(kb, "dve", self.zl, self.zl[:], 0.0)
        memset(kb, "dve", self.zr, self.zr[:], 0.0)
        self.psS = Ring([kb.ps([128, 512], F32) for _ in range(3)])
        self.psO = Ring([kb.ps([128, 4, 128], F32) for _ in range(3)])
        self.pT = Ring([kb.sb([128, 512], BF16) for _ in range(3)])
        self.den = Ring([kb.sb([128, 4], F32) for _ in range(3)])
        self.coef = Ring([kb.sb([128, 4], F32) for _ in range(3)])
        self.tmp = Ring([kb.sb([128, 4, 64], F32) for _ in range(2)])
        self.mask_eng = Ring(["pool", "dve"])
        self.pend = []
        self.LA = 2

    def push(self, fn):
        self.pend.append(fn)
        while len(self.pend) > self.LA:
            self.pend.pop(0)()

    def flush_all(self):
        while self.pend:
            self.pend.pop(0)()


def attn_branch(A, Kc, q_t, k_t, v_t, nkeys_last, sched, Q, acc_t, first, gate_t=None, gate_ap=None, after=None):
    kb = A.kb
    po = A.psO.next()
    mm(kb, po, po[:].rearrange("p a b -> p (a b)"), A.zl, A.zl[:], A.zr, A.zr[:], start=True, stop=True)

    def fin():
        den, coef = A.den.next(), A.coef.next()
        ts(kb, "dve", den, den[:], po, po[:, :, 64], 1e-30, None, ALU.max)
        kb.op("dve", lambda g: g.reciprocal(out=coef[:], in_=den[:]), [den], [coef])
        if gate_t is not None:
            tt(kb, "dve", coef, coef[:], coef, coef[:], gate_t, gate_ap, ALU.mult)
        cb = coef[:].unsqueeze(2).to_broadcast([128, 4, 64])
        if first:
            tt(kb, "dve", acc_t, acc_t[:], po, po[:, :, 0:64], coef, cb, ALU.mult)
        else:
            tmp = A.tmp.next()
            tt(kb, "dve", tmp, tmp[:], po, po[:, :, 0:64], coef, cb, ALU.mult)
            tt(kb, "pool", acc_t, acc_t[:], acc_t, acc_t[:], tmp, tmp[:], ALU.add)
        if after is not None:
            after()

    items = sched(Q)
    for idx, (kt, s0, s1, masks, full) in enumerate(items):
        ksz = 128
        if nkeys_last is not None and (kt + 1) * 128 > nkeys_last:
            ksz = nkeys_last - kt * 128
        c0, c1 = s0 * 128, s1 * 128
        ps = A.psS.next()
        mm(kb, ps, ps[0:ksz, c0:c1], k_t, k_t[0:Kc, kt * 128:kt * 128 + ksz], q_t, q_t[0:Kc, Q * 512 + c0:Q * 512 + c1])
        p = A.pT.next()
        act(kb, p, p[0:ksz, c0:c1], ps, ps[0:ksz, c0:c1], AF.Exp, scale=0.125)
        for (s, name) in masks:
            m = A.tri if name == "tri" else A.ntri
            tt(kb, A.mask_eng.next(), p, p[0:ksz, s * 128:(s + 1) * 128], p, p[0:ksz, s * 128:(s + 1) * 128], m, m[0:ksz, :], ALU.mult)
        if full == "cmp":
            tt(kb, A.mask_eng.next(), p, p[0:ksz, c0:c1], p, p[0:ksz, c0:c1], A.cmpm, A.cmpm[0:ksz, kt, Q * 512 + c0:Q * 512 + c1], ALU.mult)
        last = idx == len(items) - 1

        def pv(p=p, ksz=ksz, kt=kt, s0=s0, s1=s1, last=last):
            for s in range(s0, s1):
                mm(kb, po, po[:, s, 0:65], p, p[0:ksz, s * 128:(s + 1) * 128], v_t, v_t[0:ksz, kt, :], start=False, stop=False)
            if last:
                fin()

        A.push(pv)


def make_ramp(kb, t, ap, parts, n, start=0.0):
    ones = kb.sb([parts, n], F32)
    memset(kb, "dve", ones, ones[:], 1.0)
    kb.op("dve", lambda g: g.tensor_tensor_scan(out=ap, data0=ones[:], data1=ones[:], initial=float(start) - 1.0,
                                                op0=ALU.mult, op1=ALU.add), [ones], [t])


def make_ident(kb, dtype=F32):
    ident = kb.sb([128, 128], dtype)
    memset(kb, "dve", ident, ident[:], 1.0)
    kb.op("pool", lambda g: g.affine_select(out=ident[:], in_=ident[:], pattern=[[-1, 128]],
                                            compare_op=ALU.is_equal, fill=0.0, base=0, channel_multiplier=1),
          [ident], [ident])
    return ident


def gelu_tanh(kb, z, zap, out_t, out_ap, shape, tmp1, tmp2):
    a1 = tmp1[tuple(slice(0, s) for s in shape)]
    a2 = tmp2[tuple(slice(0, s) for s in shape)]
    act(kb, tmp1, a1, z, zap, AF.Square)
    ts(kb, "dve", tmp1, a1, tmp1, a1, 0.044715, 1.0, ALU.mult, ALU.add)
    tt(kb, "dve", tmp1, a1, tmp1, a1, z, zap, ALU.mult)
    act(kb, tmp2, a2, tmp1, a1, AF.Sigmoid, scale=1.5957691216057308)
    tt(kb, "dve", out_t, out_ap, z, zap, tmp2, a2, ALU.mult)


def nsa_compress(kb, G):
    with kb.phase():
        ident = make_ident(kb)
        pps = kb.ps([64, 32], F32)
        pb = kb.ps([128, 1], F32)
        ph = kb.ps([128, 256], F32)
        pk = kb.ps([64, 256], F32)
        pv = kb.ps([128, 64], F32)
        for which in ("k", "v"):
            w1_d, w2_d, pe_d = G["cmp_%s_w1" % which], G["cmp_%s_w2" % which], G["pe_%s" % which]
            w1 = kb.sb([128, 32, 128], BF16)
            w1v = w1_d.rearrange("(l d) f -> d l f", d=64)
            kb.dma("pool", w1[0:64], w1v, writes=[w1])
            kb.dma("pool", w1[64:128], w1v, writes=[w1])
            w2 = kb.sb([128, 64], BF16)
            kb.dma("pool", w2[:], w2_d, writes=[w2])
            pe = kb.sb([32, 64], F32)
            kb.dma("sp", pe[:], pe_d, writes=[pe])
            kb.op("pe", lambda e: e.transpose(pps[:], pe[:], ident[0:32, 0:32]), [pe, ident], [pps])
            peT = kb.sb([64, 32], BF16)
            cp(kb, "act", peT, peT[:], pps, pps[:])
            for l in range(32):
                mm(kb, pb, pb[:], w1, w1[0:64, l, :], peT, peT[:, l:l + 1], start=(l == 0), stop=(l == 31))
            bias = kb.sb([128, 1], F32)
            cp(kb, "act", bias, bias[:], pb, pb[:])
            src = kb.sb([128, S], BF16)
            if which == "k":
                kb.dma("sp", src[0:64], G["kcT"][0], reads=[G["b_qk0"]], writes=[src])
                kb.dma("sp", src[64:128], G["kcT"][1], reads=[G["b_qk0"]], writes=[src])
            else:
                kb.dma("sp", src[:], G["vcT"], reads=[G["b_qk0"]], writes=[src])
            for g in range(2):
                for l in range(32):
                    mm(kb, ph, ph[:, 0:255], w1, w1[g * 64:(g + 1) * 64, l, :], src, src[g * 64:(g + 1) * 64, l:l + 4065:16],
                       start=(l == 0), stop=(l == 31))
                z = kb.sb([128, 255], F32)
                act(kb, z, z[:], ph, ph[:, 0:255], AF.Identity, bias=bias[:, 0:1], extra=[bias])
                hid = kb.sb([128, 256], BF16)
                t1, t2 = kb.sb([128, 255], F32), kb.sb([128, 255], F32)
                gelu_tanh(kb, z, z[:], hid, hid[:, 0:255], (128, 255), t1, t2)
                if which == "k":
                    mm(kb, pk, pk[:, 0:255], w2, w2[:], hid, hid[:, 0:255])
                    kc = kb.sb([64, 256], BF16)
                    memset(kb, "dve", kc, kc[:], 0.0)
                    cp(kb, "act", kc, kc[:, 0:255], pk, pk[:, 0:255])
                    kb.dma("sp", G["kcmpT"][g], kc[:], reads=[kc], writes=[G["b_cmp"]])
                else:
                    vcs = kb.sb([128, 2, 65], BF16)
                    memset(kb, "dve", vcs, vcs[:], 0.0)
                    memset(kb, "dve", vcs, vcs[:, :, 64:65], 1.0)
                    for j in range(2):
                        nsz = 128 if j == 0 else 127
                        mm(kb, pv, pv[0:nsz, :], hid, hid[:, j * 128:j * 128 + nsz], w2, w2[:])
                        cp(kb, "act", vcs, vcs[0:nsz, j, 0:64], pv, pv[0:nsz, :])
                    kb.dma("sp", G["vcmp"][g], vcs[:], reads=[vcs], writes=[G["b_cmp"]])


def nsa_select(kb, G):
    with kb.phase():
        identb = make_ident(kb, BF16)
        val = kb.sb([128, 128], F32)
        make_ramp(kb, val, val[:], 128, 128, start=-64.0)
        ts(kb, "dve", val, val[64:128, :], val, val[64:128, :], -1.0, None, ALU.add)
        KM, AM, fut = kb.sb([128, 128], F32), kb.sb([128, 128], F32), kb.sb([128, 128], F32)
        ts(kb, "dve", KM, KM[:], val, val[:], -2.0, None, ALU.is_le)
        ts(kb, "dve", fut, fut[:], val, val[:], 1.0, None, ALU.is_ge)
        ts(kb, "dve", AM, AM[:], KM, KM[:], -1000.0, 1000.0, ALU.mult, ALU.add)
        stt(kb, "dve", AM, AM[:], fut, fut[:], -1001.0, AM, AM[:], ALU.mult, ALU.add)
        Mc = kb.sb([128, 8], F32)
        memset(kb, "dve", Mc, Mc[:], 1.0)
        kb.op("pool", lambda g: g.affine_select(out=Mc[:], in_=Mc[:], pattern=[[-16, 8]], compare_op=ALU.is_ge,
                                                fill=0.0, base=-15, channel_multiplier=1), [Mc], [Mc])
        q4 = kb.sb([64, 4, S], BF16)
        kcm = kb.sb([64, 256], BF16)
        negT = kb.sb([64, S], BF16)
        psc = Ring([kb.ps([128, 4, 256], F32) for _ in range(2)])
        ptr = Ring([kb.ps([64, 128], BF16) for _ in range(2)])
        pcs = Ring([kb.sb([128, 4, 256], F32) for _ in range(2)])
        Psum = Ring([kb.sb([128, 256], F32) for _ in range(2)])
        sm = Ring([kb.sb([128, 160], F32) for _ in range(2)])
        sc = Ring([kb.sb([128, 64], F32) for _ in range(2)])
        ngb = Ring([kb.sb([128, 64], BF16) for _ in range(2)])
        for g in range(2):
            for z in range(4):
                kb.dma("sp", q4[:, z, :], G["qaug0"][g * 4 + z, 0:64, :], reads=[G["b_qk0"]], writes=[q4])
            kb.dma("sp", kcm[:], G["kcmpT"][g], reads=[G["b_cmp"]], writes=[kcm])
            for it in pcs.items + Psum.items:
                memset(kb, "pool", it, it[:], 0.0)
            for c in range(32):
                ncv = min(255, 8 * c + 7)
                p = psc.next()
                for z in range(4):
                    mm(kb, p, p[:, z, 0:ncv], q4, q4[:, z, c * 128:(c + 1) * 128], kcm, kcm[:, 0:ncv])
                pc = pcs.next()
                act(kb, pc, pc[:, :, 0:ncv], p, p[:, :, 0:ncv], AF.Exp, scale=0.125)
                if c == 0:
                    tt(kb, "dve", pc, pc[:, :, 0:7], pc, pc[:, :, 0:7], Mc, Mc[:, 1:8].unsqueeze(1).to_broadcast([128, 4, 7]), ALU.mult)
                else:
                    tt(kb, "dve", pc, pc[:, :, ncv - 8:ncv], pc, pc[:, :, ncv - 8:ncv], Mc,
                       Mc[:, 0:8].unsqueeze(1).to_broadcast([128, 4, 8]), ALU.mult)
                w = sm.next()
                kb.op("dve", lambda e: e.tensor_reduce(out=w[:, 0:4], in_=pc[:, :, 0:ncv], axis=AX.X, op=ALU.add), [pc], [w])
                ts(kb, "dve", w, w[:, 4:8], w, w[:, 0:4], 1e-30, None, ALU.max)
                kb.op("dve", lambda e: e.reciprocal(out=w[:, 8:12], in_=w[:, 4:8]), [w], [w])
                P_ = Psum.next()
                ts(kb, "dve", P_, P_[:, 0:ncv], pc, pc[:, 0, 0:ncv], w[:, 8:9], None, ALU.mult, extra=[w])
                for z in range(1, 4):
                    stt(kb, "dve", P_, P_[:, 0:ncv], pc, pc[:, z, 0:ncv], w[:, 8 + z:9 + z], P_, P_[:, 0:ncv], ALU.mult, ALU.add,
                        extra=[w])
                s_ = sc.next()
                kb.op("dve", lambda e: e.tensor_reduce(out=w[:, 16:80], in_=P_[:].rearrange("p (j r) -> p j r", r=4),
                                                       axis=AX.X, op=ALU.add), [P_], [w])
                stt(kb, "dve", s_, s_[:], P_, P_[:, 3:256:4], -0.5, w, w[:, 16:80], ALU.mult, ALU.add)
                stt(kb, "dve", s_, s_[:, 1:64], P_, P_[:, 3:252:4], 0.5, s_, s_[:, 1:64], ALU.mult, ALU.add)
                tt(kb, "dve", s_, s_[:], s_, s_[:], KM, KM[:, 64 - 2 * c:128 - 2 * c], ALU.mult)
                tt(kb, "dve", s_, s_[:], s_, s_[:], AM, AM[:, 64 - 2 * c:128 - 2 * c], ALU.add)
                memset(kb, "dve", s_, s_[:, 0:1], 1000.0)
                kb.op("dve", lambda e: e.max(out=w[:, 80:88], in_=s_[:]), [s_], [w])
                ts(kb, "dve", w, w[:, 88:89], w, w[:, 87:88], 0.0, None, ALU.max)
                ts(kb, "dve", s_, s_[:], s_, s_[:], w[:, 88:89], None, ALU.is_ge, extra=[w])
                nb = ngb.next()
                ts(kb, "dve", nb, nb[:], s_, s_[:], -1.0, BIG, ALU.add, ALU.mult)
                pT = ptr.next()
                kb.op("pe", lambda e: e.transpose(pT[:], nb[:], identb[:]), [nb, identb], [pT])
                cp(kb, "act", negT, negT[:, c * 128:(c + 1) * 128], pT, pT[:])
            for z in range(4):
                kb.dma("sp", G["qaug0"][g * 4 + z, 64:128, :], negT[:], reads=[negT], writes=[G["b_neg0"]])


def nsa_attn(kb, G):
    with kb.phase():
        A = AttnCtx(kb, need_cmp=True)
        identb = make_ident(kb, BF16)
        Ec = kb.sb([128, S], BF16)
        memset(kb, "dve", Ec, Ec[:], 1.0)
        kb.op("pool", lambda g: g.affine_select(out=Ec[64:128, :], in_=Ec[64:128, :], pattern=[[1, S]], compare_op=ALU.is_ge,
                                                fill=0.0, base=0, channel_multiplier=-64), [Ec], [Ec])
        kb.op("pool", lambda g: g.affine_select(out=Ec[64:128, :], in_=Ec[64:128, :], pattern=[[-1, S]], compare_op=ALU.is_ge,
                                                fill=0.0, base=63, channel_multiplier=64), [Ec], [Ec])
        gates = kb.sb([128, 32, 24], F32)
        kb.dma("sp", gates[:], G["gates"].rearrange("(c p) n -> p c n", p=128), reads=[G["b_qk0"]], writes=[gates])
        ks = kb.sb([128, S], BF16)
        kw = kb.sb([64, S], BF16)
        kcm = kb.sb([64, 256], BF16)
        vs = kb.sb([128, 32, 65], BF16)
        vw = kb.sb([128, 32, 65], BF16)
        vcm = kb.sb([128, 2, 65], BF16)
        qr = Ring([kb.sb([128, S], BF16) for _ in range(2)])
        accr = Ring([kb.sb([128, 4, 64], F32) for _ in range(2)])
        otok = kb.sb([128, 8, 4, 128], BF16)
        ptr = Ring([kb.ps([128, 4, 128], BF16) for _ in range(2)])
        oT = Ring([kb.sb([128, 512], BF16) for _ in range(2)])
        v3v = G["v3"].rearrange("(kt p) b c -> p kt b c", p=128)
        for hp in range(4):
            g = hp // 2
            if hp % 2 == 0:
                kb.dma("sp", ks[0:64], G["ksT"][g], reads=[G["b_qk0"]], writes=[ks])
                cp(kb, "pool", ks, ks[64:128, :], Ec, Ec[64:128, :])
                kb.dma("sp", kw[:], G["kwT"][g], reads=[G["b_qk0"]], writes=[kw])
                kb.dma("sp", kcm[:], G["kcmpT"][g], reads=[G["b_cmp"]], writes=[kcm])
                kb.dma("sp", vs[:], v3v[:, :, 2 + g, :], reads=[G["b_qk0"]], writes=[vs])
                kb.dma("sp", vw[:], v3v[:, :, 4 + g, :], reads=[G["b_qk0"]], writes=[vw])
                kb.dma("sp", vcm[:], G["vcmp"][g], reads=[G["b_cmp"]], writes=[vcm])
            for hh in range(2):
                h = hp * 2 + hh
                q = qr.next()
                kb.dma("sp", q[:], G["qaug0"][h], reads=[G["b_qk0"], G["b_neg0"]], writes=[q])
                for Q in range(8):
                    acc = accr.next()
                    attn_branch(A, 64, q, kcm, vcm, 255, sched_cmp, Q, acc, True, gates, gates[:, 4 * Q:4 * Q + 4, h * 3 + 0])
                    attn_branch(A, 128, q, ks, vs, None, sched_causal, Q, acc, False, gates, gates[:, 4 * Q:4 * Q + 4, h * 3 + 1])
                    attn_branch(A, 64, q, kw, vw, None, sched_win, Q, acc, False, gates, gates[:, 4 * Q:4 * Q + 4, h * 3 + 2],
                                after=lambda acc=acc, Q=Q, hh=hh: cp(kb, "act", otok, otok[:, Q, :, hh * 64:(hh + 1) * 64], acc, acc[:]))
            A.flush_all()
            for Q in range(8):
                pT = ptr.next()
                for s in range(4):
                    kb.op("pe", lambda e, s=s: e.transpose(pT[:, s, :], otok[:, Q, s, :], identb[:]), [otok, identb], [pT])
                o = oT.next()
                cp(kb, "dve", o, o[:].rearrange("p (s q) -> p s q", s=4), pT, pT[:])
                kb.dma("sp", G["o0T"][hp * 128:(hp + 1) * 128, Q * 512:(Q + 1) * 512], o[:], reads=[o], writes=[G["b_o0"]])


def sincos_from_rev(kb, r, rap, shape, tmp, out_sin, out_sin_ap, out_cos, out_cos_ap):
    a1 = tmp[0][tuple(slice(0, s) for s in shape)]
    a2 = tmp[1][tuple(slice(0, s) for s in shape)]
    ts(kb, "dve", tmp[0], a1, r, rap, MAGIC, MAGIC, ALU.add, ALU.subtract)
    tt(kb, "dve", tmp[0], a1, r, rap, tmp[0], a1, ALU.subtract)
    act(kb, out_sin, out_sin_ap, tmp[0], a1, AF.Sin, scale=TWO_PI)
    ts(kb, "dve", tmp[1], a2, r, rap, 0.25, None, ALU.add)
    ts(kb, "dve", tmp[0], a1, tmp[1], a2, MAGIC, MAGIC, ALU.add, ALU.subtract)
    tt(kb, "dve", tmp[0], a1, tmp[1], a2, tmp[0], a1, ALU.subtract)
    act(kb, out_cos, out_cos_ap, tmp[0], a1, AF.Sin, scale=TWO_PI)


def s5(kb, G):
    with kb.phase():
        ident = make_ident(kb)
        identb = make_ident(kb, BF16)
        par = kb.sb([128, 4, 16], F32)
        with contextlib.ExitStack() as rs:
            old = kb.stack
            kb.stack = rs
            lr, li = kb.sb([16, 128], F32), kb.sb([16, 128], F32)
            kb.dma("sp", lr[:], G["s5_lre"].rearrange("(s w) p -> s (w p)", w=2), writes=[lr])
            kb.dma("sp", li[:], G["s5_lim"].rearrange("(s w) p -> s (w p)", w=2), writes=[li])
            ls = kb.sb([16, 2], F32)
            kb.dma("sp", ls[:], G["s5_ls"].rearrange("(s w) -> s w", w=2), writes=[ls])
            act(kb, ls, ls[:], ls, ls[:], AF.Exp)
            W = [kb.sb([16, 128], F32) for _ in range(12)]
            stepb, mag, thr, sn, cs, ar1, ai, den, fr, fi, ta, tb = W
            cp(kb, "dve", stepb, stepb[:].rearrange("s (w p) -> s w p", w=2), ls, ls[:].unsqueeze(2).to_broadcast([16, 2, 64]))
            tt(kb, "dve", ta, ta[:], lr, lr[:], stepb, stepb[:], ALU.mult)
            act(kb, mag, mag[:], ta, ta[:], AF.Exp)
            tt(kb, "dve", thr, thr[:], li, li[:], stepb, stepb[:], ALU.mult)
            ts(kb, "dve", thr, thr[:], thr, thr[:], 1.0 / TWO_PI, None, ALU.mult)
            sincos_from_rev(kb, thr, thr[:], (16, 128), (ta, tb), sn, sn[:], cs, cs[:])
            tt(kb, "dve", ai, ai[:], mag, mag[:], sn, sn[:], ALU.mult)
            tt(kb, "dve", ar1, ar1[:], mag, mag[:], cs, cs[:], ALU.mult)
            ts(kb, "dve", ar1, ar1[:], ar1, ar1[:], -1.0, None, ALU.add)
            tt(kb, "dve", ta, ta[:], lr, lr[:], lr, lr[:], ALU.mult)
            tt(kb, "dve", tb, tb[:], li, li[:], li, li[:], ALU.mult)
            tt(kb, "dve", den, den[:], ta, ta[:], tb, tb[:], ALU.add)
            kb.op("dve", lambda e: e.reciprocal(out=den[:], in_=den[:]), [den], [den])
            tt(kb, "dve", ta, ta[:], ar1, ar1[:], lr, lr[:], ALU.mult)
            tt(kb, "dve", tb, tb[:], ai, ai[:], li, li[:], ALU.mult)
            tt(kb, "dve", fr, fr[:], ta, ta[:], tb, tb[:], ALU.add)
            tt(kb, "dve", fr, fr[:], fr, fr[:], den, den[:], ALU.mult)
            tt(kb, "dve", ta, ta[:], ai, ai[:], lr, lr[:], ALU.mult)
            tt(kb, "dve", tb, tb[:], ar1, ar1[:], li, li[:], ALU.mult)
            tt(kb, "dve", fi, fi[:], ta, ta[:], tb, tb[:], ALU.subtract)
            tt(kb, "dve", fi, fi[:], fi, fi[:], den, den[:], ALU.mult)
            pp = kb.ps([128, 4, 16], F32)
            for i, src in enumerate((mag, thr, fr, fi)):
                kb.op("pe", lambda e, i=i, src=src: e.transpose(pp[:, i, :], src[:], ident[0:16, 0:16]), [src, ident], [pp])
            cp(kb, "act", par, par[:], pp, pp[:])
            kb.barrier()
            kb.stack = old
        BTr, BTi = kb.sb([128, 4, 128], BF16), kb.sb([128, 4, 128], BF16)
        CTr, CTi = kb.sb([128, 4, 128], BF16), kb.sb([128, 4, 128], BF16)
        with contextlib.ExitStack() as rs:
            old = kb.stack
            kb.stack = rs
            br, bi = kb.sb([128, 16, 16], F32), kb.sb([128, 16, 16], F32)
            kb.dma("sp", br[:], G["s5_bre"].rearrange("(s w) p c -> (w p) s c", w=2), writes=[br])
            kb.dma("sp", bi[:], G["s5_bim"].rearrange("(s w) p c -> (w p) s c", w=2), writes=[bi])
            frb = par[:, 2, :].unsqueeze(2).to_broadcast([128, 16, 16])
            fib = par[:, 3, :].unsqueeze(2).to_broadcast([128, 16, 16])
            t1, t2 = kb.sb([128, 16, 16], F32), kb.sb([128, 16, 16], F32)
            Bb = [kb.sb([128, 16, 16], F32), kb.sb([128, 16, 16], F32)]
            tt(kb, "dve", t1, t1[:], br, br[:], par, frb, ALU.mult)
            tt(kb, "dve", t2, t2[:], bi, bi[:], par, fib, ALU.mult)
            tt(kb, "dve", Bb[0], Bb[0][:], t1, t1[:], t2, t2[:], ALU.subtract)
            tt(kb, "dve", t1, t1[:], bi, bi[:], par, frb, ALU.mult)
            tt(kb, "dve", t2, t2[:], br, br[:], par, fib, ALU.mult)
            tt(kb, "dve", Bb[1], Bb[1][:], t1, t1[:], t2, t2[:], ALU.add)
            for ri, dstT in ((0, BTr), (1, BTi)):
                BD = kb.sb([128, 16, 32], BF16)
                memset(kb, "dve", BD, BD[:], 0.0)
                cp(kb, "dve", BD, BD[0:64, :, 0:16], Bb[ri], Bb[ri][0:64])
                cp(kb, "dve", BD, BD[64:128, :, 16:32], Bb[ri], Bb[ri][64:128])
                pT = kb.ps([128, 4, 128], BF16)
                for j in range(4):
                    kb.op("pe", lambda e, j=j: e.transpose(pT[:, j, :], BD[:, 4 * j:4 * j + 4, :].rearrange("p a b -> p (a b)"), identb[:]),
                          [BD, identb], [pT])
                cp(kb, "act", dstT, dstT[:], pT, pT[:])
            m0, m1 = kb.sb([128, 1], F32), kb.sb([128, 1], F32)
            memset(kb, "dve", m0, m0[:], 0.0)
            memset(kb, "dve", m1, m1[:], 1.0)
            for q in range(4):
                memset(kb, "dve", m0, m0[32 * q:32 * q + 16, :], 1.0)
                memset(kb, "dve", m1, m1[32 * q:32 * q + 16, :], 0.0)
            for ri, dstT, key in ((0, CTr, "s5_cre"), (1, CTi, "s5_cim")):
                ct = kb.sb([128, 4, 64], F32)
                kb.dma("sp", ct[:], G[key].rearrange("g c p -> (g c) p").rearrange("(j r) p -> r j p", r=128), writes=[ct])
                BD = kb.sb([128, 4, 128], BF16)
                ts(kb, "dve", BD, BD[:, :, 0:64], ct, ct[:], m0[:, 0:1], None, ALU.mult, extra=[m0])
                ts(kb, "dve", BD, BD[:, :, 64:128], ct, ct[:], m1[:, 0:1], None, ALU.mult, extra=[m1])
                pT = kb.ps([128, 4, 128], BF16)
                for j in range(4):
                    kb.op("pe", lambda e, j=j: e.transpose(pT[:, j, :], BD[:, j, :], identb[:]), [BD, identb], [pT])
                if ri == 0:
                    cp(kb, "act", dstT, dstT[:], pT, pT[:])
                else:
                    ts(kb, "dve", dstT, dstT[:], pT, pT[:], -1.0, None, ALU.mult)
            kb.barrier()
            kb.stack = old
        with contextlib.ExitStack() as rs:
            old = kb.stack
            kb.stack = rs
            uT = kb.sb([128, 4, S], BF16)
            kb.dma("sp", uT[:], fm(G["uT"]), reads=[G["b_qk0"]], writes=[uT])
            uTa = kb.sb([32, 4, S], BF16)
            cp(kb, "pool", uTa, uTa[:], uT, uT[96:128])
            BTra, BTia = kb.sb([32, 4, 128], BF16), kb.sb([32, 4, 128], BF16)
            cp(kb, "pool", BTra, BTra[:], BTr, BTr[96:128])
            cp(kb, "pool", BTia, BTia[:], BTi, BTi[96:128])
            jt = kb.sb([128, 513], F32)
            make_ramp(kb, jt, jt[:], 128, 513)
            rT = kb.sb([128, 513], F32)
            tmpA, tmpB = kb.sb([128, 513], F32), kb.sb([128, 513], F32)
            cTr = Ring([kb.sb([128, 513], F32) for _ in range(2)])
            sTr = Ring([kb.sb([128, 513], F32) for _ in range(2)])
            pbr = Ring([kb.ps([128, 512], F32) for _ in range(2)])
            pbi = Ring([kb.ps([128, 512], F32) for _ in range(2)])
            pyr = Ring([kb.ps([128, 4, 32], F32) for _ in range(2)])
            E = [Ring([kb.sb([128, 512], F32) for _ in range(2)]) for _ in range(6)]
            zrr = Ring([kb.sb([128, 512], F32) for _ in range(2)])
            zir = Ring([kb.sb([128, 512], F32) for _ in range(2)])
            xrr = Ring([kb.sb([128, 512], BF16) for _ in range(2)])
            xir = Ring([kb.sb([128, 512], BF16) for _ in range(2)])
            car = Ring([kb.sb([128, 4], F32) for _ in range(2)])
            ysr = Ring([kb.sb([128, 4, 32], F32) for _ in range(2)])
            yv = G["ytm"].rearrange("(c p) f -> p c f", p=128)
            for st in range(16):
                ts(kb, "dve", rT, rT[:], jt, jt[:], par[:, 1, st:st + 1], None, ALU.mult, extra=[par])
                cT, sT = cTr.next(), sTr.next()
                sincos_from_rev(kb, rT, rT[:], (128, 513), (tmpA, tmpB), sT, sT[:], cT, cT[:])
                magb = par[:, 0, st:st + 1].to_broadcast([128, 512])
                po = (st % 4) * 32
                prev = None
                for ck in range(8):
                    a, b = pbr.next(), pbi.next()
                    csl = slice(ck * 512, (ck + 1) * 512)
                    if st % 4 == 3:
                        mm(kb, a, a[:], BTra, BTra[0:32, st // 4, :], uTa, uTa[0:32, st // 4, csl])
                        mm(kb, b, b[:], BTia, BTia[0:32, st // 4, :], uTa, uTa[0:32, st // 4, csl])
                    else:
                        mm(kb, a, a[:], BTr, BTr[po:po + 32, st // 4, :], uT, uT[po:po + 32, st // 4, csl])
                        mm(kb, b, b[:], BTi, BTi[po:po + 32, st // 4, :], uT, uT[po:po + 32, st // 4, csl])
                    e = [r.next() for r in E]
                    tt(kb, "dve", e[0], e[0][:], a, a[:], cT, cT[:, 0:512], ALU.mult)
                    tt(kb, "dve", e[1], e[1][:], b, b[:], sT, sT[:, 0:512], ALU.mult)
                    tt(kb, "pool", e[0], e[0][:], e[0], e[0][:], e[1], e[1][:], ALU.add)
                    tt(kb, "dve", e[2], e[2][:], b, b[:], cT, cT[:, 0:512], ALU.mult)
                    tt(kb, "dve", e[3], e[3][:], a, a[:], sT, sT[:, 0:512], ALU.mult)
                    tt(kb, "pool", e[2], e[2][:], e[2], e[2][:], e[3], e[3][:], ALU.subtract)
                    zr, zi = zrr.next(), zir.next()
                    if prev is None:
                        i0, i1, ex = 0.0, 0.0, []
                    else:
                        pzr, pzi = prev
                        ca = car.next()
                        ts(kb, "dve", ca, ca[:, 0:1], pzi, pzi[:, 511:512], sT[:, 512:513], None, ALU.mult, extra=[sT])
                        stt(kb, "dve", ca, ca[:, 1:2], pzr, pzr[:, 511:512], cT[:, 512:513], ca, ca[:, 0:1], ALU.mult, ALU.subtract,
                            extra=[cT])
                        ts(kb, "dve", ca, ca[:, 2:3], pzi, pzi[:, 511:512], cT[:, 512:513], None, ALU.mult, extra=[cT])
                        stt(kb, "dve", ca, ca[:, 3:4], pzr, pzr[:, 511:512], sT[:, 512:513], ca, ca[:, 2:3], ALU.mult, ALU.add,
                            extra=[sT])
                        i0, i1, ex = ca[:, 1:2], ca[:, 3:4], [ca]
                    kb.op("dve", lambda g, i0=i0: g.tensor_tensor_scan(out=zr[:], data0=magb, data1=e[0][:], initial=i0,
                                                                       op0=ALU.mult, op1=ALU.add), [e[0], par] + ex, [zr])
                    kb.op("dve", lambda g, i1=i1: g.tensor_tensor_scan(out=zi[:], data0=magb, data1=e[2][:], initial=i1,
                                                                       op0=ALU.mult, op1=ALU.add), [e[2], par] + ex, [zi])
                    prev = (zr, zi)
                    xr, xi = xrr.next(), xir.next()
                    tt(kb, "pool", e[4], e[4][:], zr, zr[:], cT, cT[:, 0:512], ALU.mult)
                    tt(kb, "pool", e[5], e[5][:], zi, zi[:], sT, sT[:, 0:512], ALU.mult)
                    tt(kb, "pool", xr, xr[:], e[4], e[4][:], e[5], e[5][:], ALU.subtract)
                    tt(kb, "dve", e[1], e[1][:], zr, zr[:], sT, sT[:, 0:512], ALU.mult)
                    tt(kb, "pool", e[3], e[3][:], zi, zi[:], cT, cT[:, 0:512], ALU.mult)
                    tt(kb, "pool", xi, xi[:], e[1], e[1][:], e[3], e[3][:], ALU.add)
                    yp = pyr.next()
                    for tq in range(4):
                        mm(kb, yp, yp[:, tq, :], xr, xr[:, tq * 128:(tq + 1) * 128], CTr, CTr[:, st // 4, po:po + 32], start=True, stop=False)
                        mm(kb, yp, yp[:, tq, :], xi, xi[:, tq * 128:(tq + 1) * 128], CTi, CTi[:, st // 4, po:po + 32], start=False, stop=True)
                    ys = ysr.next()
                    cp(kb, "act", ys, ys[:], yp, yp[:])
                    kb.dma("sp", yv[:, ck * 4:ck * 4 + 4, st * 32:(st + 1) * 32], ys[:], reads=[ys], writes=[G["b_y"]])
            kb.barrier()
            kb.stack = old
        dbc = kb.sb([128, 512], F32)
        kb.dma("sp", dbc[:], G["s5_d"].to_broadcast([128, 512]), writes=[dbc])
        glw = kb.sb([128, 4, 512], BF16)
        kb.dma("pool", glw[:], G["s5_glw"].rearrange("(c p) n -> p c n", p=128), writes=[glw])
        glb = kb.sb([128, 4], F32)
        with kb.nc.allow_non_contiguous_dma(reason="tiny"):
            kb.dma("sp", glb[:], G["s5_glb"].rearrange("(c p) -> p c", p=128), writes=[glb])
        yr = Ring([kb.sb([128, 512], F32) for _ in range(2)])
        ur = Ring([kb.sb([128, 512], F32) for _ in range(2)])
        g1, g2 = kb.sb([128, 512], F32), kb.sb([128, 512], F32)
        gbr = Ring([kb.sb([128, 512], BF16) for _ in range(2)])
        pT = Ring([kb.ps([128, 4, 128], BF16) for _ in range(2)])
        gTr = Ring([kb.sb([128, 4, 128], BF16) for _ in range(2)])
        pz = Ring([kb.ps([128, 128], F32) for _ in range(2)])
        sgr = Ring([kb.sb([128, 128], F32) for _ in range(2)])
        osr = Ring([kb.sb([128, 4, 128], BF16) for _ in range(2)])
        ytv = G["ytm"].rearrange("(c p) f -> p c f", p=128)
        utv = G["utm"].rearrange("(c p) f -> p c f", p=128)
        o0v = G["o0T"][512:1024, :].rearrange("(c p) t -> p c t", p=128)
        for tk in range(32):
            y, u = yr.next(), ur.next()
            kb.dma("sp", y[:], ytv[:, tk, :], reads=[G["b_y"]], writes=[y])
            kb.dma("act", u[:], utv[:, tk, :], reads=[G["b_qk0"]], writes=[u])
            tt(kb, "dve", u, u[:], u, u[:], dbc, dbc[:], ALU.mult)
            tt(kb, "dve", y, y[:], y, y[:], u, u[:], ALU.add)
            gb_ = gbr.next()
            gelu_tanh(kb, y, y[:], gb_, gb_[:], (128, 512), g1, g2)
            p = pT.next()
            for c in range(4):
                kb.op("pe", lambda e, c=c: e.transpose(p[:, c, :], gb_[:, c * 128:(c + 1) * 128], identb[:]), [gb_, identb], [p])
            gT = gTr.next()
            cp(kb, "act", gT, gT[:], p, p[:])
            os_ = osr.next()
            for fo in range(4):
                z = pz.next()
                for c in range(4):
                    mm(kb, z, z[:], glw, glw[:, c, fo * 128:(fo + 1) * 128], gT, gT[:, c, :], start=(c == 0), stop=(c == 3))
                sg = sgr.next()
                act(kb, sg, sg[:], z, z[:], AF.Sigmoid, bias=glb[:, fo:fo + 1], extra=[glb])
                tt(kb, "dve", os_, os_[:, fo, :], gT, gT[:, fo, :], sg, sg[:], ALU.mult)
            kb.dma("sp", o0v[:, :, tk * 128:(tk + 1) * 128], os_[:], reads=[os_], writes=[G["b_o0"]])


def moba_select(kb, G):
    with kb.phase():
        identb = make_ident(kb, BF16)
        val = kb.sb([128, 32, 16], F32)
        nv = kb.sb([128, 16], F32)
        make_ramp(kb, nv, nv[:], 128, 16)
        tt(kb, "dve", val, val[:].rearrange("p (a b) n -> p a b n", b=2),
           nv, nv[:].unsqueeze(1).unsqueeze(1).to_broadcast([128, 16, 2, 16]),
           nv, nv[:].unsqueeze(2).unsqueeze(3).to_broadcast([128, 16, 2, 16]), ALU.subtract)
        addm, notown = kb.sb([128, 32, 16], F32), kb.sb([128, 32, 16], F32)
        ts(kb, "dve", addm, addm[:], val, val[:], 0.0, -1e30, ALU.is_ge, ALU.mult)
        ts(kb, "dve", notown, notown[:], val, val[:], 0.0, None, ALU.not_equal)
        qr = Ring([kb.sb([64, S], BF16) for _ in range(2)])
        kr = Ring([kb.sb([64, S], BF16) for _ in range(2)])
        km = Ring([kb.sb([64, 16], F32) for _ in range(2)])
        kmb = Ring([kb.sb([64, 16], BF16) for _ in range(2)])
        pg = Ring([kb.ps([128, 32, 16], F32) for _ in range(2)])
        gs = Ring([kb.sb([128, 32, 16], F32) for _ in range(2)])
        eq = kb.sb([128, 32, 16], F32)
        mx = Ring([kb.sb([128, 32], F32) for _ in range(2)])
        ngr = Ring([kb.sb([128, 32, 16], BF16) for _ in range(2)])
        ptr = Ring([kb.ps([16, 8, 128], BF16) for _ in range(2)])
        ngT = Ring([kb.sb([16, S], BF16) for _ in range(2)])
        for h in range(16):
            q, k = qr.next(), kr.next()
            kb.dma("sp", q[:], G["qaug1"][h, 0:64, :], reads=[G["b_qk1"]], writes=[q])
            kb.dma("act", k[:], G["kaug1"][h, 0:64, :], reads=[G["b_qk1"]], writes=[k])
            m_, mb = km.next(), kmb.next()
            kb.op("dve", lambda e: e.tensor_reduce(out=m_[:], in_=k[:].rearrange("p (n j) -> p n j", j=256), axis=AX.X, op=ALU.add),
                  [k], [m_])
            ts(kb, "dve", mb, mb[:], m_, m_[:], 1.0 / 256.0, None, ALU.mult)
            p = pg.next()
            for c in range(32):
                mm(kb, p, p[:, c, :], q, q[:, c * 128:(c + 1) * 128], mb, mb[:])
            g_ = gs.next()
            tt(kb, "dve", g_, g_[:], p, p[:], addm, addm[:], ALU.add)
            m = mx.next()
            for it in range(3):
                kb.op("dve", lambda e: e.tensor_reduce(out=m[:], in_=g_[:], axis=AX.X, op=ALU.max), [g_], [m])
                if it < 2:
                    tt(kb, "dve", eq, eq[:], g_, g_[:], m, m[:].unsqueeze(2).to_broadcast([128, 32, 16]), ALU.is_equal)
                    stt(kb, "dve", g_, g_[:], eq, eq[:], -1e30, g_, g_[:], ALU.mult, ALU.add)
            ts(kb, "dve", m, m[:], m, m[:], -1e29, None, ALU.max)
            tt(kb, "dve", eq, eq[:], p, p[:], addm, addm[:], ALU.add)
            tt(kb, "dve", eq, eq[:], eq, eq[:], m, m[:].unsqueeze(2).to_broadcast([128, 32, 16]), ALU.is_ge)
            ts(kb, "dve", eq, eq[:], eq, eq[:], -1.0, BIG, ALU.add, ALU.mult)
            ng = ngr.next()
            tt(kb, "dve", ng, ng[:], eq, eq[:], notown, notown[:], ALU.mult)
            nT = ngT.next()
            for c8 in range(4):
                pT = ptr.next()
                for j in range(8):
                    c = c8 * 8 + j
                    kb.op("pe", lambda e, c=c, j=j: e.transpose(pT[:, j, :], ng[:, c, :], identb[:]), [ng, identb], [pT])
                cp(kb, "act", nT, nT[:, c8 * 1024:(c8 + 1) * 1024].rearrange("p (j q) -> p j q", j=8), pT, pT[:])
            kb.dma("sp", G["qaug1"][h, 64:80, :], nT[:], reads=[nT], writes=[G["b_neg1"]])


def moba_attn(kb, G):
    with kb.phase():
        A = AttnCtx(kb, need_cmp=False)
        identb = make_ident(kb, BF16)
        Ec = kb.sb([128, S], BF16)
        memset(kb, "dve", Ec, Ec[:], 1.0)
        kb.op("pool", lambda g: g.affine_select(out=Ec[64:80, :], in_=Ec[64:80, :], pattern=[[1, S]], compare_op=ALU.is_ge,
                                                fill=0.0, base=0, channel_multiplier=-256), [Ec], [Ec])
        kb.op("pool", lambda g: g.affine_select(out=Ec[64:80, :], in_=Ec[64:80, :], pattern=[[-1, S]], compare_op=ALU.is_ge,
                                                fill=0.0, base=255, channel_multiplier=256), [Ec], [Ec])
        qr = Ring([kb.sb([80, S], BF16) for _ in range(2)])
        kr = Ring([kb.sb([80, S], BF16) for _ in range(2)])
        vr = Ring([kb.sb([128, 32, 65], BF16) for _ in range(2)])
        accr = Ring([kb.sb([128, 4, 64], F32) for _ in range(2)])
        otok = kb.sb([128, 8, 4, 128], BF16)
        ptr = Ring([kb.ps([128, 4, 128], BF16) for _ in range(2)])
        oT = Ring([kb.sb([128, 512], BF16) for _ in range(2)])
        v1v = G["v1"].rearrange("(kt p) b c -> p kt b c", p=128)
        for hp in range(8):
            for hh in range(2):
                h = hp * 2 + hh
                q, k, v = qr.next(), kr.next(), vr.next()
                kb.dma("sp", q[:], G["qaug1"][h], reads=[G["b_qk1"], G["b_neg1"]], writes=[q])
                kb.dma("act", k[0:64], G["kaug1"][h, 0:64, :], reads=[G["b_qk1"]], writes=[k])
                cp(kb, "pool", k, k[64:80, :], Ec, Ec[64:80, :])
                kb.dma("sp", v[:], v1v[:, :, h, :], reads=[G["b_qk1"]], writes=[v])
                for Q in range(8):
                    acc = accr.next()
                    attn_branch(A, 80, q, k, v, None, sched_causal, Q, acc, True,
                                after=lambda acc=acc, Q=Q, hh=hh: cp(kb, "act", otok, otok[:, Q, :, hh * 64:(hh + 1) * 64], acc, acc[:]))
            A.flush_all()
            for Q in range(8):
                pT = ptr.next()
                for s in range(4):
                    kb.op("pe", lambda e, s=s: e.transpose(pT[:, s, :], otok[:, Q, s, :], identb[:]), [otok, identb], [pT])
                o = oT.next()
                cp(kb, "dve", o, o[:].rearrange("p (s q) -> p s q", s=4), pT, pT[:])
                kb.dma("sp", G["o1T"][hp * 128:(hp + 1) * 128, Q * 512:(Q + 1) * 512], o[:], reads=[o], writes=[G["b_o1"]])


def build_program(debug=False, upto=99):
    nc = bass.Bass("TRN2", target_bir_lowering=False)
    G = {}

    def din(name, shape, dt=F32):
        return nc.dram_tensor(name, list(shape), dt, kind="ExternalInput").ap()

    def scr(name, shape, dt=F32, out=False):
        isout = out or (debug and (debug is True or name in debug))
        return nc.dram_tensor(name, list(shape), dt, kind="ExternalOutput" if isout else "Internal").ap()

    xT = din("xT", [D, S])
    w0r, w0s = din("w0r", [D, 14 * 64]), din("w0s", [D, 14 * 64])
    w0f, w0t = din("w0f", [D, 5 * 128]), din("w0t", [D, 920])
    w1r, w1s, w1t = din("w1r", [D, 32 * 64]), din("w1s", [D, 32 * 64]), din("w1t", [D, 1024])
    for wh in ("k", "v"):
        G["cmp_%s_w1" % wh] = din("cmp_%s_w1" % wh, [2048, 128])
        G["cmp_%s_w2" % wh] = din("cmp_%s_w2" % wh, [128, 64])
        G["pe_%s" % wh] = din("pe_%s" % wh, [32, 64])
    G["s5_lre"], G["s5_lim"], G["s5_ls"] = din("s5_lre", [32, 64]), din("s5_lim", [32, 64]), din("s5_ls", [32])
    G["s5_bre"], G["s5_bim"] = din("s5_bre", [32, 64, 16]), din("s5_bim", [32, 64, 16])
    G["s5_cre"], G["s5_cim"] = din("s5_cre", [32, 16, 64]), din("s5_cim", [32, 16, 64])
    G["s5_d"], G["s5_glw"], G["s5_glb"] = din("s5_d", [1, 512]), din("s5_glw", [512, 512]), din("s5_glb", [512])
    ev_wo, od_wo = din("ev_wo", [D, D]), din("od_wo", [D, D])
    ln = {k: din(k, [2, D]) for k in ("ln_mix_g", "ln_mix_b", "ln_ffn_g", "ln_ffn_b")}
    G["moe_wr"] = [din("moe_wr%d" % L, [D, 36]) for L in range(2)]
    G["moe_br"] = [din("moe_br%d" % L, [1, 36]) for L in range(2)]
    G["moe_wg"] = [din("moe_wg%d" % L, [32, D, 128]) for L in range(2)]
    G["moe_wu"] = [din("moe_wu%d" % L, [32, D, 128]) for L in range(2)]
    G["moe_wd"] = [din("moe_wd%d" % L, [32, 128, D]) for L in range(2)]
    yT = scr("yT", [D, S], out=True)
    G["ropeC"], G["ropeS"] = scr("ropeC", [64, S]), scr("ropeS", [64, S])
    G["qaug0"] = scr("qaug0", [8, 128, S], BF16)
    G["ksT"], G["kwT"], G["kcT"] = scr("ksT", [2, 64, S], BF16), scr("kwT", [2, 64, S], BF16), scr("kcT", [2, 64, S], BF16)
    G["vcT"], G["uT"] = scr("vcT", [128, S], BF16), scr("uT", [512, S], BF16)
    G["v3"] = scr("v3", [S, 6, 65], BF16)
    G["gates"], G["utm"], G["ytm"] = scr("gates", [S, 24]), scr("utm", [S, 512]), scr("ytm", [S, 512])
    G["kcmpT"], G["vcmp"] = scr("kcmpT", [2, 64, 256], BF16), scr("vcmp", [2, 128, 2, 65], BF16)
    G["o0T"], G["o1T"] = scr("o0T", [D, S], BF16), scr("o1T", [D, S], BF16)
    mixT = scr("mixT", [D, S])
    x1T, x2T, x3T = scr("x1T", [D, S]), scr("x2T", [D, S]), scr("x3T", [D, S])
    G["qaug1"], G["kaug1"] = scr("qaug1", [16, 80, S], BF16), scr("kaug1", [16, 64, S], BF16)
    G["v1"] = scr("v1", [S, 16, 65], BF16)
    for b in ("b_rope", "b_qk0", "b_cmp", "b_neg0", "b_o0", "b_y", "b_qk1", "b_neg1", "b_o1"):
        G[b] = Buf(b)
    bx, bmix, b1, b2, b3, by = Buf(), Buf(), Buf(), Buf(), Buf(), Buf()

    with contextlib.ExitStack() as st:
        kb = KB(nc, st)
        build_consts(kb, G)
        v3r = Ring([kb.sb([128, 6, 65], BF16) for _ in range(2)])
        gtr = Ring([kb.sb([128, 24], F32) for _ in range(2)])
        utr = Ring([kb.sb([128, 512], F32) for _ in range(2)])
        for it in v3r.items:
            memset(kb, "dve", it, it[:], 1.0)

        def rope_dst0(h):
            if h < 8:
                return G["qaug0"][h, 0:64, :], G["b_qk0"]
            g = (h - 8) % 2
            return (G["kcT"], G["ksT"], G["kwT"])[(h - 8) // 2][g], G["b_qk0"]

        def fm_dst0(c):
            if c == 0:
                return G["vcT"], G["b_qk0"]
            return G["uT"][(c - 1) * 128:c * 128, :], G["b_qk0"]

        def tm_cb0(kb, pt, ti):
            import os
            TM = os.environ.get("TM_PARTS", "vgu")
            v, g_, u = v3r.next(), gtr.next(), utr.next()
            rs = slice(ti * 128, (ti + 1) * 128)
            if "v" in TM:
                cp(kb, "act", v, v[:, :, 0:64], pt[0], pt[0][:, 0:384].rearrange("p (b c) -> p b c", b=6))
                kb.dma("sp", G["v3"][rs], v[:], reads=[v], writes=[G["b_qk0"]])
            if "g" in TM:
                act(kb, g_, g_[:], pt[0], pt[0][:, 384:408], AF.Sigmoid)
                kb.dma("sp", G["gates"][rs], g_[:], reads=[g_], writes=[G["b_qk0"]])
            if "u" in TM:
                cp(kb, "dve", u, u[:], pt[1], pt[1][:])
                kb.dma("sp", G["utm"][rs], u[:], reads=[u], writes=[G["b_qk0"]])

        if upto >= 1:
            inproj(kb, G, xT, bx, w0r, w0s, 14, rope_dst0, w0f, 5, fm_dst0, w0t, 920, tm_cb0, [(0, 408), (408, 920)])
        if upto >= 2:
            nsa_compress(kb, G)
            nsa_select(kb, G)
        if upto >= 3:
            nsa_attn(kb, G)
        if upto >= 4:
            s5(kb, G)
        if upto >= 5:
            linear_res_ln(kb, G, G["o0T"], ev_wo, xT, ln["ln_mix_g"][0], ln["ln_mix_b"][0], x1T, G["b_o0"], bx, b1)
            moe(kb, G, x1T, b1, 0, mixT, bmix)
            res_ln(kb, G, x1T, mixT, ln["ln_ffn_g"][0], ln["ln_ffn_b"][0], x2T, b1, bmix, b2)
        v1r = Ring([kb.sb([128, 16, 65], BF16) for _ in range(2)])
        for it in v1r.items:
            memset(kb, "dve", it, it[:], 1.0)

        def rope_dst1(h):
            if h < 16:
                return G["qaug1"][h, 0:64, :], G["b_qk1"]
            return G["kaug1"][h - 16], G["b_qk1"]

        def tm_cb1(kb, pt, ti):
            v = v1r.next()
            cp(kb, "act", v, v[:, 0:8, 0:64], pt[0], pt[0][:].rearrange("p (b c) -> p b c", b=8))
            cp(kb, "dve", v, v[:, 8:16, 0:64], pt[1], pt[1][:].rearrange("p (b c) -> p b c", b=8))
            kb.dma("sp", G["v1"][ti * 128:(ti + 1) * 128], v[:], reads=[v], writes=[G["b_qk1"]])

        if upto >= 6:
            inproj(kb, G, x2T, b2, w1r, w1s, 32, rope_dst1, None, 0, None, w1t, 1024, tm_cb1, [(0, 512), (512, 1024)])
            moba_select(kb, G)
            moba_attn(kb, G)
        if upto >= 7:
            linear_res_ln(kb, G, G["o1T"], od_wo, x2T, ln["ln_mix_g"][1], ln["ln_mix_b"][1], x3T, G["b_o1"], b2, b3)
            moe(kb, G, x3T, b3, 1, mixT, bmix)
            res_ln(kb, G, x3T, mixT, ln["ln_ffn_g"][1], ln["ln_ffn_b"][1], yT, b3, bmix, by)
        kb.barrier()
        print("instructions", kb.n_ins, "waits", kb.n_wait, flush=True)
    return nc


def _swap_cols(w, nh):
    w = w.reshape(w.shape[0], nh, 2, 32)
    return np.ascontiguousarray(w[:, :, ::-1, :].reshape(w.shape[0], nh * 64))


def prep_weights(inp):
    c = np.ascontiguousarray
    W = inp["ev_w_in"][0]
    q, kc, vc, ks, vs, kw, vw, gl, u = np.split(W, [512, 640, 768, 896, 1024, 1152, 1280, 1304], axis=1)
    m = {}
    rope = np.concatenate([q, kc, ks, kw], axis=1)
    m["w0r"] = c(rope)
    m["w0s"] = _swap_cols(rope, 14)
    m["w0f"] = c(np.concatenate([vc, u], axis=1))
    m["w0t"] = c(np.concatenate([vc, vs, vw, gl, u], axis=1))
    W1 = inp["od_w_in"][0]
    m["w1r"] = c(W1[:, 0:2048])
    m["w1s"] = _swap_cols(W1[:, 0:2048], 32)
    m["w1t"] = c(W1[:, 2048:3072])
    m["cmp_k_w1"], m["cmp_k_w2"], m["pe_k"] = c(inp["nsa_cmp_k_w1"][0]), c(inp["nsa_cmp_k_w2"][0]), c(inp["nsa_pe_k"][0])
    m["cmp_v_w1"], m["cmp_v_w2"], m["pe_v"] = c(inp["nsa_cmp_v_w1"][0]), c(inp["nsa_cmp_v_w2"][0]), c(inp["nsa_pe_v"][0])
    m["s5_lre"], m["s5_lim"], m["s5_ls"] = c(inp["s5_lambda_re"][0]), c(inp["s5_lambda_im"][0]), c(inp["s5_log_step"][0])
    m["s5_bre"], m["s5_bim"] = c(inp["s5_b_re"][0]), c(inp["s5_b_im"][0])
    m["s5_cre"], m["s5_cim"] = c(inp["s5_c_re"][0]), c(inp["s5_c_im"][0])
    m["s5_d"], m["s5_glw"], m["s5_glb"] = c(inp["s5_d"][0][None, :]), c(inp["s5_glu_w"][0]), c(inp["s5_glu_b"][0])
    m["ev_wo"], m["od_wo"] = c(inp["ev_w_out"][0]), c(inp["od_w_out"][0])
    for k in ("ln_mix_g", "ln_mix_b", "ln_ffn_g", "ln_ffn_b"):
        m[k] = c(inp[k])
    for L in range(2):
        m["moe_wr%d" % L] = c(np.concatenate([inp["moe_w_coarse"][L]] + [inp["moe_w_fine"][L, g] for g in range(4)], axis=1))
        m["moe_br%d" % L] = c(np.concatenate([inp["moe_b_coarse"][L]] + [inp["moe_b_fine"][L, g] for g in range(4)])[None, :])
        m["moe_wg%d" % L] = c(inp["moe_w_gate"][L].reshape(32, D, 128))
        m["moe_wu%d" % L] = c(inp["moe_w_up"][L].reshape(32, D, 128))
        m["moe_wd%d" % L] = c(inp["moe_w_down"][L].reshape(32, 128, D))
    return {k: np.asarray(v, dtype=np.float32) for k, v in m.items()}


def kernel(**inputs):
    inp = {k: np.asarray(v) for k, v in inputs.items()}
    nc = build_program()
    wm = prep_weights(inp)
    in_maps = []
    for b in range(8):
        m = dict(wm)
        m["xT"] = np.ascontiguousarray(inp["x"][b].T)
        in_maps.append(m)
    res = run_bass_kernel_spmd(nc, in_maps, core_ids=list(range(8)))
    out = np.stack([np.ascontiguousarray(res.results[b]["yT"].T) for b in range(8)], axis=0)
    return out.astype(np.float32)
```

```python
import contextlib
import math
import numpy as np
import concourse.bass as bass
import concourse.mybir as mybir
from concourse.bass_utils import run_bass_kernel_spmd

F32 = mybir.dt.float32
BF16 = mybir.dt.bfloat16
I32 = mybir.dt.int32
ALU = mybir.AluOpType
AF = mybir.ActivationFunctionType
AX = mybir.AxisListType

S = 4096
D = 1024
NT = 8
ALPHA = 4.0 ** 0.25
EPS = 1e-5
BIG = 240000.0
MAGIC = 12582912.0
TWO_PI = 2.0 * math.pi


class Buf:
    __slots__ = ("name", "last_w", "readers")

    def __init__(self, name=""):
        self.name = name
        self.last_w = None
        self.readers = {}


class T:
    __slots__ = ("t", "buf")

    def __init__(self, t, name=""):
        self.t = t
        self.buf = Buf(name)

    def __getitem__(self, idx):
        return self.t[idx]


class KB:
    NDMA = 48

    def __init__(self, nc, stack):
        self.nc = nc
        self.stack = stack
        self.eng = {"pe": nc.tensor, "act": nc.scalar, "dve": nc.vector,
                    "pool": nc.gpsimd, "sp": nc.sync}
        self.sems = {}
        for e in self.eng:
            self.sems[e] = stack.enter_context(nc.semaphore("s_" + e))
        self.cnt = {e: 0 for e in self.eng}
        self.dsem = [stack.enter_context(nc.semaphore("d%d" % i)) for i in range(self.NDMA)]
        self.dcnt = [0] * self.NDMA
        self.dnext = 0
        self.dnext_sw = 0
        self.waited = {}
        self.n_ins = 0
        self.n_wait = 0
        self._uid = 0

    def sb(self, shape, dtype=F32, name=None):
        self._uid += 1
        name = (name or "t") + "_%d" % self._uid
        t = self.stack.enter_context(self.nc.sbuf_tensor(name, list(shape), dtype))
        return T(t, name)

    def ps(self, shape, dtype=F32, name=None):
        self._uid += 1
        name = (name or "p") + "_%d" % self._uid
        t = self.stack.enter_context(self.nc.psum_tensor(name, list(shape), dtype))
        return T(t, name)

    def _sem(self, key):
        return self.sems[key] if isinstance(key, str) else self.dsem[key]

    def _wait(self, e, ev):
        key, val = ev
        if e == "pe" and key == "pe":
            return
        k = (e, key)
        if self.waited.get(k, 0) >= val:
            return
        self.waited[k] = val
        self.eng[e].wait_ge(self._sem(key), val)
        self.n_wait += 1

    @staticmethod
    def _b(b):
        return b.buf if isinstance(b, T) else b

    def _deps(self, e, reads, writes):
        for b in reads:
            b = self._b(b)
            if b.last_w is not None:
                self._wait(e, b.last_w)
        for b in writes:
            b = self._b(b)
            if b.last_w is not None:
                self._wait(e, b.last_w)
            for k, v in b.readers.items():
                self._wait(e, (k, v))

    def _mark(self, ev, reads, writes):
        key, val = ev
        for b in reads:
            b = self._b(b)
            if b.readers.get(key, 0) < val:
                b.readers[key] = val
        for b in writes:
            b = self._b(b)
            b.last_w = ev
            b.readers = {}

    def op(self, e, fn, reads=(), writes=()):
        self._deps(e, reads, writes)
        ins = fn(self.eng[e])
        self.cnt[e] += 1
        ins.then_inc(self.sems[e], 1)
        self._mark((e, self.cnt[e]), reads, writes)
        self.n_ins += 1
        return ins

    def dma(self, q, out, in_, reads=(), writes=(), **kw):
        self._deps(q, reads, writes)
        half = self.NDMA // 2
        if q == "pool":
            i = half + self.dnext_sw
            self.dnext_sw = (self.dnext_sw + 1) % (self.NDMA - half)
        else:
            i = self.dnext
            self.dnext = (self.dnext + 1) % half
        if self.dcnt[i] > 0:
            self._wait(q, (i, self.dcnt[i]))
        ins = self.eng[q].dma_start(out=out, in_=in_, **kw)
        self.dcnt[i] += 16
        ins.then_inc(self.dsem[i], 16)
        self._mark((i, self.dcnt[i]), reads, writes)
        self.n_ins += 1
        return ins

    def barrier(self):
        for e in self.eng:
            for k in self.eng:
                if self.cnt[k] > 0:
                    self._wait(e, (k, self.cnt[k]))
            for i in range(self.NDMA):
                if self.dcnt[i] > 0:
                    self._wait(e, (i, self.dcnt[i]))

    @contextlib.contextmanager
    def phase(self):
        with contextlib.ExitStack() as ps:
            old = self.stack
            self.stack = ps
            yield
            self.barrier()
            self.stack = old


class Ring:
    def __init__(self, items):
        self.items = items
        self.i = 0

    def next(self):
        it = self.items[self.i % len(self.items)]
        self.i += 1
        return it


def mm(kb, ot, oap, lt, lap, rt, rap, start=True, stop=True):
    kb.op("pe", lambda e: e.matmul(oap, lhsT=lap, rhs=rap, start=start, stop=stop,
                                   skip_group_check=True), [lt, rt], [ot])


def tt(kb, e, ot, oap, at, aap, bt, bap, op):
    kb.op(e, lambda g: g.tensor_tensor(out=oap, in0=aap, in1=bap, op=op), [at, bt], [ot])


def ts(kb, e, ot, oap, at, aap, s1, s2, op0, op1=None, extra=()):
    if op1 is None:
        kb.op(e, lambda g: g.tensor_scalar(out=oap, in0=aap, scalar1=s1, scalar2=None, op0=op0),
              [at] + list(extra), [ot])
    else:
        kb.op(e, lambda g: g.tensor_scalar(out=oap, in0=aap, scalar1=s1, scalar2=s2, op0=op0, op1=op1),
              [at] + list(extra), [ot])


def stt(kb, e, ot, oap, at, aap, sc, bt, bap, op0, op1, extra=()):
    kb.op(e, lambda g: g.scalar_tensor_tensor(out=oap, in0=aap, scalar=sc, in1=bap, op0=op0, op1=op1),
          [at, bt] + list(extra), [ot])


def act(kb, ot, oap, it, iap, func, scale=1.0, bias=None, extra=()):
    if bias is None:
        kb.op("act", lambda g: g.activation(out=oap, in_=iap, func=func, scale=scale), [it] + list(extra), [ot])
    else:
        kb.op("act", lambda g: g.activation(out=oap, in_=iap, func=func, scale=scale, bias=bias),
              [it] + list(extra), [ot])


def cp(kb, e, ot, oap, it, iap):
    if e == "act":
        kb.op("act", lambda g: g.activation(out=oap, in_=iap, func=AF.Copy), [it], [ot])
    elif e == "dve":
        kb.op("dve", lambda g: g.tensor_scalar(out=oap, in0=iap, scalar1=1.0, scalar2=None, op0=ALU.mult), [it], [ot])
    else:
        kb.op(e, lambda g: g.tensor_copy(out=oap, in_=iap), [it], [ot])


def memset(kb, e, ot, oap, val):
    kb.op(e, lambda g: g.memset(oap, val), [], [ot])


def fm(ap):
    return ap.rearrange("(c p) t -> p c t", p=128)


def build_consts(kb, G):
    with kb.phase():
        row = kb.sb([1, 64], F32)
        make_ramp(kb, row, row[:, 0:32], 1, 32)
        make_ramp(kb, row, row[:, 32:64], 1, 32)
        one1 = kb.sb([1, 1], F32)
        memset(kb, "dve", one1, one1[:], 1.0)
        pidx = kb.ps([64, 1], F32)
        mm(kb, pidx, pidx[:], row, row[:], one1, one1[:])
        idx = kb.sb([64, 1], F32)
        cp(kb, "act", idx, idx[:], pidx, pidx[:])
        inv = kb.sb([64, 1], F32)
        act(kb, inv, inv[:], idx, idx[:], AF.Exp, scale=-math.log(10000.0) / 32.0)
        ts(kb, "dve", inv, inv[:], inv, inv[:], 1.0 / TWO_PI, None, ALU.mult)
        tpos = kb.sb([64, S], F32)
        make_ramp(kb, tpos, tpos[:], 64, S)
        r = kb.sb([64, S], F32)
        rr = kb.sb([64, S], F32)
        tab = kb.sb([64, S], F32)
        for which in ("sin", "cos"):
            if which == "sin":
                ts(kb, "dve", r, r[:], tpos, tpos[:], inv[:, 0:1], None, ALU.mult, extra=[inv])
            else:
                ts(kb, "dve", r, r[:], tpos, tpos[:], inv[:, 0:1], None, ALU.mult, extra=[inv])
                ts(kb, "dve", r, r[:], r, r[:], 0.25, None, ALU.add)
            ts(kb, "dve", rr, rr[:], r, r[:], MAGIC, MAGIC, ALU.add, ALU.subtract)
            tt(kb, "dve", r, r[:], r, r[:], rr, rr[:], ALU.subtract)
            act(kb, tab, tab[:], r, r[:], AF.Sin, scale=TWO_PI)
            if which == "sin":
                ts(kb, "dve", tab, tab[0:32, :], tab, tab[0:32, :], -1.0, None, ALU.mult)
                kb.dma("sp", G["ropeS"], tab[:], reads=[tab], writes=[G["b_rope"]])
            else:
                kb.dma("sp", G["ropeC"], tab[:], reads=[tab], writes=[G["b_rope"]])


def linear_fm(kb, G, inT, w_dram, outT, n_in_chunks=8, n_out_chunks=8, in_buf=None, out_buf=None):
    with kb.phase():
        w = kb.sb([128, n_in_chunks, n_out_chunks * 128], BF16)
        kb.dma("pool", w[:], w_dram.rearrange("(c p) n -> p c n", p=128), reads=[], writes=[w])
        xin = Ring([kb.sb([128, n_in_chunks, 512], BF16) for _ in range(2)])
        ob = Ring([kb.sb([128, n_out_chunks, 512], F32) for _ in range(2)])
        pss = Ring([kb.ps([128, 512], F32) for _ in range(4)])
        inv = fm(inT)
        outv = fm(outT)
        for t in range(NT):
            x = xin.next()
            kb.dma("sp", x[:], inv[:, :, t * 512:(t + 1) * 512], reads=[in_buf], writes=[x])
            o = ob.next()
            for f in range(n_out_chunks):
                p = pss.next()
                for c in range(n_in_chunks):
                    mm(kb, p, p[:], w, w[:, c, f * 128:(f + 1) * 128], x, x[:, c, :],
                       start=(c == 0), stop=(c == n_in_chunks - 1))
                cp(kb, "act" if f % 2 == 0 else "dve", o, o[:, f, :], p, p[:])
            kb.dma("sp", outv[:, :, t * 512:(t + 1) * 512], o[:], reads=[o], writes=[out_buf])


def res_ln(kb, G, resT, addT, g_dram, b_dram, outT, res_buf, add_buf, out_buf):
    with kb.phase():
        ones = kb.sb([128, 128], F32)
        memset(kb, "dve", ones, ones[:], 1.0 / D)
        gb = kb.sb([128, 2, 8], F32)
        with kb.nc.allow_non_contiguous_dma(reason="tiny ln params"):
            kb.dma("sp", gb[:, 0, :], g_dram.rearrange("(c p) -> p c", p=128), writes=[gb])
            kb.dma("sp", gb[:, 1, :], b_dram.rearrange("(c p) -> p c", p=128), writes=[gb])
        rin = Ring([kb.sb([128, 8, 512], F32) for _ in range(2)])
        ain = Ring([kb.sb([128, 8, 512], F32) for _ in range(2)])
        zsq = Ring([kb.sb([128, 512], F32) for _ in range(2)])
        oo = Ring([kb.sb([128, 8, 512], F32) for _ in range(2)])
        ps1 = Ring([kb.ps([128, 512], F32) for _ in range(2)])
        ps2 = Ring([kb.ps([128, 512], F32) for _ in range(2)])
        mean = kb.sb([128, 512], F32)
        rstd = kb.sb([128, 512], F32)
        tmp = kb.sb([128, 512], F32)
        rv, av, ov = fm(resT), fm(addT), fm(outT)
        for t in range(NT):
            sl = slice(t * 512, (t + 1) * 512)
            r = rin.next()
            a = ain.next()
            kb.dma("sp", r[:], rv[:, :, sl], reads=[res_buf], writes=[r])
            kb.dma("act", a[:], av[:, :, sl], reads=[add_buf], writes=[a])
            p1, p2 = ps1.next(), ps2.next()
            for c in range(8):
                stt(kb, "dve", r, r[:, c, :], r, r[:, c, :], ALPHA, a, a[:, c, :], ALU.mult, ALU.add)
                z2 = zsq.next()
                act(kb, z2, z2[:], r, r[:, c, :], AF.Square)
                mm(kb, p1, p1[:], ones, ones[:], r, r[:, c, :], start=(c == 0), stop=(c == 7))
                mm(kb, p2, p2[:], ones, ones[:], z2, z2[:], start=(c == 0), stop=(c == 7))
            cp(kb, "act", mean, mean[:], p1, p1[:])
            tt(kb, "dve", tmp, tmp[:], mean, mean[:], mean, mean[:], ALU.mult)
            tt(kb, "dve", tmp, tmp[:], p2, p2[:], tmp, tmp[:], ALU.subtract)
            ts(kb, "dve", tmp, tmp[:], tmp, tmp[:], EPS, None, ALU.add)
            act(kb, tmp, tmp[:], tmp, tmp[:], AF.Sqrt)
            kb.op("dve", lambda g: g.reciprocal(out=rstd[:], in_=tmp[:]), [tmp], [rstd])
            o = oo.next()
            for c in range(8):
                e = "dve" if c % 2 == 0 else "pool"
                tt(kb, e, r, r[:, c, :], r, r[:, c, :], mean, mean[:], ALU.subtract)
                tt(kb, e, r, r[:, c, :], r, r[:, c, :], rstd, rstd[:], ALU.mult)
                ts(kb, e, o, o[:, c, :], r, r[:, c, :], gb[:, 0, c:c + 1], gb[:, 1, c:c + 1], ALU.mult, ALU.add,
                   extra=[gb])
            kb.dma("sp", ov[:, :, sl], o[:], reads=[o], writes=[out_buf])


def linear_res_ln(kb, G, inT, w_dram, resT, g_dram, b_dram, outT, in_buf, res_buf, out_buf):
    with kb.phase():
        w = kb.sb([128, 8, 1024], BF16)
        kb.dma("pool", w[:], w_dram.rearrange("(c p) n -> p c n", p=128), reads=[], writes=[w])
        ones = kb.sb([128, 128], F32)
        memset(kb, "dve", ones, ones[:], 1.0 / D)
        gb = kb.sb([128, 2, 8], F32)
        with kb.nc.allow_non_contiguous_dma(reason="tiny ln params"):
            kb.dma("sp", gb[:, 0, :], g_dram.rearrange("(c p) -> p c", p=128), writes=[gb])
            kb.dma("sp", gb[:, 1, :], b_dram.rearrange("(c p) -> p c", p=128), writes=[gb])
        xin = Ring([kb.sb([128, 8, 512], BF16) for _ in range(2)])
        rin = Ring([kb.sb([128, 8, 512], F32) for _ in range(2)])
        zsq = Ring([kb.sb([128, 512], F32) for _ in range(2)])
        oo = Ring([kb.sb([128, 8, 512], F32) for _ in range(2)])
        pss = Ring([kb.ps([128, 512], F32) for _ in range(4)])
        ps1 = Ring([kb.ps([128, 512], F32) for _ in range(2)])
        ps2 = Ring([kb.ps([128, 512], F32) for _ in range(2)])
        mean = kb.sb([128, 512], F32)
        rstd = kb.sb([128, 512], F32)
        tmp = kb.sb([128, 512], F32)
        inv, rv, ov = fm(inT), fm(resT), fm(outT)
        for t in range(NT):
            sl = slice(t * 512, (t + 1) * 512)
            x = xin.next()
            kb.dma("sp", x[:], inv[:, :, sl], reads=[in_buf], writes=[x])
            r = rin.next()
            kb.dma("act", r[:], rv[:, :, sl], reads=[res_buf], writes=[r])
            p1, p2 = ps1.next(), ps2.next()
            pend = None
            for f in range(8):
                p = pss.next()
                for c in range(8):
                    mm(kb, p, p[:], w, w[:, c, f * 128:(f + 1) * 128], x, x[:, c, :], start=(c == 0), stop=(c == 7))
                if pend is not None:
                    pend()
                stt(kb, "dve", r, r[:, f, :], r, r[:, f, :], ALPHA, p, p[:], ALU.mult, ALU.add)
                z2 = zsq.next()
                act(kb, z2, z2[:], r, r[:, f, :], AF.Square)

                def pend(f=f, z2=z2, r=r, p1=p1, p2=p2):
                    mm(kb, p1, p1[:], ones, ones[:], r, r[:, f, :], start=(f == 0), stop=(f == 7))
                    mm(kb, p2, p2[:], ones, ones[:], z2, z2[:], start=(f == 0), stop=(f == 7))
            pend()
            cp(kb, "act", mean, mean[:], p1, p1[:])
            tt(kb, "dve", tmp, tmp[:], mean, mean[:], mean, mean[:], ALU.mult)
            tt(kb, "dve", tmp, tmp[:], p2, p2[:], tmp, tmp[:], ALU.subtract)
            ts(kb, "dve", tmp, tmp[:], tmp, tmp[:], EPS, None, ALU.add)
            act(kb, tmp, tmp[:], tmp, tmp[:], AF.Sqrt)
            kb.op("dve", lambda g: g.reciprocal(out=rstd[:], in_=tmp[:]), [tmp], [rstd])
            o = oo.next()
            for c in range(8):
                e = "dve" if c % 2 == 0 else "pool"
                tt(kb, e, r, r[:, c, :], r, r[:, c, :], mean, mean[:], ALU.subtract)
                tt(kb, e, r, r[:, c, :], r, r[:, c, :], rstd, rstd[:], ALU.mult)
                ts(kb, e, o, o[:, c, :], r, r[:, c, :], gb[:, 0, c:c + 1], gb[:, 1, c:c + 1], ALU.mult, ALU.add,
                   extra=[gb])
            kb.dma("sp", ov[:, :, sl], o[:], reads=[o], writes=[out_buf])


def inproj(kb, G, xT, x_buf, w_rope, w_swap, n_rope, rope_dst, w_fm, n_fm_chunks, fm_dst, w_tm, tm_cols, tm_cb, tm_splits):
    with kb.phase():
        wr = kb.sb([128, 8, n_rope * 64], BF16)
        ws = kb.sb([128, 8, n_rope * 64], BF16)
        kb.dma("pool", wr[:], w_rope.rearrange("(c p) n -> p c n", p=128), writes=[wr])
        wrv = wr[:].rearrange("p c (h t k) -> p c h t k", t=2, k=32)
        wsv = ws[:].rearrange("p c (h t k) -> p c h t k", t=2, k=32)
        for c in range(8):
            cp(kb, "act", ws, wsv[:, c, :, 0, :], wr, wrv[:, c, :, 1, :])
            cp(kb, "dve", ws, wsv[:, c, :, 1, :], wr, wrv[:, c, :, 0, :])
        if n_fm_chunks:
            wf = kb.sb([128, 8, n_fm_chunks * 128], BF16)
            kb.dma("pool", wf[:], w_fm.rearrange("(c p) n -> p c n", p=128), writes=[wf])
        wt = kb.sb([128, 8, tm_cols], BF16)
        kb.dma("pool", wt[:], w_tm.rearrange("(c p) n -> p c n", p=128), writes=[wt])
        xin = Ring([kb.sb([128, 8, 512], BF16) for _ in range(2)])
        assert n_rope % 2 == 0
        cc = Ring([kb.sb([128, 512], F32) for _ in range(2)])
        ss = Ring([kb.sb([128, 512], F32) for _ in range(2)])
        pa = Ring([kb.ps([128, 512], F32) for _ in range(2)])
        pb = Ring([kb.ps([128, 512], F32) for _ in range(2)])
        pf = Ring([kb.ps([128, 512], F32) for _ in range(2)])
        ntm = len(tm_splits)
        pt = [kb.ps([128, 512], F32) for _ in range(ntm)]
        t1 = Ring([kb.sb([128, 512], F32) for _ in range(2)])
        t2 = Ring([kb.sb([128, 512], F32) for _ in range(2)])
        ro = Ring([kb.sb([128, 512], BF16) for _ in range(3)])
        fo = Ring([kb.sb([128, 512], BF16) for _ in range(2)])
        xv = fm(xT)
        import os
        SK = os.environ.get("INPROJ_SKIP", "")
        for t in range(NT):
            sl = slice(t * 512, (t + 1) * 512)
            x = xin.next()
            kb.dma("pool", x[:], xv[:, :, sl], reads=[x_buf], writes=[x])
            c_, s_ = cc.next(), ss.next()
            for k in range(2):
                kb.dma("sp", c_[64 * k:64 * k + 64], G["ropeC"][:, sl], reads=[G["b_rope"]], writes=[c_])
                kb.dma("sp", s_[64 * k:64 * k + 64], G["ropeS"][:, sl], reads=[G["b_rope"]], writes=[s_])
            for hp in range(0 if "r" in SK else n_rope // 2):
                a, b = pa.next(), pb.next()
                for c in range(8):
                    mm(kb, a, a[:], wr, wr[:, c, hp * 128:(hp + 1) * 128], x, x[:, c, :], start=(c == 0), stop=(c == 7))
                for c in range(8):
                    mm(kb, b, b[:], ws, ws[:, c, hp * 128:(hp + 1) * 128], x, x[:, c, :], start=(c == 0), stop=(c == 7))
                u1, u2, o = t1.next(), t2.next(), ro.next()
                tt(kb, "dve", u1, u1[:], a, a[:], c_, c_[:], ALU.mult)
                tt(kb, "dve", u2, u2[:], b, b[:], s_, s_[:], ALU.mult)
                tt(kb, "dve" if "p" in SK else "pool", o, o[:], u1, u1[:], u2, u2[:], ALU.add)
                for k in range(2):
                    dst, dbuf = rope_dst(2 * hp + k)
                    kb.dma("sp", dst[:, sl], o[64 * k:64 * k + 64, :], reads=[o], writes=[dbuf])
            for f in range(0 if "f" in SK else n_fm_chunks):
                p = pf.next()
                for c in range(8):
                    mm(kb, p, p[:], wf, wf[:, c, f * 128:(f + 1) * 128], x, x[:, c, :], start=(c == 0), stop=(c == 7))
                o = fo.next()
                cp(kb, "act", o, o[:], p, p[:])
                dst, dbuf = fm_dst(f)
                kb.dma("sp", dst[:, sl], o[:], reads=[o], writes=[dbuf])
            for sub in range(0 if "t" in SK else 4):
                for j in range(ntm):
                    n0, n1 = tm_splits[j]
                    for c in range(8):
                        mm(kb, pt[j], pt[j][:, 0:n1 - n0], x, x[:, c, sub * 128:(sub + 1) * 128], wt, wt[:, c, n0:n1],
                           start=(c == 0), stop=(c == 7))
                tm_cb(kb, pt, t * 4 + sub)


def moe(kb, G, xT, x_buf, L, outT, out_buf):
    wr_d, br_d = G["moe_wr"][L], G["moe_br"][L]
    wg_d, wu_d, wd_d = G["moe_wg"][L], G["moe_wu"][L], G["moe_wd"][L]
    xv = fm(xT)
    with kb.phase():
        gT = kb.sb([32, S], BF16)
        with contextlib.ExitStack() as rs:
            old = kb.stack
            kb.stack = rs
            wr = kb.sb([128, 8, 36], F32)
            kb.dma("sp", wr[:], wr_d.rearrange("(c p) n -> p c n", p=128), writes=[wr])
            br = kb.sb([1, 36], F32)
            kb.dma("sp", br[:], br_d, writes=[br])
            ones1 = kb.sb([1, 128], F32)
            memset(kb, "dve", ones1, ones1[:], 1.0)
            ident = kb.sb([128, 128], F32)
            memset(kb, "dve", ident, ident[:], 1.0)
            kb.op("pool", lambda g: g.affine_select(out=ident[:], in_=ident[:], pattern=[[-1, 128]],
                                                    compare_op=ALU.is_equal, fill=0.0, base=0, channel_multiplier=1),
                  [ident], [ident])
            xin = Ring([kb.sb([128, 8, 512], F32) for _ in range(2)])
            pl = Ring([kb.ps([128, 36], F32) for _ in range(2)])
            ptr = Ring([kb.ps([32, 128], F32) for _ in range(2)])
            sm = Ring([kb.sb([128, 160], F32) for _ in range(2)])
            gt = Ring([kb.sb([128, 32], F32) for _ in range(2)])
            for t in range(NT):
                x = xin.next()
                kb.dma("sp", x[:], xv[:, :, t * 512:(t + 1) * 512], reads=[x_buf], writes=[x])
                for sub in range(4):
                    p = pl.next()
                    for c in range(8):
                        mm(kb, p, p[:], x, x[:, c, sub * 128:(sub + 1) * 128], wr, wr[:, c, :], start=(c == 0), stop=False)
                    mm(kb, p, p[:], ones1, ones1[:], br, br[:], start=False, stop=True)
                    w = sm.next()
                    cp(kb, "act", w, w[:, 0:36], p, p[:])
                    kb.op("dve", lambda g: g.tensor_reduce(out=w[:, 36:37], in_=w[:, 0:4], axis=AX.X, op=ALU.max), [w], [w])
                    ts(kb, "dve", w, w[:, 37:38], w, w[:, 36:37], -1.0, None, ALU.mult)
                    act(kb, w, w[:, 38:42], w, w[:, 0:4], AF.Exp, bias=w[:, 37:38])
                    kb.op("dve", lambda g: g.tensor_reduce(out=w[:, 42:43], in_=w[:, 38:42], axis=AX.X, op=ALU.add), [w], [w])
                    kb.op("dve", lambda g: g.reciprocal(out=w[:, 43:44], in_=w[:, 42:43]), [w], [w])
                    ts(kb, "dve", w, w[:, 44:48], w, w[:, 0:4], w[:, 36:37], None, ALU.is_equal)
                    tt(kb, "dve", w, w[:, 48:80].rearrange("p (g e) -> p g e", g=4),
                       w, w[:, 4:36].rearrange("p (g e) -> p g e", g=4),
                       w, w[:, 44:48].unsqueeze(2).to_broadcast([128, 4, 8]), ALU.mult)
                    kb.op("dve", lambda g: g.tensor_reduce(out=w[:, 80:88], in_=w[:, 48:80].rearrange("p (g e) -> p e g", g=4),
                                                           axis=AX.X, op=ALU.add), [w], [w])
                    kb.op("dve", lambda g: g.max(out=w[:, 88:96], in_=w[:, 80:88]), [w], [w])
                    tt(kb, "dve", w, w[:, 96:97], w, w[:, 88:89], w, w[:, 89:90], ALU.subtract)
                    act(kb, w, w[:, 97:98], w, w[:, 96:97], AF.Sigmoid)
                    tt(kb, "dve", w, w[:, 98:99], w, w[:, 97:98], w, w[:, 43:44], ALU.mult)
                    tt(kb, "dve", w, w[:, 99:100], w, w[:, 43:44], w, w[:, 98:99], ALU.subtract)
                    ts(kb, "dve", w, w[:, 100:108], w, w[:, 80:88], w[:, 88:89], w[:, 98:99], ALU.is_equal, ALU.mult)
                    ts(kb, "dve", w, w[:, 108:116], w, w[:, 80:88], w[:, 89:90], w[:, 99:100], ALU.is_equal, ALU.mult)
                    tt(kb, "dve", w, w[:, 116:124], w, w[:, 100:108], w, w[:, 108:116], ALU.add)
                    gg = gt.next()
                    tt(kb, "dve", gg, gg[:].rearrange("p (g e) -> p g e", g=4),
                       w, w[:, 44:48].unsqueeze(2).to_broadcast([128, 4, 8]),
                       w, w[:, 116:124].unsqueeze(1).to_broadcast([128, 4, 8]), ALU.mult)
                    pT = ptr.next()
                    kb.op("pe", lambda e: e.transpose(pT[:], gg[:], ident[:]), [gg, ident], [pT])
                    q0 = (t * 4 + sub) * 128
                    cp(kb, "act", gT, gT[:, q0:q0 + 128], pT, pT[:])
            kb.barrier()
            kb.stack = old
        sel = kb.sb([32, 32, 128], BF16)
        memset(kb, "dve", sel, sel[:], 1.0)
        kb.op("pool", lambda g: g.affine_select(out=sel[:], in_=sel[:], pattern=[[-1, 32], [0, 128]],
                                                compare_op=ALU.is_equal, fill=0.0, base=0, channel_multiplier=1),
              [sel], [sel])
        TS = 2048
        xb = kb.sb([128, 8, TS], BF16)
        yacc = kb.sb([128, 8, TS], F32)
        hq = kb.sb([128, 4, TS], BF16)
        wgr = Ring([kb.sb([128, 8, 128], BF16) for _ in range(2)])
        wur = Ring([kb.sb([128, 8, 128], BF16) for _ in range(2)])
        wdr = Ring([kb.sb([128, 1024], BF16) for _ in range(8)])
        pg = Ring([kb.ps([128, 512], F32) for _ in range(2)])
        pu = Ring([kb.ps([128, 512], F32) for _ in range(2)])
        pc = Ring([kb.ps([128, 512], F32) for _ in range(2)])
        py = Ring([kb.ps([128, 512], F32) for _ in range(2)])
        sl_ = Ring([kb.sb([128, 512], F32) for _ in range(2)])
        t1_ = Ring([kb.sb([128, 512], F32) for _ in range(2)])
        ov = fm(outT)
        for st in range(S // TS):
            kb.dma("pool", xb[:], xv[:, :, st * TS:(st + 1) * TS], reads=[x_buf], writes=[xb])
            for q in range(8):
                wds = []
                for ei in range(4):
                    e = q * 4 + ei
                    wg, wu, wd = wgr.next(), wur.next(), wdr.next()
                    kb.dma("pool", wg[:], wg_d[e].rearrange("(c p) n -> p c n", p=128), writes=[wg])
                    kb.dma("pool", wu[:], wu_d[e].rearrange("(c p) n -> p c n", p=128), writes=[wu])
                    kb.dma("pool", wd[:], wd_d[e], writes=[wd])
                    wds.append(wd)
                    for tq in range(TS // 512):
                        tsl = slice(tq * 512, (tq + 1) * 512)
                        a, b, c_ = pg.next(), pu.next(), pc.next()
                        for c in range(8):
                            mm(kb, a, a[:], wg, wg[:, c, :], xb, xb[:, c, tsl], start=(c == 0), stop=(c == 7))
                        for c in range(8):
                            mm(kb, b, b[:], wu, wu[:, c, :], xb, xb[:, c, tsl], start=(c == 0), stop=(c == 7))
                        g0 = st * TS + tq * 512
                        mm(kb, c_, c_[:], sel, sel[:, e, :], gT, gT[:, g0:g0 + 512])
                        s1, u1 = sl_.next(), t1_.next()
                        act(kb, s1, s1[:], a, a[:], AF.Silu)
                        tt(kb, "dve", u1, u1[:], c_, c_[:], s1, s1[:], ALU.mult)
                        tt(kb, "dve", hq, hq[:, ei, tsl], b, b[:], u1, u1[:], ALU.mult)
                for tq in range(TS // 512):
                    tsl = slice(tq * 512, (tq + 1) * 512)
                    for f in range(8):
                        y = py.next()
                        for ei in range(4):
                            mm(kb, y, y[:], wds[ei], wds[ei][:, f * 128:(f + 1) * 128], hq, hq[:, ei, tsl],
                               start=(ei == 0), stop=(ei == 3))
                        if q == 0:
                            cp(kb, "act", yacc, yacc[:, f, tsl], y, y[:])
                        else:
                            tt(kb, "pool" if False else "dve", yacc, yacc[:, f, tsl], y, y[:], yacc, yacc[:, f, tsl], ALU.add)
            kb.dma("sp", ov[:, :, st * TS:(st + 1) * TS], yacc[:], reads=[yacc], writes=[out_buf])


def sched_causal(Q):
    out = []
    for kt in range(4 * Q + 4):
        r = kt - 4 * Q
        out.append((kt, max(0, r), 4, [(r, "tri")] if r >= 0 else [], None))
    return out


def sched_win(Q):
    out = []
    for kt in range(max(0, 4 * Q - 2), 4 * Q + 4):
        r = kt - 4 * Q
        s0, s1 = max(0, r), min(3, r + 2) + 1
        m = []
        if r >= 0:
            m.append((r, "tri"))
        if 0 <= r + 2 <= 3:
            m.append((r + 2, "ntri"))
        out.append((kt, s0, s1, m, None))
    return out


def sched_cmp(Q):
    out = [(0, 0, 4, [], "cmp")]
    if Q >= 4:
        out.append((1, 0, 4, [], "cmp"))
    return out


class AttnCtx:
    def __init__(self, kb, need_cmp):
        self.kb = kb
        self.tri = kb.sb([128, 128], BF16)
        self.ntri = kb.sb([128, 128], BF16)
        memset(kb, "dve", self.tri, self.tri[:], 1.0)
        memset(kb, "dve", self.ntri, self.ntri[:], 1.0)
        kb.op("pool", lambda g: g.affine_select(out=self.tri[:], in_=self.tri[:], pattern=[[1, 128]],
                                                compare_op=ALU.is_ge, fill=0.0, base=0, channel_multiplier=-1),
              [self.tri], [self.tri])
        kb.op("pool", lambda g: g.affine_select(out=self.ntri[:], in_=self.ntri[:], pattern=[[-1, 128]],
                                                compare_op=ALU.is_gt, fill=0.0, base=0, channel_multiplier=1),
              [self.ntri], [self.ntri])
        self.cmpm = None
        if need_cmp:
            self.cmpm = kb.sb([128, 2, S], BF16)
            memset(kb, "dve", self.cmpm, self.cmpm[:], 1.0)
            for j in range(2):
                kb.op("pool", lambda g, j=j: g.affine_select(out=self.cmpm[:, j, :], in_=self.cmpm[:, j, :], pattern=[[1, S]],
                                                             compare_op=ALU.is_ge, fill=0.0, base=-31 - 2048 * j,
                                                             channel_multiplier=-16), [self.cmpm], [self.cmpm])
        self.zl = kb.sb([128, 128], BF16)
        self.zr = kb.sb([128, 512], BF16)
        memset(kb, "dve", self.zl, self.zl[:], 0.0)
        memset(kb, "dve", self.zr, self.zr[:], 0.0)
        self.psS = Ring([kb.ps([128, 512], F32) for _ in range(3)])
        self.psO = Ring([kb.ps([128, 4, 128], F32) for _ in range(3)])
        self.pT = Ring([kb.sb([128, 512], BF16) for _ in range(3)])
        self.den = Ring([kb.sb([128, 4], F32) for _ in range(3)])
        self.coef = Ring([kb.sb([128, 4], F32) for _ in range(3)])
        self.tmp = Ring([kb.sb([128, 4, 64], F32) for _ in range(2)])
        self.mask_eng = Ring(["pool", "dve"])
        self.pend = []
        self.LA = 2

    def push(self, fn):
        self.pend.append(fn)
        while len(self.pend) > self.LA:
            self.pend.pop(0)()

    def flush_all(self):
        while self.pend:
            self.pend.pop(0)()


def attn_branch(A, Kc, q_t, k_t, v_t, nkeys_last, sched, Q, acc_t, first, gate_t=None, gate_ap=None, after=None):
    kb = A.kb
    po = A.psO.next()
    mm(kb, po, po[:].rearrange("p a b -> p (a b)"), A.zl, A.zl[:], A.zr, A.zr[:], start=True, stop=True)

    def fin():
        den, coef = A.den.next(), A.coef.next()
        ts(kb, "dve", den, den[:], po, po[:, :, 64], 1e-30, None, ALU.max)
        kb.op("dve", lambda g: g.reciprocal(out=coef[:], in_=den[:]), [den], [coef])
        if gate_t is not None:
            tt(kb, "dve", coef, coef[:], coef, coef[:], gate_t, gate_ap, ALU.mult)
        cb = coef[:].unsqueeze(2).to_broadcast([128, 4, 64])
        if first:
            tt(kb, "dve", acc_t, acc_t[:], po, po[:, :, 0:64], coef, cb, ALU.mult)
        else:
            tmp = A.tmp.next()
            tt(kb, "dve", tmp, tmp[:], po, po[:, :, 0:64], coef, cb, ALU.mult)
            tt(kb, "pool", acc_t, acc_t[:], acc_t, acc_t[:], tmp, tmp[:], ALU.add)
        if after is not None:
            after()

    items = sched(Q)
    for idx, (kt, s0, s1, masks, full) in enumerate(items):
        ksz = 128
        if nkeys_last is not None and (kt + 1) * 128 > nkeys_last:
            ksz = nkeys_last - kt * 128
        c0, c1 = s0 * 128, s1 * 128
        ps = A.psS.next()
        mm(kb, ps, ps[0:ksz, c0:c1], k_t, k_t[0:Kc, kt * 128:kt * 128 + ksz], q_t, q_t[0:Kc, Q * 512 + c0:Q * 512 + c1])
        p = A.pT.next()
        act(kb, p, p[0:ksz, c0:c1], ps, ps[0:ksz, c0:c1], AF.Exp, scale=0.125)
        for (s, name) in masks:
            m = A.tri if name == "tri" else A.ntri
            tt(kb, A.mask_eng.next(), p, p[0:ksz, s * 128:(s + 1) * 128], p, p[0:ksz, s * 128:(s + 1) * 128], m, m[0:ksz, :], ALU.mult)
        if full == "cmp":
            tt(kb, A.mask_eng.next(), p, p[0:ksz, c0:c1], p, p[0:ksz, c0:c1], A.cmpm, A.cmpm[0:ksz, kt, Q * 512 + c0:Q * 512 + c1], ALU.mult)
        last = idx == len(items) - 1

        def pv(p=p, ksz=ksz, kt=kt, s0=s0, s1=s1, last=last):
            for s in range(s0, s1):
                mm(kb, po, po[:, s, 0:65], p, p[0:ksz, s * 128:(s + 1) * 128], v_t, v_t[0:ksz, kt, :], start=False, stop=False)
            if last:
                fin()

        A.push(pv)


def make_ramp(kb, t, ap, parts, n, start=0.0):
    ones = kb.sb([parts, n], F32)
    memset(kb, "dve", ones, ones[:], 1.0)
    kb.op("dve", lambda g: g.tensor_tensor_scan(out=ap, data0=ones[:], data1=ones[:], initial=float(start) - 1.0,
                                                op0=ALU.mult, op1=ALU.add), [ones], [t])


def make_ident(kb, dtype=F32):
    ident = kb.sb([128, 128], dtype)
    memset(kb, "dve", ident, ident[:], 1.0)
    kb.op("pool", lambda g: g.affine_select(out=ident[:], in_=ident[:], pattern=[[-1, 128]],
                                            compare_op=ALU.is_equal, fill=0.0, base=0, channel_multiplier=1),
          [ident], [ident])
    return ident


def gelu_tanh(kb, z, zap, out_t, out_ap, shape, tmp1, tmp2):
    a1 = tmp1[tuple(slice(0, s) for s in shape)]
    a2 = tmp2[tuple(slice(0, s) for s in shape)]
    act(kb, tmp1, a1, z, zap, AF.Square)
    ts(kb, "dve", tmp1, a1, tmp1, a1, 0.044715, 1.0, ALU.mult, ALU.add)
    tt(kb, "dve", tmp1, a1, tmp1, a1, z, zap, ALU.mult)
    act(kb, tmp2, a2, tmp1, a1, AF.Sigmoid, scale=1.5957691216057308)
    tt(kb, "dve", out_t, out_ap, z, zap, tmp2, a2, ALU.mult)


def nsa_compress(kb, G):
    with kb.phase():
        ident = make_ident(kb)
        pps = kb.ps([64, 32], F32)
        pb = kb.ps([128, 1], F32)
        ph = kb.ps([128, 256], F32)
        pk = kb.ps([64, 256], F32)
        pv = kb.ps([128, 64], F32)
        for which in ("k", "v"):
            w1_d, w2_d, pe_d = G["cmp_%s_w1" % which], G["cmp_%s_w2" % which], G["pe_%s" % which]
            w1 = kb.sb([128, 32, 128], BF16)
            w1v = w1_d.rearrange("(l d) f -> d l f", d=64)
            kb.dma("pool", w1[0:64], w1v, writes=[w1])
            kb.dma("pool", w1[64:128], w1v, writes=[w1])
            w2 = kb.sb([128, 64], BF16)
            kb.dma("pool", w2[:], w2_d, writes=[w2])
            pe = kb.sb([32, 64], F32)
            kb.dma("sp", pe[:], pe_d, writes=[pe])
            kb.op("pe", lambda e: e.transpose(pps[:], pe[:], ident[0:32, 0:32]), [pe, ident], [pps])
            peT = kb.sb([64, 32], BF16)
            cp(kb, "act", peT, peT[:], pps, pps[:])
            for l in range(32):
                mm(kb, pb, pb[:], w1, w1[0:64, l, :], peT, peT[:, l:l + 1], start=(l == 0), stop=(l == 31))
            bias = kb.sb([128, 1], F32)
            cp(kb, "act", bias, bias[:], pb, pb[:])
            src = kb.sb([128, S], BF16)
            if which == "k":
                kb.dma("sp", src[0:64], G["kcT"][0], reads=[G["b_qk0"]], writes=[src])
                kb.dma("sp", src[64:128], G["kcT"][1], reads=[G["b_qk0"]], writes=[src])
            else:
                kb.dma("sp", src[:], G["vcT"], reads=[G["b_qk0"]], writes=[src])
            for g in range(2):
                for l in range(32):
                    mm(kb, ph, ph[:, 0:255], w1, w1[g * 64:(g + 1) * 64, l, :], src, src[g * 64:(g + 1) * 64, l:l + 4065:16],
                       start=(l == 0), stop=(l == 31))
                z = kb.sb([128, 255], F32)
                act(kb, z, z[:], ph, ph[:, 0:255], AF.Identity, bias=bias[:, 0:1], extra=[bias])
                hid = kb.sb([128, 256], BF16)
                t1, t2 = kb.sb([128, 255], F32), kb.sb([128, 255], F32)
                gelu_tanh(kb, z, z[:], hid, hid[:, 0:255], (128, 255), t1, t2)
                if which == "k":
                    mm(kb, pk, pk[:, 0:255], w2, w2[:], hid, hid[:, 0:255])
                    kc = kb.sb([64, 256], BF16)
                    memset(kb, "dve", kc, kc[:], 0.0)
                    cp(kb, "act", kc, kc[:, 0:255], pk, pk[:, 0:255])
                    kb.dma("sp", G["kcmpT"][g], kc[:], reads=[kc], writes=[G["b_cmp"]])
                else:
                    vcs = kb.sb([128, 2, 65], BF16)
                    memset(kb, "dve", vcs, vcs[:], 0.0)
                    memset(kb, "dve", vcs, vcs[:, :, 64:65], 1.0)
                    for j in range(2):
                        nsz = 128 if j == 0 else 127
                        mm(kb, pv, pv[0:nsz, :], hid, hid[:, j * 128:j * 128 + nsz], w2, w2[:])
                        cp(kb, "act", vcs, vcs[0:nsz, j, 0:64], pv, pv[0:nsz, :])
                    kb.dma("sp", G["vcmp"][g], vcs[:], reads=[vcs], writes=[G["b_cmp"]])


def nsa_select(kb, G):
    with kb.phase():
        identb = make_ident(kb, BF16)
        val = kb.sb([128, 128], F32)
        make_ramp(kb, val, val[:], 128, 128, start=-64.0)
        ts(kb, "dve", val, val[64:128, :], val, val[64:128, :], -1.0, None, ALU.add)
        KM, AM, fut = kb.sb([128, 128], F32), kb.sb([128, 128], F32), kb.sb([128, 128], F32)
        ts(kb, "dve", KM, KM[:], val, val[:], -2.0, None, ALU.is_le)
        ts(kb, "dve", fut, fut[:], val, val[:], 1.0, None, ALU.is_ge)
        ts(kb, "dve", AM, AM[:], KM, KM[:], -1000.0, 1000.0, ALU.mult, ALU.add)
        stt(kb, "dve", AM, AM[:], fut, fut[:], -1001.0, AM, AM[:], ALU.mult, ALU.add)
        Mc = kb.sb([128, 8], F32)
        memset(kb, "dve", Mc, Mc[:], 1.0)
        kb.op("pool", lambda g: g.affine_select(out=Mc[:], in_=Mc[:], pattern=[[-16, 8]], compare_op=ALU.is_ge,
                                                fill=0.0, base=-15, channel_multiplier=1), [Mc], [Mc])
        q4 = kb.sb([64, 4, S], BF16)
        kcm = kb.sb([64, 256], BF16)
        negT = kb.sb([64, S], BF16)
        psc = Ring([kb.ps([128, 4, 256], F32) for _ in range(2)])
        ptr = Ring([kb.ps([64, 128], BF16) for _ in range(2)])
        pcs = Ring([kb.sb([128, 4, 256], F32) for _ in range(2)])
        Psum = Ring([kb.sb([128, 256], F32) for _ in range(2)])
        sm = Ring([kb.sb([128, 160], F32) for _ in range(2)])
        sc = Ring([kb.sb([128, 64], F32) for _ in range(2)])
        ngb = Ring([kb.sb([128, 64], BF16) for _ in range(2)])
        for g in range(2):
            for z in range(4):
                kb.dma("sp", q4[:, z, :], G["qaug0"][g * 4 + z, 0:64, :], reads=[G["b_qk0"]], writes=[q4])
            kb.dma("sp", kcm[:], G["kcmpT"][g], reads=[G["b_cmp"]], writes=[kcm])
            for it in pcs.items + Psum.items:
                memset(kb, "pool", it, it[:], 0.0)
            for c in range(32):
                ncv = min(255, 8 * c + 7)
                p = psc.next()
                for z in range(4):
                    mm(kb, p, p[:, z, 0:ncv], q4, q4[:, z, c * 128:(c + 1) * 128], kcm, kcm[:, 0:ncv])
                pc = pcs.next()
                act(kb, pc, pc[:, :, 0:ncv], p, p[:, :, 0:ncv], AF.Exp, scale=0.125)
                if c == 0:
                    tt(kb, "dve", pc, pc[:, :, 0:7], pc, pc[:, :, 0:7], Mc, Mc[:, 1:8].unsqueeze(1).to_broadcast([128, 4, 7]), ALU.mult)
                else:
                    tt(kb, "dve", pc, pc[:, :, ncv - 8:ncv], pc, pc[:, :, ncv - 8:ncv], Mc,
                       Mc[:, 0:8].unsqueeze(1).to_broadcast([128, 4, 8]), ALU.mult)
                w = sm.next()
                kb.op("dve", lambda e: e.tensor_reduce(out=w[:, 0:4], in_=pc[:, :, 0:ncv], axis=AX.X, op=ALU.add), [pc], [w])
                ts(kb, "dve", w, w[:, 4:8], w, w[:, 0:4], 1e-30, None, ALU.max)
                kb.op("dve", lambda e: e.reciprocal(out=w[:, 8:12], in_=w[:, 4:8]), [w], [w])
                P_ = Psum.next()
                ts(kb, "dve", P_, P_[:, 0:ncv], pc, pc[:, 0, 0:ncv], w[:, 8:9], None, ALU.mult, extra=[w])
                for z in range(1, 4):
                    stt(kb, "dve", P_, P_[:, 0:ncv], pc, pc[:, z, 0:ncv], w[:, 8 + z:9 + z], P_, P_[:, 0:ncv], ALU.mult, ALU.add,
                        extra=[w])
                s_ = sc.next()
                kb.op("dve", lambda e: e.tensor_reduce(out=w[:, 16:80], in_=P_[:].rearrange("p (j r) -> p j r", r=4),
                                                       axis=AX.X, op=ALU.add), [P_], [w])
                stt(kb, "dve", s_, s_[:], P_, P_[:, 3:256:4], -0.5, w, w[:, 16:80], ALU.mult, ALU.add)
                stt(kb, "dve", s_, s_[:, 1:64], P_, P_[:, 3:252:4], 0.5, s_, s_[:, 1:64], ALU.mult, ALU.add)
                tt(kb, "dve", s_, s_[:], s_, s_[:], KM, KM[:, 64 - 2 * c:128 - 2 * c], ALU.mult)
                tt(kb, "dve", s_, s_[:], s_, s_[:], AM, AM[:, 64 - 2 * c:128 - 2 * c], ALU.add)
                memset(kb, "dve", s_, s_[:, 0:1], 1000.0)
                kb.op("dve", lambda e: e.max(out=w[:, 80:88], in_=s_[:]), [s_], [w])
                ts(kb, "dve", w, w[:, 88:89], w, w[:, 87:88], 0.0, None, ALU.max)
                ts(kb, "dve", s_, s_[:], s_, s_[:], w[:, 88:89], None, ALU.is_ge, extra=[w])
                nb = ngb.next()
                ts(kb, "dve", nb, nb[:], s_, s_[:], -1.0, BIG, ALU.add, ALU.mult)
                pT = ptr.next()
                kb.op("pe", lambda e: e.transpose(pT[:], nb[:], identb[:]), [nb, identb], [pT])
                cp(kb, "act", negT, negT[:, c * 128:(c + 1) * 128], pT, pT[:])
            for z in range(4):
                kb.dma("sp", G["qaug0"][g * 4 + z, 64:128, :], negT[:], reads=[negT], writes=[G["b_neg0"]])


def nsa_attn(kb, G):
    with kb.phase():
        A = AttnCtx(kb, need_cmp=True)
        identb = make_ident(kb, BF16)
        Ec = kb.sb([128, S], BF16)
        memset(kb, "dve", Ec, Ec[:], 1.0)
        kb.op("pool", lambda g: g.affine_select(out=Ec[64:128, :], in_=Ec[64:128, :], pattern=[[1, S]], compare_op=ALU.is_ge,
                                                fill=0.0, base=0, channel_multiplier=-64), [Ec], [Ec])
        kb.op("pool", lambda g: g.affine_select(out=Ec[64:128, :], in_=Ec[64:128, :], pattern=[[-1, S]], compare_op=ALU.is_ge,
                                                fill=0.0, base=63, channel_multiplier=64), [Ec], [Ec])
        gates = kb.sb([128, 32, 24], F32)
        kb.dma("sp", gates[:], G["gates"].rearrange("(c p) n -> p c n", p=128), reads=[G["b_qk0"]], writes=[gates])
        ks = kb.sb([128, S], BF16)
        kw = kb.sb([64, S], BF16)
        kcm = kb.sb([64, 256], BF16)
        vs = kb.sb([128, 32, 65], BF16)
        vw = kb.sb([128, 32, 65], BF16)
        vcm = kb.sb([128, 2, 65], BF16)
        qr = Ring([kb.sb([128, S], BF16) for _ in range(2)])
        accr = Ring([kb.sb([128, 4, 64], F32) for _ in range(2)])
        otok = kb.sb([128, 8, 4, 128], BF16)
        ptr = Ring([kb.ps([128, 4, 128], BF16) for _ in range(2)])
        oT = Ring([kb.sb([128, 512], BF16) for _ in range(2)])
        v3v = G["v3"].rearrange("(kt p) b c -> p kt b c", p=128)
        for hp in range(4):
            g = hp // 2
            if hp % 2 == 0:
                kb.dma("sp", ks[0:64], G["ksT"][g], reads=[G["b_qk0"]], writes=[ks])
                cp(kb, "pool", ks, ks[64:128, :], Ec, Ec[64:128, :])
                kb.dma("sp", kw[:], G["kwT"][g], reads=[G["b_qk0"]], writes=[kw])
                kb.dma("sp", kcm[:], G["kcmpT"][g], reads=[G["b_cmp"]], writes=[kcm])
                kb.dma("sp", vs[:], v3v[:, :, 2 + g, :], reads=[G["b_qk0"]], writes=[vs])
                kb.dma("sp", vw[:], v3v[:, :, 4 + g, :], reads=[G["b_qk0"]], writes=[vw])
                kb.dma("sp", vcm[:], G["vcmp"][g], reads=[G["b_cmp"]], writes=[vcm])
            for hh in range(2):
                h = hp * 2 + hh
                q = qr.next()
                kb.dma("sp", q[:], G["qaug0"][h], reads=[G["b_qk0"], G["b_neg0"]], writes=[q])
                for Q in range(8):
                    acc = accr.next()
                    attn_branch(A, 64, q, kcm, vcm, 255, sched_cmp, Q, acc, True, gates, gates[:, 4 * Q:4 * Q + 4, h * 3 + 0])
                    attn_branch(A, 128, q, ks, vs, None, sched_causal, Q, acc, False, gates, gates[:, 4 * Q:4 * Q + 4, h * 3 + 1])
                    attn_branch(A, 64, q, kw, vw, None, sched_win, Q, acc, False, gates, gates[:, 4 * Q:4 * Q + 4, h * 3 + 2],
                                after=lambda acc=acc, Q=Q, hh=hh: cp(kb, "act", otok, otok[:, Q, :, hh * 64:(hh + 1) * 64], acc, acc[:]))
            A.flush_all()
            for Q in range(8):
                pT = ptr.next()
                for s in range(4):
                    kb.op("pe", lambda e, s=s: e.transpose(pT[:, s, :], otok[:, Q, s, :], identb[:]), [otok, identb], [pT])
                o = oT.next()
                cp(kb, "dve", o, o[:].rearrange("p (s q) -> p s q", s=4), pT, pT[:])
                kb.dma("sp", G["o0T"][hp * 128:(hp + 1) * 128, Q * 512:(Q + 1) * 512], o[:], reads=[o], writes=[G["b_o0"]])


def sincos_from_rev(kb, r, rap, shape, tmp, out_sin, out_sin_ap, out_cos, out_cos_ap):
    a1 = tmp[0][tuple(slice(0, s) for s in shape)]
    a2 = tmp[1][tuple(slice(0, s) for s in shape)]
    ts(kb, "dve", tmp[0], a1, r, rap, MAGIC, MAGIC, ALU.add, ALU.subtract)
    tt(kb, "dve", tmp[0], a1, r, rap, tmp[0], a1, ALU.subtract)
    act(kb, out_sin, out_sin_ap, tmp[0], a1, AF.Sin, scale=TWO_PI)
    ts(kb, "dve", tmp[1], a2, r, rap, 0.25, None, ALU.add)
    ts(kb, "dve", tmp[0], a1, tmp[1], a2, MAGIC, MAGIC, ALU.add, ALU.subtract)
    tt(kb, "dve", tmp[0], a1, tmp[1], a2, tmp[0], a1, ALU.subtract)
    act(kb, out_cos, out_cos_ap, tmp[0], a1, AF.Sin, scale=TWO_PI)


def s5(kb, G):
    with kb.phase():
        ident = make_ident(kb)
        identb = make_ident(kb, BF16)
        par = kb.sb([128, 4, 16], F32)
        with contextlib.ExitStack() as rs:
            old = kb.stack
            kb.stack = rs
            lr, li = kb.sb([16, 128], F32), kb.sb([16, 128], F32)
            kb.dma("sp", lr[:], G["s5_lre"].rearrange("(s w) p -> s (w p)", w=2), writes=[lr])
            kb.dma("sp", li[:], G["s5_lim"].rearrange("(s w) p -> s (w p)", w=2), writes=[li])
            ls = kb.sb([16, 2], F32)
            kb.dma("sp", ls[:], G["s5_ls"].rearrange("(s w) -> s w", w=2), writes=[ls])
            act(kb, ls, ls[:], ls, ls[:], AF.Exp)
            W = [kb.sb([16, 128], F32) for _ in range(12)]
            stepb, mag, thr, sn, cs, ar1, ai, den, fr, fi, ta, tb = W
            cp(kb, "dve", stepb, stepb[:].rearrange("s (w p) -> s w p", w=2), ls, ls[:].unsqueeze(2).to_broadcast([16, 2, 64]))
            tt(kb, "dve", ta, ta[:], lr, lr[:], stepb, stepb[:], ALU.mult)
            act(kb, mag, mag[:], ta, ta[:], AF.Exp)
            tt(kb, "dve", thr, thr[:], li, li[:], stepb, stepb[:], ALU.mult)
            ts(kb, "dve", thr, thr[:], thr, thr[:], 1.0 / TWO_PI, None, ALU.mult)
            sincos_from_rev(kb, thr, thr[:], (16, 128), (ta, tb), sn, sn[:], cs, cs[:])
            tt(kb, "dve", ai, ai[:], mag, mag[:], sn, sn[:], ALU.mult)
            tt(kb, "dve", ar1, ar1[:], mag, mag[:], cs, cs[:], ALU.mult)
            ts(kb, "dve", ar1, ar1[:], ar1, ar1[:], -1.0, None, ALU.add)
            tt(kb, "dve", ta, ta[:], lr, lr[:], lr, lr[:], ALU.mult)
            tt(kb, "dve", tb, tb[:], li, li[:], li, li[:], ALU.mult)
            tt(kb, "dve", den, den[:], ta, ta[:], tb, tb[:], ALU.add)
            kb.op("dve", lambda e: e.reciprocal(out=den[:], in_=den[:]), [den], [den])
            tt(kb, "dve", ta, ta[:], ar1, ar1[:], lr, lr[:], ALU.mult)
            tt(kb, "dve", tb, tb[:], ai, ai[:], li, li[:], ALU.mult)
            tt(kb, "dve", fr, fr[:], ta, ta[:], tb, tb[:], ALU.add)
            tt(kb, "dve", fr, fr[:], fr, fr[:], den, den[:], ALU.mult)
            tt(kb, "dve", ta, ta[:], ai, ai[:], lr, lr[:], ALU.mult)
            tt(kb, "dve", tb, tb[:], ar1, ar1[:], li, li[:], ALU.mult)
            tt(kb, "dve", fi, fi[:], ta, ta[:], tb, tb[:], ALU.subtract)
            tt(kb, "dve", fi, fi[:], fi, fi[:], den, den[:], ALU.mult)
            pp = kb.ps([128, 4, 16], F32)
            for i, src in enumerate((mag, thr, fr, fi)):
                kb.op("pe", lambda e, i=i, src=src: e.transpose(pp[:, i, :], src[:], ident[0:16, 0:16]), [src, ident], [pp])
            cp(kb, "act", par, par[:], pp, pp[:])
            kb.barrier()
            kb.stack = old
        BTr, BTi = kb.sb([128, 4, 128], BF16), kb.sb([128, 4, 128], BF16)
        CTr, CTi = kb.sb([128, 4, 128], BF16), kb.sb([128, 4, 128], BF16)
        with contextlib.ExitStack() as rs:
            old = kb.stack
            kb.stack = rs
            br, bi = kb.sb([128, 16, 16], F32), kb.sb([128, 16, 16], F32)
            kb.dma("sp", br[:], G["s5_bre"].rearrange("(s w) p c -> (w p) s c", w=2), writes=[br])
            kb.dma("sp", bi[:], G["s5_bim"].rearrange("(s w) p c -> (w p) s c", w=2), writes=[bi])
            frb = par[:, 2, :].unsqueeze(2).to_broadcast([128, 16, 16])
            fib = par[:, 3, :].unsqueeze(2).to_broadcast([128, 16, 16])
            t1, t2 = kb.sb([128, 16, 16], F32), kb.sb([128, 16, 16], F32)
            Bb = [kb.sb([128, 16, 16], F32), kb.sb([128, 16, 16], F32)]
            tt(kb, "dve", t1, t1[:], br, br[:], par, frb, ALU.mult)
            tt(kb, "dve", t2, t2[:], bi, bi[:], par, fib, ALU.mult)
            tt(kb, "dve", Bb[0], Bb[0][:], t1, t1[:], t2, t2[:], ALU.subtract)
            tt(kb, "dve", t1, t1[:], bi, bi[:], par, frb, ALU.mult)
            tt(kb, "dve", t2, t2[:], br, br[:], par, fib, ALU.mult)
            tt(kb, "dve", Bb[1], Bb[1][:], t1, t1[:], t2, t2[:], ALU.add)
            for ri, dstT in ((0, BTr), (1, BTi)):
                BD = kb.sb([128, 16, 32], BF16)
                memset(kb, "dve", BD, BD[:], 0.0)
                cp(kb, "dve", BD, BD[0:64, :, 0:16], Bb[ri], Bb[ri][0:64])
                cp(kb, "dve", BD, BD[64:128, :, 16:32], Bb[ri], Bb[ri][64:128])
                pT = kb.ps([128, 4, 128], BF16)
                for j in range(4):
                    kb.op("pe", lambda e, j=j: e.transpose(pT[:, j, :], BD[:, 4 * j:4 * j + 4, :].rearrange("p a b -> p (a b)"), identb[:]),
                          [BD, identb], [pT])
                cp(kb, "act", dstT, dstT[:], pT, pT[:])
            m0, m1 = kb.sb([128, 1], F32), kb.sb([128, 1], F32)
            memset(kb, "dve", m0, m0[:], 0.0)
            memset(kb, "dve", m1, m1[:], 1.0)
            for q in range(4):
                memset(kb, "dve", m0, m0[32 * q:32 * q + 16, :], 1.0)
                memset(kb, "dve", m1, m1[32 * q:32 * q + 16, :], 0.0)
            for ri, dstT, key in ((0, CTr, "s5_cre"), (1, CTi, "s5_cim")):
                ct = kb.sb([128, 4, 64], F32)
                kb.dma("sp", ct[:], G[key].rearrange("g c p -> (g c) p").rearrange("(j r) p -> r j p", r=128), writes=[ct])
                BD = kb.sb([128, 4, 128], BF16)
                ts(kb, "dve", BD, BD[:, :, 0:64], ct, ct[:], m0[:, 0:1], None, ALU.mult, extra=[m0])
                ts(kb, "dve", BD, BD[:, :, 64:128], ct, ct[:], m1[:, 0:1], None, ALU.mult, extra=[m1])
                pT = kb.ps([128, 4, 128], BF16)
                for j in range(4):
                    kb.op("pe", lambda e, j=j: e.transpose(pT[:, j, :], BD[:, j, :], identb[:]), [BD, identb], [pT])
                if ri == 0:
                    cp(kb, "act", dstT, dstT[:], pT, pT[:])
                else:
                    ts(kb, "dve", dstT, dstT[:], pT, pT[:], -1.0, None, ALU.mult)
            kb.barrier()
            kb.stack = old
        with contextlib.ExitStack() as rs:
            old = kb.stack
            kb.stack = rs
            uT = kb.sb([128, 4, S], BF16)
            kb.dma("sp", uT[:], fm(G["uT"]), reads=[G["b_qk0"]], writes=[uT])
            uTa = kb.sb([32, 4, S], BF16)
            cp(kb, "pool", uTa, uTa[:], uT, uT[96:128])
            BTra, BTia = kb.sb([32, 4, 128], BF16), kb.sb([32, 4, 128], BF16)
            cp(kb, "pool", BTra, BTra[:], BTr, BTr[96:128])
            cp(kb, "pool", BTia, BTia[:], BTi, BTi[96:128])
            jt = kb.sb([128, 513], F32)
            make_ramp(kb, jt, jt[:], 128, 513)
            rT = kb.sb([128, 513], F32)
            tmpA, tmpB = kb.sb([128, 513], F32), kb.sb([128, 513], F32)
            cTr = Ring([kb.sb([128, 513], F32) for _ in range(2)])
            sTr = Ring([kb.sb([128, 513], F32) for _ in range(2)])
            pbr = Ring([kb.ps([128, 512], F32) for _ in range(2)])
            pbi = Ring([kb.ps([128, 512], F32) for _ in range(2)])
            pyr = Ring([kb.ps([128, 4, 32], F32) for _ in range(2)])
            E = [Ring([kb.sb([128, 512], F32) for _ in range(2)]) for _ in range(6)]
            zrr = Ring([kb.sb([128, 512], F32) for _ in range(2)])
            zir = Ring([kb.sb([128, 512], F32) for _ in range(2)])
            xrr = Ring([kb.sb([128, 512], BF16) for _ in range(2)])
            xir = Ring([kb.sb([128, 512], BF16) for _ in range(2)])
            car = Ring([kb.sb([128, 4], F32) for _ in range(2)])
            ysr = Ring([kb.sb([128, 4, 32], F32) for _ in range(2)])
            yv = G["ytm"].rearrange("(c p) f -> p c f", p=128)
            for st in range(16):
                ts(kb, "dve", rT, rT[:], jt, jt[:], par[:, 1, st:st + 1], None, ALU.mult, extra=[par])
                cT, sT = cTr.next(), sTr.next()
                sincos_from_rev(kb, rT, rT[:], (128, 513), (tmpA, tmpB), sT, sT[:], cT, cT[:])
                magb = par[:, 0, st:st + 1].to_broadcast([128, 512])
                po = (st % 4) * 32
                prev = None
                for ck in range(8):
                    a, b = pbr.next(), pbi.next()
                    csl = slice(ck * 512, (ck + 1) * 512)
                    if st % 4 == 3:
                        mm(kb, a, a[:], BTra, BTra[0:32, st // 4, :], uTa, uTa[0:32, st // 4, csl])
                        mm(kb, b, b[:], BTia, BTia[0:32, st // 4, :], uTa, uTa[0:32, st // 4, csl])
                    else:
                        mm(kb, a, a[:], BTr, BTr[po:po + 32, st // 4, :], uT, uT[po:po + 32, st // 4, csl])
                        mm(kb, b, b[:], BTi, BTi[po:po + 32, st // 4, :], uT, uT[po:po + 32, st // 4, csl])
                    e = [r.next() for r in E]
                    tt(kb, "dve", e[0], e[0][:], a, a[:], cT, cT[:, 0:512], ALU.mult)
                    tt(kb, "dve", e[1], e[1][:], b, b[:], sT, sT[:, 0:512], ALU.mult)
                    tt(kb, "pool", e[0], e[0][:], e[0], e[0][:], e[1], e[1][:], ALU.add)
                    tt(kb, "dve", e[2], e[2][:], b, b[:], cT, cT[:, 0:512], ALU.mult)
                    tt(kb, "dve", e[3], e[3][:], a, a[:], sT, sT[:, 0:512], ALU.mult)
                    tt(kb, "pool", e[2], e[2][:], e[2], e[2][:], e[3], e[3][:], ALU.subtract)
                    zr, zi = zrr.next(), zir.next()
                    if prev is None:
                        i0, i1, ex = 0.0, 0.0, []
                    else:
                        pzr, pzi = prev
                        ca = car.next()
                        ts(kb, "dve", ca, ca[:, 0:1], pzi, pzi[:, 511:512], sT[:, 512:513], None, ALU.mult, extra=[sT])
                        stt(kb, "dve", ca, ca[:, 1:2], pzr, pzr[:, 511:512], cT[:, 512:513], ca, ca[:, 0:1], ALU.mult, ALU.subtract,
                            extra=[cT])
                        ts(kb, "dve", ca, ca[:, 2:3], pzi, pzi[:, 511:512], cT[:, 512:513], None, ALU.mult, extra=[cT])
                        stt(kb, "dve", ca, ca[:, 3:4], pzr, pzr[:, 511:512], sT[:, 512:513], ca, ca[:, 2:3], ALU.mult, ALU.add,
                            extra=[sT])
                        i0, i1, ex = ca[:, 1:2], ca[:, 3:4], [ca]
                    kb.op("dve", lambda g, i0=i0: g.tensor_tensor_scan(out=zr[:], data0=magb, data1=e[0][:], initial=i0,
                                                                       op0=ALU.mult, op1=ALU.add), [e[0], par] + ex, [zr])
                    kb.op("dve", lambda g, i1=i1: g.tensor_tensor_scan(out=zi[:], data0=magb, data1=e[2][:], initial=i1,
                                                                       op0=ALU.mult, op1=ALU.add), [e[2], par] + ex, [zi])
                    prev = (zr, zi)
                    xr, xi = xrr.next(), xir.next()
                    tt(kb, "pool", e[4], e[4][:], zr, zr[:], cT, cT[:, 0:512], ALU.mult)
                    tt(kb, "pool", e[5], e[5][:], zi, zi[:], sT, sT[:, 0:512], ALU.mult)
                    tt(kb, "pool", xr, xr[:], e[4], e[4][:], e[5], e[5][:], ALU.subtract)
                    tt(kb, "dve", e[1], e[1][:], zr, zr[:], sT, sT[:, 0:512], ALU.mult)
                    tt(kb, "pool", e[3], e[3][:], zi, zi[:], cT, cT[:, 0:512], ALU.mult)
                    tt(kb, "pool", xi, xi[:], e[1], e[1][:], e[3], e[3][:], ALU.add)
                    yp = pyr.next()
                    for tq in range(4):
                        mm(kb, yp, yp[:, tq, :], xr, xr[:, tq * 128:(tq + 1) * 128], CTr, CTr[:, st // 4, po:po + 32], start=True, stop=False)
                        mm(kb, yp, yp[:, tq, :], xi, xi[:, tq * 128:(tq + 1) * 128], CTi, CTi[:, st // 4, po:po + 32], start=False, stop=True)
                    ys = ysr.next()
                    cp(kb, "act", ys, ys[:], yp, yp[:])
                    kb.dma("sp", yv[:, ck * 4:ck * 4 + 4, st * 32:(st + 1) * 32], ys[:], reads=[ys], writes=[G["b_y"]])
            kb.barrier()
            kb.stack = old
        dbc = kb.sb([128, 512], F32)
        kb.dma("sp", dbc[:], G["s5_d"].to_broadcast([128, 512]), writes=[dbc])
        glw = kb.sb([128, 4, 512], BF16)
        kb.dma("pool", glw[:], G["s5_glw"].rearrange("(c p) n -> p c n", p=128), writes=[glw])
        glb = kb.sb([128, 4], F32)
        with kb.nc.allow_non_contiguous_dma(reason="tiny"):
            kb.dma("sp", glb[:], G["s5_glb"].rearrange("(c p) -> p c", p=128), writes=[glb])
        yr = Ring([kb.sb([128, 512], F32) for _ in range(2)])
        ur = Ring([kb.sb([128, 512], F32) for _ in range(2)])
        g1, g2 = kb.sb([128, 512], F32), kb.sb([128, 512], F32)
        gbr = Ring([kb.sb([128, 512], BF16) for _ in range(2)])
        pT = Ring([kb.ps([128, 4, 128], BF16) for _ in range(2)])
        gTr = Ring([kb.sb([128, 4, 128], BF16) for _ in range(2)])
        pz = Ring([kb.ps([128, 128], F32) for _ in range(2)])
        sgr = Ring([kb.sb([128, 128], F32) for _ in range(2)])
        osr = Ring([kb.sb([128, 4, 128], BF16) for _ in range(2)])
        ytv = G["ytm"].rearrange("(c p) f -> p c f", p=128)
        utv = G["utm"].rearrange("(c p) f -> p c f", p=128)
        o0v = G["o0T"][512:1024, :].rearrange("(c p) t -> p c t", p=128)
        for tk in range(32):
            y, u = yr.next(), ur.next()
            kb.dma("sp", y[:], ytv[:, tk, :], reads=[G["b_y"]], writes=[y])
            kb.dma("act", u[:], utv[:, tk, :], reads=[G["b_qk0"]], writes=[u])
            tt(kb, "dve", u, u[:], u, u[:], dbc, dbc[:], ALU.mult)
            tt(kb, "dve", y, y[:], y, y[:], u, u[:], ALU.add)
            gb_ = gbr.next()
            gelu_tanh(kb, y, y[:], gb_, gb_[:], (128, 512), g1, g2)
            p = pT.next()
            for c in range(4):
                kb.op("pe", lambda e, c=c: e.transpose(p[:, c, :], gb_[:, c * 128:(c + 1) * 128], identb[:]), [gb_, identb], [p])
            gT = gTr.next()
            cp(kb, "act", gT, gT[:], p, p[:])
            os_ = osr.next()
            for fo in range(4):
                z = pz.next()
                for c in range(4):
                    mm(kb, z, z[:], glw, glw[:, c, fo * 128:(fo + 1) * 128], gT, gT[:, c, :], start=(c == 0), stop=(c == 3))
                sg = sgr.next()
                act(kb, sg, sg[:], z, z[:], AF.Sigmoid, bias=glb[:, fo:fo + 1], extra=[glb])
                tt(kb, "dve", os_, os_[:, fo, :], gT, gT[:, fo, :], sg, sg[:], ALU.mult)
            kb.dma("sp", o0v[:, :, tk * 128:(tk + 1) * 128], os_[:], reads=[os_], writes=[G["b_o0"]])


def moba_select(kb, G):
    with kb.phase():
        identb = make_ident(kb, BF16)
        val = kb.sb([128, 32, 16], F32)
        nv = kb.sb([128, 16], F32)
        make_ramp(kb, nv, nv[:], 128, 16)
        tt(kb, "dve", val, val[:].rearrange("p (a b) n -> p a b n", b=2),
           nv, nv[:].unsqueeze(1).unsqueeze(1).to_broadcast([128, 16, 2, 16]),
           nv, nv[:].unsqueeze(2).unsqueeze(3).to_broadcast([128, 16, 2, 16]), ALU.subtract)
        addm, notown = kb.sb([128, 32, 16], F32), kb.sb([128, 32, 16], F32)
        ts(kb, "dve", addm, addm[:], val, val[:], 0.0, -1e30, ALU.is_ge, ALU.mult)
        ts(kb, "dve", notown, notown[:], val, val[:], 0.0, None, ALU.not_equal)
        qr = Ring([kb.sb([64, S], BF16) for _ in range(2)])
        kr = Ring([kb.sb([64, S], BF16) for _ in range(2)])
        km = Ring([kb.sb([64, 16], F32) for _ in range(2)])
        kmb = Ring([kb.sb([64, 16], BF16) for _ in range(2)])
        pg = Ring([kb.ps([128, 32, 16], F32) for _ in range(2)])
        gs = Ring([kb.sb([128, 32, 16], F32) for _ in range(2)])
        eq = kb.sb([128, 32, 16], F32)
        mx = Ring([kb.sb([128, 32], F32) for _ in range(2)])
        ngr = Ring([kb.sb([128, 32, 16], BF16) for _ in range(2)])
        ptr = Ring([kb.ps([16, 8, 128], BF16) for _ in range(2)])
        ngT = Ring([kb.sb([16, S], BF16) for _ in range(2)])
        for h in range(16):
            q, k = qr.next(), kr.next()
            kb.dma("sp", q[:], G["qaug1"][h, 0:64, :], reads=[G["b_qk1"]], writes=[q])
            kb.dma("act", k[:], G["kaug1"][h, 0:64, :], reads=[G["b_qk1"]], writes=[k])
            m_, mb = km.next(), kmb.next()
            kb.op("dve", lambda e: e.tensor_reduce(out=m_[:], in_=k[:].rearrange("p (n j) -> p n j", j=256), axis=AX.X, op=ALU.add),
                  [k], [m_])
            ts(kb, "dve", mb, mb[:], m_, m_[:], 1.0 / 256.0, None, ALU.mult)
            p = pg.next()
            for c in range(32):
                mm(kb, p, p[:, c, :], q, q[:, c * 128:(c + 1) * 128], mb, mb[:])
            g_ = gs.next()
            tt(kb, "dve", g_, g_[:], p, p[:], addm, addm[:], ALU.add)
            m = mx.next()
            for it in range(3):
                kb.op("dve", lambda e: e.tensor_reduce(out=m[:], in_=g_[:], axis=AX.X, op=ALU.max), [g_], [m])
                if it < 2:
                    tt(kb, "dve", eq, eq[:], g_, g_[:], m, m[:].unsqueeze(2).to_broadcast([128, 32, 16]), ALU.is_equal)
                    stt(kb, "dve", g_, g_[:], eq, eq[:], -1e30, g_, g_[:], ALU.mult, ALU.add)
            ts(kb, "dve", m, m[:], m, m[:], -1e29, None, ALU.max)
            tt(kb, "dve", eq, eq[:], p, p[:], addm, addm[:], ALU.add)
            tt(kb, "dve", eq, eq[:], eq, eq[:], m, m[:].unsqueeze(2).to_broadcast([128, 32, 16]), ALU.is_ge)
            ts(kb, "dve", eq, eq[:], eq, eq[:], -1.0, BIG, ALU.add, ALU.mult)
            ng = ngr.next()
            tt(kb, "dve", ng, ng[:], eq, eq[:], notown, notown[:], ALU.mult)
            nT = ngT.next()
            for c8 in range(4):
                pT = ptr.next()
                for j in range(8):
                    c = c8 * 8 + j
                    kb.op("pe", lambda e, c=c, j=j: e.transpose(pT[:, j, :], ng[:, c, :], identb[:]), [ng, identb], [pT])
                cp(kb, "act", nT, nT[:, c8 * 1024:(c8 + 1) * 1024].rearrange("p (j q) -> p j q", j=8), pT, pT[:])
            kb.dma("sp", G["qaug1"][h, 64:80, :], nT[:], reads=[nT], writes=[G["b_neg1"]])


def moba_attn(kb, G):
    with kb.phase():
        A = AttnCtx(kb, need_cmp=False)
        identb = make_ident(kb, BF16)
        Ec = kb.sb([128, S], BF16)
        memset(kb, "dve", Ec, Ec[:], 1.0)
        kb.op("pool", lambda g: g.affine_select(out=Ec[64:80, :], in_=Ec[64:80, :], pattern=[[1, S]], compare_op=ALU.is_ge,
                                                fill=0.0, base=0, channel_multiplier=-256), [Ec], [Ec])
        kb.op("pool", lambda g: g.affine_select(out=Ec[64:80, :], in_=Ec[64:80, :], pattern=[[-1, S]], compare_op=ALU.is_ge,
                                                fill=0.0, base=255, channel_multiplier=256), [Ec], [Ec])
        qr = Ring([kb.sb([80, S], BF16) for _ in range(2)])
        kr = Ring([kb.sb([80, S], BF16) for _ in range(2)])
        vr = Ring([kb.sb([128, 32, 65], BF16) for _ in range(2)])
        accr = Ring([kb.sb([128, 4, 64], F32) for _ in range(2)])
        otok = kb.sb([128, 8, 4, 128], BF16)
        ptr = Ring([kb.ps([128, 4, 128], BF16) for _ in range(2)])
        oT = Ring([kb.sb([128, 512], BF16) for _ in range(2)])
        v1v = G["v1"].rearrange("(kt p) b c -> p kt b c", p=128)
        for hp in range(8):
            for hh in range(2):
                h = hp * 2 + hh
                q, k, v = qr.next(), kr.next(), vr.next()
                kb.dma("sp", q[:], G["qaug1"][h], reads=[G["b_qk1"], G["b_neg1"]], writes=[q])
                kb.dma("act", k[0:64], G["kaug1"][h, 0:64, :], reads=[G["b_qk1"]], writes=[k])
                cp(kb, "pool", k, k[64:80, :], Ec, Ec[64:80, :])
                kb.dma("sp", v[:], v1v[:, :, h, :], reads=[G["b_qk1"]], writes=[v])
                for Q in range(8):
                    acc = accr.next()
                    attn_branch(A, 80, q, k, v, None, sched_causal, Q, acc, True,
                                after=lambda acc=acc, Q=Q, hh=hh: cp(kb, "act", otok, otok[:, Q, :, hh * 64:(hh + 1) * 64], acc, acc[:]))
            A.flush_all()
            for Q in range(8):
                pT = ptr.next()
                for s in range(4):
                    kb.op("pe", lambda e, s=s: e.transpose(pT[:, s, :], otok[:, Q, s, :], identb[:]), [otok, identb], [pT])
                o = oT.next()
                cp(kb, "dve", o, o[:].rearrange("p (s q) -> p s q", s=4), pT, pT[:])
                kb.dma("sp", G["o1T"][hp * 128:(hp + 1) * 128, Q * 512:(Q + 1) * 512], o[:], reads=[o], writes=[G["b_o1"]])


def build_program(debug=False, upto=99):
    nc = bass.Bass("TRN2", target_bir_lowering=False)
    G = {}

    def din(name, shape, dt=F32):
        return nc.dram_tensor(name, list(shape), dt, kind="ExternalInput").ap()

    def scr(name, shape, dt=F32, out=False):
        isout = out or (debug and (debug is True or name in debug))
        return nc.dram_tensor(name, list(shape), dt, kind="ExternalOutput" if isout else "Internal").ap()

    xT = din("xT", [D, S])
    w0r, w0s = din("w0r", [D, 14 * 64]), din("w0s", [D, 14 * 64])
    w0f, w0t = din("w0f", [D, 5 * 128]), din("w0t", [D, 920])
    w1r, w1s, w1t = din("w1r", [D, 32 * 64]), din("w1s", [D, 32 * 64]), din("w1t", [D, 1024])
    for wh in ("k", "v"):
        G["cmp_%s_w1" % wh] = din("cmp_%s_w1" % wh, [2048, 128])
        G["cmp_%s_w2" % wh] = din("cmp_%s_w2" % wh, [128, 64])
        G["pe_%s" % wh] = din("pe_%s" % wh, [32, 64])
    G["s5_lre"], G["s5_lim"], G["s5_ls"] = din("s5_lre", [32, 64]), din("s5_lim", [32, 64]), din("s5_ls", [32])
    G["s5_bre"], G["s5_bim"] = din("s5_bre", [32, 64, 16]), din("s5_bim", [32, 64, 16])
    G["s5_cre"], G["s5_cim"] = din("s5_cre", [32, 16, 64]), din("s5_cim", [32, 16, 64])
    G["s5_d"], G["s5_glw"], G["s5_glb"] = din("s5_d", [1, 512]), din("s5_glw", [512, 512]), din("s5_glb", [512])
    ev_wo, od_wo = din("ev_wo", [D, D]), din("od_wo", [D, D])
    ln = {k: din(k, [2, D]) for k in ("ln_mix_g", "ln_mix_b", "ln_ffn_g", "ln_ffn_b")}
    G["moe_wr"] = [din("moe_wr%d" % L, [D, 36]) for L in range(2)]
    G["moe_br"] = [din("moe_br%d" % L, [1, 36]) for L in range(2)]
    G["moe_wg"] = [din("moe_wg%d" % L, [32, D, 128]) for L in range(2)]
    G["moe_wu"] = [din("moe_wu%d" % L, [32, D, 128]) for L in range(2)]
    G["moe_wd"] = [din("moe_wd%d" % L, [32, 128, D]) for L in range(2)]
    yT = scr("yT", [D, S], out=True)
    G["ropeC"], G["ropeS"] = scr("ropeC", [64, S]), scr("ropeS", [64, S])
    G["qaug0"] = scr("qaug0", [8, 128, S], BF16)
    G["ksT"], G["kwT"], G["kcT"] = scr("ksT", [2, 64, S], BF16), scr("kwT", [2, 64, S], BF16), scr("kcT", [2, 64, S], BF16)
    G["vcT"], G["uT"] = scr("vcT", [128, S], BF16), scr("uT", [512, S], BF16)
    G["v3"] = scr("v3", [S, 6, 65], BF16)
    G["gates"], G["utm"], G["ytm"] = scr("gates", [S, 24]), scr("utm", [S, 512]), scr("ytm", [S, 512])
    G["kcmpT"], G["vcmp"] = scr("kcmpT", [2, 64, 256], BF16), scr("vcmp", [2, 128, 2, 65], BF16)
    G["o0T"], G["o1T"] = scr("o0T", [D, S], BF16), scr("o1T", [D, S], BF16)
    mixT = scr("mixT", [D, S])
    x1T, x2T, x3T = scr("x1T", [D, S]), scr("x2T", [D, S]), scr("x3T", [D, S])
    G["qaug1"], G["kaug1"] = scr("qaug1", [16, 80, S], BF16), scr("kaug1", [16, 64, S], BF16)
    G["v1"] = scr("v1", [S, 16, 65], BF16)
    for b in ("b_rope", "b_qk0", "b_cmp", "b_neg0", "b_o0", "b_y", "b_qk1", "b_neg1", "b_o1"):
        G[b] = Buf(b)
    bx, bmix, b1, b2, b3, by = Buf(), Buf(), Buf(), Buf(), Buf(), Buf()

    with contextlib.ExitStack() as st:
        kb = KB(nc, st)
        build_consts(kb, G)
        v3r = Ring([kb.sb([128, 6, 65], BF16) for _ in range(2)])
        gtr = Ring([kb.sb([128, 24], F32) for _ in range(2)])
        utr = Ring([kb.sb([128, 512], F32) for _ in range(2)])
        for it in v3r.items:
            memset(kb, "dve", it, it[:], 1.0)

        def rope_dst0(h):
            if h < 8:
                return G["qaug0"][h, 0:64, :], G["b_qk0"]
            g = (h - 8) % 2
            return (G["kcT"], G["ksT"], G["kwT"])[(h - 8) // 2][g], G["b_qk0"]

        def fm_dst0(c):
            if c == 0:
                return G["vcT"], G["b_qk0"]
            return G["uT"][(c - 1) * 128:c * 128, :], G["b_qk0"]

        def tm_cb0(kb, pt, ti):
            import os
            TM = os.environ.get("TM_PARTS", "vgu")
            v, g_, u = v3r.next(), gtr.next(), utr.next()
            rs = slice(ti * 128, (ti + 1) * 128)
            if "v" in TM:
                cp(kb, "act", v, v[:, :, 0:64], pt[0], pt[0][:, 0:384].rearrange("p (b c) -> p b c", b=6))
                kb.dma("sp", G["v3"][rs], v[:], reads=[v], writes=[G["b_qk0"]])
            if "g" in TM:
                act(kb, g_, g_[:], pt[0], pt[0][:, 384:408], AF.Sigmoid)
                kb.dma("sp", G["gates"][rs], g_[:], reads=[g_], writes=[G["b_qk0"]])
            if "u" in TM:
                cp(kb, "dve", u, u[:], pt[1], pt[1][:])
                kb.dma("sp", G["utm"][rs], u[:], reads=[u], writes=[G["b_qk0"]])

        if upto >= 1:
            inproj(kb, G, xT, bx, w0r, w0s, 14, rope_dst0, w0f, 5, fm_dst0, w0t, 920, tm_cb0, [(0, 408), (408, 920)])
        if upto >= 2:
            nsa_compress(kb, G)
            nsa_select(kb, G)
        if upto >= 3:
            nsa_attn(kb, G)
        if upto >= 4:
            s5(kb, G)
        if upto >= 5:
            linear_res_ln(kb, G, G["o0T"], ev_wo, xT, ln["ln_mix_g"][0], ln["ln_mix_b"][0], x1T, G["b_o0"], bx, b1)
            moe(kb, G, x1T, b1, 0, mixT, bmix)
            res_ln(kb, G, x1T, mixT, ln["ln_ffn_g"][0], ln["ln_ffn_b"][0], x2T, b1, bmix, b2)
        v1r = Ring([kb.sb([128, 16, 65], BF16) for _ in range(2)])
        for it in v1r.items:
            memset(kb, "dve", it, it[:], 1.0)

        def rope_dst1(h):
            if h < 16:
                return G["qaug1"][h, 0:64, :], G["b_qk1"]
            return G["kaug1"][h - 16], G["b_qk1"]

        def tm_cb1(kb, pt, ti):
            v = v1r.next()
            cp(kb, "act", v, v[:, 0:8, 0:64], pt[0], pt[0][:].rearrange("p (b c) -> p b c", b=8))
            cp(kb, "dve", v, v[:, 8:16, 0:64], pt[1], pt[1][:].rearrange("p (b c) -> p b c", b=8))
            kb.dma("sp", G["v1"][ti * 128:(ti + 1) * 128], v[:], reads=[v], writes=[G["b_qk1"]])

        if upto >= 6:
            inproj(kb, G, x2T, b2, w1r, w1s, 32, rope_dst1, None, 0, None, w1t, 1024, tm_cb1, [(0, 512), (512, 1024)])
            moba_select(kb, G)
            moba_attn(kb, G)
        if upto >= 7:
            linear_res_ln(kb, G, G["o1T"], od_wo, x2T, ln["ln_mix_g"][1], ln["ln_mix_b"][1], x3T, G["b_o1"], b2, b3)
            moe(kb, G, x3T, b3, 1, mixT, bmix)
            res_ln(kb, G, x3T, mixT, ln["ln_ffn_g"][1], ln["ln_ffn_b"][1], yT, b3, bmix, by)
        kb.barrier()
        print("instructions", kb.n_ins, "waits", kb.n_wait, flush=True)
    return nc


def _swap_cols(w, nh):
    w = w.reshape(w.shape[0], nh, 2, 32)
    return np.ascontiguousarray(w[:, :, ::-1, :].reshape(w.shape[0], nh * 64))


def prep_weights(inp):
    c = np.ascontiguousarray
    W = inp["ev_w_in"][0]
    q, kc, vc, ks, vs, kw, vw, gl, u = np.split(W, [512, 640, 768, 896, 1024, 1152, 1280, 1304], axis=1)
    m = {}
    rope = np.concatenate([q, kc, ks, kw], axis=1)
    m["w0r"] = c(rope)
    m["w0s"] = _swap_cols(rope, 14)
    m["w0f"] = c(np.concatenate([vc, u], axis=1))
    m["w0t"] = c(np.concatenate([vc, vs, vw, gl, u], axis=1))
    W1 = inp["od_w_in"][0]
    m["w1r"] = c(W1[:, 0:2048])
    m["w1s"] = _swap_cols(W1[:, 0:2048], 32)
    m["w1t"] = c(W1[:, 2048:3072])
    m["cmp_k_w1"], m["cmp_k_w2"], m["pe_k"] = c(inp["nsa_cmp_k_w1"][0]), c(inp["nsa_cmp_k_w2"][0]), c(inp["nsa_pe_k"][0])
    m["cmp_v_w1"], m["cmp_v_w2"], m["pe_v"] = c(inp["nsa_cmp_v_w1"][0]), c(inp["nsa_cmp_v_w2"][0]), c(inp["nsa_pe_v"][0])
    m["s5_lre"], m["s5_lim"], m["s5_ls"] = c(inp["s5_lambda_re"][0]), c(inp["s5_lambda_im"][0]), c(inp["s5_log_step"][0])
    m["s5_bre"], m["s5_bim"] = c(inp["s5_b_re"][0]), c(inp["s5_b_im"][0])
    m["s5_cre"], m["s5_cim"] = c(inp["s5_c_re"][0]), c(inp["s5_c_im"][0])
    m["s5_d"], m["s5_glw"], m["s5_glb"] = c(inp["s5_d"][0][None, :]), c(inp["s5_glu_w"][0]), c(inp["s5_glu_b"][0])
    m["ev_wo"], m["od_wo"] = c(inp["ev_w_out"][0]), c(inp["od_w_out"][0])
    for k in ("ln_mix_g", "ln_mix_b", "ln_ffn_g", "ln_ffn_b"):
        m[k] = c(inp[k])
    for L in range(2):
        m["moe_wr%d" % L] = c(np.concatenate([inp["moe_w_coarse"][L]] + [inp["moe_w_fine"][L, g] for g in range(4)], axis=1))
        m["moe_br%d" % L] = c(np.concatenate([inp["moe_b_coarse"][L]] + [inp["moe_b_fine"][L, g] for g in range(4)])[None, :])
        m["moe_wg%d" % L] = c(inp["moe_w_gate"][L].reshape(32, D, 128))
        m["moe_wu%d" % L] = c(inp["moe_w_up"][L].reshape(32, D, 128))
        m["moe_wd%d" % L] = c(inp["moe_w_down"][L].reshape(32, 128, D))
    return {k: np.asarray(v, dtype=np.float32) for k, v in m.items()}


def kernel(**inputs):
    inp = {k: np.asarray(v) for k, v in inputs.items()}
    nc = build_program()
    wm = prep_weights(inp)
    in_maps = []
    for b in range(8):
        m = dict(wm)
        m["xT"] = np.ascontiguousarray(inp["x"][b].T)
        in_maps.append(m)
    res = run_bass_kernel_spmd(nc, in_maps, core_ids=list(range(8)))
    out = np.stack([np.ascontiguousarray(res.results[b]["yT"].T) for b in range(8)], axis=0)
    return out.astype(np.float32)
```
